# Optimizing a Trainium2 kernel written in Bass

```python
import jax
import jax.numpy as jnp
from jax import lax
import numpy as np

D_MODEL = 1024
BATCH = 4
SEQ = 8192
DEPTH = 4

CTX_LEN = 256
GRID_W = 64
HEAD_DIM = 64
NA_HEADS = 4
NA_KH = 8
NA_KW = 16
WA_HEADS = 4
WA_KV_HEADS = 2
WA_WINDOW = 128
WA_BLOCK = 128
MLA_HEADS = 4
MLA_Q_RANK = 256
MLA_KV_RANK = 128
MLA_NOPE = 32
MLA_ROPE = 32
MLA_V = 64
MLA_BLOCK = 128
GLA_HEADS = 4
GLA_DK = 32
GLA_DV = 64
GLA_GATE_RANK = 16
GLA_GATE_NORM = 16.0
GLA_CHUNK = 64
N_EXPERTS = 128
TOP_K = 8
D_EXPERT = 256
D_SHARED = 256
ROUTED_SCALE = 2.5
MOE_BLOCK = 128

ROPE_THETA = 10000.0
LN_EPS = 1e-5
RMS_EPS = 1e-6
DEEPNORM_ALPHA = (2 * DEPTH) ** 0.25
DEEPNORM_BETA = (8 * DEPTH) ** -0.25
MIX_WIDTH = NA_HEADS * HEAD_DIM + WA_HEADS * HEAD_DIM + MLA_HEADS * MLA_V + GLA_HEADS * GLA_DV
IN_SIZES = (
    NA_HEADS * HEAD_DIM, NA_HEADS * HEAD_DIM, NA_HEADS * HEAD_DIM,
    WA_HEADS * HEAD_DIM, WA_KV_HEADS * HEAD_DIM, WA_KV_HEADS * HEAD_DIM,
    MLA_Q_RANK, MLA_KV_RANK, MLA_ROPE,
    GLA_HEADS * GLA_DK, GLA_HEADS * GLA_DK, GLA_HEADS * GLA_DV,
    GLA_HEADS * GLA_DV, GLA_GATE_RANK, GLA_GATE_RANK,
)
IN_WIDTH = sum(IN_SIZES)

kernel_name = 'hybrid_dit_parallel_heads_moe'


def layer_norm(x):
    xf = x.astype(jnp.float32)
    mu = jnp.mean(xf, -1, keepdims=True)
    var = jnp.mean(jnp.square(xf - mu), -1, keepdims=True)
    return ((xf - mu) * lax.rsqrt(var + LN_EPS)).astype(x.dtype)


def rms_norm(x, g):
    xf = x.astype(jnp.float32)
    y = xf * lax.rsqrt(jnp.mean(xf * xf, -1, keepdims=True) + RMS_EPS)
    return y.astype(x.dtype) * g


def modulate(x, shift, scale):
    return layer_norm(x) * (1 + scale) + shift


def post_norm(z, g, b):
    return layer_norm(z) * g + b


def heads(a, n):
    return a.reshape(a.shape[:-1] + (n, a.shape[-1] // n))


def rope_1d(x, pos):
    half = x.shape[-1] // 2
    inv_freq = ROPE_THETA ** (-jnp.arange(half, dtype=jnp.float32) / half)
    ang = pos.astype(jnp.float32)[:, None, None] * inv_freq
    cos, sin = jnp.cos(ang), jnp.sin(ang)
    x1, x2 = x[..., :half].astype(jnp.float32), x[..., half:].astype(jnp.float32)
    return jnp.concatenate([x1 * cos - x2 * sin, x2 * cos + x1 * sin], -1).astype(x.dtype)


def rope_2d(x):
    t = jnp.arange(x.shape[1])
    d = x.shape[-1] // 2
    return jnp.concatenate([rope_1d(x[..., :d], t // GRID_W), rope_1d(x[..., d:], t % GRID_W)], -1)


def context_attention(qc, kc, vc, sink=None):
    s = jnp.einsum('bqhd,bkhd->bhqk', qc, kc).astype(jnp.float32) * qc.shape[-1] ** -0.5
    if sink is not None:
        s = jnp.concatenate([s, jnp.broadcast_to(sink.astype(jnp.float32)[None, :, None, None], s.shape[:-1] + (1,))], -1)
    p = jax.nn.softmax(s, axis=-1)[..., :kc.shape[1]].astype(vc.dtype)
    o = jnp.einsum('bhqk,bkhd->bqhd', p, vc)
    return o.reshape(o.shape[:2] + (-1,))


def neighbourhood_attention(q, k, v, kc, vc, rpb):
    B, T, H, d = q.shape
    rows = T // GRID_W
    kh = min(NA_KH, rows)
    scale = d ** -0.5

    def to_grid(a):
        return a.reshape(B, rows, GRID_W, H, d).transpose(0, 3, 1, 2, 4)

    kg, vg = to_grid(k), to_grid(v)
    q_rows = jnp.moveaxis(to_grid(q), 2, 0)
    r_idx = jnp.arange(rows)
    c_idx = jnp.arange(GRID_W)
    row_start = jnp.clip(r_idx - kh // 2, 0, rows - kh)
    col_keys = jnp.clip(c_idx - NA_KW // 2, 0, GRID_W - NA_KW)[:, None] + jnp.arange(NA_KW)
    col_off = col_keys - c_idx[:, None] + (NA_KW - 1)
    kc_t, vc_t = kc.transpose(0, 2, 1, 3), vc.transpose(0, 2, 1, 3)
    n_nb = kh * NA_KW

    def one_row(inp):
        r, rs, q_row = inp
        k_nb = lax.dynamic_slice_in_dim(kg, rs, kh, axis=2)[:, :, :, col_keys]
        v_nb = lax.dynamic_slice_in_dim(vg, rs, kh, axis=2)[:, :, :, col_keys]
        row_off = rs + jnp.arange(kh) - r + (NA_KH - 1)
        bias = rpb[:, row_off][:, :, col_off].transpose(0, 2, 1, 3)
        s_nb = jnp.einsum('bhcd,bhicjd->bhcij', q_row, k_nb).astype(jnp.float32) * scale + bias.astype(jnp.float32)
        s_ctx = jnp.einsum('bhcd,bhnd->bhcn', q_row, kc_t).astype(jnp.float32) * scale
        p = jax.nn.softmax(jnp.concatenate([s_nb.reshape(B, H, GRID_W, n_nb), s_ctx], -1), axis=-1).astype(v.dtype)
        return (jnp.einsum('bhcij,bhicjd->bhcd', p[..., :n_nb].reshape(B, H, GRID_W, kh, NA_KW), v_nb)
                + jnp.einsum('bhcn,bhnd->bhcd', p[..., n_nb:], vc_t))

    o = lax.map(one_row, (r_idx, row_start, q_rows))
    return o.transpose(1, 0, 3, 2, 4).reshape(B, T, H * d)


def window_attention(q, k, v, kc, vc, sink):
    B, T, H, d = q.shape
    hkv = k.shape[2]
    grp = H // hkv
    nb = T // WA_BLOCK
    band = WA_BLOCK + 2 * WA_WINDOW
    scale = d ** -0.5
    pad = ((0, 0), (WA_WINDOW, WA_WINDOW), (0, 0), (0, 0))
    kp, vp = jnp.pad(k, pad), jnp.pad(v, pad)
    qb = q.reshape(B, nb, WA_BLOCK, hkv, grp, d).transpose(1, 0, 2, 3, 4, 5)
    k_off = jnp.arange(band)
    rel = k_off[None, :] - WA_WINDOW - jnp.arange(WA_BLOCK)[:, None]
    sink_l = jnp.broadcast_to(sink.reshape(hkv, grp).astype(jnp.float32)[None, :, :, None, None], (B, hkv, grp, WA_BLOCK, 1))

    def one_block(inp):
        i, q_blk = inp
        start = i * WA_BLOCK
        k_blk = lax.dynamic_slice_in_dim(kp, start, band, axis=1)
        v_blk = lax.dynamic_slice_in_dim(vp, start, band, axis=1)
        kpos = start - WA_WINDOW + k_off
        valid = (jnp.abs(rel) <= WA_WINDOW) & ((kpos >= 0) & (kpos < T))[None, :]
        s = jnp.einsum('bqhgd,bkhd->bhgqk', q_blk, k_blk).astype(jnp.float32) * scale
        s = jnp.where(valid, s, -jnp.inf)
        s_ctx = jnp.einsum('bqhgd,bchd->bhgqc', q_blk, kc).astype(jnp.float32) * scale
        p = jax.nn.softmax(jnp.concatenate([s, s_ctx, sink_l], -1), axis=-1).astype(v.dtype)
        return (jnp.einsum('bhgqk,bkhd->bqhgd', p[..., :band], v_blk)
                + jnp.einsum('bhgqc,bchd->bqhgd', p[..., band:band + kc.shape[1]], vc))

    o = lax.map(one_block, (jnp.arange(nb), qb))
    return o.transpose(1, 0, 2, 3, 4, 5).reshape(B, T, H * d)


def mla_q(cq, g_q, w_uq, rotary):
    q = heads(rms_norm(cq, g_q) @ w_uq, MLA_HEADS)
    q_nope, q_rope = q[..., :MLA_NOPE], q[..., MLA_NOPE:]
    if rotary:
        q_rope = rope_2d(q_rope)
    return jnp.concatenate([q_nope, q_rope], -1)


def mla_kv(ckv, k_rope, g_kv, w_ukv, rotary):
    kv = heads(rms_norm(ckv, g_kv) @ w_ukv, MLA_HEADS)
    k_nope, v = kv[..., :MLA_NOPE], kv[..., MLA_NOPE:]
    kr = k_rope[:, :, None, :]
    if rotary:
        kr = rope_2d(kr)
    k = jnp.concatenate([k_nope, jnp.broadcast_to(kr, k_nope.shape[:-1] + (MLA_ROPE,))], -1)
    return k, v


def mla_attention(q, k, v, kc, vc):
    B, T, H, dqk = q.shape
    nb = T // MLA_BLOCK
    scale = dqk ** -0.5
    k_all = jnp.concatenate([k, kc], axis=1)
    v_all = jnp.concatenate([v, vc], axis=1)
    qb = q.reshape(B, nb, MLA_BLOCK, H, dqk).transpose(1, 0, 2, 3, 4)

    def one_block(q_blk):
        s = jnp.einsum('bqhd,bkhd->bhqk', q_blk, k_all).astype(jnp.float32) * scale
        p = jax.nn.softmax(s, axis=-1).astype(v.dtype)
        return jnp.einsum('bhqk,bkhd->bqhd', p, v_all)

    o = lax.map(one_block, qb)
    return o.transpose(1, 0, 2, 3, 4).reshape(B, T, H * v.shape[-1])


def gla_chunked(q, k, v, g, s0, with_output):
    B, H, T, dk = q.shape
    n = T // GLA_CHUNK

    def ch(a):
        return a.reshape(B, H, n, GLA_CHUNK, a.shape[-1])

    q, k, v, g = ch(q), ch(k), ch(v), ch(g)
    b = jnp.cumsum(g, axis=3)
    b_end = b[:, :, :, -1]
    u = jnp.einsum('bhnck,bhncv->bhnkv', k * jnp.exp(b_end[:, :, :, None] - b), v)

    def step(s, inp):
        dec, du = inp
        return s * dec[..., None] + du, s

    s_final, s_before = lax.scan(step, s0, (jnp.moveaxis(jnp.exp(b_end), 2, 0), jnp.moveaxis(u, 2, 0)))
    if not with_output:
        return None, s_final
    s_before = jnp.moveaxis(s_before, 0, 2)
    q_dec = q * jnp.exp(b)
    att = jnp.einsum('bhnik,bhnjk->bhnij', q_dec, k * jnp.exp(-b))
    att = jnp.where(jnp.tril(jnp.ones((GLA_CHUNK, GLA_CHUNK), dtype=bool)), att, 0.0)
    o = jnp.einsum('bhnij,bhnjv->bhniv', att, v) + jnp.einsum('bhnik,bhnkv->bhniv', q_dec, s_before)
    return o.reshape(B, H, T, v.shape[-1]), s_final


def gla_mixer(lat, ctx, w_gf, b_gf, w_gb, b_gb, g_norm, ctx_out):
    def prep(q, k, v, zf, zb):
        def f(a, dh):
            return heads(a, GLA_HEADS).transpose(0, 2, 1, 3).astype(jnp.float32)
        gf = jax.nn.log_sigmoid(zf @ w_gf + b_gf) / GLA_GATE_NORM
        gb = jax.nn.log_sigmoid(zb @ w_gb + b_gb) / GLA_GATE_NORM
        return f(q, GLA_DK) * GLA_DK ** -0.5, f(k, GLA_DK), f(v, GLA_DV), f(gf, GLA_DK), f(gb, GLA_DK)

    q, k, v, gf, gb = prep(lat[0], lat[1], lat[2], lat[4], lat[5])
    qc, kc, vc, gfc, gbc = prep(ctx[0], ctx[1], ctx[2], ctx[4], ctx[5])
    B = q.shape[0]
    s0 = jnp.zeros((B, GLA_HEADS, GLA_DK, GLA_DV), jnp.float32)

    def flip(a):
        return jnp.flip(a, axis=2)

    oc_f, sc_f = gla_chunked(qc, kc, vc, gfc, s0, ctx_out)
    oc_b, sc_b = gla_chunked(flip(qc), flip(kc), flip(vc), flip(gbc), s0, ctx_out)
    o_f, _ = gla_chunked(q, k, v, gf, sc_f, True)
    o_b, _ = gla_chunked(flip(q), flip(k), flip(v), flip(gb), sc_b, True)

    def finish(of, ob, r):
        o = rms_norm((of + flip(ob)).transpose(0, 2, 1, 3), g_norm).astype(r.dtype)
        return o.reshape(r.shape[:-1] + (-1,)) * jax.nn.silu(r)

    o_lat = finish(o_f, o_b, lat[3])
    o_ctx = finish(oc_f, oc_b, ctx[3]) if ctx_out else None
    return o_lat, o_ctx


def moe_ffn(h, router_w, router_bias, w1, w3, w2, sw1, sw3, sw2):
    N, D = h.shape
    E = w1.shape[0]
    scores = jax.nn.sigmoid((h @ router_w).astype(jnp.float32))
    _, sel = lax.top_k(scores + router_bias.astype(jnp.float32), TOP_K)
    s_sel = jnp.take_along_axis(scores, sel, axis=-1)
    gates = ROUTED_SCALE * s_sel / jnp.sum(s_sel, -1, keepdims=True)
    e_flat = sel.reshape(-1)
    tok_flat = jnp.repeat(jnp.arange(N, dtype=jnp.int32), TOP_K)
    g_flat = gates.reshape(-1).astype(h.dtype)
    order = jnp.argsort(e_flat)
    e_s, tok_s, g_s = e_flat[order], tok_flat[order], g_flat[order]
    counts = jnp.bincount(e_flat, length=E)
    padded = (counts + MOE_BLOCK - 1) // MOE_BLOCK * MOE_BLOCK
    pad_end = jnp.cumsum(padded)
    pad_start = pad_end - padded
    grp_start = jnp.cumsum(counts) - counts
    dest = pad_start[e_s] + jnp.arange(e_s.shape[0]) - grp_start[e_s]
    n_blocks = -(-(N * TOP_K) // MOE_BLOCK) + E
    buf_tok = jnp.full((n_blocks * MOE_BLOCK,), N, jnp.int32).at[dest].set(tok_s)
    buf_gate = jnp.zeros((n_blocks * MOE_BLOCK,), h.dtype).at[dest].set(g_s)
    blk_exp = jnp.minimum(jnp.searchsorted(pad_end, jnp.arange(n_blocks) * MOE_BLOCK, side='right'), E - 1)
    h_pad = jnp.concatenate([h, jnp.zeros((1, D), h.dtype)], 0)

    def expert_block(acc, inp):
        e, tok, g = inp
        xb = h_pad[tok]
        y = (jax.nn.silu(xb @ w1[e]) * (xb @ w3[e])) @ w2[e]
        return acc.at[tok].add(y * g[:, None]), None

    acc, _ = lax.scan(expert_block, jnp.zeros((N + 1, D), h.dtype),
                      (blk_exp, buf_tok.reshape(n_blocks, MOE_BLOCK), buf_gate.reshape(n_blocks, MOE_BLOCK)))
    shared = (jax.nn.silu(h @ sw1) * (h @ sw3)) @ sw2
    return acc[:N] + shared


def setup_inputs(seed: int = 0) -> dict:
    key = jax.random.key(seed)
    keys = iter(jax.random.split(key, 40))

    def nrm(shape, scale):
        return scale * jax.random.normal(next(keys), shape, jnp.float32)

    L, D, E, F = DEPTH, D_MODEL, N_EXPERTS, D_EXPERT
    return {
        'x': nrm((BATCH, SEQ, D), 1.0),
        'c': nrm((BATCH, D), 1.0),
        'ctx': nrm((BATCH, CTX_LEN, D), 1.0),
        'c_ctx': nrm((D,), 1.0),
        'w_ada': nrm((L, D, 6 * D), 0.5 * D ** -0.5),
        'b_ada': nrm((L, 6 * D), 0.02),
        'w_in': nrm((L, D, IN_WIDTH), D ** -0.5),
        'na_rpb': nrm((L, NA_HEADS, 2 * NA_KH - 1, 2 * NA_KW - 1), 0.3),
        'wa_sink': nrm((L, WA_HEADS), 1.0),
        'mla_g_q': 1.0 + nrm((L, MLA_Q_RANK), 0.02),
        'mla_g_kv': 1.0 + nrm((L, MLA_KV_RANK), 0.02),
        'mla_w_uq': nrm((L, MLA_Q_RANK, MLA_HEADS * (MLA_NOPE + MLA_ROPE)), MLA_Q_RANK ** -0.5),
        'mla_w_ukv': nrm((L, MLA_KV_RANK, MLA_HEADS * (MLA_NOPE + MLA_V)), MLA_KV_RANK ** -0.5),
        'gla_w_gf': nrm((L, GLA_GATE_RANK, GLA_HEADS * GLA_DK), GLA_GATE_RANK ** -0.5),
        'gla_b_gf': nrm((L, GLA_HEADS * GLA_DK), 0.5),
        'gla_w_gb': nrm((L, GLA_GATE_RANK, GLA_HEADS * GLA_DK), GLA_GATE_RANK ** -0.5),
        'gla_b_gb': nrm((L, GLA_HEADS * GLA_DK), 0.5),
        'gla_g_norm': 1.0 + nrm((L, GLA_DV), 0.02),
        'w_out': nrm((L, MIX_WIDTH, D), DEEPNORM_BETA * MIX_WIDTH ** -0.5),
        'ln1_g': 1.0 + nrm((L, D), 0.02),
        'ln1_b': nrm((L, D), 0.02),
        'ln2_g': 1.0 + nrm((L, D), 0.02),
        'ln2_b': nrm((L, D), 0.02),
        'router_w': nrm((L, D, E), D ** -0.5),
        'router_bias': nrm((L, E), 0.01),
        'exp_w1': nrm((L, E, D, F), D ** -0.5),
        'exp_w3': nrm((L, E, D, F), D ** -0.5),
        'exp_w2': nrm((L, E, F, D), DEEPNORM_BETA * F ** -0.5),
        'sh_w1': nrm((L, D, D_SHARED), D ** -0.5),
        'sh_w3': nrm((L, D, D_SHARED), D ** -0.5),
        'sh_w2': nrm((L, D_SHARED, D), DEEPNORM_BETA * D_SHARED ** -0.5),
    }


def reference(x, c, ctx, c_ctx, w_ada, b_ada, w_in, na_rpb, wa_sink, mla_g_q, mla_g_kv, mla_w_uq, mla_w_ukv,
              gla_w_gf, gla_b_gf, gla_w_gb, gla_b_gb, gla_g_norm, w_out, ln1_g, ln1_b, ln2_g, ln2_b,
              router_w, router_bias, exp_w1, exp_w3, exp_w2, sh_w1, sh_w3, sh_w2):
    B, T, D = x.shape
    C = ctx.shape[1]
    offs = np.cumsum(IN_SIZES)[:-1].tolist()
    grp = WA_HEADS // WA_KV_HEADS
    xc = ctx
    for l in range(DEPTH):
        last = l == DEPTH - 1
        mod = jnp.split((jax.nn.silu(c) @ w_ada[l] + b_ada[l])[:, None, :], 6, axis=-1)
        mod_c = jnp.split(jax.nn.silu(c_ctx) @ w_ada[l] + b_ada[l], 6, axis=-1)
        h = modulate(x, mod[0], mod[1])
        hc = modulate(xc, mod_c[0], mod_c[1])
        P = jnp.split(h @ w_in[l], offs, axis=-1)
        Pc = jnp.split(hc @ w_in[l], offs, axis=-1)

        kc_a, vc_a = heads(Pc[1], NA_HEADS), heads(Pc[2], NA_HEADS)
        o_a = neighbourhood_attention(heads(P[0], NA_HEADS), heads(P[1], NA_HEADS), heads(P[2], NA_HEADS),
                                      kc_a, vc_a, na_rpb[l])
        kc_b, vc_b = heads(Pc[4], WA_KV_HEADS), heads(Pc[5], WA_KV_HEADS)
        o_b = window_attention(rope_2d(heads(P[3], WA_HEADS)), rope_2d(heads(P[4], WA_KV_HEADS)),
                               heads(P[5], WA_KV_HEADS), kc_b, vc_b, wa_sink[l])
        k_c, v_c = mla_kv(P[7], P[8], mla_g_kv[l], mla_w_ukv[l], True)
        kc_c, vc_c = mla_kv(Pc[7], Pc[8], mla_g_kv[l], mla_w_ukv[l], False)
        o_c = mla_attention(mla_q(P[6], mla_g_q[l], mla_w_uq[l], True), k_c, v_c, kc_c, vc_c)
        o_d, oc_d = gla_mixer(P[9:15], Pc[9:15], gla_w_gf[l], gla_b_gf[l], gla_w_gb[l], gla_b_gb[l],
                              gla_g_norm[l], not last)

        o = jnp.concatenate([o_a, o_b, o_c, o_d], -1) @ w_out[l]
        x = post_norm(DEEPNORM_ALPHA * x + mod[2] * o, ln1_g[l], ln1_b[l])
        if not last:
            oc = jnp.concatenate([
                context_attention(heads(Pc[0], NA_HEADS), kc_a, vc_a),
                context_attention(heads(Pc[3], WA_HEADS), jnp.repeat(kc_b, grp, axis=2),
                                  jnp.repeat(vc_b, grp, axis=2), wa_sink[l]),
                context_attention(mla_q(Pc[6], mla_g_q[l], mla_w_uq[l], False), kc_c, vc_c),
                oc_d], -1) @ w_out[l]
            xc = post_norm(DEEPNORM_ALPHA * xc + mod_c[2] * oc, ln1_g[l], ln1_b[l])

        h = modulate(x, mod[3], mod[4])
        moe_w = (router_w[l], router_bias[l], exp_w1[l], exp_w3[l], exp_w2[l], sh_w1[l], sh_w3[l], sh_w2[l])
        if last:
            f = moe_ffn(h.reshape(B * T, D), *moe_w).reshape(B, T, D)
        else:
            hc = modulate(xc, mod_c[3], mod_c[4])
            f_all = moe_ffn(jnp.concatenate([h.reshape(B * T, D), hc.reshape(B * C, D)], 0), *moe_w)
            f = f_all[:B * T].reshape(B, T, D)
            xc = post_norm(DEEPNORM_ALPHA * xc + mod_c[5] * f_all[B * T:].reshape(B, C, D), ln2_g[l], ln2_b[l])
        x = post_norm(DEEPNORM_ALPHA * x + mod[5] * f, ln2_g[l], ln2_b[l])
    return x
```

```python
import numpy as np
import ml_dtypes
import concourse.bass as bass
import concourse.mybir as mybir
from concourse.bass_utils import run_bass_kernel_spmd

F32 = mybir.dt.float32
BF16 = mybir.dt.bfloat16
I32 = mybir.dt.int32
AF = mybir.ActivationFunctionType
ALU = mybir.AluOpType
AX = mybir.AxisListType

D = 1024
C = 256
NE = 128
LN_EPS = 1e-5
RMS_EPS = 1e-6
ALPHA = 8 ** 0.25
IN_W = 2496
O_AQ, O_AK, O_AV = 0, 256, 512
O_BQ, O_BK, O_BV = 768, 1024, 1152
O_CQ, O_CKV, O_CKR = 1280, 1536, 1664
O_DQ, O_DK, O_DV, O_DR, O_ZF, O_ZB = 1696, 1824, 1952, 2208, 2464, 2480


class Buf:
    def __init__(self, t, name):
        self.t = t
        self.name = name
        self.w = None
        self.r = []

    def __getitem__(self, k):
        return self.t[k]


class FW:
    def __init__(self, nc):
        self.nc = nc
        self.eng = {"pe": nc.tensor, "act": nc.scalar, "dve": nc.vector, "pool": nc.gpsimd, "sp": nc.sync}
        self.sem = {}
        self.cnt = {}
        self.waited = {}
        for e in self.eng:
            self.sem[e] = nc.alloc_semaphore("s_" + e)
            self.cnt[e] = 0
            self.waited[e] = {}
        self.pend = {e: False for e in self.eng}
        self.dsem = {}
        self.dval = {}
        self.drr = {}
        for q in ("sp", "pool", "act"):
            self.dsem[q] = [nc.alloc_semaphore("d_%s%d" % (q, i)) for i in range(12)]
            self.dval[q] = [0] * 12
            self.drr[q] = 0
        self.semkey = {}
        self.nbuf = 0

    def sb(self, shape, dt, name=None):
        self.nbuf += 1
        name = (name or "t") + "_%d" % self.nbuf
        return Buf(self.stack.enter_context(self.nc.sbuf_tensor(name, list(shape), dt)), name)

    def ps(self, shape, dt=F32, name=None):
        self.nbuf += 1
        name = (name or "p") + "_%d" % self.nbuf
        return Buf(self.stack.enter_context(self.nc.psum_tensor(name, list(shape), dt)), name)

    def _wait(self, e, tok):
        if tok is None:
            return
        sem, val, key = tok
        if self.waited[e].get(key, 0) >= val:
            return
        self.eng[e].wait_ge(sem, val)
        self.waited[e][key] = val

    def _deps(self, e, reads, writes):
        for b in reads:
            if b.w is not None:
                if not (e == "pe" and b.w[2] == "pe"):
                    self._wait(e, b.w)
        for b in writes:
            if b.w is not None and b.w[2] != e:
                self._wait(e, b.w)
            for tok in b.r:
                if tok[2] != e:
                    self._wait(e, tok)

    def _mark(self, tok, reads, writes):
        for b in reads:
            b.r.append(tok)
            if len(b.r) > 24:
                last = {}
                for t in b.r:
                    if t[2] not in last or last[t[2]][1] < t[1]:
                        last[t[2]] = t
                b.r = list(last.values())
        for b in writes:
            b.w = tok
            b.r = []

    def op(self, e, fn, reads=(), writes=(), inc=True):
        self._deps(e, reads, writes)
        ins = fn()
        if inc:
            self.cnt[e] += 1
            ins.then_inc(self.sem[e], 1)
            self.pend[e] = False
            tok = (self.sem[e], self.cnt[e], e)
        else:
            self.pend[e] = True
            tok = (self.sem[e], self.cnt[e] + 1, e)
        self._mark(tok, reads, writes)
        return tok

    def dma(self, q, out, in_, reads=(), writes=()):
        self._deps(q, reads, writes)
        i = self.drr[q]
        self.drr[q] = (i + 1) % len(self.dsem[q])
        sem = self.dsem[q][i]
        key = "d_%s%d" % (q, i)
        if self.dval[q][i] > 0:
            self._wait(q, (sem, self.dval[q][i], key))
        self.dval[q][i] += 16
        self.eng[q].dma_start(out=out, in_=in_).then_inc(sem, 16)
        tok = (sem, self.dval[q][i], key)
        self._mark(tok, reads, writes)
        return tok

    def barrier(self):
        for e in self.eng:
            assert not self.pend[e], "pending non-inc op on " + e
        toks = []
        for e in self.eng:
            if self.cnt[e] > 0:
                toks.append((self.sem[e], self.cnt[e], e))
        for q in self.dsem:
            for i, s in enumerate(self.dsem[q]):
                if self.dval[q][i] > 0:
                    toks.append((s, self.dval[q][i], "d_%s%d" % (q, i)))
        for e in self.eng:
            for t in toks:
                if t[2] != e:
                    self._wait(e, t)


class Ring:
    def __init__(self, bufs):
        self.bufs = bufs
        self.i = 0

    def next(self):
        b = self.bufs[self.i]
        self.i = (self.i + 1) % len(self.bufs)
        return b


def host_consts(T):
    cs = {}
    cs["ident"] = np.eye(128, dtype=np.float32)
    t = np.arange(T)
    row, col = (t // 64).astype(np.float32), (t % 64).astype(np.float32)

    def rope_tabs(d):
        half = d // 2
        inv = (10000.0 ** (-np.arange(half, dtype=np.float32) / half)).astype(np.float32)
        cos = np.zeros((2 * d, T), np.float32)
        sin = np.zeros((2 * d, T), np.float32)
        R = np.zeros((2 * d, 2 * d), np.float32)
        for a, pos in enumerate((row, col)):
            ang = pos[None, :] * inv[:, None]
            for hh in range(2):
                cos[a * d + hh * half:a * d + (hh + 1) * half] = np.cos(ang)
                sin[a * d + hh * half:a * d + (hh + 1) * half] = np.sin(ang)
            for i in range(half):
                R[a * d + i, a * d + half + i] = -1.0
                R[a * d + half + i, a * d + i] = 1.0
        return cos, sin, R

    cw, sw, Rw = rope_tabs(32)
    cs["cos_wa"], cs["sin_wa"], cs["rt_wa"] = cw, sw, np.ascontiguousarray(Rw.T)
    cm, sm, Rm = rope_tabs(16)
    cos_m = np.ones((64, T), np.float32); sin_m = np.zeros((64, T), np.float32); R64 = np.zeros((64, 64), np.float32)
    cos_m[32:], sin_m[32:], R64[32:, 32:] = cm, sm, Rm
    cs["cos_mla"], cs["sin_mla"], cs["rt_mla"] = cos_m, sin_m, np.ascontiguousarray(R64.T)
    kk = np.arange(128)[:, None]; qq = np.arange(512)[None, :]
    m = np.zeros((6, 128, 512), np.float32)
    for r in range(-1, 5):
        m[r + 1] = (np.abs(r * 128 + kk - qq) <= 128)
    cs["maskwa"] = m
    oh = np.zeros((31, 64, 64), np.float32)
    cmask = np.zeros((64, 15, 64), np.float32)
    for c_ in range(64):
        cs0 = min(max(c_ - 8, 0), 48)
        for kc in range(64):
            j = kc - c_ + 15
            if 0 <= j < 31:
                oh[j, c_, kc] = 1.0
            if cs0 <= kc < cs0 + 16:
                cmask[kc, :, c_] = 1.0
    cs["na_oh"] = oh
    cs["na_cmask"] = cmask
    cs["j15"] = np.ascontiguousarray(np.eye(15, dtype=np.float32)[::-1])
    a = np.arange(128)
    same = (a[:, None] // 64) == (a[None, :] // 64)
    le = a[:, None] <= a[None, :]
    lt = a[:, None] < a[None, :]
    g = np.zeros((4, 128, 128), np.float32)
    g[0] = same & le
    g[1] = same & le.T
    g[2] = same & lt.T
    g[3] = same & lt
    cs["gla_tri"] = (g * (-1.0 / 16.0)).astype(np.float32)
    mk = np.zeros((2, 128, 4, 128), np.float32)
    mk[0] = (same & le)[:, None, :]
    mk[1] = (same & le.T)[:, None, :]
    cs["gla_mask"] = mk
    return cs


class K:
    def __init__(self, T, L, debug=()):
        self.T, self.L = T, L
        self.NT = T + C
        self.debug = set(debug)
        nc = bass.Bass("TRN2", target_bir_lowering=False)
        self.nc = nc
        self.fw = FW(nc)
        self.din = {}
        self.scr = {}

    def inp(self, name, shape, dt=F32):
        self.din[name] = self.nc.dram_tensor(name, list(shape), dt, kind="ExternalInput").ap()
        return self.din[name]

    def scratch(self, name, shape, dt):
        kind = "ExternalOutput" if name in self.debug else "Internal"
        self.scr[name] = self.nc.dram_tensor(name, list(shape), dt, kind=kind).ap()
        return self.scr[name]

    def chunks(self, n=512):
        out = []
        t = 0
        while t < self.NT:
            m = min(n, self.NT - t)
            out.append((t, m))
            t += m
        return out

    def declare(self):
        T, L, NT = self.T, self.L, self.NT
        i = self.inp
        i("x", [T, D]); i("ctx", [C, D]); i("cv", [128, 8, 2])
        i("w_ada", [L, D, 6 * D]); i("b_ada_pj", [L, 128, 48]); i("b_ada", [L, 6 * D])
        i("w_in", [L, D, IN_W])
        i("ident", [128, 128])
        i("cos_wa", [64, T]); i("sin_wa", [64, T]); i("rt_wa", [64, 64])
        i("cos_mla", [64, T]); i("sin_mla", [64, T]); i("rt_mla", [64, 64])
        i("maskwa", [6, 128, 512]); i("na_oh", [31, 64, 64]); i("na_cmask", [64, 15, 64]); i("j15", [15, 15])
        i("gla_tri", [4, 128, 128]); i("gla_mask", [2, 128, 4, 128])
        i("na_rpb", [L, 4, 15, 31]); i("wa_sink", [L, 4])
        i("mla_g_q", [L, 128, 2]); i("mla_g_kv", [L, 128, 1]); i("mla_w_uq", [L, 256, 256]); i("mla_w_ukv", [L, 128, 384])
        i("gla_w_gf", [L, 16, 128]); i("gla_b_gf", [L, 128]); i("gla_w_gb", [L, 16, 128]); i("gla_b_gb", [L, 128])
        i("gla_g_norm", [L, 64, 1])
        i("w_out", [L, D, D]); i("ln1_g", [L, D]); i("ln1_b", [L, D]); i("ln2_g", [L, D]); i("ln2_b", [L, D])
        i("router_w", [L, D, NE]); i("router_bias", [L, NE])
        i("exp_w1", [L, NE, D, 256]); i("exp_w3", [L, NE, D, 256]); i("exp_w2", [L, NE, 256, D])
        i("sh_w1", [L, D, 256]); i("sh_w3", [L, D, 256]); i("sh_w2", [L, 256, D])
        self.out = self.nc.dram_tensor("out", [T, D], F32, kind="ExternalOutput").ap()
        s = self.scratch
        s("XR", [NT, D], F32)
        s("PT", [IN_W, NT], BF16)
        s("VT", [NT, 768], BF16)
        s("QTb", [4, 64, NT], BF16); s("KTb", [2, 64, NT], BF16)
        s("QTc", [4, 64, NT], BF16); s("KTc", [4, 64, NT], BF16); s("Vc", [NT, 4, 64], BF16)
        s("OT", [D, NT], BF16)
        s("OF", [64, 4, NT], F32)
        s("HT", [D, NT], BF16)
        s("GT", [NT // 128, 128, NE + 1], F32)

    def setup_global(self, stack):
        fw, nc = self.fw, self.nc
        self.gstack = stack
        fw.stack = stack
        self.ident = fw.sb([128, 128], F32, "ident")
        fw.dma("sp", self.ident[:], self.din["ident"], writes=[self.ident])
        self.ident_bf = fw.sb([128, 128], BF16, "identb")
        fw.op("dve", lambda: nc.vector.tensor_copy(out=self.ident_bf[:], in_=self.ident[:]),
              reads=[self.ident], writes=[self.ident_bf])
        self.ones = fw.sb([128, 128], F32, "ones")
        fw.op("dve", lambda: nc.vector.memset(self.ones[:], 1.0), writes=[self.ones])
        self.epsln = fw.sb([128, 2], F32, "eps")
        fw.op("dve", lambda: nc.vector.memset(self.epsln[:, 0:1], LN_EPS), writes=[self.epsln])
        fw.op("dve", lambda: nc.vector.memset(self.epsln[:, 1:2], RMS_EPS), writes=[self.epsln])
        self.cvs = fw.sb([128, 8, 2], F32, "cvs")
        cv_raw = fw.sb([128, 8, 2], F32, "cvraw")
        fw.dma("sp", cv_raw[:], self.din["cv"], writes=[cv_raw])
        fw.op("act", lambda: nc.scalar.activation(out=self.cvs[:], in_=cv_raw[:], func=AF.Silu),
              reads=[cv_raw], writes=[self.cvs])
        self.modv = fw.sb([128, 48, 2], F32, "modv")
        self.psum_banks = None

    def stage_mod(self, l):
        fw, nc = self.fw, self.nc
        from contextlib import ExitStack
        with ExitStack() as st:
            fw.stack = st
            wj = Ring([fw.sb([128, 8, 128], F32, "wj") for _ in range(3)])
            pm = fw.ps([128, 48, 2], F32, "pm")
            bpj = fw.sb([128, 48], F32, "bpj")
            fw.dma("sp", bpj[:], self.din["b_ada_pj"][l], writes=[bpj])
            wsrc = self.din["w_ada"][l].rearrange("(k p) n -> p k n", p=128)
            for j in range(48):
                w = wj.next()
                fw.dma("sp", w[:], wsrc[:, :, j * 128:(j + 1) * 128], writes=[w])
                for k in range(8):
                    fw.op("pe", lambda k=k, w=w: nc.tensor.matmul(pm[:, j, :], lhsT=w[:, k, :], rhs=self.cvs[:, k, :],
                                                                   start=(k == 0), stop=(k == 7)),
                          reads=[w, self.cvs], writes=[pm], inc=(k == 7))
            for wh in range(2):
                fw.op("dve", lambda wh=wh: nc.vector.tensor_tensor(out=self.modv[:, :, wh], in0=pm[:, :, wh], in1=bpj[:],
                                                                    op=ALU.add),
                      reads=[pm, bpj], writes=[self.modv])
            for j0 in (8, 32):
                fw.op("dve", lambda j0=j0: nc.vector.tensor_scalar_add(out=self.modv[:, j0:j0 + 8, :],
                                                                        in0=self.modv[:, j0:j0 + 8, :], scalar1=1.0),
                      reads=[self.modv], writes=[self.modv])
            fw.barrier()
        fw.stack = self.gstack

    def stage_inproj(self, l):
        fw, nc = self.fw, self.nc
        T, NT = self.T, self.NT
        from contextlib import ExitStack
        with ExitStack() as st:
            fw.stack = st
            win = fw.sb([128, 8, IN_W], BF16, "win")
            wsrc = self.din["w_in"][l].rearrange("(k p) n -> p k n", p=128)
            for k in range(8):
                fw.dma("pool", win[:, k, :], wsrc[:, k, :], writes=[win])
            xr = Ring([fw.sb([128, D], F32, "x") for _ in range(3)])
            xnr = Ring([fw.sb([128, D], F32, "xn") for _ in range(2)])
            str_ = Ring([fw.sb([128, 16], F32, "st") for _ in range(3)])
            hTr = Ring([fw.sb([128, 8, 512], BF16, "hT") for _ in range(2)])
            ptr = Ring([fw.sb([128, 512], BF16, "pt") for _ in range(4)])
            vtr = Ring([fw.sb([128, 768], BF16, "vt") for _ in range(2)])
            tpr = Ring([fw.ps([128, 8, 128], F32, "tp") for _ in range(2)])
            ppr = Ring([fw.ps([128, 512], F32, "pp") for _ in range(2)])
            pvr = Ring([fw.ps([128, 2, 512], F32, "pv") for _ in range(1)])
            ev = 0
            for (t0, n) in self.chunks(512):
                hT = hTr.next()
                for ti in range(n // 128):
                    tok = t0 + ti * 128
                    lat = tok < T
                    wh = 0 if lat else 1
                    if l == 0:
                        src = self.din["x"][tok:tok + 128, :] if lat else self.din["ctx"][tok - T:tok - T + 128, :]
                    else:
                        src = self.scr["XR"][tok:tok + 128, :]
                    x = xr.next()
                    fw.dma("sp", x[:], src, writes=[x])
                    s = str_.next()
                    for hh in range(2):
                        fw.op("dve", lambda x=x, s=s, hh=hh: nc.vector.bn_stats(out=s[:, hh * 6:hh * 6 + 6],
                                                                                 in_=x[:, hh * 512:(hh + 1) * 512]),
                              reads=[x], writes=[s])
                    fw.op("dve", lambda s=s: nc.vector.bn_aggr(out=s[:, 12:14], in_=s[:, 0:12]), reads=[s], writes=[s])
                    fw.op("act", lambda s=s: nc.scalar.activation(out=s[:, 15:16], in_=s[:, 13:14], func=AF.Sqrt,
                                                                   bias=self.epsln[:, 0:1], scale=1.0),
                          reads=[s, self.epsln], writes=[s])
                    fw.op("dve", lambda s=s: nc.vector.reciprocal(out=s[:, 14:15], in_=s[:, 15:16]), reads=[s], writes=[s])
                    xn = xnr.next()
                    fw.op("dve", lambda x=x, s=s, xn=xn: nc.vector.tensor_scalar(out=xn[:], in0=x[:], scalar1=s[:, 12:13],
                                                                                  scalar2=s[:, 14:15], op0=ALU.subtract,
                                                                                  op1=ALU.mult),
                          reads=[x, s], writes=[xn])
                    tp = tpr.next()
                    for k in range(8):
                        fw.op("pe", lambda k=k, tp=tp, xn=xn: nc.tensor.transpose(out=tp[:, k, :], in_=xn[:, k * 128:(k + 1) * 128],
                                                                                   identity=self.ident[:]),
                              reads=[xn, self.ident], writes=[tp], inc=(k == 7))
                    for k in range(8):
                        sc = self.modv[:, 8 + k, wh:wh + 1]
                        sh = self.modv[:, k, wh:wh + 1]
                        o = hT[:, k, ti * 128:(ti + 1) * 128]
                        if k % 2 == 0:
                            fw.op("act", lambda o=o, tp=tp, k=k, sc=sc, sh=sh: nc.scalar.activation(
                                out=o, in_=tp[:, k, :], func=AF.Identity, bias=sh, scale=sc),
                                reads=[tp, self.modv], writes=[hT])
                        else:
                            fw.op("dve", lambda o=o, tp=tp, k=k, sc=sc, sh=sh: nc.vector.tensor_scalar(
                                out=o, in0=tp[:, k, :], scalar1=sc, scalar2=sh, op0=ALU.mult, op1=ALU.add),
                                reads=[tp, self.modv], writes=[hT])
                    pv = pvr.next()
                    for (c0, c1, bank, off) in ((O_AV, O_AV + 256, 0, 0), (O_BV, O_BV + 128, 0, 256), (O_DK, O_DK + 384, 1, 0)):
                        for k in range(8):
                            fw.op("pe", lambda k=k, c0=c0, c1=c1, bank=bank, off=off, pv=pv, hT=hT, ti=ti: nc.tensor.matmul(
                                pv[:, bank, off:off + (c1 - c0)], lhsT=hT[:, k, ti * 128:(ti + 1) * 128], rhs=win[:, k, c0:c1],
                                start=(k == 0), stop=(k == 7)),
                                reads=[hT, win], writes=[pv], inc=(k == 7))
                    vt = vtr.next()
                    fw.op("act", lambda vt=vt, pv=pv: nc.scalar.copy(out=vt[:, 0:384], in_=pv[:, 0, 0:384]), reads=[pv], writes=[vt])
                    fw.op("dve", lambda vt=vt, pv=pv: nc.vector.tensor_copy(out=vt[:, 384:768], in_=pv[:, 1, 0:384]), reads=[pv], writes=[vt])
                    fw.dma("pool", self.scr["VT"][tok:tok + 128, :], vt[:], reads=[vt])
                for jc in range(20):
                    m = min(128, IN_W - jc * 128)
                    pp = ppr.next()
                    for k in range(8):
                        fw.op("pe", lambda k=k, jc=jc, m=m, pp=pp, hT=hT: nc.tensor.matmul(
                            pp[0:m, 0:n], lhsT=win[:, k, jc * 128:jc * 128 + m], rhs=hT[:, k, 0:n],
                            start=(k == 0), stop=(k == 7)),
                            reads=[hT, win], writes=[pp], inc=(k == 7))
                    pt = ptr.next()
                    if ev % 2 == 0:
                        fw.op("act", lambda pt=pt, pp=pp, m=m: nc.scalar.copy(out=pt[0:m, 0:n], in_=pp[0:m, 0:n]), reads=[pp], writes=[pt])
                    else:
                        fw.op("dve", lambda pt=pt, pp=pp, m=m: nc.vector.tensor_copy(out=pt[0:m, 0:n], in_=pp[0:m, 0:n]), reads=[pp], writes=[pt])
                    ev += 1
                    fw.dma("pool", self.scr["PT"][jc * 128:jc * 128 + m, t0:t0 + n], pt[0:m, 0:n], reads=[pt])
            fw.barrier()
        fw.stack = self.gstack


def _stage(fn):
    def wrap(self, *a, **kw):
        from contextlib import ExitStack
        with ExitStack() as st:
            self.fw.stack = st
            fn(self, *a, **kw)
            self.fw.barrier()
        self.fw.stack = self.gstack
    return wrap


class KA(K):
    def lat_chunks(self, n=512):
        return [(t, min(n, self.T - t)) for t in range(0, self.T, n)]

    def attn_finish(self, po, n, dst, rings, sink=None):
        fw, nc = self.fw, self.nc
        rden, pbr, osbr, obr = rings
        rd = rden.next()
        if sink is not None:
            fw.op("dve", lambda: nc.vector.tensor_scalar(out=rd[64:65, 0:n], in0=po[64:65, 0:n], scalar1=sink, scalar2=None,
                                                          op0=ALU.add), reads=[po, self.sinkexp], writes=[rd])
            fw.op("dve", lambda: nc.vector.reciprocal(out=rd[64:65, 0:n], in_=rd[64:65, 0:n]), reads=[rd], writes=[rd])
        else:
            fw.op("dve", lambda: nc.vector.reciprocal(out=rd[64:65, 0:n], in_=po[64:65, 0:n]), reads=[po], writes=[rd])
        pb = pbr.next()
        fw.op("pe", lambda: nc.tensor.matmul(pb[0:64, 0:n], lhsT=self.ones[64:65, 0:64], rhs=rd[64:65, 0:n], start=True, stop=True),
              reads=[rd, self.ones], writes=[pb])
        osb = osbr.next()
        fw.op("act", lambda: nc.scalar.copy(out=osb[0:64, 0:n], in_=po[0:64, 0:n]), reads=[po], writes=[osb])
        ob = obr.next()
        fw.op("dve", lambda: nc.vector.tensor_tensor(out=ob[0:64, 0:n], in0=osb[0:64, 0:n], in1=pb[0:64, 0:n], op=ALU.mult),
              reads=[osb, pb], writes=[ob])
        fw.dma("pool", dst, ob[0:64, 0:n], reads=[ob])

    def finish_rings(self):
        fw = self.fw
        return (Ring([fw.sb([128, 512], F32, "rden") for _ in range(2)]),
                Ring([fw.ps([128, 512], F32, "pb") for _ in range(1)]),
                Ring([fw.sb([64, 512], F32, "osb") for _ in range(2)]),
                Ring([fw.sb([64, 512], BF16, "ob") for _ in range(2)]))

    def rope64(self, src, srcbufs, n, t0, rot, which, rings):
        fw, nc = self.fw, self.nc
        qsr, prr, cosr, sinr, t1r, qfr = rings
        qs = qsr.next()
        fw.op("act", lambda: nc.scalar.copy(out=qs[0:64, 0:n], in_=src), reads=srcbufs, writes=[qs])
        if not rot:
            return qs
        pr = prr.next()
        rt = self.rt_wa if which == "wa" else self.rt_mla
        fw.op("pe", lambda: nc.tensor.matmul(pr[0:64, 0:n], lhsT=rt[:], rhs=qs[0:64, 0:n], start=True, stop=True),
              reads=[qs, rt], writes=[pr])
        cos, sin = cosr.next(), sinr.next()
        fw.dma("sp", cos[0:64, 0:n], self.din["cos_" + which][:, t0:t0 + n], writes=[cos])
        fw.dma("sp", sin[0:64, 0:n], self.din["sin_" + which][:, t0:t0 + n], writes=[sin])
        t1 = t1r.next()
        fw.op("pool", lambda: nc.gpsimd.tensor_tensor(out=t1[0:64, 0:n], in0=qs[0:64, 0:n], in1=cos[0:64, 0:n], op=ALU.mult),
              reads=[qs, cos], writes=[t1])
        t2 = t1r.next()
        fw.op("dve", lambda: nc.vector.tensor_tensor(out=t2[0:64, 0:n], in0=pr[0:64, 0:n], in1=sin[0:64, 0:n], op=ALU.mult),
              reads=[pr, sin], writes=[t2])
        qf = qfr.next()
        fw.op("dve", lambda: nc.vector.tensor_tensor(out=qf[0:64, 0:n], in0=t1[0:64, 0:n], in1=t2[0:64, 0:n], op=ALU.add),
              reads=[t1, t2], writes=[qf])
        return qf

    def rope_rings(self):
        fw = self.fw
        return (Ring([fw.sb([64, 512], BF16, "qs") for _ in range(3)]),
                Ring([fw.ps([64, 512], F32, "pr") for _ in range(2)]),
                Ring([fw.sb([64, 512], F32, "cos") for _ in range(2)]),
                Ring([fw.sb([64, 512], F32, "sin") for _ in range(2)]),
                Ring([fw.sb([64, 512], F32, "t1") for _ in range(4)]),
                Ring([fw.sb([64, 512], BF16, "qf") for _ in range(3)]))

    def load_rt(self):
        fw, nc = self.fw, self.nc
        for nm in ("rt_wa", "rt_mla"):
            t = fw.sb([64, 64], BF16, nm)
            fw.dma("pool", t[:], self.din[nm], writes=[t])
            setattr(self, nm, t)

    @_stage
    def stage_wa_prep(self, l):
        fw, nc = self.fw, self.nc
        T = self.T
        self.load_rt()
        rr = self.rope_rings()
        inr = Ring([fw.sb([64, 512], BF16, "in") for _ in range(3)])
        for (t0, n) in self.chunks(512):
            rot = t0 < T
            for (src_row, dst) in [(O_BQ + 64 * h, self.scr["QTb"][h]) for h in range(4)] + \
                                  [(O_BK + 64 * h, self.scr["KTb"][h]) for h in range(2)]:
                a = inr.next()
                fw.dma("sp", a[0:64, 0:n], self.scr["PT"][src_row:src_row + 64, t0:t0 + n], writes=[a])
                if rot:
                    qf = self.rope64(a[0:64, 0:n], [a], n, t0, True, "wa", rr)
                    fw.dma("pool", dst[:, t0:t0 + n], qf[0:64, 0:n], reads=[qf])
                else:
                    fw.dma("pool", dst[:, t0:t0 + n], a[0:64, 0:n], reads=[a])

    @_stage
    def stage_mla_prep(self, l):
        fw, nc = self.fw, self.nc
        T = self.T
        self.load_rt()
        rr = self.rope_rings()
        onesb = fw.sb([128, 128], BF16, "onesb")
        fw.op("dve", lambda: nc.vector.memset(onesb[:], 1.0), writes=[onesb])
        gq = fw.sb([128, 2], F32, "gq"); gkv = fw.sb([128, 1], F32, "gkv")
        fw.dma("sp", gq[:], self.din["mla_g_q"][l], writes=[gq])
        fw.dma("sp", gkv[:], self.din["mla_g_kv"][l], writes=[gkv])
        wuq = fw.sb([128, 2, 256], BF16, "wuq"); wukv = fw.sb([128, 384], BF16, "wukv")
        fw.dma("pool", wuq[:], self.din["mla_w_uq"][l].rearrange("(k p) n -> p k n", p=128), writes=[wuq])
        fw.dma("pool", wukv[:], self.din["mla_w_ukv"][l], writes=[wukv])
        cqr = Ring([fw.sb([128, 3, 512], BF16, "cq") for _ in range(2)])
        sqr = Ring([fw.sb([128, 3, 512], BF16, "sq") for _ in range(2)])
        rsr = Ring([fw.sb([128, 2, 512], F32, "rs") for _ in range(2)])
        cnr = Ring([fw.sb([128, 3, 512], BF16, "cn") for _ in range(2)])
        krr = Ring([fw.sb([64, 512], BF16, "kr") for _ in range(2)])
        knr = Ring([fw.sb([32, 512], BF16, "kn") for _ in range(3)])
        vcr = Ring([fw.sb([128, 384], BF16, "vc") for _ in range(3)])
        pss = Ring([fw.ps([128, 2, 512], F32, "pss") for _ in range(1)])
        pq = Ring([fw.ps([64, 512], F32, "pq") for _ in range(2)])
        pv = Ring([fw.ps([128, 512], F32, "pvc") for _ in range(1)])
        PT = self.scr["PT"]
        for (t0, n) in self.chunks(512):
            rot = t0 < T
            cq = cqr.next()
            fw.dma("sp", cq[:, 0:2, 0:n], PT[O_CQ:O_CQ + 256, t0:t0 + n].rearrange("(k p) t -> p k t", p=128), writes=[cq])
            fw.dma("sp", cq[:, 2, 0:n], PT[O_CKV:O_CKV + 128, t0:t0 + n], writes=[cq])
            sq = sqr.next()
            fw.op("pool", lambda: nc.gpsimd.tensor_tensor(out=sq[:, :, 0:n], in0=cq[:, :, 0:n], in1=cq[:, :, 0:n], op=ALU.mult),
                  reads=[cq], writes=[sq])
            ps = pss.next()
            for k in range(2):
                fw.op("pe", lambda k=k: nc.tensor.matmul(ps[:, 0, 0:n], lhsT=onesb[:], rhs=sq[:, k, 0:n], start=(k == 0), stop=(k == 1)),
                      reads=[sq, onesb], writes=[ps], inc=(k == 1))
            fw.op("pe", lambda: nc.tensor.matmul(ps[:, 1, 0:n], lhsT=onesb[:], rhs=sq[:, 2, 0:n], start=True, stop=True),
                  reads=[sq, onesb], writes=[ps])
            rs = rsr.next()
            fw.op("act", lambda: nc.scalar.activation(out=rs[:, 0, 0:n], in_=ps[:, 0, 0:n], func=AF.Sqrt, bias=self.epsln[:, 1:2], scale=1.0 / 256),
                  reads=[ps, self.epsln], writes=[rs])
            fw.op("act", lambda: nc.scalar.activation(out=rs[:, 1, 0:n], in_=ps[:, 1, 0:n], func=AF.Sqrt, bias=self.epsln[:, 1:2], scale=1.0 / 128),
                  reads=[ps, self.epsln], writes=[rs])
            fw.op("dve", lambda: nc.vector.reciprocal(out=rs[:, :, 0:n], in_=rs[:, :, 0:n]), reads=[rs], writes=[rs])
            cn = cnr.next()
            for k in range(3):
                g = gq[:, k:k + 1] if k < 2 else gkv[:, 0:1]
                fw.op("dve", lambda k=k, g=g: nc.vector.scalar_tensor_tensor(out=cn[:, k, 0:n], in0=cq[:, k, 0:n], scalar=g,
                                                                             in1=rs[:, (0 if k < 2 else 1), 0:n], op0=ALU.mult, op1=ALU.mult),
                      reads=[cq, rs, gq, gkv], writes=[cn])
            for h in range(4):
                p = pq.next()
                for k in range(2):
                    fw.op("pe", lambda k=k, h=h, p=p: nc.tensor.matmul(p[0:64, 0:n], lhsT=wuq[:, k, h * 64:(h + 1) * 64], rhs=cn[:, k, 0:n],
                                                                       start=(k == 0), stop=(k == 1)),
                          reads=[cn, wuq], writes=[p], inc=(k == 1))
                qf = self.rope64(p[0:64, 0:n], [p], n, t0, rot, "mla", rr)
                fw.dma("pool", self.scr["QTc"][h][:, t0:t0 + n], qf[0:64, 0:n], reads=[qf])
            kr = krr.next()
            if rot:
                fw.op("pool", lambda: nc.gpsimd.memset(kr[0:32, 0:n], 0.0), writes=[kr])
            fw.dma("sp", kr[32:64, 0:n], PT[O_CKR:O_CKR + 32, t0:t0 + n], writes=[kr])
            if rot:
                kf = self.rope64(kr[0:64, 0:n], [kr], n, t0, True, "mla", rr)
            else:
                kf = kr
            for h in range(4):
                fw.dma("pool", self.scr["KTc"][h][32:64, t0:t0 + n], kf[32:64, 0:n], reads=[kf])
            for h in range(4):
                p = pq.next()
                fw.op("pe", lambda h=h, p=p: nc.tensor.matmul(p[0:32, 0:n], lhsT=wukv[:, h * 96:h * 96 + 32], rhs=cn[:, 2, 0:n], start=True, stop=True),
                      reads=[cn, wukv], writes=[p])
                kn = knr.next()
                fw.op("act", lambda p=p, kn=kn: nc.scalar.copy(out=kn[0:32, 0:n], in_=p[0:32, 0:n]), reads=[p], writes=[kn])
                fw.dma("pool", self.scr["KTc"][h][0:32, t0:t0 + n], kn[0:32, 0:n], reads=[kn])
            for ti in range(n // 128):
                p = pv.next()
                fw.op("pe", lambda p=p, ti=ti: nc.tensor.matmul(p[:, 0:384], lhsT=cn[:, 2, ti * 128:(ti + 1) * 128], rhs=wukv[:], start=True, stop=True),
                      reads=[cn, wukv], writes=[p])
                vc = vcr.next()
                fw.op("dve", lambda p=p, vc=vc: nc.vector.tensor_copy(out=vc[:], in_=p[:, 0:384]), reads=[p], writes=[vc])
                tok = t0 + ti * 128
                fw.dma("pool", self.scr["Vc"][tok:tok + 128], vc[:].rearrange("p (h c) -> p h c", h=4)[:, :, 32:96], reads=[vc])

    def exp_block(self, ps, kp, c0, c1, er, mask=None, mring=None):
        fw, nc = self.fw, self.nc
        e = er.next()
        fw.op("act", lambda: nc.scalar.activation(out=e[0:kp, c0:c1], in_=ps[0:kp, c0:c1], func=AF.Exp, scale=0.125),
              reads=[ps], writes=[e])
        if mask is None:
            return e
        mbufs, map_ = mask
        e2 = mring.next()
        fw.op("pool", lambda: nc.gpsimd.tensor_tensor(out=e2[0:kp, c0:c1], in0=e[0:kp, c0:c1], in1=map_, op=ALU.mult),
              reads=[e] + mbufs, writes=[e2])
        return e2

    @_stage
    def stage_mla_attn(self, l, ctx_out):
        fw, nc = self.fw, self.nc
        T, NT = self.T, self.NT
        nkb = NT // 128
        fr = self.finish_rings()
        kT = fw.sb([64, NT], BF16, "kT")
        va = fw.sb([128, nkb, 65], BF16, "va")
        fw.op("dve", lambda: nc.vector.memset(va[:, :, 64:65], 1.0), writes=[va])
        qr = Ring([fw.sb([64, 512], BF16, "q") for _ in range(2)])
        er = Ring([fw.sb([128, 512], BF16, "e") for _ in range(3)])
        psr = Ring([fw.ps([128, 512], F32, "ps") for _ in range(3)])
        por = Ring([fw.ps([128, 512], F32, "po") for _ in range(2)])
        for h in range(4):
            fw.dma("sp", kT[:], self.scr["KTc"][h], writes=[kT])
            fw.dma("sp", va[:, :, 0:64], self.scr["Vc"][:, h, :].rearrange("(b p) d -> p b d", p=128), writes=[va])
            qchunks = [(t0, n, list(range(nkb))) for (t0, n) in self.lat_chunks(512)]
            if ctx_out:
                qchunks.append((T, C, [nkb - 2, nkb - 1]))
            for (t0, n, kbs) in qchunks:
                q = qr.next()
                fw.dma("sp", q[0:64, 0:n], self.scr["QTc"][h][:, t0:t0 + n], writes=[q])
                po = por.next()
                for i, kb in enumerate(kbs):
                    ps = psr.next()
                    fw.op("pe", lambda ps=ps, kb=kb: nc.tensor.matmul(ps[:, 0:n], lhsT=kT[:, kb * 128:(kb + 1) * 128], rhs=q[0:64, 0:n], start=True, stop=True),
                          reads=[kT, q], writes=[ps])
                    e = self.exp_block(ps, 128, 0, n, er)
                    fw.op("pe", lambda e=e, kb=kb, i=i: nc.tensor.matmul(po[0:65, 0:n], lhsT=va[:, kb, :], rhs=e[:, 0:n], start=(i == 0), stop=(i == len(kbs) - 1)),
                          reads=[e, va], writes=[po], inc=(i == len(kbs) - 1))
                self.attn_finish(po, n, self.scr["OT"][512 + h * 64:512 + (h + 1) * 64, t0:t0 + n], fr)

    @_stage
    def stage_wa_attn(self, l, ctx_out):
        fw, nc = self.fw, self.nc
        T, NT = self.T, self.NT
        nb = T // 128
        fr = self.finish_rings()
        self.sinkexp = fw.sb([128, 4], F32, "sinkexp")
        fw.dma("sp", self.sinkexp[:], self.din["wa_sink"][l].partition_broadcast(128), writes=[self.sinkexp])
        fw.op("act", lambda: nc.scalar.activation(out=self.sinkexp[:], in_=self.sinkexp[:], func=AF.Exp), reads=[self.sinkexp], writes=[self.sinkexp])
        mask = fw.sb([128, 6, 512], BF16, "mask")
        fw.dma("pool", mask[:], self.din["maskwa"].rearrange("r p q -> p r q"), writes=[mask])
        kT = fw.sb([64, NT], BF16, "kT")
        va = fw.sb([128, NT // 128, 65], BF16, "va")
        fw.op("dve", lambda: nc.vector.memset(va[:, :, 64:65], 1.0), writes=[va])
        qr = Ring([fw.sb([64, 512], BF16, "q") for _ in range(2)])
        er = Ring([fw.sb([128, 512], BF16, "e") for _ in range(3)])
        mr = Ring([fw.sb([128, 512], BF16, "em") for _ in range(3)])
        psr = Ring([fw.ps([128, 512], F32, "ps") for _ in range(3)])
        por = Ring([fw.ps([128, 512], F32, "po") for _ in range(2)])
        for g in range(2):
            fw.dma("sp", kT[:], self.scr["KTb"][g], writes=[kT])
            fw.dma("sp", va[:, :, 0:64], self.scr["VT"][:, 256 + g * 64:256 + (g + 1) * 64].rearrange("(b p) d -> p b d", p=128), writes=[va])
            for h in (2 * g, 2 * g + 1):
                qchunks = [(t0, n, True) for (t0, n) in self.lat_chunks(512)]
                if ctx_out:
                    qchunks.append((T, C, False))
                for (t0, n, lat) in qchunks:
                    q = qr.next()
                    fw.dma("sp", q[0:64, 0:n], self.scr["QTb"][h][:, t0:t0 + n], writes=[q])
                    po = por.next()
                    items = [(nb, 0, n, None)]
                    if lat:
                        i0 = t0 // 128
                        nqb = n // 128
                        for r in range(-1, nqb + 1):
                            j = i0 + r
                            if j < 0 or j >= nb:
                                continue
                            qlo, qhi = max(0, r - 1), min(nqb, r + 2)
                            items.append((j, qlo * 128, qhi * 128, r + 1))
                    items.append((nb + 1, 0, n, None))
                    for i, (kb, c0, c1, mi) in enumerate(items):
                        ps = psr.next()
                        fw.op("pe", lambda ps=ps, kb=kb, c0=c0, c1=c1: nc.tensor.matmul(ps[:, c0:c1], lhsT=kT[:, kb * 128:(kb + 1) * 128], rhs=q[0:64, c0:c1], start=True, stop=True),
                              reads=[kT, q], writes=[ps])
                        e = self.exp_block(ps, 128, c0, c1, er, mask=None if mi is None else ([mask], mask[:, mi, c0:c1]), mring=mr)
                        fw.op("pe", lambda e=e, kb=kb, c0=c0, c1=c1, i=i: nc.tensor.matmul(po[0:65, c0:c1], lhsT=va[:, kb, :], rhs=e[:, c0:c1], start=(i == 0), stop=(i == len(items) - 1)),
                              reads=[e, va], writes=[po], inc=(i == len(items) - 1))
                    self.attn_finish(po, n, self.scr["OT"][256 + h * 64:256 + (h + 1) * 64, t0:t0 + n], fr, sink=self.sinkexp[64:65, h:h + 1])

    @_stage
    def stage_na_attn(self, l, ctx_out):
        fw, nc = self.fw, self.nc
        T, NT = self.T, self.NT
        R = T // 64
        fr = self.finish_rings()
        oh = fw.sb([31, 64, 64], F32, "oh")
        fw.dma("sp", oh[:], self.din["na_oh"], writes=[oh])
        cmask = fw.sb([64, 960], F32, "cmask")
        fw.dma("sp", cmask[:], self.din["na_cmask"].rearrange("k m c -> k (m c)"), writes=[cmask])
        j15 = fw.sb([15, 15], F32, "j15")
        fw.dma("sp", j15[:], self.din["j15"], writes=[j15])
        rpb = fw.sb([15, 4, 31], F32, "rpb")
        fw.dma("sp", rpb[:], self.din["na_rpb"][l].rearrange("h r j -> r h j"), writes=[rpb])
        rrr = Ring([fw.sb([31, 15], F32, "rr") for _ in range(2)])
        etz = [fw.sb([64, 960], BF16, "etz") for _ in range(4)]
        etmp = fw.sb([64, 960], F32, "etmp")
        prr = fw.ps([31, 15], F32, "prr")
        pz = fw.ps([64, 2, 512], F32, "pz")
        for h in range(4):
            fw.op("pe", lambda h=h: nc.tensor.matmul(prr[:, :], lhsT=rpb[:, h, :], rhs=j15[:], start=True, stop=True), reads=[rpb, j15], writes=[prr])
            rr = rrr.next()
            fw.op("dve", lambda rr=rr: nc.vector.tensor_copy(out=rr[:], in_=prr[:]), reads=[prr], writes=[rr])
            for c_ in range(64):
                for (bk, m0, m1) in ((0, 0, 8), (1, 8, 15)):
                    fw.op("pe", lambda c_=c_, bk=bk, m0=m0, m1=m1, rr=rr: nc.tensor.matmul(
                        pz[:, bk, :].rearrange("k (m c) -> k m c", c=64)[:, 0:m1 - m0, c_], lhsT=oh[:, c_, :], rhs=rr[:, m0:m1], start=True, stop=True),
                        reads=[oh, rr], writes=[pz], inc=(c_ == 63 and bk == 1))
            fw.op("act", lambda: nc.scalar.activation(out=etmp[:, 0:512], in_=pz[:, 0, :], func=AF.Exp), reads=[pz], writes=[etmp])
            fw.op("act", lambda: nc.scalar.activation(out=etmp[:, 512:960], in_=pz[:, 1, 0:448], func=AF.Exp), reads=[pz], writes=[etmp])
            fw.op("dve", lambda h=h: nc.vector.tensor_tensor(out=etz[h][:], in0=etmp[:], in1=cmask[:], op=ALU.mult), reads=[etmp, cmask], writes=[etz[h]])
        kTr = Ring([fw.sb([64, 1024], BF16, "kT") for _ in range(2)])
        kcr = Ring([fw.sb([64, 256], BF16, "kc") for _ in range(2)])
        var = Ring([fw.sb([64, 16, 65], BF16, "va") for _ in range(2)])
        vcr = Ring([fw.sb([128, 2, 65], BF16, "vca") for _ in range(2)])
        for b in var.bufs:
            fw.op("dve", lambda b=b: nc.vector.memset(b[:, :, 64:65], 1.0), writes=[b])
        for b in vcr.bufs:
            fw.op("dve", lambda b=b: nc.vector.memset(b[:, :, 64:65], 1.0), writes=[b])
        qr = Ring([fw.sb([64, 512], BF16, "q") for _ in range(2)])
        er = Ring([fw.sb([128, 512], BF16, "e") for _ in range(3)])
        mr = Ring([fw.sb([128, 512], BF16, "em") for _ in range(3)])
        psr = Ring([fw.ps([128, 512], F32, "ps") for _ in range(2)])
        por = Ring([fw.ps([128, 512], F32, "po") for _ in range(2)])
        PT, VT = self.scr["PT"], self.scr["VT"]
        for h in range(4):
            kc = kcr.next()
            fw.dma("sp", kc[:], PT[O_AK + h * 64:O_AK + (h + 1) * 64, T:T + C], writes=[kc])
            vc = vcr.next()
            fw.dma("sp", vc[:, :, 0:64], VT[T:T + C, h * 64:(h + 1) * 64].rearrange("(b p) d -> p b d", p=128), writes=[vc])
            qchunks = [(t0, n, True) for (t0, n) in self.lat_chunks(512)]
            if ctx_out:
                qchunks.append((T, C, False))
            for (t0, n, lat) in qchunks:
                q = qr.next()
                fw.dma("sp", q[0:64, 0:n], PT[O_AQ + h * 64:O_AQ + (h + 1) * 64, t0:t0 + n], writes=[q])
                po = por.next()
                items = [("c", 0, 0, n, None)]
                if lat:
                    r0 = t0 // 64
                    nqr = n // 64
                    klo, khi = max(0, r0 - 4), min(R - 1, r0 + nqr - 1 + 3 + 0)
                    rs_all = [min(max(r0 + rq - 4, 0), R - 8) for rq in range(nqr)]
                    klo, khi = min(rs_all), max(rs_all) + 7
                    kT = kTr.next()
                    nk = khi - klo + 1
                    fw.dma("sp", kT[:, 0:nk * 64], PT[O_AK + h * 64:O_AK + (h + 1) * 64, klo * 64:(khi + 1) * 64], writes=[kT])
                    va = var.next()
                    fw.dma("sp", va[:, 0:nk, 0:64], VT[klo * 64:(khi + 1) * 64, h * 64:(h + 1) * 64].rearrange("(r p) d -> p r d", p=64), writes=[va])
                    for kr in range(klo, khi + 1):
                        val = [rq for rq in range(nqr) if rs_all[rq] <= kr <= rs_all[rq] + 7]
                        if not val:
                            continue
                        lo, hi = val[0], val[-1] + 1
                        assert val == list(range(lo, hi))
                        m0 = lo + 7 - kr + r0
                        assert 0 <= m0 and m0 + (hi - lo) <= 15
                        items.append(("k", kr - klo, lo * 64, hi * 64, m0))
                items.append(("c", 1, 0, n, None))
                for i, (kind, idx, c0, c1, m0) in enumerate(items):
                    ps = psr.next()
                    first, last = i == 0, i == len(items) - 1
                    if kind == "c":
                        fw.op("pe", lambda ps=ps, idx=idx: nc.tensor.matmul(ps[:, c0:c1], lhsT=kc[:, idx * 128:(idx + 1) * 128], rhs=q[0:64, c0:c1], start=True, stop=True),
                              reads=[kc, q], writes=[ps])
                        e = self.exp_block(ps, 128, c0, c1, er)
                        fw.op("pe", lambda e=e, idx=idx, first=first, last=last: nc.tensor.matmul(po[0:65, c0:c1], lhsT=vc[:, idx, :], rhs=e[:, c0:c1], start=first, stop=last),
                              reads=[e, vc], writes=[po], inc=last)
                    else:
                        fw.op("pe", lambda ps=ps, idx=idx, c0=c0, c1=c1: nc.tensor.matmul(ps[0:64, c0:c1], lhsT=kT[:, idx * 64:(idx + 1) * 64], rhs=q[0:64, c0:c1], start=True, stop=True),
                              reads=[kT, q], writes=[ps])
                        nrow = (c1 - c0) // 64
                        e = self.exp_block(ps, 64, c0, c1, er, mask=([etz[h]], etz[h][:, m0 * 64:(m0 + nrow) * 64]), mring=mr)
                        fw.op("pe", lambda e=e, idx=idx, c0=c0, c1=c1: nc.tensor.matmul(po[0:65, c0:c1], lhsT=va[:, idx, :], rhs=e[0:64, c0:c1], start=False, stop=False),
                              reads=[e, va], writes=[po], inc=False)
                self.attn_finish(po, n, self.scr["OT"][h * 64:(h + 1) * 64, t0:t0 + n], fr)

    @_stage
    def stage_gla(self, l):
        fw, nc = self.fw, self.nc
        T, NT = self.T, self.NT
        PT, VT, OT, OF = self.scr["PT"], self.scr["VT"], self.scr["OT"], self.scr["OF"]
        tri = fw.sb([128, 4, 128], F32, "tri")
        fw.dma("sp", tri[:], self.din["gla_tri"].rearrange("g a b -> a g b"), writes=[tri])
        maskg = fw.sb([128, 2, 512], BF16, "maskg")
        fw.dma("pool", maskg[:], self.din["gla_mask"].rearrange("d j h i -> j d (h i)"), writes=[maskg])
        wg = fw.sb([32, 2, 128], BF16, "wg")
        for d, (wn, bn) in enumerate((("gla_w_gf", "gla_b_gf"), ("gla_w_gb", "gla_b_gb"))):
            fw.dma("pool", wg[0:16, d, :], self.din[wn][l], writes=[wg])
            fw.dma("pool", wg[16:17, d, :], self.din[bn][l:l + 1, :], writes=[wg])
        gn = fw.sb([64, 1], F32, "gn")
        fw.dma("sp", gn[:], self.din["gla_g_norm"][l], writes=[gn])
        onesb = fw.sb([64, 64], BF16, "onesb")
        fw.op("dve", lambda: nc.vector.memset(onesb[:], 1.0), writes=[onesb])
        zr = Ring([fw.sb([32, 128], BF16, "zaug") for _ in range(3)])
        for b in zr.bufs:
            fw.op("dve", lambda b=b: nc.vector.memset(b[:], 1.0), writes=[b])
        qkr = Ring([fw.sb([32, 2, 4, 128], BF16, "qk") for _ in range(3)])
        ktr = Ring([fw.sb([128, 128], BF16, "ktok") for _ in range(3)])
        vtr = Ring([fw.sb([128, 4, 64], BF16, "vtok") for _ in range(3)])
        exr = Ring([fw.sb([128, 128], F32, "ex") for _ in range(2)])
        Lr = Ring([fw.sb([128, 128], F32, "L") for _ in range(2)])
        ebr = Ring([fw.sb([32, 2, 512], F32, "eb") for _ in range(2)])
        esr = Ring([fw.sb([128, 128], F32, "es") for _ in range(2)])
        qdr = Ring([fw.sb([32, 2, 512], BF16, "qd") for _ in range(2)])
        ker = Ring([fw.sb([128, 2, 128], BF16, "kend") for _ in range(2)])
        cm = fw.sb([128, 2], F32, "cm")
        fw.op("dve", lambda: nc.vector.memset(cm[:], 0.0), writes=[cm])
        fw.op("dve", lambda: nc.vector.memset(cm[0:64, 0:1], 1.0), writes=[cm])
        fw.op("dve", lambda: nc.vector.memset(cm[64:128, 1:2], 1.0), writes=[cm])
        amr = Ring([fw.sb([128, 512], BF16, "am") for _ in range(2)])
        Sr = Ring([fw.sb([32, 4, 64], F32, "S") for _ in range(4)])
        Sbr = Ring([fw.sb([32, 4, 64], BF16, "Sb") for _ in range(4)])
        ofr = Ring([fw.sb([64, 512], F32, "of") for _ in range(2)])
        osr = Ring([fw.sb([64, 512], F32, "os") for _ in range(2)])
        sqr = Ring([fw.sb([64, 512], BF16, "sq") for _ in range(2)])
        rsr = Ring([fw.sb([64, 512], F32, "rs") for _ in range(2)])
        rtr = Ring([fw.sb([64, 512], BF16, "rT") for _ in range(2)])
        srr = Ring([fw.sb([64, 512], F32, "sr") for _ in range(2)])
        fnr = Ring([fw.sb([64, 512], BF16, "fin") for _ in range(2)])
        pz = fw.ps([128, 128], F32, "pz")
        pbT = fw.ps([32, 512], F32, "pbT")
        pbs = fw.ps([128, 128], F32, "pbs")
        pa = fw.ps([128, 512], F32, "pa")
        pu = fw.ps([32, 2, 4, 64], F32, "pu")
        po = fw.ps([64, 512], F32, "po")
        pss = fw.ps([64, 512], F32, "pss")
        nl, nt = T // 128, NT // 128
        orders = [[nt - 2, nt - 1] + list(range(nl)), [nt - 1, nt - 2] + list(range(nl - 1, -1, -1))]
        for d in (0, 1):
            if d == 1:
                fw.barrier()
            zrow = O_ZF if d == 0 else O_ZB
            S = Sr.next()
            fw.op("dve", lambda: nc.vector.memset(S[:], 0.0), writes=[S])
            for tile in orders[d]:
                tok = tile * 128
                za = zr.next()
                fw.dma("sp", za[0:16, :], PT[zrow:zrow + 16, tok:tok + 128], writes=[za])
                qk = qkr.next()
                fw.dma("sp", qk[:, 0, :, :], PT[O_DQ:O_DQ + 128, tok:tok + 128].rearrange("(h d) t -> d h t", d=32), writes=[qk])
                fw.dma("sp", qk[:, 1, :, :], PT[O_DK:O_DK + 128, tok:tok + 128].rearrange("(h d) t -> d h t", d=32), writes=[qk])
                ktok = ktr.next()
                fw.dma("sp", ktok[:], VT[tok:tok + 128, 384:512], writes=[ktok])
                vtok = vtr.next()
                fw.dma("sp", vtok[:], VT[tok:tok + 128, 512:768].rearrange("p (h d) -> p h d", h=4), writes=[vtok])
                if getattr(self, 'gla_cut', 99) <= 1:
                    continue
                fw.op("pe", lambda: nc.tensor.matmul(pz[:, :], lhsT=za[0:17, :], rhs=wg[0:17, d, :], start=True, stop=True), reads=[za, wg], writes=[pz])
                ex = exr.next()
                fw.op("act", lambda: nc.scalar.activation(out=ex[:], in_=pz[:], func=AF.Exp, scale=-1.0), reads=[pz], writes=[ex])
                L = Lr.next()
                fw.op("act", lambda: nc.scalar.activation(out=L[:], in_=ex[:], func=AF.Ln, bias=self.ones[:, 0:1], scale=1.0), reads=[ex, self.ones], writes=[L])
                if getattr(self, 'gla_cut', 99) <= 2:
                    continue
                for h in range(4):
                    fw.op("pe", lambda h=h: nc.tensor.matmul(pbT[0:32, h * 128:(h + 1) * 128], lhsT=L[:, h * 32:(h + 1) * 32], rhs=tri[:, d, :], start=True, stop=True),
                          reads=[L, tri], writes=[pbT], inc=(h == 3))
                fw.op("pe", lambda: nc.tensor.matmul(pbs[:, :], lhsT=tri[:, 2 + d, :], rhs=L[:], start=True, stop=True), reads=[L, tri], writes=[pbs])
                eb = ebr.next()
                fw.op("act", lambda: nc.scalar.activation(out=eb[:, 0, :], in_=pbT[0:32, :], func=AF.Exp), reads=[pbT], writes=[eb])
                fw.op("act", lambda: nc.scalar.activation(out=eb[:, 1, :], in_=pbT[0:32, :], func=AF.Exp, scale=-1.0), reads=[pbT], writes=[eb])
                es = esr.next()
                fw.op("act", lambda: nc.scalar.activation(out=es[:], in_=pbs[:], func=AF.Exp), reads=[pbs], writes=[es])
                if getattr(self, 'gla_cut', 99) <= 3:
                    continue
                qd = qdr.next()
                fw.op("dve", lambda: nc.vector.scalar_tensor_tensor(out=qd[:, 0, :], in0=qk[:, 0, :, :].rearrange("p h t -> p (h t)"), scalar=32 ** -0.5,
                                                                     in1=eb[:, 0, :], op0=ALU.mult, op1=ALU.mult), reads=[qk, eb], writes=[qd])
                fw.op("dve", lambda: nc.vector.tensor_tensor(out=qd[:, 1, :], in0=qk[:, 1, :, :].rearrange("p h t -> p (h t)"), in1=eb[:, 1, :], op=ALU.mult),
                      reads=[qk, eb], writes=[qd])
                kend = ker.next()
                for cc in range(2):
                    fw.op("dve", lambda cc=cc: nc.vector.scalar_tensor_tensor(out=kend[:, cc, :], in0=ktok[:], scalar=cm[:, cc:cc + 1], in1=es[:],
                                                                               op0=ALU.mult, op1=ALU.mult), reads=[ktok, es, cm], writes=[kend])
                if getattr(self, 'gla_cut', 99) <= 4:
                    continue
                for h in range(4):
                    fw.op("pe", lambda h=h: nc.tensor.matmul(pa[:, h * 128:(h + 1) * 128], lhsT=qd[:, 1, h * 128:(h + 1) * 128], rhs=qd[:, 0, h * 128:(h + 1) * 128], start=True, stop=True),
                          reads=[qd], writes=[pa], inc=(h == 3))
                am = amr.next()
                fw.op("dve", lambda: nc.vector.tensor_tensor(out=am[:], in0=pa[:], in1=maskg[:, d, :], op=ALU.mult), reads=[pa, maskg], writes=[am])
                if getattr(self, 'gla_cut', 99) <= 5:
                    continue
                for cc in range(2):
                    for h in range(4):
                        fw.op("pe", lambda cc=cc, h=h: nc.tensor.matmul(pu[0:32, cc, h, :], lhsT=kend[:, cc, h * 32:(h + 1) * 32],
                                                                         rhs=vtok[:, h, :], start=True, stop=True),
                              reads=[kend, vtok], writes=[pu], inc=(cc == 1 and h == 3))
                if getattr(self, 'gla_cut', 99) <= 6:
                    continue
                Sb = {}
                for cc in ((0, 1) if d == 0 else (1, 0)):
                    sb_ = Sbr.next()
                    fw.op("act", lambda sb_=sb_, S=S: nc.scalar.copy(out=sb_[:], in_=S[:]), reads=[S], writes=[sb_])
                    Sb[cc] = sb_
                    S2 = Sr.next()
                    tidx = cc * 64 + (63 if d == 0 else 0)
                    for h in range(4):
                        fw.op("dve", lambda h=h, S=S, S2=S2, cc=cc, tidx=tidx: nc.vector.scalar_tensor_tensor(
                            out=S2[:, h, :], in0=S[:, h, :], scalar=eb[:, 0, h * 128 + tidx:h * 128 + tidx + 1], in1=pu[0:32, cc, h, :],
                            op0=ALU.mult, op1=ALU.add), reads=[S, eb, pu], writes=[S2])
                    S = S2
                if getattr(self, 'gla_cut', 99) <= 7:
                    continue
                for h in range(4):
                    fw.op("pe", lambda h=h: nc.tensor.matmul(po[0:64, h * 128:(h + 1) * 128], lhsT=vtok[:, h, :], rhs=am[:, h * 128:(h + 1) * 128], start=True, stop=False),
                          reads=[vtok, am], writes=[po], inc=False)
                    for cc in range(2):
                        fw.op("pe", lambda h=h, cc=cc: nc.tensor.matmul(po[0:64, h * 128 + cc * 64:h * 128 + (cc + 1) * 64], lhsT=Sb[cc][:, h, :],
                                                                         rhs=qd[:, 0, h * 128 + cc * 64:h * 128 + (cc + 1) * 64], start=False, stop=(cc == 1)),
                              reads=[Sb[cc], qd], writes=[po], inc=(cc == 1))
                if getattr(self, 'gla_cut', 99) <= 8:
                    continue
                if d == 0:
                    of = ofr.next()
                    fw.op("act", lambda: nc.scalar.copy(out=of[:], in_=po[:]), reads=[po], writes=[of])
                    fw.dma("pool", OF[:, :, tok:tok + 128], of[:].rearrange("p (h t) -> p h t", h=4), reads=[of])
                else:
                    of = ofr.next()
                    fw.dma("sp", of[:].rearrange("p (h t) -> p h t", h=4), OF[:, :, tok:tok + 128], writes=[of])
                    rT = rtr.next()
                    fw.dma("sp", rT[:].rearrange("p (h t) -> p h t", h=4), PT[O_DR:O_DR + 256, tok:tok + 128].rearrange("(h d) t -> d h t", d=64), writes=[rT])
                    os_ = osr.next()
                    fw.op("dve", lambda: nc.vector.tensor_tensor(out=os_[:], in0=po[:], in1=of[:], op=ALU.add), reads=[po, of], writes=[os_])
                    sq = sqr.next()
                    fw.op("pool", lambda: nc.gpsimd.tensor_tensor(out=sq[:], in0=os_[:], in1=os_[:], op=ALU.mult), reads=[os_], writes=[sq])
                    fw.op("pe", lambda: nc.tensor.matmul(pss[:, :], lhsT=onesb[:], rhs=sq[:], start=True, stop=True), reads=[sq, onesb], writes=[pss])
                    rs = rsr.next()
                    fw.op("act", lambda: nc.scalar.activation(out=rs[:], in_=pss[:], func=AF.Sqrt, bias=self.epsln[0:64, 1:2], scale=1.0 / 64),
                          reads=[pss, self.epsln], writes=[rs])
                    fw.op("dve", lambda: nc.vector.reciprocal(out=rs[:], in_=rs[:]), reads=[rs], writes=[rs])
                    sr = srr.next()
                    fw.op("act", lambda: nc.scalar.activation(out=sr[:], in_=rT[:], func=AF.Silu), reads=[rT], writes=[sr])
                    fw.op("dve", lambda: nc.vector.scalar_tensor_tensor(out=os_[:], in0=os_[:], scalar=gn[:, 0:1], in1=rs[:], op0=ALU.mult, op1=ALU.mult),
                          reads=[os_, gn, rs], writes=[os_])
                    fin = fnr.next()
                    fw.op("dve", lambda: nc.vector.tensor_tensor(out=fin[:], in0=os_[:], in1=sr[:], op=ALU.mult), reads=[os_, sr], writes=[fin])
                    fw.dma("pool", OT[768:1024, tok:tok + 128].rearrange("(h d) t -> d h t", d=64), fin[:].rearrange("p (h t) -> p h t", h=4), reads=[fin])


class KB(KA):
    def bcast_mod(self, j0, dst_lat, dst_ctx, pg, repr_):
        fw, nc = self.fw, self.nc
        for wh, dst in ((0, dst_lat), (1, dst_ctx)):
            for jj in range(8):
                rep = repr_.next()
                fw.op("act", lambda rep=rep, jj=jj, wh=wh: nc.scalar.activation(out=rep[:], in_=self.ones[:], func=AF.Copy,
                                                                                 scale=self.modv[:, j0 + jj, wh:wh + 1]),
                      reads=[self.ones, self.modv], writes=[rep])
                fw.op("pe", lambda rep=rep, jj=jj: nc.tensor.matmul(pg[:, jj // 4, (jj % 4) * 128:(jj % 4 + 1) * 128], lhsT=rep[:], rhs=self.ident[:],
                                                                    start=True, stop=True), reads=[rep, self.ident], writes=[pg])
            fw.op("dve", lambda dst=dst: nc.vector.tensor_copy(out=dst[:].rearrange("p (a b) -> p a b", a=2), in_=pg[:]), reads=[pg], writes=[dst])

    def load_ln(self, l, which):
        fw = self.fw
        g = fw.sb([128, D], F32, "lng"); b = fw.sb([128, D], F32, "lnb")
        fw.dma("sp", g[:], self.din[which + "_g"][l].partition_broadcast(128), writes=[g])
        fw.dma("sp", b[:], self.din[which + "_b"][l].partition_broadcast(128), writes=[b])
        return g, b

    def ln_stats(self, z, s):
        fw, nc = self.fw, self.nc
        for hh in range(2):
            fw.op("dve", lambda hh=hh: nc.vector.bn_stats(out=s[:, hh * 6:hh * 6 + 6], in_=z[:, hh * 512:(hh + 1) * 512]), reads=[z], writes=[s])
        fw.op("dve", lambda: nc.vector.bn_aggr(out=s[:, 12:14], in_=s[:, 0:12]), reads=[s], writes=[s])
        fw.op("act", lambda: nc.scalar.activation(out=s[:, 15:16], in_=s[:, 13:14], func=AF.Sqrt, bias=self.epsln[:, 0:1], scale=1.0),
              reads=[s, self.epsln], writes=[s])
        fw.op("dve", lambda: nc.vector.reciprocal(out=s[:, 14:15], in_=s[:, 15:16]), reads=[s], writes=[s])

    def post_norm_store(self, z, s, g, b, dst):
        fw, nc = self.fw, self.nc
        self.ln_stats(z, s)
        fw.op("dve", lambda: nc.vector.tensor_scalar(out=z[:], in0=z[:], scalar1=s[:, 12:13], scalar2=s[:, 14:15], op0=ALU.subtract, op1=ALU.mult),
              reads=[z, s], writes=[z])
        fw.op("pool", lambda: nc.gpsimd.tensor_tensor(out=z[:], in0=z[:], in1=g[:], op=ALU.mult), reads=[z, g], writes=[z])
        fw.op("pool", lambda: nc.gpsimd.tensor_tensor(out=z[:], in0=z[:], in1=b[:], op=ALU.add), reads=[z, b], writes=[z])
        fw.dma("pool", dst, z[:], reads=[z])

    def xsrc(self, l, tok, first):
        if l == 0 and first:
            return self.din["x"][tok:tok + 128, :] if tok < self.T else self.din["ctx"][tok - self.T:tok - self.T + 128, :]
        return self.scr["XR"][tok:tok + 128, :]

    @_stage
    def stage_outproj(self, l, last):
        fw, nc = self.fw, self.nc
        T, NT = self.T, self.NT
        g_lat = fw.sb([128, D], F32, "glat"); g_ctx = fw.sb([128, D], F32, "gctx")
        pg = fw.ps([128, 2, 512], F32, "pg")
        repr_ = Ring([fw.sb([128, 128], F32, "rep") for _ in range(2)])
        self.bcast_mod(16, g_lat, g_ctx, pg, repr_)
        lg, lb = self.load_ln(l, "ln1")
        wout = fw.sb([128, 8, D], BF16, "wout")
        wsrc = self.din["w_out"][l].rearrange("(k p) n -> p k n", p=128)
        for k in range(8):
            fw.dma("pool", wout[:, k, :], wsrc[:, k, :], writes=[wout])
        otr = Ring([fw.sb([128, 8, 128], BF16, "oT") for _ in range(3)])
        xr = Ring([fw.sb([128, D], F32, "x") for _ in range(3)])
        zr = Ring([fw.sb([128, D], F32, "z") for _ in range(3)])
        sr = Ring([fw.sb([128, 16], F32, "st") for _ in range(3)])
        pyr = Ring([fw.ps([128, 2, 512], F32, "py") for _ in range(2)])
        ntile = (T if last else NT) // 128
        for ti in range(ntile):
            tok = ti * 128
            oT = otr.next()
            fw.dma("sp", oT[:], self.scr["OT"][:, tok:tok + 128].rearrange("(k p) t -> p k t", p=128), writes=[oT])
            x = xr.next()
            fw.dma("sp", x[:], self.xsrc(l, tok, True), writes=[x])
            py = pyr.next()
            for half in range(2):
                for k in range(8):
                    fw.op("pe", lambda half=half, k=k: nc.tensor.matmul(py[:, half, :], lhsT=oT[:, k, :], rhs=wout[:, k, half * 512:(half + 1) * 512],
                                                                         start=(k == 0), stop=(k == 7)), reads=[oT, wout], writes=[py], inc=(k == 7))
            gb = g_lat if tok < T else g_ctx
            z = zr.next()
            fw.op("dve", lambda: nc.vector.tensor_tensor(out=z[:].rearrange("p (a b) -> p a b", a=2), in0=py[:], in1=gb[:].rearrange("p (a b) -> p a b", a=2), op=ALU.mult),
                  reads=[py, gb], writes=[z])
            fw.op("dve", lambda: nc.vector.scalar_tensor_tensor(out=z[:], in0=x[:], scalar=ALPHA, in1=z[:], op0=ALU.mult, op1=ALU.add),
                  reads=[x, z], writes=[z])
            self.post_norm_store(z, sr.next(), lg, lb, self.scr["XR"][tok:tok + 128, :])

    @_stage
    def stage_router(self, l, last):
        fw, nc = self.fw, self.nc
        T, NT = self.T, self.NT
        rw = fw.sb([128, 8, NE], F32, "rw")
        fw.dma("sp", rw[:], self.din["router_w"][l].rearrange("(k p) e -> p k e", p=128), writes=[rw])
        rb = fw.sb([128, NE], F32, "rb")
        fw.dma("sp", rb[:], self.din["router_bias"][l].partition_broadcast(128), writes=[rb])
        xr = Ring([fw.sb([128, D], F32, "x") for _ in range(3)])
        xnr = Ring([fw.sb([128, D], F32, "xn") for _ in range(2)])
        sr = Ring([fw.sb([128, 16], F32, "st") for _ in range(3)])
        hbr = Ring([fw.sb([128, 8, 128], BF16, "hb") for _ in range(3)])
        hfr = Ring([fw.sb([128, 8, 128], F32, "hf") for _ in range(2)])
        scr_ = Ring([fw.sb([128, NE], F32, "sc") for _ in range(2)])
        bir = Ring([fw.sb([128, NE], F32, "bi") for _ in range(2)])
        m8r = Ring([fw.sb([128, 16], F32, "m8") for _ in range(2)])
        gtr = Ring([fw.sb([128, NE + 1], F32, "gt") for _ in range(3)])
        tpr = Ring([fw.ps([128, 8, 128], F32, "tp") for _ in range(2)])
        plr = Ring([fw.ps([128, NE], F32, "pl") for _ in range(2)])
        ntile = (T if last else NT) // 128
        for ti in range(ntile):
            tok = ti * 128
            wh = 0 if tok < T else 1
            x = xr.next()
            fw.dma("sp", x[:], self.scr["XR"][tok:tok + 128, :], writes=[x])
            s = sr.next()
            self.ln_stats(x, s)
            xn = xnr.next()
            fw.op("dve", lambda: nc.vector.tensor_scalar(out=xn[:], in0=x[:], scalar1=s[:, 12:13], scalar2=s[:, 14:15], op0=ALU.subtract, op1=ALU.mult),
                  reads=[x, s], writes=[xn])
            tp = tpr.next()
            for k in range(8):
                fw.op("pe", lambda k=k: nc.tensor.transpose(out=tp[:, k, :], in_=xn[:, k * 128:(k + 1) * 128], identity=self.ident[:]),
                      reads=[xn, self.ident], writes=[tp], inc=(k == 7))
            hb, hf = hbr.next(), hfr.next()
            for k in range(8):
                sc_ = self.modv[:, 32 + k, wh:wh + 1]
                sh_ = self.modv[:, 24 + k, wh:wh + 1]
                fw.op("act", lambda k=k, sc_=sc_, sh_=sh_: nc.scalar.activation(out=hf[:, k, :], in_=tp[:, k, :], func=AF.Identity, bias=sh_, scale=sc_),
                      reads=[tp, self.modv], writes=[hf])
            fw.op("dve", lambda: nc.vector.tensor_copy(out=hb[:], in_=hf[:]), reads=[hf], writes=[hb])
            fw.dma("pool", self.scr["HT"][:, tok:tok + 128].rearrange("(k p) t -> p k t", p=128), hb[:], reads=[hb])
            pl = plr.next()
            for k in range(8):
                fw.op("pe", lambda k=k: nc.tensor.matmul(pl[:, :], lhsT=hf[:, k, :], rhs=rw[:, k, :], start=(k == 0), stop=(k == 7)),
                      reads=[hf, rw], writes=[pl], inc=(k == 7))
            sc = scr_.next()
            fw.op("act", lambda: nc.scalar.activation(out=sc[:], in_=pl[:], func=AF.Sigmoid), reads=[pl], writes=[sc])
            bi = bir.next()
            fw.op("dve", lambda: nc.vector.tensor_tensor(out=bi[:], in0=sc[:], in1=rb[:], op=ALU.add), reads=[sc, rb], writes=[bi])
            m8 = m8r.next()
            fw.op("dve", lambda: nc.vector.max(out=m8[:, 0:8], in_=bi[:]), reads=[bi], writes=[m8])
            fw.op("dve", lambda: nc.vector.tensor_reduce(out=m8[:, 8:9], in_=m8[:, 0:8], axis=AX.X, op=ALU.min), reads=[m8], writes=[m8])
            fw.op("dve", lambda: nc.vector.tensor_scalar(out=bi[:], in0=bi[:], scalar1=m8[:, 8:9], scalar2=None, op0=ALU.is_ge), reads=[bi, m8], writes=[bi])
            fw.op("dve", lambda: nc.vector.tensor_tensor(out=sc[:], in0=sc[:], in1=bi[:], op=ALU.mult), reads=[sc, bi], writes=[sc])
            fw.op("dve", lambda: nc.vector.reduce_sum(out=m8[:, 9:10], in_=sc[:], axis=AX.X), reads=[sc], writes=[m8])
            fw.op("dve", lambda: nc.vector.reciprocal(out=m8[:, 10:11], in_=m8[:, 9:10]), reads=[m8], writes=[m8])
            gt = gtr.next()
            fw.op("dve", lambda: nc.vector.tensor_scalar(out=gt[:, 0:NE], in0=sc[:], scalar1=m8[:, 10:11], scalar2=2.5, op0=ALU.mult, op1=ALU.mult),
                  reads=[sc, m8], writes=[gt])
            fw.op("pool", lambda: nc.gpsimd.memset(gt[:, NE:NE + 1], 1.0), writes=[gt])
            fw.dma("pool", self.scr["GT"][ti], gt[:], reads=[gt])

    @_stage
    def stage_experts(self, l, last, G=16):
        fw, nc = self.fw, self.nc
        T, NT = self.T, self.NT
        g_lat = fw.sb([128, D], F32, "glat"); g_ctx = fw.sb([128, D], F32, "gctx")
        pg = fw.ps([128, 2, 512], F32, "pg")
        repr_ = Ring([fw.sb([128, 128], F32, "rep") for _ in range(2)])
        self.bcast_mod(40, g_lat, g_ctx, pg, repr_)
        lg, lb = self.load_ln(l, "ln2")
        ntile = (T if last else NT) // 128
        acc = fw.sb([128, G, D], F32, "acc")
        hT = fw.sb([128, 8, G * 128], BF16, "hTg")
        gts = fw.sb([128, G, NE + 1], F32, "gts")
        w1r = Ring([fw.sb([128, 8, 256], BF16, "w1") for _ in range(2)])
        w3r = Ring([fw.sb([128, 8, 256], BF16, "w3") for _ in range(2)])
        w2r = Ring([fw.sb([128, 2, D], BF16, "w2") for _ in range(2)])
        sar = Ring([fw.sb([128, 2, 512], BF16, "sa") for _ in range(2)])
        hdr = Ring([fw.sb([128, 2, 512], BF16, "hd") for _ in range(2)])
        xr = Ring([fw.sb([128, D], F32, "x") for _ in range(2)])
        sr = Ring([fw.sb([128, 16], F32, "st") for _ in range(2)])
        par = Ring([fw.ps([128, 2, 512], F32, "pa") for _ in range(1)])
        pbr = Ring([fw.ps([128, 2, 512], F32, "pb3") for _ in range(1)])
        pyr = Ring([fw.ps([128, 512], F32, "py") for _ in range(2)])
        dn = self.din
        for g0 in range(0, ntile, G):
            gn = min(G, ntile - g0)
            tok0 = g0 * 128
            for k in range(8):
                fw.dma("sp", hT[:, k, 0:gn * 128], self.scr["HT"][k * 128:(k + 1) * 128, tok0:tok0 + gn * 128], writes=[hT])
            fw.dma("sp", gts[:, 0:gn, :], self.scr["GT"][g0:g0 + gn].rearrange("g p e -> p g e"), writes=[gts])
            for e in range(NE + 1):
                w1, w3, w2 = w1r.next(), w3r.next(), w2r.next()
                if e < NE:
                    s1, s3, s2 = dn["exp_w1"][l, e], dn["exp_w3"][l, e], dn["exp_w2"][l, e]
                else:
                    s1, s3, s2 = dn["sh_w1"][l], dn["sh_w3"][l], dn["sh_w2"][l]
                fw.dma("pool", w1[:], s1.rearrange("(k p) f -> p k f", p=128), writes=[w1])
                fw.dma("pool", w3[:], s3.rearrange("(k p) f -> p k f", p=128), writes=[w3])
                fw.dma("pool", w2[:], s2.rearrange("(k p) n -> p k n", p=128), writes=[w2])
                for c0 in range(0, gn, 4):
                    cn_ = min(4, gn - c0)
                    n = cn_ * 128
                    pa, pb = par.next(), pbr.next()
                    for (w, p) in ((w1, pa), (w3, pb)):
                        for hc in range(2):
                            for k in range(8):
                                fw.op("pe", lambda w=w, p=p, hc=hc, k=k: nc.tensor.matmul(p[:, hc, 0:n], lhsT=w[:, k, hc * 128:(hc + 1) * 128],
                                                                                         rhs=hT[:, k, c0 * 128:c0 * 128 + n], start=(k == 0), stop=(k == 7)),
                                      reads=[w, hT], writes=[p], inc=(k == 7))
                    sa, hd = sar.next(), hdr.next()
                    for hc in range(2):
                        fw.op("act", lambda hc=hc: nc.scalar.activation(out=sa[:, hc, 0:n], in_=pa[:, hc, 0:n], func=AF.Silu), reads=[pa], writes=[sa])
                        fw.op("dve", lambda hc=hc: nc.vector.tensor_tensor(out=hd[:, hc, 0:n], in0=sa[:, hc, 0:n], in1=pb[:, hc, 0:n], op=ALU.mult),
                              reads=[sa, pb], writes=[hd])
                    for t in range(cn_):
                        gi = c0 + t
                        for half in range(2):
                            py = pyr.next()
                            for hc in range(2):
                                fw.op("pe", lambda t=t, half=half, hc=hc, py=py: nc.tensor.matmul(py[:, :], lhsT=hd[:, hc, t * 128:(t + 1) * 128],
                                                                                                 rhs=w2[:, hc, half * 512:(half + 1) * 512], start=(hc == 0), stop=(hc == 1)),
                                      reads=[hd, w2], writes=[py], inc=(hc == 1))
                            a = acc[:, gi, half * 512:(half + 1) * 512]
                            gsc = gts[:, gi, e:e + 1]
                            if e == 0:
                                fw.op("dve", lambda a=a, py=py, gsc=gsc: nc.vector.tensor_scalar(out=a, in0=py[:], scalar1=gsc, scalar2=None, op0=ALU.mult),
                                      reads=[py, gts], writes=[acc])
                            else:
                                fw.op("dve", lambda a=a, py=py, gsc=gsc: nc.vector.scalar_tensor_tensor(out=a, in0=py[:], scalar=gsc, in1=a, op0=ALU.mult, op1=ALU.add),
                                      reads=[py, gts, acc], writes=[acc])
            for gi in range(gn):
                tok = tok0 + gi * 128
                x = xr.next()
                fw.dma("sp", x[:], self.scr["XR"][tok:tok + 128, :], writes=[x])
                gb = g_lat if tok < T else g_ctx
                fw.op("dve", lambda gi=gi, gb=gb: nc.vector.tensor_tensor(out=acc[:, gi, :], in0=acc[:, gi, :], in1=gb[:], op=ALU.mult), reads=[acc, gb], writes=[acc])
                fw.op("dve", lambda gi=gi, x=x: nc.vector.scalar_tensor_tensor(out=x[:], in0=x[:], scalar=ALPHA, in1=acc[:, gi, :], op0=ALU.mult, op1=ALU.add),
                      reads=[x, acc], writes=[x])
                if last:
                    dst = self.out[tok:tok + 128, :]
                else:
                    dst = self.scr["XR"][tok:tok + 128, :]
                self.post_norm_store(x, sr.next(), lg, lb, dst)

    def build(self, upto=None):
        from contextlib import ExitStack
        self.declare()
        names = ["mod", "inproj", "wa_prep", "mla_prep", "na_attn", "wa_attn", "mla_attn", "gla", "outproj", "router", "experts"]
        cnt = 0
        with ExitStack() as gst:
            self.setup_global(gst)
            self.fw.barrier()
            for l in range(self.L):
                last = l == self.L - 1
                for nm in names:
                    if upto is not None and cnt >= upto:
                        break
                    cnt += 1
                    fn = getattr(self, "stage_" + nm)
                    if nm in ("na_attn", "wa_attn", "mla_attn"):
                        fn(l, not last)
                    elif nm in ("outproj", "router", "experts"):
                        fn(l, last)
                    else:
                        fn(l)
            self.fw.barrier()
        return self.nc


def prep_shared(inp, T, L):
    f = lambda a: np.ascontiguousarray(np.asarray(a, dtype=np.float32))
    m = dict(host_consts(T))
    m["w_ada"] = f(inp["w_ada"][:L])
    m["b_ada"] = f(inp["b_ada"][:L])
    m["b_ada_pj"] = f(np.asarray(inp["b_ada"][:L]).reshape(L, 48, 128).transpose(0, 2, 1))
    m["w_in"] = f(inp["w_in"][:L])
    m["na_rpb"] = f(inp["na_rpb"][:L]); m["wa_sink"] = f(inp["wa_sink"][:L])
    m["mla_g_q"] = f(np.asarray(inp["mla_g_q"][:L]).reshape(L, 2, 128).transpose(0, 2, 1))
    m["mla_g_kv"] = f(np.asarray(inp["mla_g_kv"][:L]).reshape(L, 128, 1))
    m["mla_w_uq"] = f(inp["mla_w_uq"][:L]); m["mla_w_ukv"] = f(inp["mla_w_ukv"][:L])
    for k in ("gla_w_gf", "gla_b_gf", "gla_w_gb", "gla_b_gb"):
        m[k] = f(inp[k][:L])
    m["gla_g_norm"] = f(np.asarray(inp["gla_g_norm"][:L]).reshape(L, 64, 1))
    for k in ("w_out", "ln1_g", "ln1_b", "ln2_g", "ln2_b", "router_w", "router_bias",
              "exp_w1", "exp_w3", "exp_w2", "sh_w1", "sh_w3", "sh_w2"):
        m[k] = f(inp[k][:L])
    return m


def prep_core(inp, b):
    f = lambda a: np.ascontiguousarray(np.asarray(a, dtype=np.float32))
    c = np.asarray(inp["c"][b], dtype=np.float32)
    cc = np.asarray(inp["c_ctx"], dtype=np.float32)
    cv = np.stack([c.reshape(8, 128).T, cc.reshape(8, 128).T], axis=-1)
    return {"x": f(inp["x"][b]), "ctx": f(inp["ctx"][b]), "cv": f(cv)}


_CACHE = {}


def kernel(**inputs):
    B, T, _ = inputs["x"].shape
    L = inputs["w_ada"].shape[0]
    key = (T, L)
    if key not in _CACHE:
        _CACHE[key] = KB(T, L).build()
    nc = _CACHE[key]
    shared = prep_shared(inputs, T, L)
    in_maps = []
    for b in range(B):
        m = dict(shared)
        m.update(prep_core(inputs, b))
        in_maps.append(m)
    res = run_bass_kernel_spmd(nc, in_maps, core_ids=list(range(B)))
    return np.stack([np.asarray(res.results[b]["out"], dtype=np.float32) for b in range(B)], axis=0)
```

```python
import numpy as np
import ml_dtypes
import concourse.bass as bass
import concourse.mybir as mybir
from concourse.bass_utils import run_bass_kernel_spmd

F32 = mybir.dt.float32
BF16 = mybir.dt.bfloat16
I32 = mybir.dt.int32
AF = mybir.ActivationFunctionType
ALU = mybir.AluOpType
AX = mybir.AxisListType

D = 1024
C = 256
NE = 128
LN_EPS = 1e-5
RMS_EPS = 1e-6
ALPHA = 8 ** 0.25
IN_W = 2496
O_AQ, O_AK, O_AV = 0, 256, 512
O_BQ, O_BK, O_BV = 768, 1024, 1152
O_CQ, O_CKV, O_CKR = 1280, 1536, 1664
O_DQ, O_DK, O_DV, O_DR, O_ZF, O_ZB = 1696, 1824, 1952, 2208, 2464, 2480


class Buf:
    def __init__(self, t, name):
        self.t = t
        self.name = name
        self.w = None
        self.r = []

    def __getitem__(self, k):
        return self.t[k]


class FW:
    def __init__(self, nc):
        self.nc = nc
        self.eng = {"pe": nc.tensor, "act": nc.scalar, "dve": nc.vector, "pool": nc.gpsimd, "sp": nc.sync}
        self.sem = {}
        self.cnt = {}
        self.waited = {}
        for e in self.eng:
            self.sem[e] = nc.alloc_semaphore("s_" + e)
            self.cnt[e] = 0
            self.waited[e] = {}
        self.pend = {e: False for e in self.eng}
        self.dsem = {}
        self.dval = {}
        self.drr = {}
        for q in ("sp", "pool", "act"):
            self.dsem[q] = [nc.alloc_semaphore("d_%s%d" % (q, i)) for i in range(12)]
            self.dval[q] = [0] * 12
            self.drr[q] = 0
        self.semkey = {}
        self.nbuf = 0

    def sb(self, shape, dt, name=None):
        self.nbuf += 1
        name = (name or "t") + "_%d" % self.nbuf
        return Buf(self.stack.enter_context(self.nc.sbuf_tensor(name, list(shape), dt)), name)

    def ps(self, shape, dt=F32, name=None):
        self.nbuf += 1
        name = (name or "p") + "_%d" % self.nbuf
        return Buf(self.stack.enter_context(self.nc.psum_tensor(name, list(shape), dt)), name)

    def _wait(self, e, tok):
        if tok is None:
            return
        sem, val, key = tok
        if self.waited[e].get(key, 0) >= val:
            return
        self.eng[e].wait_ge(sem, val)
        self.waited[e][key] = val

    def _deps(self, e, reads, writes):
        for b in reads:
            if b.w is not None:
                if not (e == "pe" and b.w[2] == "pe"):
                    self._wait(e, b.w)
        for b in writes:
            if b.w is not None and b.w[2] != e:
                self._wait(e, b.w)
            for tok in b.r:
                if tok[2] != e:
                    self._wait(e, tok)

    def _mark(self, tok, reads, writes):
        for b in reads:
            b.r.append(tok)
            if len(b.r) > 24:
                last = {}
                for t in b.r:
                    if t[2] not in last or last[t[2]][1] < t[1]:
                        last[t[2]] = t
                b.r = list(last.values())
        for b in writes:
            b.w = tok
            b.r = []

    def op(self, e, fn, reads=(), writes=(), inc=True):
        self._deps(e, reads, writes)
        ins = fn()
        if inc:
            self.cnt[e] += 1
            ins.then_inc(self.sem[e], 1)
            self.pend[e] = False
            tok = (self.sem[e], self.cnt[e], e)
        else:
            self.pend[e] = True
            tok = (self.sem[e], self.cnt[e] + 1, e)
        self._mark(tok, reads, writes)
        return tok

    def dma(self, q, out, in_, reads=(), writes=()):
        self._deps(q, reads, writes)
        i = self.drr[q]
        self.drr[q] = (i + 1) % len(self.dsem[q])
        sem = self.dsem[q][i]
        key = "d_%s%d" % (q, i)
        if self.dval[q][i] > 0:
            self._wait(q, (sem, self.dval[q][i], key))
        self.dval[q][i] += 16
        self.eng[q].dma_start(out=out, in_=in_).then_inc(sem, 16)
        tok = (sem, self.dval[q][i], key)
        self._mark(tok, reads, writes)
        return tok

    def idma(self, out, out_off, in_, in_off, reads=(), writes=(), **kw):
        q = "pool"
        self._deps(q, reads, writes)
        i = self.drr[q]
        self.drr[q] = (i + 1) % len(self.dsem[q])
        sem = self.dsem[q][i]
        key = "d_%s%d" % (q, i)
        if self.dval[q][i] > 0:
            self._wait(q, (sem, self.dval[q][i], key))
        self.dval[q][i] += 16
        self.nc.gpsimd.indirect_dma_start(out=out, out_offset=out_off, in_=in_, in_offset=in_off, **kw).then_inc(sem, 16)
        tok = (sem, self.dval[q][i], key)
        self._mark(tok, reads, writes)
        return tok

    def barrier(self):
        for e in self.eng:
            assert not self.pend[e], "pending non-inc op on " + e
        toks = []
        for e in self.eng:
            if self.cnt[e] > 0:
                toks.append((self.sem[e], self.cnt[e], e))
        for q in self.dsem:
            for i, s in enumerate(self.dsem[q]):
                if self.dval[q][i] > 0:
                    toks.append((s, self.dval[q][i], "d_%s%d" % (q, i)))
        for e in self.eng:
            for t in toks:
                if t[2] != e:
                    self._wait(e, t)


class Ring:
    def __init__(self, bufs):
        self.bufs = bufs
        self.i = 0

    def next(self):
        b = self.bufs[self.i]
        self.i = (self.i + 1) % len(self.bufs)
        return b


def host_consts(T):
    cs = {}
    cs["ident"] = np.eye(128, dtype=np.float32)
    t = np.arange(T)
    row, col = (t // 64).astype(np.float32), (t % 64).astype(np.float32)

    def rope_tabs(d):
        half = d // 2
        inv = (10000.0 ** (-np.arange(half, dtype=np.float32) / half)).astype(np.float32)
        cos = np.zeros((2 * d, T), np.float32)
        sin = np.zeros((2 * d, T), np.float32)
        R = np.zeros((2 * d, 2 * d), np.float32)
        for a, pos in enumerate((row, col)):
            ang = pos[None, :] * inv[:, None]
            for hh in range(2):
                cos[a * d + hh * half:a * d + (hh + 1) * half] = np.cos(ang)
                sin[a * d + hh * half:a * d + (hh + 1) * half] = np.sin(ang)
            for i in range(half):
                R[a * d + i, a * d + half + i] = -1.0
                R[a * d + half + i, a * d + i] = 1.0
        return cos, sin, R

    cw, sw, Rw = rope_tabs(32)
    cs["cos_wa"], cs["sin_wa"], cs["rt_wa"] = cw, sw, np.ascontiguousarray(Rw.T)
    cm, sm, Rm = rope_tabs(16)
    cos_m = np.ones((64, T), np.float32); sin_m = np.zeros((64, T), np.float32); R64 = np.zeros((64, 64), np.float32)
    cos_m[32:], sin_m[32:], R64[32:, 32:] = cm, sm, Rm
    cs["cos_mla"], cs["sin_mla"], cs["rt_mla"] = cos_m, sin_m, np.ascontiguousarray(R64.T)
    kk = np.arange(128)[:, None]; qq = np.arange(512)[None, :]
    m = np.zeros((6, 128, 512), np.float32)
    for r in range(-1, 5):
        m[r + 1] = (np.abs(r * 128 + kk - qq) <= 128)
    cs["maskwa"] = m
    oh = np.zeros((31, 64, 64), np.float32)
    cmask = np.zeros((64, 15, 64), np.float32)
    for c_ in range(64):
        cs0 = min(max(c_ - 8, 0), 48)
        for kc in range(64):
            j = kc - c_ + 15
            if 0 <= j < 31:
                oh[j, c_, kc] = 1.0
            if cs0 <= kc < cs0 + 16:
                cmask[kc, :, c_] = 1.0
    cs["na_oh"] = oh
    cs["na_cmask"] = cmask
    cs["j15"] = np.ascontiguousarray(np.eye(15, dtype=np.float32)[::-1])
    a = np.arange(128)
    same = (a[:, None] // 64) == (a[None, :] // 64)
    le = a[:, None] <= a[None, :]
    lt = a[:, None] < a[None, :]
    g = np.zeros((4, 128, 128), np.float32)
    g[0] = same & le
    g[1] = same & le.T
    g[2] = same & lt.T
    g[3] = same & lt
    cs["gla_tri"] = (g * (-1.0 / 16.0)).astype(np.float32)
    mk = np.zeros((2, 128, 4, 128), np.float32)
    mk[0] = (same & le)[:, None, :]
    mk[1] = (same & le.T)[:, None, :]
    cs["gla_mask"] = mk
    NB = (T + C) // 128 * 8 + NE
    cs["moe_sl"] = np.triu(np.ones((128, 128), np.float32), 1)
    cs["moe_biota"] = np.tile((np.arange(NB, dtype=np.float32) * 128.0)[None, :], (128, 1))
    cs["moe_piota"] = np.arange(128, dtype=np.float32).reshape(128, 1)
    cs["moe_eiota"] = np.tile((128.0 - np.arange(128, dtype=np.float32))[None, :], (128, 1))
    return cs


class K:
    def __init__(self, T, L, debug=()):
        self.T, self.L = T, L
        self.NT = T + C
        self.debug = set(debug)
        nc = bass.Bass("TRN2", target_bir_lowering=False)
        self.nc = nc
        self.fw = FW(nc)
        self.din = {}
        self.scr = {}

    def inp(self, name, shape, dt=F32):
        self.din[name] = self.nc.dram_tensor(name, list(shape), dt, kind="ExternalInput").ap()
        return self.din[name]

    def scratch(self, name, shape, dt):
        kind = "ExternalOutput" if name in self.debug else "Internal"
        self.scr[name] = self.nc.dram_tensor(name, list(shape), dt, kind=kind).ap()
        return self.scr[name]

    def chunks(self, n=512):
        out = []
        t = 0
        while t < self.NT:
            m = min(n, self.NT - t)
            out.append((t, m))
            t += m
        return out

    def declare(self):
        T, L, NT = self.T, self.L, self.NT
        i = self.inp
        i("x", [T, D]); i("ctx", [C, D]); i("cv", [128, 8, 2])
        i("w_ada", [L, D, 6 * D]); i("b_ada_pj", [L, 128, 48]); i("b_ada", [L, 6 * D])
        i("w_in", [L, D, IN_W])
        i("ident", [128, 128])
        i("cos_wa", [64, T]); i("sin_wa", [64, T]); i("rt_wa", [64, 64])
        i("cos_mla", [64, T]); i("sin_mla", [64, T]); i("rt_mla", [64, 64])
        i("maskwa", [6, 128, 512]); i("na_oh", [31, 64, 64]); i("na_cmask", [64, 15, 64]); i("j15", [15, 15])
        i("gla_tri", [4, 128, 128]); i("gla_mask", [2, 128, 4, 128])
        self.NB = NT // 128 * 8 + NE
        i("moe_sl", [128, 128]); i("moe_biota", [128, self.NB]); i("moe_piota", [128, 1]); i("moe_eiota", [128, 128])
        i("na_rpb", [L, 4, 15, 31]); i("wa_sink", [L, 4])
        i("mla_g_q", [L, 128, 2]); i("mla_g_kv", [L, 128, 1]); i("mla_w_uq", [L, 256, 256]); i("mla_w_ukv", [L, 128, 384])
        i("gla_w_gf", [L, 16, 128]); i("gla_b_gf", [L, 128]); i("gla_w_gb", [L, 16, 128]); i("gla_b_gb", [L, 128])
        i("gla_g_norm", [L, 64, 1])
        i("w_out", [L, D, D]); i("ln1_g", [L, D]); i("ln1_b", [L, D]); i("ln2_g", [L, D]); i("ln2_b", [L, D])
        i("router_w", [L, D, NE]); i("router_bias", [L, NE])
        i("exp_w1", [L * NE * 128, 2048]); i("exp_w3", [L * NE * 128, 2048]); i("exp_w2", [L * NE * 128, 2048])
        i("sh_w1", [L, D, 256]); i("sh_w3", [L, D, 256]); i("sh_w2", [L, 256, D])
        self.out = self.nc.dram_tensor("out", [T, D], F32, kind="ExternalOutput").ap()
        s = self.scratch
        s("XR", [NT, D], F32)
        s("PT", [IN_W, NT], BF16)
        s("VT", [NT, 768], BF16)
        s("QTb", [4, 64, NT], BF16); s("KTb", [2, 64, NT], BF16)
        s("QTc", [4, 64, NT], BF16); s("KTc", [4, 64, NT], BF16); s("Vc", [NT, 4, 64], BF16)
        s("OT", [D, NT], BF16)
        s("OF", [64, 4, NT], F32)
        s("HT", [D, NT], BF16)
        s("GT", [NT // 128, 128, NE + 1], F32)
        s("POS", [NT // 128, 128, NE], F32)
        s("XB", [NT, D], BF16)
        s("XS", [self.NB * 128, D], BF16)
        s("YS", [self.NB * 128, D], BF16)

    def setup_global(self, stack):
        fw, nc = self.fw, self.nc
        self.gstack = stack
        fw.stack = stack
        self.ident = fw.sb([128, 128], F32, "ident")
        fw.dma("sp", self.ident[:], self.din["ident"], writes=[self.ident])
        self.ident_bf = fw.sb([128, 128], BF16, "identb")
        fw.op("dve", lambda: nc.vector.tensor_copy(out=self.ident_bf[:], in_=self.ident[:]),
              reads=[self.ident], writes=[self.ident_bf])
        self.ones = fw.sb([128, 128], F32, "ones")
        fw.op("dve", lambda: nc.vector.memset(self.ones[:], 1.0), writes=[self.ones])
        self.epsln = fw.sb([128, 2], F32, "eps")
        fw.op("dve", lambda: nc.vector.memset(self.epsln[:, 0:1], LN_EPS), writes=[self.epsln])
        fw.op("dve", lambda: nc.vector.memset(self.epsln[:, 1:2], RMS_EPS), writes=[self.epsln])
        self.cvs = fw.sb([128, 8, 2], F32, "cvs")
        cv_raw = fw.sb([128, 8, 2], F32, "cvraw")
        fw.dma("sp", cv_raw[:], self.din["cv"], writes=[cv_raw])
        fw.op("act", lambda: nc.scalar.activation(out=self.cvs[:], in_=cv_raw[:], func=AF.Silu),
              reads=[cv_raw], writes=[self.cvs])
        self.modv = fw.sb([128, 48, 2], F32, "modv")
        self.psum_banks = None
        U32 = mybir.dt.uint32
        nt = self.NT // 128
        self.moe_run = fw.sb([128, NE], F32, "moerun")
        self.widx = fw.sb([128, self.NB], U32, "widx")
        self.dest8 = fw.sb([128, nt, 8], U32, "dest8")
        self.gate8 = fw.sb([128, nt, 8], F32, "gate8")
        zt = fw.sb([128, D], BF16, "zt")
        fw.op("dve", lambda: nc.vector.memset(zt[:], 0.0), writes=[zt])
        for b in range(self.NB):
            fw.dma("sp", self.scr["XS"][b * 128:(b + 1) * 128, :], zt[:], reads=[zt])

    def stage_mod(self, l):
        fw, nc = self.fw, self.nc
        from contextlib import ExitStack
        with ExitStack() as st:
            fw.stack = st
            wj = Ring([fw.sb([128, 8, 128], F32, "wj") for _ in range(3)])
            pm = fw.ps([128, 48, 2], F32, "pm")
            bpj = fw.sb([128, 48], F32, "bpj")
            fw.dma("sp", bpj[:], self.din["b_ada_pj"][l], writes=[bpj])
            wsrc = self.din["w_ada"][l].rearrange("(k p) n -> p k n", p=128)
            for j in range(48):
                w = wj.next()
                fw.dma("sp", w[:], wsrc[:, :, j * 128:(j + 1) * 128], writes=[w])
                for k in range(8):
                    fw.op("pe", lambda k=k, w=w: nc.tensor.matmul(pm[:, j, :], lhsT=w[:, k, :], rhs=self.cvs[:, k, :],
                                                                   start=(k == 0), stop=(k == 7)),
                          reads=[w, self.cvs], writes=[pm], inc=(k == 7))
            for wh in range(2):
                fw.op("dve", lambda wh=wh: nc.vector.tensor_tensor(out=self.modv[:, :, wh], in0=pm[:, :, wh], in1=bpj[:],
                                                                    op=ALU.add),
                      reads=[pm, bpj], writes=[self.modv])
            for j0 in (8, 32):
                fw.op("dve", lambda j0=j0: nc.vector.tensor_scalar_add(out=self.modv[:, j0:j0 + 8, :],
                                                                        in0=self.modv[:, j0:j0 + 8, :], scalar1=1.0),
                      reads=[self.modv], writes=[self.modv])
            fw.barrier()
        fw.stack = self.gstack

    def stage_inproj(self, l):
        fw, nc = self.fw, self.nc
        T, NT = self.T, self.NT
        from contextlib import ExitStack
        with ExitStack() as st:
            fw.stack = st
            win = fw.sb([128, 8, IN_W], BF16, "win")
            wsrc = self.din["w_in"][l].rearrange("(k p) n -> p k n", p=128)
            for k in range(8):
                fw.dma("pool", win[:, k, :], wsrc[:, k, :], writes=[win])
            xr = Ring([fw.sb([128, D], F32, "x") for _ in range(3)])
            xnr = Ring([fw.sb([128, D], F32, "xn") for _ in range(2)])
            str_ = Ring([fw.sb([128, 16], F32, "st") for _ in range(3)])
            hTr = Ring([fw.sb([128, 8, 512], BF16, "hT") for _ in range(2)])
            ptr = Ring([fw.sb([128, 512], BF16, "pt") for _ in range(4)])
            vtr = Ring([fw.sb([128, 768], BF16, "vt") for _ in range(2)])
            tpr = Ring([fw.ps([128, 8, 128], F32, "tp") for _ in range(2)])
            ppr = Ring([fw.ps([128, 512], F32, "pp") for _ in range(2)])
            pvr = Ring([fw.ps([128, 2, 512], F32, "pv") for _ in range(1)])
            ev = 0
            for (t0, n) in self.chunks(512):
                hT = hTr.next()
                for ti in range(n // 128):
                    tok = t0 + ti * 128
                    lat = tok < T
                    wh = 0 if lat else 1
                    if l == 0:
                        src = self.din["x"][tok:tok + 128, :] if lat else self.din["ctx"][tok - T:tok - T + 128, :]
                    else:
                        src = self.scr["XR"][tok:tok + 128, :]
                    x = xr.next()
                    fw.dma("sp", x[:], src, writes=[x])
                    s = str_.next()
                    for hh in range(2):
                        fw.op("dve", lambda x=x, s=s, hh=hh: nc.vector.bn_stats(out=s[:, hh * 6:hh * 6 + 6],
                                                                                 in_=x[:, hh * 512:(hh + 1) * 512]),
                              reads=[x], writes=[s])
                    fw.op("dve", lambda s=s: nc.vector.bn_aggr(out=s[:, 12:14], in_=s[:, 0:12]), reads=[s], writes=[s])
                    fw.op("act", lambda s=s: nc.scalar.activation(out=s[:, 15:16], in_=s[:, 13:14], func=AF.Sqrt,
                                                                   bias=self.epsln[:, 0:1], scale=1.0),
                          reads=[s, self.epsln], writes=[s])
                    fw.op("dve", lambda s=s: nc.vector.reciprocal(out=s[:, 14:15], in_=s[:, 15:16]), reads=[s], writes=[s])
                    xn = xnr.next()
                    fw.op("dve", lambda x=x, s=s, xn=xn: nc.vector.tensor_scalar(out=xn[:], in0=x[:], scalar1=s[:, 12:13],
                                                                                  scalar2=s[:, 14:15], op0=ALU.subtract,
                                                                                  op1=ALU.mult),
                          reads=[x, s], writes=[xn])
                    tp = tpr.next()
                    for k in range(8):
                        fw.op("pe", lambda k=k, tp=tp, xn=xn: nc.tensor.transpose(out=tp[:, k, :], in_=xn[:, k * 128:(k + 1) * 128],
                                                                                   identity=self.ident[:]),
                              reads=[xn, self.ident], writes=[tp], inc=(k == 7))
                    for k in range(8):
                        sc = self.modv[:, 8 + k, wh:wh + 1]
                        sh = self.modv[:, k, wh:wh + 1]
                        o = hT[:, k, ti * 128:(ti + 1) * 128]
                        if k % 2 == 0:
                            fw.op("act", lambda o=o, tp=tp, k=k, sc=sc, sh=sh: nc.scalar.activation(
                                out=o, in_=tp[:, k, :], func=AF.Identity, bias=sh, scale=sc),
                                reads=[tp, self.modv], writes=[hT])
                        else:
                            fw.op("dve", lambda o=o, tp=tp, k=k, sc=sc, sh=sh: nc.vector.tensor_scalar(
                                out=o, in0=tp[:, k, :], scalar1=sc, scalar2=sh, op0=ALU.mult, op1=ALU.add),
                                reads=[tp, self.modv], writes=[hT])
                    pv = pvr.next()
                    for (c0, c1, bank, off) in ((O_AV, O_AV + 256, 0, 0), (O_BV, O_BV + 128, 0, 256), (O_DK, O_DK + 384, 1, 0)):
                        for k in range(8):
                            fw.op("pe", lambda k=k, c0=c0, c1=c1, bank=bank, off=off, pv=pv, hT=hT, ti=ti: nc.tensor.matmul(
                                pv[:, bank, off:off + (c1 - c0)], lhsT=hT[:, k, ti * 128:(ti + 1) * 128], rhs=win[:, k, c0:c1],
                                start=(k == 0), stop=(k == 7)),
                                reads=[hT, win], writes=[pv], inc=(k == 7))
                    vt = vtr.next()
                    fw.op("act", lambda vt=vt, pv=pv: nc.scalar.copy(out=vt[:, 0:384], in_=pv[:, 0, 0:384]), reads=[pv], writes=[vt])
                    fw.op("dve", lambda vt=vt, pv=pv: nc.vector.tensor_copy(out=vt[:, 384:768], in_=pv[:, 1, 0:384]), reads=[pv], writes=[vt])
                    fw.dma("pool", self.scr["VT"][tok:tok + 128, :], vt[:], reads=[vt])
                for jc in range(20):
                    m = min(128, IN_W - jc * 128)
                    pp = ppr.next()
                    for k in range(8):
                        fw.op("pe", lambda k=k, jc=jc, m=m, pp=pp, hT=hT: nc.tensor.matmul(
                            pp[0:m, 0:n], lhsT=win[:, k, jc * 128:jc * 128 + m], rhs=hT[:, k, 0:n],
                            start=(k == 0), stop=(k == 7)),
                            reads=[hT, win], writes=[pp], inc=(k == 7))
                    pt = ptr.next()
                    if ev % 2 == 0:
                        fw.op("act", lambda pt=pt, pp=pp, m=m: nc.scalar.copy(out=pt[0:m, 0:n], in_=pp[0:m, 0:n]), reads=[pp], writes=[pt])
                    else:
                        fw.op("dve", lambda pt=pt, pp=pp, m=m: nc.vector.tensor_copy(out=pt[0:m, 0:n], in_=pp[0:m, 0:n]), reads=[pp], writes=[pt])
                    ev += 1
                    fw.dma("pool", self.scr["PT"][jc * 128:jc * 128 + m, t0:t0 + n], pt[0:m, 0:n], reads=[pt])
            fw.barrier()
        fw.stack = self.gstack


def _stage(fn):
    def wrap(self, *a, **kw):
        from contextlib import ExitStack
        with ExitStack() as st:
            self.fw.stack = st
            fn(self, *a, **kw)
            self.fw.barrier()
        self.fw.stack = self.gstack
    return wrap


class KA(K):
    def lat_chunks(self, n=512):
        return [(t, min(n, self.T - t)) for t in range(0, self.T, n)]

    def attn_finish(self, po, n, dst, rings, sink=None):
        fw, nc = self.fw, self.nc
        rden, pbr, osbr, obr = rings
        rd = rden.next()
        if sink is not None:
            fw.op("dve", lambda: nc.vector.tensor_scalar(out=rd[64:65, 0:n], in0=po[64:65, 0:n], scalar1=sink, scalar2=None,
                                                          op0=ALU.add), reads=[po, self.sinkexp], writes=[rd])
            fw.op("dve", lambda: nc.vector.reciprocal(out=rd[64:65, 0:n], in_=rd[64:65, 0:n]), reads=[rd], writes=[rd])
        else:
            fw.op("dve", lambda: nc.vector.reciprocal(out=rd[64:65, 0:n], in_=po[64:65, 0:n]), reads=[po], writes=[rd])
        pb = pbr.next()
        fw.op("pe", lambda: nc.tensor.matmul(pb[0:64, 0:n], lhsT=self.ones[64:65, 0:64], rhs=rd[64:65, 0:n], start=True, stop=True),
              reads=[rd, self.ones], writes=[pb])
        osb = osbr.next()
        fw.op("act", lambda: nc.scalar.copy(out=osb[0:64, 0:n], in_=po[0:64, 0:n]), reads=[po], writes=[osb])
        ob = obr.next()
        fw.op("dve", lambda: nc.vector.tensor_tensor(out=ob[0:64, 0:n], in0=osb[0:64, 0:n], in1=pb[0:64, 0:n], op=ALU.mult),
              reads=[osb, pb], writes=[ob])
        fw.dma("pool", dst, ob[0:64, 0:n], reads=[ob])

    def finish_rings(self):
        fw = self.fw
        return (Ring([fw.sb([128, 512], F32, "rden") for _ in range(2)]),
                Ring([fw.ps([128, 512], F32, "pb") for _ in range(1)]),
                Ring([fw.sb([64, 512], F32, "osb") for _ in range(2)]),
                Ring([fw.sb([64, 512], BF16, "ob") for _ in range(2)]))

    def rope64(self, src, srcbufs, n, t0, rot, which, rings):
        fw, nc = self.fw, self.nc
        qsr, prr, cosr, sinr, t1r, qfr = rings
        qs = qsr.next()
        fw.op("act", lambda: nc.scalar.copy(out=qs[0:64, 0:n], in_=src), reads=srcbufs, writes=[qs])
        if not rot:
            return qs
        pr = prr.next()
        rt = self.rt_wa if which == "wa" else self.rt_mla
        fw.op("pe", lambda: nc.tensor.matmul(pr[0:64, 0:n], lhsT=rt[:], rhs=qs[0:64, 0:n], start=True, stop=True),
              reads=[qs, rt], writes=[pr])
        cos, sin = cosr.next(), sinr.next()
        fw.dma("sp", cos[0:64, 0:n], self.din["cos_" + which][:, t0:t0 + n], writes=[cos])
        fw.dma("sp", sin[0:64, 0:n], self.din["sin_" + which][:, t0:t0 + n], writes=[sin])
        t1 = t1r.next()
        fw.op("pool", lambda: nc.gpsimd.tensor_tensor(out=t1[0:64, 0:n], in0=qs[0:64, 0:n], in1=cos[0:64, 0:n], op=ALU.mult),
              reads=[qs, cos], writes=[t1])
        t2 = t1r.next()
        fw.op("dve", lambda: nc.vector.tensor_tensor(out=t2[0:64, 0:n], in0=pr[0:64, 0:n], in1=sin[0:64, 0:n], op=ALU.mult),
              reads=[pr, sin], writes=[t2])
        qf = qfr.next()
        fw.op("dve", lambda: nc.vector.tensor_tensor(out=qf[0:64, 0:n], in0=t1[0:64, 0:n], in1=t2[0:64, 0:n], op=ALU.add),
              reads=[t1, t2], writes=[qf])
        return qf

    def rope_rings(self):
        fw = self.fw
        return (Ring([fw.sb([64, 512], BF16, "qs") for _ in range(3)]),
                Ring([fw.ps([64, 512], F32, "pr") for _ in range(2)]),
                Ring([fw.sb([64, 512], F32, "cos") for _ in range(2)]),
                Ring([fw.sb([64, 512], F32, "sin") for _ in range(2)]),
                Ring([fw.sb([64, 512], F32, "t1") for _ in range(4)]),
                Ring([fw.sb([64, 512], BF16, "qf") for _ in range(3)]))

    def load_rt(self):
        fw, nc = self.fw, self.nc
        for nm in ("rt_wa", "rt_mla"):
            t = fw.sb([64, 64], BF16, nm)
            fw.dma("pool", t[:], self.din[nm], writes=[t])
            setattr(self, nm, t)

    @_stage
    def stage_wa_prep(self, l):
        fw, nc = self.fw, self.nc
        T = self.T
        self.load_rt()
        rr = self.rope_rings()
        inr = Ring([fw.sb([64, 512], BF16, "in") for _ in range(3)])
        for (t0, n) in self.chunks(512):
            rot = t0 < T
            for (src_row, dst) in [(O_BQ + 64 * h, self.scr["QTb"][h]) for h in range(4)] + \
                                  [(O_BK + 64 * h, self.scr["KTb"][h]) for h in range(2)]:
                a = inr.next()
                fw.dma("sp", a[0:64, 0:n], self.scr["PT"][src_row:src_row + 64, t0:t0 + n], writes=[a])
                if rot:
                    qf = self.rope64(a[0:64, 0:n], [a], n, t0, True, "wa", rr)
                    fw.dma("pool", dst[:, t0:t0 + n], qf[0:64, 0:n], reads=[qf])
                else:
                    fw.dma("pool", dst[:, t0:t0 + n], a[0:64, 0:n], reads=[a])

    @_stage
    def stage_mla_prep(self, l):
        fw, nc = self.fw, self.nc
        T = self.T
        self.load_rt()
        rr = self.rope_rings()
        onesb = fw.sb([128, 128], BF16, "onesb")
        fw.op("dve", lambda: nc.vector.memset(onesb[:], 1.0), writes=[onesb])
        gq = fw.sb([128, 2], F32, "gq"); gkv = fw.sb([128, 1], F32, "gkv")
        fw.dma("sp", gq[:], self.din["mla_g_q"][l], writes=[gq])
        fw.dma("sp", gkv[:], self.din["mla_g_kv"][l], writes=[gkv])
        wuq = fw.sb([128, 2, 256], BF16, "wuq"); wukv = fw.sb([128, 384], BF16, "wukv")
        fw.dma("pool", wuq[:], self.din["mla_w_uq"][l].rearrange("(k p) n -> p k n", p=128), writes=[wuq])
        fw.dma("pool", wukv[:], self.din["mla_w_ukv"][l], writes=[wukv])
        cqr = Ring([fw.sb([128, 3, 512], BF16, "cq") for _ in range(2)])
        sqr = Ring([fw.sb([128, 3, 512], BF16, "sq") for _ in range(2)])
        rsr = Ring([fw.sb([128, 2, 512], F32, "rs") for _ in range(2)])
        cnr = Ring([fw.sb([128, 3, 512], BF16, "cn") for _ in range(2)])
        krr = Ring([fw.sb([64, 512], BF16, "kr") for _ in range(2)])
        knr = Ring([fw.sb([32, 512], BF16, "kn") for _ in range(3)])
        vcr = Ring([fw.sb([128, 384], BF16, "vc") for _ in range(3)])
        pss = Ring([fw.ps([128, 2, 512], F32, "pss") for _ in range(1)])
        pq = Ring([fw.ps([64, 512], F32, "pq") for _ in range(2)])
        pv = Ring([fw.ps([128, 512], F32, "pvc") for _ in range(1)])
        PT = self.scr["PT"]
        for (t0, n) in self.chunks(512):
            rot = t0 < T
            cq = cqr.next()
            fw.dma("sp", cq[:, 0:2, 0:n], PT[O_CQ:O_CQ + 256, t0:t0 + n].rearrange("(k p) t -> p k t", p=128), writes=[cq])
            fw.dma("sp", cq[:, 2, 0:n], PT[O_CKV:O_CKV + 128, t0:t0 + n], writes=[cq])
            sq = sqr.next()
            fw.op("pool", lambda: nc.gpsimd.tensor_tensor(out=sq[:, :, 0:n], in0=cq[:, :, 0:n], in1=cq[:, :, 0:n], op=ALU.mult),
                  reads=[cq], writes=[sq])
            ps = pss.next()
            for k in range(2):
                fw.op("pe", lambda k=k: nc.tensor.matmul(ps[:, 0, 0:n], lhsT=onesb[:], rhs=sq[:, k, 0:n], start=(k == 0), stop=(k == 1)),
                      reads=[sq, onesb], writes=[ps], inc=(k == 1))
            fw.op("pe", lambda: nc.tensor.matmul(ps[:, 1, 0:n], lhsT=onesb[:], rhs=sq[:, 2, 0:n], start=True, stop=True),
                  reads=[sq, onesb], writes=[ps])
            rs = rsr.next()
            fw.op("act", lambda: nc.scalar.activation(out=rs[:, 0, 0:n], in_=ps[:, 0, 0:n], func=AF.Sqrt, bias=self.epsln[:, 1:2], scale=1.0 / 256),
                  reads=[ps, self.epsln], writes=[rs])
            fw.op("act", lambda: nc.scalar.activation(out=rs[:, 1, 0:n], in_=ps[:, 1, 0:n], func=AF.Sqrt, bias=self.epsln[:, 1:2], scale=1.0 / 128),
                  reads=[ps, self.epsln], writes=[rs])
            fw.op("dve", lambda: nc.vector.reciprocal(out=rs[:, :, 0:n], in_=rs[:, :, 0:n]), reads=[rs], writes=[rs])
            cn = cnr.next()
            for k in range(3):
                g = gq[:, k:k + 1] if k < 2 else gkv[:, 0:1]
                fw.op("dve", lambda k=k, g=g: nc.vector.scalar_tensor_tensor(out=cn[:, k, 0:n], in0=cq[:, k, 0:n], scalar=g,
                                                                             in1=rs[:, (0 if k < 2 else 1), 0:n], op0=ALU.mult, op1=ALU.mult),
                      reads=[cq, rs, gq, gkv], writes=[cn])
            for h in range(4):
                p = pq.next()
                for k in range(2):
                    fw.op("pe", lambda k=k, h=h, p=p: nc.tensor.matmul(p[0:64, 0:n], lhsT=wuq[:, k, h * 64:(h + 1) * 64], rhs=cn[:, k, 0:n],
                                                                       start=(k == 0), stop=(k == 1)),
                          reads=[cn, wuq], writes=[p], inc=(k == 1))
                qf = self.rope64(p[0:64, 0:n], [p], n, t0, rot, "mla", rr)
                fw.dma("pool", self.scr["QTc"][h][:, t0:t0 + n], qf[0:64, 0:n], reads=[qf])
            kr = krr.next()
            if rot:
                fw.op("pool", lambda: nc.gpsimd.memset(kr[0:32, 0:n], 0.0), writes=[kr])
            fw.dma("sp", kr[32:64, 0:n], PT[O_CKR:O_CKR + 32, t0:t0 + n], writes=[kr])
            if rot:
                kf = self.rope64(kr[0:64, 0:n], [kr], n, t0, True, "mla", rr)
            else:
                kf = kr
            for h in range(4):
                fw.dma("pool", self.scr["KTc"][h][32:64, t0:t0 + n], kf[32:64, 0:n], reads=[kf])
            for h in range(4):
                p = pq.next()
                fw.op("pe", lambda h=h, p=p: nc.tensor.matmul(p[0:32, 0:n], lhsT=wukv[:, h * 96:h * 96 + 32], rhs=cn[:, 2, 0:n], start=True, stop=True),
                      reads=[cn, wukv], writes=[p])
                kn = knr.next()
                fw.op("act", lambda p=p, kn=kn: nc.scalar.copy(out=kn[0:32, 0:n], in_=p[0:32, 0:n]), reads=[p], writes=[kn])
                fw.dma("pool", self.scr["KTc"][h][0:32, t0:t0 + n], kn[0:32, 0:n], reads=[kn])
            for ti in range(n // 128):
                p = pv.next()
                fw.op("pe", lambda p=p, ti=ti: nc.tensor.matmul(p[:, 0:384], lhsT=cn[:, 2, ti * 128:(ti + 1) * 128], rhs=wukv[:], start=True, stop=True),
                      reads=[cn, wukv], writes=[p])
                vc = vcr.next()
                fw.op("dve", lambda p=p, vc=vc: nc.vector.tensor_copy(out=vc[:], in_=p[:, 0:384]), reads=[p], writes=[vc])
                tok = t0 + ti * 128
                fw.dma("pool", self.scr["Vc"][tok:tok + 128], vc[:].rearrange("p (h c) -> p h c", h=4)[:, :, 32:96], reads=[vc])

    def exp_block(self, ps, kp, c0, c1, er, mask=None, mring=None):
        fw, nc = self.fw, self.nc
        e = er.next()
        fw.op("act", lambda: nc.scalar.activation(out=e[0:kp, c0:c1], in_=ps[0:kp, c0:c1], func=AF.Exp, scale=0.125),
              reads=[ps], writes=[e])
        if mask is None:
            return e
        mbufs, map_ = mask
        e2 = mring.next()
        fw.op("pool", lambda: nc.gpsimd.tensor_tensor(out=e2[0:kp, c0:c1], in0=e[0:kp, c0:c1], in1=map_, op=ALU.mult),
              reads=[e] + mbufs, writes=[e2])
        return e2

    @_stage
    def stage_mla_attn(self, l, ctx_out):
        fw, nc = self.fw, self.nc
        T, NT = self.T, self.NT
        nkb = NT // 128
        fr = self.finish_rings()
        kT = fw.sb([64, NT], BF16, "kT")
        va = fw.sb([128, nkb, 65], BF16, "va")
        fw.op("dve", lambda: nc.vector.memset(va[:, :, 64:65], 1.0), writes=[va])
        qr = Ring([fw.sb([64, 512], BF16, "q") for _ in range(2)])
        er = Ring([fw.sb([128, 512], BF16, "e") for _ in range(3)])
        psr = Ring([fw.ps([128, 512], F32, "ps") for _ in range(3)])
        por = Ring([fw.ps([128, 512], F32, "po") for _ in range(2)])
        for h in range(4):
            fw.dma("sp", kT[:], self.scr["KTc"][h], writes=[kT])
            fw.dma("sp", va[:, :, 0:64], self.scr["Vc"][:, h, :].rearrange("(b p) d -> p b d", p=128), writes=[va])
            qchunks = [(t0, n, list(range(nkb))) for (t0, n) in self.lat_chunks(512)]
            if ctx_out:
                qchunks.append((T, C, [nkb - 2, nkb - 1]))
            for (t0, n, kbs) in qchunks:
                q = qr.next()
                fw.dma("sp", q[0:64, 0:n], self.scr["QTc"][h][:, t0:t0 + n], writes=[q])
                po = por.next()
                for i, kb in enumerate(kbs):
                    ps = psr.next()
                    fw.op("pe", lambda ps=ps, kb=kb: nc.tensor.matmul(ps[:, 0:n], lhsT=kT[:, kb * 128:(kb + 1) * 128], rhs=q[0:64, 0:n], start=True, stop=True),
                          reads=[kT, q], writes=[ps])
                    e = self.exp_block(ps, 128, 0, n, er)
                    fw.op("pe", lambda e=e, kb=kb, i=i: nc.tensor.matmul(po[0:65, 0:n], lhsT=va[:, kb, :], rhs=e[:, 0:n], start=(i == 0), stop=(i == len(kbs) - 1)),
                          reads=[e, va], writes=[po], inc=(i == len(kbs) - 1))
                self.attn_finish(po, n, self.scr["OT"][512 + h * 64:512 + (h + 1) * 64, t0:t0 + n], fr)

    @_stage
    def stage_wa_attn(self, l, ctx_out):
        fw, nc = self.fw, self.nc
        T, NT = self.T, self.NT
        nb = T // 128
        fr = self.finish_rings()
        self.sinkexp = fw.sb([128, 4], F32, "sinkexp")
        fw.dma("sp", self.sinkexp[:], self.din["wa_sink"][l].partition_broadcast(128), writes=[self.sinkexp])
        fw.op("act", lambda: nc.scalar.activation(out=self.sinkexp[:], in_=self.sinkexp[:], func=AF.Exp), reads=[self.sinkexp], writes=[self.sinkexp])
        mask = fw.sb([128, 6, 512], BF16, "mask")
        fw.dma("pool", mask[:], self.din["maskwa"].rearrange("r p q -> p r q"), writes=[mask])
        kT = fw.sb([64, NT], BF16, "kT")
        va = fw.sb([128, NT // 128, 65], BF16, "va")
        fw.op("dve", lambda: nc.vector.memset(va[:, :, 64:65], 1.0), writes=[va])
        qr = Ring([fw.sb([64, 512], BF16, "q") for _ in range(2)])
        er = Ring([fw.sb([128, 512], BF16, "e") for _ in range(3)])
        mr = Ring([fw.sb([128, 512], BF16, "em") for _ in range(3)])
        psr = Ring([fw.ps([128, 512], F32, "ps") for _ in range(3)])
        por = Ring([fw.ps([128, 512], F32, "po") for _ in range(2)])
        for g in range(2):
            fw.dma("sp", kT[:], self.scr["KTb"][g], writes=[kT])
            fw.dma("sp", va[:, :, 0:64], self.scr["VT"][:, 256 + g * 64:256 + (g + 1) * 64].rearrange("(b p) d -> p b d", p=128), writes=[va])
            for h in (2 * g, 2 * g + 1):
                qchunks = [(t0, n, True) for (t0, n) in self.lat_chunks(512)]
                if ctx_out:
                    qchunks.append((T, C, False))
                for (t0, n, lat) in qchunks:
                    q = qr.next()
                    fw.dma("sp", q[0:64, 0:n], self.scr["QTb"][h][:, t0:t0 + n], writes=[q])
                    po = por.next()
                    items = [(nb, 0, n, None)]
                    if lat:
                        i0 = t0 // 128
                        nqb = n // 128
                        for r in range(-1, nqb + 1):
                            j = i0 + r
                            if j < 0 or j >= nb:
                                continue
                            qlo, qhi = max(0, r - 1), min(nqb, r + 2)
                            items.append((j, qlo * 128, qhi * 128, r + 1))
                    items.append((nb + 1, 0, n, None))
                    for i, (kb, c0, c1, mi) in enumerate(items):
                        ps = psr.next()
                        fw.op("pe", lambda ps=ps, kb=kb, c0=c0, c1=c1: nc.tensor.matmul(ps[:, c0:c1], lhsT=kT[:, kb * 128:(kb + 1) * 128], rhs=q[0:64, c0:c1], start=True, stop=True),
                              reads=[kT, q], writes=[ps])
                        e = self.exp_block(ps, 128, c0, c1, er, mask=None if mi is None else ([mask], mask[:, mi, c0:c1]), mring=mr)
                        fw.op("pe", lambda e=e, kb=kb, c0=c0, c1=c1, i=i: nc.tensor.matmul(po[0:65, c0:c1], lhsT=va[:, kb, :], rhs=e[:, c0:c1], start=(i == 0), stop=(i == len(items) - 1)),
                              reads=[e, va], writes=[po], inc=(i == len(items) - 1))
                    self.attn_finish(po, n, self.scr["OT"][256 + h * 64:256 + (h + 1) * 64, t0:t0 + n], fr, sink=self.sinkexp[64:65, h:h + 1])

    @_stage
    def stage_na_attn(self, l, ctx_out):
        fw, nc = self.fw, self.nc
        T, NT = self.T, self.NT
        R = T // 64
        fr = self.finish_rings()
        oh = fw.sb([31, 64, 64], F32, "oh")
        fw.dma("sp", oh[:], self.din["na_oh"], writes=[oh])
        cmask = fw.sb([64, 960], F32, "cmask")
        fw.dma("sp", cmask[:], self.din["na_cmask"].rearrange("k m c -> k (m c)"), writes=[cmask])
        j15 = fw.sb([15, 15], F32, "j15")
        fw.dma("sp", j15[:], self.din["j15"], writes=[j15])
        rpb = fw.sb([15, 4, 31], F32, "rpb")
        fw.dma("sp", rpb[:], self.din["na_rpb"][l].rearrange("h r j -> r h j"), writes=[rpb])
        rrr = Ring([fw.sb([31, 15], F32, "rr") for _ in range(2)])
        etz = [fw.sb([64, 960], BF16, "etz") for _ in range(4)]
        etmp = fw.sb([64, 960], F32, "etmp")
        prr = fw.ps([31, 15], F32, "prr")
        pz = fw.ps([64, 2, 512], F32, "pz")
        for h in range(4):
            fw.op("pe", lambda h=h: nc.tensor.matmul(prr[:, :], lhsT=rpb[:, h, :], rhs=j15[:], start=True, stop=True), reads=[rpb, j15], writes=[prr])
            rr = rrr.next()
            fw.op("dve", lambda rr=rr: nc.vector.tensor_copy(out=rr[:], in_=prr[:]), reads=[prr], writes=[rr])
            for c_ in range(64):
                for (bk, m0, m1) in ((0, 0, 8), (1, 8, 15)):
                    fw.op("pe", lambda c_=c_, bk=bk, m0=m0, m1=m1, rr=rr: nc.tensor.matmul(
                        pz[:, bk, :].rearrange("k (m c) -> k m c", c=64)[:, 0:m1 - m0, c_], lhsT=oh[:, c_, :], rhs=rr[:, m0:m1], start=True, stop=True),
                        reads=[oh, rr], writes=[pz], inc=(c_ == 63 and bk == 1))
            fw.op("act", lambda: nc.scalar.activation(out=etmp[:, 0:512], in_=pz[:, 0, :], func=AF.Exp), reads=[pz], writes=[etmp])
            fw.op("act", lambda: nc.scalar.activation(out=etmp[:, 512:960], in_=pz[:, 1, 0:448], func=AF.Exp), reads=[pz], writes=[etmp])
            fw.op("dve", lambda h=h: nc.vector.tensor_tensor(out=etz[h][:], in0=etmp[:], in1=cmask[:], op=ALU.mult), reads=[etmp, cmask], writes=[etz[h]])
        kTr = Ring([fw.sb([64, 1024], BF16, "kT") for _ in range(2)])
        kcr = Ring([fw.sb([64, 256], BF16, "kc") for _ in range(2)])
        var = Ring([fw.sb([64, 16, 65], BF16, "va") for _ in range(2)])
        vcr = Ring([fw.sb([128, 2, 65], BF16, "vca") for _ in range(2)])
        for b in var.bufs:
            fw.op("dve", lambda b=b: nc.vector.memset(b[:, :, 64:65], 1.0), writes=[b])
        for b in vcr.bufs:
            fw.op("dve", lambda b=b: nc.vector.memset(b[:, :, 64:65], 1.0), writes=[b])
        qr = Ring([fw.sb([64, 512], BF16, "q") for _ in range(2)])
        er = Ring([fw.sb([128, 512], BF16, "e") for _ in range(3)])
        mr = Ring([fw.sb([128, 512], BF16, "em") for _ in range(3)])
        psr = Ring([fw.ps([128, 512], F32, "ps") for _ in range(2)])
        por = Ring([fw.ps([128, 512], F32, "po") for _ in range(2)])
        PT, VT = self.scr["PT"], self.scr["VT"]
        for h in range(4):
            kc = kcr.next()
            fw.dma("sp", kc[:], PT[O_AK + h * 64:O_AK + (h + 1) * 64, T:T + C], writes=[kc])
            vc = vcr.next()
            fw.dma("sp", vc[:, :, 0:64], VT[T:T + C, h * 64:(h + 1) * 64].rearrange("(b p) d -> p b d", p=128), writes=[vc])
            qchunks = [(t0, n, True) for (t0, n) in self.lat_chunks(512)]
            if ctx_out:
                qchunks.append((T, C, False))
            for (t0, n, lat) in qchunks:
                q = qr.next()
                fw.dma("sp", q[0:64, 0:n], PT[O_AQ + h * 64:O_AQ + (h + 1) * 64, t0:t0 + n], writes=[q])
                po = por.next()
                items = [("c", 0, 0, n, None)]
                if lat:
                    r0 = t0 // 64
                    nqr = n // 64
                    klo, khi = max(0, r0 - 4), min(R - 1, r0 + nqr - 1 + 3 + 0)
                    rs_all = [min(max(r0 + rq - 4, 0), R - 8) for rq in range(nqr)]
                    klo, khi = min(rs_all), max(rs_all) + 7
                    kT = kTr.next()
                    nk = khi - klo + 1
                    fw.dma("sp", kT[:, 0:nk * 64], PT[O_AK + h * 64:O_AK + (h + 1) * 64, klo * 64:(khi + 1) * 64], writes=[kT])
                    va = var.next()
                    fw.dma("sp", va[:, 0:nk, 0:64], VT[klo * 64:(khi + 1) * 64, h * 64:(h + 1) * 64].rearrange("(r p) d -> p r d", p=64), writes=[va])
                    for kr in range(klo, khi + 1):
                        val = [rq for rq in range(nqr) if rs_all[rq] <= kr <= rs_all[rq] + 7]
                        if not val:
                            continue
                        lo, hi = val[0], val[-1] + 1
                        assert val == list(range(lo, hi))
                        m0 = lo + 7 - kr + r0
                        assert 0 <= m0 and m0 + (hi - lo) <= 15
                        items.append(("k", kr - klo, lo * 64, hi * 64, m0))
                items.append(("c", 1, 0, n, None))
                for i, (kind, idx, c0, c1, m0) in enumerate(items):
                    ps = psr.next()
                    first, last = i == 0, i == len(items) - 1
                    if kind == "c":
                        fw.op("pe", lambda ps=ps, idx=idx: nc.tensor.matmul(ps[:, c0:c1], lhsT=kc[:, idx * 128:(idx + 1) * 128], rhs=q[0:64, c0:c1], start=True, stop=True),
                              reads=[kc, q], writes=[ps])
                        e = self.exp_block(ps, 128, c0, c1, er)
                        fw.op("pe", lambda e=e, idx=idx, first=first, last=last: nc.tensor.matmul(po[0:65, c0:c1], lhsT=vc[:, idx, :], rhs=e[:, c0:c1], start=first, stop=last),
                              reads=[e, vc], writes=[po], inc=last)
                    else:
                        fw.op("pe", lambda ps=ps, idx=idx, c0=c0, c1=c1: nc.tensor.matmul(ps[0:64, c0:c1], lhsT=kT[:, idx * 64:(idx + 1) * 64], rhs=q[0:64, c0:c1], start=True, stop=True),
                              reads=[kT, q], writes=[ps])
                        nrow = (c1 - c0) // 64
                        e = self.exp_block(ps, 64, c0, c1, er, mask=([etz[h]], etz[h][:, m0 * 64:(m0 + nrow) * 64]), mring=mr)
                        fw.op("pe", lambda e=e, idx=idx, c0=c0, c1=c1: nc.tensor.matmul(po[0:65, c0:c1], lhsT=va[:, idx, :], rhs=e[0:64, c0:c1], start=False, stop=False),
                              reads=[e, va], writes=[po], inc=False)
                self.attn_finish(po, n, self.scr["OT"][h * 64:(h + 1) * 64, t0:t0 + n], fr)

    @_stage
    def stage_gla(self, l):
        fw, nc = self.fw, self.nc
        T, NT = self.T, self.NT
        PT, VT, OT, OF = self.scr["PT"], self.scr["VT"], self.scr["OT"], self.scr["OF"]
        tri = fw.sb([128, 4, 128], F32, "tri")
        fw.dma("sp", tri[:], self.din["gla_tri"].rearrange("g a b -> a g b"), writes=[tri])
        maskg = fw.sb([128, 2, 512], BF16, "maskg")
        fw.dma("pool", maskg[:], self.din["gla_mask"].rearrange("d j h i -> j d (h i)"), writes=[maskg])
        wg = fw.sb([32, 2, 128], BF16, "wg")
        for d, (wn, bn) in enumerate((("gla_w_gf", "gla_b_gf"), ("gla_w_gb", "gla_b_gb"))):
            fw.dma("pool", wg[0:16, d, :], self.din[wn][l], writes=[wg])
            fw.dma("pool", wg[16:17, d, :], self.din[bn][l:l + 1, :], writes=[wg])
        gn = fw.sb([64, 1], F32, "gn")
        fw.dma("sp", gn[:], self.din["gla_g_norm"][l], writes=[gn])
        onesb = fw.sb([64, 64], BF16, "onesb")
        fw.op("dve", lambda: nc.vector.memset(onesb[:], 1.0), writes=[onesb])
        zr = Ring([fw.sb([32, 128], BF16, "zaug") for _ in range(3)])
        for b in zr.bufs:
            fw.op("dve", lambda b=b: nc.vector.memset(b[:], 1.0), writes=[b])
        qkr = Ring([fw.sb([32, 2, 4, 128], BF16, "qk") for _ in range(3)])
        ktr = Ring([fw.sb([128, 128], BF16, "ktok") for _ in range(3)])
        vtr = Ring([fw.sb([128, 4, 64], BF16, "vtok") for _ in range(3)])
        exr = Ring([fw.sb([128, 128], F32, "ex") for _ in range(2)])
        Lr = Ring([fw.sb([128, 128], F32, "L") for _ in range(2)])
        ebr = Ring([fw.sb([32, 2, 512], F32, "eb") for _ in range(2)])
        esr = Ring([fw.sb([128, 128], F32, "es") for _ in range(2)])
        qdr = Ring([fw.sb([32, 2, 512], BF16, "qd") for _ in range(2)])
        ker = Ring([fw.sb([128, 2, 128], BF16, "kend") for _ in range(2)])
        cm = fw.sb([128, 2], F32, "cm")
        fw.op("dve", lambda: nc.vector.memset(cm[:], 0.0), writes=[cm])
        fw.op("dve", lambda: nc.vector.memset(cm[0:64, 0:1], 1.0), writes=[cm])
        fw.op("dve", lambda: nc.vector.memset(cm[64:128, 1:2], 1.0), writes=[cm])
        amr = Ring([fw.sb([128, 512], BF16, "am") for _ in range(2)])
        Sr = Ring([fw.sb([32, 4, 64], F32, "S") for _ in range(4)])
        Sbr = Ring([fw.sb([32, 4, 64], BF16, "Sb") for _ in range(4)])
        ofr = Ring([fw.sb([64, 512], F32, "of") for _ in range(2)])
        osr = Ring([fw.sb([64, 512], F32, "os") for _ in range(2)])
        sqr = Ring([fw.sb([64, 512], BF16, "sq") for _ in range(2)])
        rsr = Ring([fw.sb([64, 512], F32, "rs") for _ in range(2)])
        rtr = Ring([fw.sb([64, 512], BF16, "rT") for _ in range(2)])
        srr = Ring([fw.sb([64, 512], F32, "sr") for _ in range(2)])
        fnr = Ring([fw.sb([64, 512], BF16, "fin") for _ in range(2)])
        pz = fw.ps([128, 128], F32, "pz")
        pbT = fw.ps([32, 512], F32, "pbT")
        pbs = fw.ps([128, 128], F32, "pbs")
        pa = fw.ps([128, 512], F32, "pa")
        pu = fw.ps([32, 2, 4, 64], F32, "pu")
        po = fw.ps([64, 512], F32, "po")
        pss = fw.ps([64, 512], F32, "pss")
        nl, nt = T // 128, NT // 128
        orders = [[nt - 2, nt - 1] + list(range(nl)), [nt - 1, nt - 2] + list(range(nl - 1, -1, -1))]
        for d in (0, 1):
            if d == 1:
                fw.barrier()
            zrow = O_ZF if d == 0 else O_ZB
            S = Sr.next()
            fw.op("dve", lambda: nc.vector.memset(S[:], 0.0), writes=[S])
            for tile in orders[d]:
                tok = tile * 128
                za = zr.next()
                fw.dma("sp", za[0:16, :], PT[zrow:zrow + 16, tok:tok + 128], writes=[za])
                qk = qkr.next()
                fw.dma("sp", qk[:, 0, :, :], PT[O_DQ:O_DQ + 128, tok:tok + 128].rearrange("(h d) t -> d h t", d=32), writes=[qk])
                fw.dma("sp", qk[:, 1, :, :], PT[O_DK:O_DK + 128, tok:tok + 128].rearrange("(h d) t -> d h t", d=32), writes=[qk])
                ktok = ktr.next()
                fw.dma("sp", ktok[:], VT[tok:tok + 128, 384:512], writes=[ktok])
                vtok = vtr.next()
                fw.dma("sp", vtok[:], VT[tok:tok + 128, 512:768].rearrange("p (h d) -> p h d", h=4), writes=[vtok])
                if getattr(self, 'gla_cut', 99) <= 1:
                    continue
                fw.op("pe", lambda: nc.tensor.matmul(pz[:, :], lhsT=za[0:17, :], rhs=wg[0:17, d, :], start=True, stop=True), reads=[za, wg], writes=[pz])
                ex = exr.next()
                fw.op("act", lambda: nc.scalar.activation(out=ex[:], in_=pz[:], func=AF.Exp, scale=-1.0), reads=[pz], writes=[ex])
                L = Lr.next()
                fw.op("act", lambda: nc.scalar.activation(out=L[:], in_=ex[:], func=AF.Ln, bias=self.ones[:, 0:1], scale=1.0), reads=[ex, self.ones], writes=[L])
                if getattr(self, 'gla_cut', 99) <= 2:
                    continue
                for h in range(4):
                    fw.op("pe", lambda h=h: nc.tensor.matmul(pbT[0:32, h * 128:(h + 1) * 128], lhsT=L[:, h * 32:(h + 1) * 32], rhs=tri[:, d, :], start=True, stop=True),
                          reads=[L, tri], writes=[pbT], inc=(h == 3))
                fw.op("pe", lambda: nc.tensor.matmul(pbs[:, :], lhsT=tri[:, 2 + d, :], rhs=L[:], start=True, stop=True), reads=[L, tri], writes=[pbs])
                eb = ebr.next()
                fw.op("act", lambda: nc.scalar.activation(out=eb[:, 0, :], in_=pbT[0:32, :], func=AF.Exp), reads=[pbT], writes=[eb])
                fw.op("act", lambda: nc.scalar.activation(out=eb[:, 1, :], in_=pbT[0:32, :], func=AF.Exp, scale=-1.0), reads=[pbT], writes=[eb])
                es = esr.next()
                fw.op("act", lambda: nc.scalar.activation(out=es[:], in_=pbs[:], func=AF.Exp), reads=[pbs], writes=[es])
                if getattr(self, 'gla_cut', 99) <= 3:
                    continue
                qd = qdr.next()
                fw.op("dve", lambda: nc.vector.scalar_tensor_tensor(out=qd[:, 0, :], in0=qk[:, 0, :, :].rearrange("p h t -> p (h t)"), scalar=32 ** -0.5,
                                                                     in1=eb[:, 0, :], op0=ALU.mult, op1=ALU.mult), reads=[qk, eb], writes=[qd])
                fw.op("dve", lambda: nc.vector.tensor_tensor(out=qd[:, 1, :], in0=qk[:, 1, :, :].rearrange("p h t -> p (h t)"), in1=eb[:, 1, :], op=ALU.mult),
                      reads=[qk, eb], writes=[qd])
                kend = ker.next()
                for cc in range(2):
                    fw.op("dve", lambda cc=cc: nc.vector.scalar_tensor_tensor(out=kend[:, cc, :], in0=ktok[:], scalar=cm[:, cc:cc + 1], in1=es[:],
                                                                               op0=ALU.mult, op1=ALU.mult), reads=[ktok, es, cm], writes=[kend])
                if getattr(self, 'gla_cut', 99) <= 4:
                    continue
                for h in range(4):
                    fw.op("pe", lambda h=h: nc.tensor.matmul(pa[:, h * 128:(h + 1) * 128], lhsT=qd[:, 1, h * 128:(h + 1) * 128], rhs=qd[:, 0, h * 128:(h + 1) * 128], start=True, stop=True),
                          reads=[qd], writes=[pa], inc=(h == 3))
                am = amr.next()
                fw.op("dve", lambda: nc.vector.tensor_tensor(out=am[:], in0=pa[:], in1=maskg[:, d, :], op=ALU.mult), reads=[pa, maskg], writes=[am])
                if getattr(self, 'gla_cut', 99) <= 5:
                    continue
                for cc in range(2):
                    for h in range(4):
                        fw.op("pe", lambda cc=cc, h=h: nc.tensor.matmul(pu[0:32, cc, h, :], lhsT=kend[:, cc, h * 32:(h + 1) * 32],
                                                                         rhs=vtok[:, h, :], start=True, stop=True),
                              reads=[kend, vtok], writes=[pu], inc=(cc == 1 and h == 3))
                if getattr(self, 'gla_cut', 99) <= 6:
                    continue
                Sb = {}
                for cc in ((0, 1) if d == 0 else (1, 0)):
                    sb_ = Sbr.next()
                    fw.op("act", lambda sb_=sb_, S=S: nc.scalar.copy(out=sb_[:], in_=S[:]), reads=[S], writes=[sb_])
                    Sb[cc] = sb_
                    S2 = Sr.next()
                    tidx = cc * 64 + (63 if d == 0 else 0)
                    for h in range(4):
                        fw.op("dve", lambda h=h, S=S, S2=S2, cc=cc, tidx=tidx: nc.vector.scalar_tensor_tensor(
                            out=S2[:, h, :], in0=S[:, h, :], scalar=eb[:, 0, h * 128 + tidx:h * 128 + tidx + 1], in1=pu[0:32, cc, h, :],
                            op0=ALU.mult, op1=ALU.add), reads=[S, eb, pu], writes=[S2])
                    S = S2
                if getattr(self, 'gla_cut', 99) <= 7:
                    continue
                for h in range(4):
                    fw.op("pe", lambda h=h: nc.tensor.matmul(po[0:64, h * 128:(h + 1) * 128], lhsT=vtok[:, h, :], rhs=am[:, h * 128:(h + 1) * 128], start=True, stop=False),
                          reads=[vtok, am], writes=[po], inc=False)
                    for cc in range(2):
                        fw.op("pe", lambda h=h, cc=cc: nc.tensor.matmul(po[0:64, h * 128 + cc * 64:h * 128 + (cc + 1) * 64], lhsT=Sb[cc][:, h, :],
                                                                         rhs=qd[:, 0, h * 128 + cc * 64:h * 128 + (cc + 1) * 64], start=False, stop=(cc == 1)),
                              reads=[Sb[cc], qd], writes=[po], inc=(cc == 1))
                if getattr(self, 'gla_cut', 99) <= 8:
                    continue
                if d == 0:
                    of = ofr.next()
                    fw.op("act", lambda: nc.scalar.copy(out=of[:], in_=po[:]), reads=[po], writes=[of])
                    fw.dma("pool", OF[:, :, tok:tok + 128], of[:].rearrange("p (h t) -> p h t", h=4), reads=[of])
                else:
                    of = ofr.next()
                    fw.dma("sp", of[:].rearrange("p (h t) -> p h t", h=4), OF[:, :, tok:tok + 128], writes=[of])
                    rT = rtr.next()
                    fw.dma("sp", rT[:].rearrange("p (h t) -> p h t", h=4), PT[O_DR:O_DR + 256, tok:tok + 128].rearrange("(h d) t -> d h t", d=64), writes=[rT])
                    os_ = osr.next()
                    fw.op("dve", lambda: nc.vector.tensor_tensor(out=os_[:], in0=po[:], in1=of[:], op=ALU.add), reads=[po, of], writes=[os_])
                    sq = sqr.next()
                    fw.op("pool", lambda: nc.gpsimd.tensor_tensor(out=sq[:], in0=os_[:], in1=os_[:], op=ALU.mult), reads=[os_], writes=[sq])
                    fw.op("pe", lambda: nc.tensor.matmul(pss[:, :], lhsT=onesb[:], rhs=sq[:], start=True, stop=True), reads=[sq, onesb], writes=[pss])
                    rs = rsr.next()
                    fw.op("act", lambda: nc.scalar.activation(out=rs[:], in_=pss[:], func=AF.Sqrt, bias=self.epsln[0:64, 1:2], scale=1.0 / 64),
                          reads=[pss, self.epsln], writes=[rs])
                    fw.op("dve", lambda: nc.vector.reciprocal(out=rs[:], in_=rs[:]), reads=[rs], writes=[rs])
                    sr = srr.next()
                    fw.op("act", lambda: nc.scalar.activation(out=sr[:], in_=rT[:], func=AF.Silu), reads=[rT], writes=[sr])
                    fw.op("dve", lambda: nc.vector.scalar_tensor_tensor(out=os_[:], in0=os_[:], scalar=gn[:, 0:1], in1=rs[:], op0=ALU.mult, op1=ALU.mult),
                          reads=[os_, gn, rs], writes=[os_])
                    fin = fnr.next()
                    fw.op("dve", lambda: nc.vector.tensor_tensor(out=fin[:], in0=os_[:], in1=sr[:], op=ALU.mult), reads=[os_, sr], writes=[fin])
                    fw.dma("pool", OT[768:1024, tok:tok + 128].rearrange("(h d) t -> d h t", d=64), fin[:].rearrange("p (h t) -> p h t", h=4), reads=[fin])


class KB(KA):
    def bcast_mod(self, j0, dst_lat, dst_ctx, pg, repr_):
        fw, nc = self.fw, self.nc
        for wh, dst in ((0, dst_lat), (1, dst_ctx)):
            for jj in range(8):
                rep = repr_.next()
                fw.op("act", lambda rep=rep, jj=jj, wh=wh: nc.scalar.activation(out=rep[:], in_=self.ones[:], func=AF.Copy,
                                                                                 scale=self.modv[:, j0 + jj, wh:wh + 1]),
                      reads=[self.ones, self.modv], writes=[rep])
                fw.op("pe", lambda rep=rep, jj=jj: nc.tensor.matmul(pg[:, jj // 4, (jj % 4) * 128:(jj % 4 + 1) * 128], lhsT=rep[:], rhs=self.ident[:],
                                                                    start=True, stop=True), reads=[rep, self.ident], writes=[pg])
            fw.op("dve", lambda dst=dst: nc.vector.tensor_copy(out=dst[:].rearrange("p (a b) -> p a b", a=2), in_=pg[:]), reads=[pg], writes=[dst])

    def load_ln(self, l, which):
        fw = self.fw
        g = fw.sb([128, D], F32, "lng"); b = fw.sb([128, D], F32, "lnb")
        fw.dma("sp", g[:], self.din[which + "_g"][l].partition_broadcast(128), writes=[g])
        fw.dma("sp", b[:], self.din[which + "_b"][l].partition_broadcast(128), writes=[b])
        return g, b

    def ln_stats(self, z, s):
        fw, nc = self.fw, self.nc
        for hh in range(2):
            fw.op("dve", lambda hh=hh: nc.vector.bn_stats(out=s[:, hh * 6:hh * 6 + 6], in_=z[:, hh * 512:(hh + 1) * 512]), reads=[z], writes=[s])
        fw.op("dve", lambda: nc.vector.bn_aggr(out=s[:, 12:14], in_=s[:, 0:12]), reads=[s], writes=[s])
        fw.op("act", lambda: nc.scalar.activation(out=s[:, 15:16], in_=s[:, 13:14], func=AF.Sqrt, bias=self.epsln[:, 0:1], scale=1.0),
              reads=[s, self.epsln], writes=[s])
        fw.op("dve", lambda: nc.vector.reciprocal(out=s[:, 14:15], in_=s[:, 15:16]), reads=[s], writes=[s])

    def post_norm_store(self, z, s, g, b, dst):
        fw, nc = self.fw, self.nc
        self.ln_stats(z, s)
        fw.op("dve", lambda: nc.vector.tensor_scalar(out=z[:], in0=z[:], scalar1=s[:, 12:13], scalar2=s[:, 14:15], op0=ALU.subtract, op1=ALU.mult),
              reads=[z, s], writes=[z])
        fw.op("pool", lambda: nc.gpsimd.tensor_tensor(out=z[:], in0=z[:], in1=g[:], op=ALU.mult), reads=[z, g], writes=[z])
        fw.op("pool", lambda: nc.gpsimd.tensor_tensor(out=z[:], in0=z[:], in1=b[:], op=ALU.add), reads=[z, b], writes=[z])
        fw.dma("pool", dst, z[:], reads=[z])

    def xsrc(self, l, tok, first):
        if l == 0 and first:
            return self.din["x"][tok:tok + 128, :] if tok < self.T else self.din["ctx"][tok - self.T:tok - self.T + 128, :]
        return self.scr["XR"][tok:tok + 128, :]

    @_stage
    def stage_outproj(self, l, last):
        fw, nc = self.fw, self.nc
        T, NT = self.T, self.NT
        g_lat = fw.sb([128, D], F32, "glat"); g_ctx = fw.sb([128, D], F32, "gctx")
        pg = fw.ps([128, 2, 512], F32, "pg")
        repr_ = Ring([fw.sb([128, 128], F32, "rep") for _ in range(2)])
        self.bcast_mod(16, g_lat, g_ctx, pg, repr_)
        lg, lb = self.load_ln(l, "ln1")
        wout = fw.sb([128, 8, D], BF16, "wout")
        wsrc = self.din["w_out"][l].rearrange("(k p) n -> p k n", p=128)
        for k in range(8):
            fw.dma("pool", wout[:, k, :], wsrc[:, k, :], writes=[wout])
        otr = Ring([fw.sb([128, 8, 128], BF16, "oT") for _ in range(3)])
        xr = Ring([fw.sb([128, D], F32, "x") for _ in range(3)])
        zr = Ring([fw.sb([128, D], F32, "z") for _ in range(3)])
        sr = Ring([fw.sb([128, 16], F32, "st") for _ in range(3)])
        pyr = Ring([fw.ps([128, 2, 512], F32, "py") for _ in range(2)])
        ntile = (T if last else NT) // 128
        for ti in range(ntile):
            tok = ti * 128
            oT = otr.next()
            fw.dma("sp", oT[:], self.scr["OT"][:, tok:tok + 128].rearrange("(k p) t -> p k t", p=128), writes=[oT])
            x = xr.next()
            fw.dma("sp", x[:], self.xsrc(l, tok, True), writes=[x])
            py = pyr.next()
            for half in range(2):
                for k in range(8):
                    fw.op("pe", lambda half=half, k=k: nc.tensor.matmul(py[:, half, :], lhsT=oT[:, k, :], rhs=wout[:, k, half * 512:(half + 1) * 512],
                                                                         start=(k == 0), stop=(k == 7)), reads=[oT, wout], writes=[py], inc=(k == 7))
            gb = g_lat if tok < T else g_ctx
            z = zr.next()
            fw.op("dve", lambda: nc.vector.tensor_tensor(out=z[:].rearrange("p (a b) -> p a b", a=2), in0=py[:], in1=gb[:].rearrange("p (a b) -> p a b", a=2), op=ALU.mult),
                  reads=[py, gb], writes=[z])
            fw.op("dve", lambda: nc.vector.scalar_tensor_tensor(out=z[:], in0=x[:], scalar=ALPHA, in1=z[:], op0=ALU.mult, op1=ALU.add),
                  reads=[x, z], writes=[z])
            self.post_norm_store(z, sr.next(), lg, lb, self.scr["XR"][tok:tok + 128, :])

    @_stage
    def stage_router(self, l, last):
        fw, nc = self.fw, self.nc
        T, NT = self.T, self.NT
        rw = fw.sb([128, 8, NE], F32, "rw")
        fw.dma("sp", rw[:], self.din["router_w"][l].rearrange("(k p) e -> p k e", p=128), writes=[rw])
        rb = fw.sb([128, NE], F32, "rb")
        fw.dma("sp", rb[:], self.din["router_bias"][l].partition_broadcast(128), writes=[rb])
        xr = Ring([fw.sb([128, D], F32, "x") for _ in range(3)])
        xnr = Ring([fw.sb([128, D], F32, "xn") for _ in range(2)])
        sr = Ring([fw.sb([128, 16], F32, "st") for _ in range(3)])
        hbr = Ring([fw.sb([128, 8, 128], BF16, "hb") for _ in range(3)])
        hfr = Ring([fw.sb([128, 8, 128], F32, "hf") for _ in range(2)])
        scr_ = Ring([fw.sb([128, NE], F32, "sc") for _ in range(2)])
        bir = Ring([fw.sb([128, NE], F32, "bi") for _ in range(2)])
        m8r = Ring([fw.sb([128, 16], F32, "m8") for _ in range(2)])
        gtr = Ring([fw.sb([128, NE + 1], F32, "gt") for _ in range(3)])
        tpr = Ring([fw.ps([128, 8, 128], F32, "tp") for _ in range(1)])
        plr = Ring([fw.ps([128, NE], F32, "pl") for _ in range(1)])
        sc_lat = fw.sb([128, D], F32, "sclat"); sc_ctx = fw.sb([128, D], F32, "scctx")
        sh_lat = fw.sb([128, D], F32, "shlat"); sh_ctx = fw.sb([128, D], F32, "shctx")
        pg = fw.ps([128, 2, 512], F32, "pg")
        repr_ = Ring([fw.sb([128, 128], F32, "rep") for _ in range(2)])
        self.bcast_mod(32, sc_lat, sc_ctx, pg, repr_)
        self.bcast_mod(24, sh_lat, sh_ctx, pg, repr_)
        slb = fw.sb([128, 128], BF16, "slb"); onesb = fw.sb([128, 128], BF16, "onesb")
        fw.dma("pool", slb[:], self.din["moe_sl"], writes=[slb])
        fw.op("dve", lambda: nc.vector.memset(onesb[:], 1.0), writes=[onesb])
        fw.op("dve", lambda: nc.vector.memset(self.moe_run[:], 0.0), writes=[self.moe_run])
        xbr = Ring([fw.sb([128, D], BF16, "xb") for _ in range(2)])
        xtr = Ring([fw.sb([128, D], F32, "xt") for _ in range(2)])
        mkr = Ring([fw.sb([128, NE], BF16, "mk") for _ in range(2)])
        posr = Ring([fw.sb([128, NE], F32, "pos") for _ in range(2)])
        ppr = Ring([fw.ps([128, 2, NE], F32, "ppos") for _ in range(1)])
        ntile = (T if last else NT) // 128
        for ti in range(ntile):
            tok = ti * 128
            wh = 0 if tok < T else 1
            x = xr.next()
            fw.dma("sp", x[:], self.scr["XR"][tok:tok + 128, :], writes=[x])
            s = sr.next()
            self.ln_stats(x, s)
            xn = xnr.next()
            fw.op("dve", lambda: nc.vector.tensor_scalar(out=xn[:], in0=x[:], scalar1=s[:, 12:13], scalar2=s[:, 14:15], op0=ALU.subtract, op1=ALU.mult),
                  reads=[x, s], writes=[xn])
            tp = tpr.next()
            for k in range(8):
                fw.op("pe", lambda k=k: nc.tensor.transpose(out=tp[:, k, :], in_=xn[:, k * 128:(k + 1) * 128], identity=self.ident[:]),
                      reads=[xn, self.ident], writes=[tp], inc=(k == 7))
            hb, hf = hbr.next(), hfr.next()
            for k in range(8):
                sc_ = self.modv[:, 32 + k, wh:wh + 1]
                sh_ = self.modv[:, 24 + k, wh:wh + 1]
                fw.op("act", lambda k=k, sc_=sc_, sh_=sh_: nc.scalar.activation(out=hf[:, k, :], in_=tp[:, k, :], func=AF.Identity, bias=sh_, scale=sc_),
                      reads=[tp, self.modv], writes=[hf])
            fw.op("dve", lambda: nc.vector.tensor_copy(out=hb[:], in_=hf[:]), reads=[hf], writes=[hb])
            fw.dma("pool", self.scr["HT"][:, tok:tok + 128].rearrange("(k p) t -> p k t", p=128), hb[:], reads=[hb])
            xt, xb = xtr.next(), xbr.next()
            scb, shb = (sc_lat, sh_lat) if wh == 0 else (sc_ctx, sh_ctx)
            fw.op("pool", lambda: nc.gpsimd.tensor_tensor(out=xt[:], in0=xn[:], in1=scb[:], op=ALU.mult), reads=[xn, scb], writes=[xt])
            fw.op("pool", lambda: nc.gpsimd.tensor_tensor(out=xb[:], in0=xt[:], in1=shb[:], op=ALU.add), reads=[xt, shb], writes=[xb])
            fw.dma("pool", self.scr["XB"][tok:tok + 128, :], xb[:], reads=[xb])
            pl = plr.next()
            for k in range(8):
                fw.op("pe", lambda k=k: nc.tensor.matmul(pl[:, :], lhsT=hf[:, k, :], rhs=rw[:, k, :], start=(k == 0), stop=(k == 7)),
                      reads=[hf, rw], writes=[pl], inc=(k == 7))
            sc = scr_.next()
            fw.op("act", lambda: nc.scalar.activation(out=sc[:], in_=pl[:], func=AF.Sigmoid), reads=[pl], writes=[sc])
            bi = bir.next()
            fw.op("dve", lambda: nc.vector.tensor_tensor(out=bi[:], in0=sc[:], in1=rb[:], op=ALU.add), reads=[sc, rb], writes=[bi])
            m8 = m8r.next()
            fw.op("dve", lambda: nc.vector.max(out=m8[:, 0:8], in_=bi[:]), reads=[bi], writes=[m8])
            fw.op("dve", lambda: nc.vector.tensor_reduce(out=m8[:, 8:9], in_=m8[:, 0:8], axis=AX.X, op=ALU.min), reads=[m8], writes=[m8])
            fw.op("dve", lambda: nc.vector.tensor_scalar(out=bi[:], in0=bi[:], scalar1=m8[:, 8:9], scalar2=None, op0=ALU.is_ge), reads=[bi, m8], writes=[bi])
            fw.op("dve", lambda: nc.vector.tensor_tensor(out=sc[:], in0=sc[:], in1=bi[:], op=ALU.mult), reads=[sc, bi], writes=[sc])
            fw.op("dve", lambda: nc.vector.reduce_sum(out=m8[:, 9:10], in_=sc[:], axis=AX.X), reads=[sc], writes=[m8])
            fw.op("dve", lambda: nc.vector.reciprocal(out=m8[:, 10:11], in_=m8[:, 9:10]), reads=[m8], writes=[m8])
            gt = gtr.next()
            fw.op("dve", lambda: nc.vector.tensor_scalar(out=gt[:, 0:NE], in0=sc[:], scalar1=m8[:, 10:11], scalar2=2.5, op0=ALU.mult, op1=ALU.mult),
                  reads=[sc, m8], writes=[gt])
            fw.op("pool", lambda: nc.gpsimd.memset(gt[:, NE:NE + 1], 1.0), writes=[gt])
            fw.dma("pool", self.scr["GT"][ti], gt[:], reads=[gt])
            mk = mkr.next()
            fw.op("dve", lambda: nc.vector.tensor_copy(out=mk[:], in_=bi[:]), reads=[bi], writes=[mk])
            pp = ppr.next()
            fw.op("pe", lambda: nc.tensor.matmul(pp[:, 0, :], lhsT=slb[:], rhs=mk[:], start=True, stop=True), reads=[slb, mk], writes=[pp], inc=False)
            fw.op("pe", lambda: nc.tensor.matmul(pp[:, 1, :], lhsT=onesb[:], rhs=mk[:], start=True, stop=True), reads=[onesb, mk], writes=[pp])
            pos = posr.next()
            fw.op("dve", lambda: nc.vector.tensor_tensor(out=pos[:], in0=pp[:, 0, :], in1=self.moe_run[:], op=ALU.add), reads=[pp, self.moe_run], writes=[pos])
            fw.op("dve", lambda: nc.vector.tensor_tensor(out=self.moe_run[:], in0=pp[:, 1, :], in1=self.moe_run[:], op=ALU.add),
                  reads=[pp, self.moe_run], writes=[self.moe_run])
            fw.dma("pool", self.scr["POS"][ti], pos[:], reads=[pos])

    @_stage
    def stage_dispatch(self, l, last):
        fw, nc = self.fw, self.nc
        T, NT, NB = self.T, self.NT, self.NB
        U32 = mybir.dt.uint32
        ntile = (T if last else NT) // 128
        run = self.moe_run
        slf = fw.sb([128, 128], F32, "slf")
        fw.dma("sp", slf[:], self.din["moe_sl"], writes=[slf])
        biota = fw.sb([128, NB], F32, "biota")
        fw.dma("sp", biota[:], self.din["moe_biota"], writes=[biota])
        piota = fw.sb([128, 1], F32, "piota")
        fw.dma("sp", piota[:], self.din["moe_piota"], writes=[piota])
        eiota = fw.sb([128, NE], F32, "eiota")
        fw.dma("sp", eiota[:], self.din["moe_eiota"], writes=[eiota])
        a = fw.sb([128, NE], F32, "a"); b_ = fw.sb([128, NE], F32, "b"); ci = fw.sb([128, NE], I32, "ci")
        cnd = fw.sb([128, NE], F32, "cnd"); padded = fw.sb([128, NE], F32, "padded")
        V = nc.vector
        fw.op("dve", lambda: V.tensor_scalar(out=a[:], in0=run[:], scalar1=127.0, scalar2=1.0 / 128, op0=ALU.add, op1=ALU.mult), reads=[run], writes=[a])
        fw.op("dve", lambda: V.tensor_scalar_add(out=a[:], in0=a[:], scalar1=-0.49609375), reads=[a], writes=[a])
        fw.op("dve", lambda: V.tensor_copy(out=ci[:], in_=a[:]), reads=[a], writes=[ci])
        fw.op("dve", lambda: V.tensor_copy(out=cnd[:], in_=ci[:]), reads=[ci], writes=[cnd])
        fw.op("dve", lambda: V.tensor_scalar(out=a[:], in0=cnd[:], scalar1=128.0, scalar2=None, op0=ALU.mult), reads=[cnd], writes=[a])
        fw.op("dve", lambda: V.tensor_tensor(out=b_[:], in0=a[:], in1=run[:], op=ALU.is_lt), reads=[a, run], writes=[b_])
        fw.op("dve", lambda: V.tensor_tensor(out=cnd[:], in0=cnd[:], in1=b_[:], op=ALU.add), reads=[cnd, b_], writes=[cnd])
        fw.op("dve", lambda: V.tensor_scalar(out=a[:], in0=cnd[:], scalar1=128.0, scalar2=-128.0, op0=ALU.mult, op1=ALU.add), reads=[cnd], writes=[a])
        fw.op("dve", lambda: V.tensor_tensor(out=b_[:], in0=a[:], in1=run[:], op=ALU.is_ge), reads=[a, run], writes=[b_])
        fw.op("dve", lambda: V.tensor_tensor(out=cnd[:], in0=cnd[:], in1=b_[:], op=ALU.subtract), reads=[cnd, b_], writes=[cnd])
        fw.op("dve", lambda: V.tensor_scalar(out=padded[:], in0=cnd[:], scalar1=128.0, scalar2=None, op0=ALU.mult), reads=[cnd], writes=[padded])
        pt = fw.ps([128, 128], F32, "pt"); pst = fw.ps([128, 128], F32, "pst")
        padT = fw.sb([128, 128], F32, "padT"); pstart = fw.sb([128, NE], F32, "pstart"); pend = fw.sb([128, NE], F32, "pend")
        pendT = fw.sb([128, 128], F32, "pendT")
        fw.op("pe", lambda: nc.tensor.transpose(out=pt[:], in_=padded[:], identity=self.ident[:]), reads=[padded, self.ident], writes=[pt])
        fw.op("act", lambda: nc.scalar.copy(out=padT[:], in_=pt[:]), reads=[pt], writes=[padT])
        fw.op("pe", lambda: nc.tensor.matmul(pst[:], lhsT=padT[:], rhs=slf[:], start=True, stop=True), reads=[padT, slf], writes=[pst])
        fw.op("act", lambda: nc.scalar.copy(out=pstart[:], in_=pst[:]), reads=[pst], writes=[pstart])
        fw.op("dve", lambda: V.tensor_tensor(out=pend[:], in0=pstart[:], in1=padded[:], op=ALU.add), reads=[pstart, padded], writes=[pend])
        fw.op("pe", lambda: nc.tensor.transpose(out=pt[:], in_=pend[:], identity=self.ident[:]), reads=[pend, self.ident], writes=[pt])
        fw.op("act", lambda: nc.scalar.copy(out=pendT[:], in_=pt[:]), reads=[pt], writes=[pendT])
        cmp_ = fw.sb([128, NB], F32, "cmp"); ebc = fw.sb([128, NB], F32, "ebc"); chg = fw.sb([128, NB], F32, "chg")
        pe_ = fw.ps([128, 2, 512], F32, "pe")
        fw.op("dve", lambda: V.tensor_scalar(out=cmp_[:], in0=biota[:], scalar1=pendT[:, 0:1], scalar2=None, op0=ALU.is_ge), reads=[biota, pendT], writes=[cmp_])
        for hb in range(2):
            c0, c1 = hb * 512, min(NB, hb * 512 + 512)
            if c0 >= NB:
                continue
            fw.op("pe", lambda hb=hb, c0=c0, c1=c1: nc.tensor.matmul(pe_[:, hb, 0:c1 - c0], lhsT=self.ones[:], rhs=cmp_[:, c0:c1], start=True, stop=True),
                  reads=[self.ones, cmp_], writes=[pe_])
            fw.op("dve", lambda hb=hb, c0=c0, c1=c1: V.tensor_scalar_min(out=ebc[:, c0:c1], in0=pe_[:, hb, 0:c1 - c0], scalar1=float(NE - 1)), reads=[pe_], writes=[ebc])
        fw.op("dve", lambda: V.memset(chg[:, 0:1], 1.0), writes=[chg])
        fw.op("dve", lambda: V.tensor_tensor(out=chg[:, 1:NB], in0=ebc[:, 1:NB], in1=ebc[:, 0:NB - 1], op=ALU.not_equal), reads=[ebc], writes=[chg])
        fw.op("dve", lambda: V.tensor_scalar(out=chg[:], in0=chg[:], scalar1=-1.0e7, scalar2=1.0e7, op0=ALU.mult, op1=ALU.add), reads=[chg], writes=[chg])
        fw.op("dve", lambda: V.scalar_tensor_tensor(out=cmp_[:], in0=ebc[:], scalar=128.0, in1=chg[:], op0=ALU.mult, op1=ALU.add), reads=[ebc, chg], writes=[cmp_])
        fw.op("dve", lambda: V.tensor_scalar_add(out=cmp_[:], in0=cmp_[:], scalar1=float(l * NE * 128)), reads=[cmp_], writes=[cmp_])
        fw.op("dve", lambda: V.tensor_scalar(out=cmp_[:], in0=cmp_[:], scalar1=piota[:, 0:1], scalar2=None, op0=ALU.add), reads=[cmp_, piota], writes=[cmp_])
        fw.op("dve", lambda: V.tensor_copy(out=self.widx[:], in_=cmp_[:]), reads=[cmp_], writes=[self.widx])
        gtr = Ring([fw.sb([128, NE + 1], F32, "gt") for _ in range(2)])
        posr = Ring([fw.sb([128, NE], F32, "pos") for _ in range(2)])
        keyr = Ring([fw.sb([128, NE], F32, "key") for _ in range(2)])
        v8r = Ring([fw.sb([128, 8], F32, "v8") for _ in range(2)])
        d8r = Ring([fw.sb([128, 8], F32, "d8") for _ in range(2)])
        jr = Ring([fw.sb([128, NE], F32, "junk") for _ in range(2)])
        xbr = Ring([fw.sb([128, D], BF16, "xb") for _ in range(3)])
        for ti in range(ntile):
            tok = ti * 128
            gt, pos = gtr.next(), posr.next()
            fw.dma("sp", gt[:], self.scr["GT"][ti], writes=[gt])
            fw.dma("sp", pos[:], self.scr["POS"][ti], writes=[pos])
            xb = xbr.next()
            fw.dma("sp", xb[:], self.scr["XB"][tok:tok + 128, :], writes=[xb])
            fw.op("dve", lambda: V.tensor_tensor(out=pos[:], in0=pos[:], in1=pstart[:], op=ALU.add), reads=[pos, pstart], writes=[pos])
            key = keyr.next()
            fw.op("dve", lambda: V.scalar_tensor_tensor(out=key[:], in0=gt[:, 0:NE], scalar=0.0, in1=eiota[:], op0=ALU.is_gt, op1=ALU.mult), reads=[gt, eiota], writes=[key])
            v8, d8 = v8r.next(), d8r.next()
            fw.op("dve", lambda: V.max(out=v8[:], in_=key[:]), reads=[key], writes=[v8])
            fw.op("dve", lambda: V.memset(d8[:], 0.0), writes=[d8])
            fw.op("dve", lambda: V.memset(self.gate8[:, ti, :], 0.0), writes=[self.gate8])
            for k in range(8):
                j1, j2 = jr.next(), jr.next()
                fw.op("dve", lambda k=k, j1=j1: V.scalar_tensor_tensor(out=j1[:], in0=key[:], scalar=v8[:, k:k + 1], in1=pos[:], op0=ALU.is_equal, op1=ALU.mult,
                                                                       accum_out=d8[:, k:k + 1]), reads=[key, v8, pos], writes=[j1, d8])
                fw.op("dve", lambda k=k, j2=j2: V.scalar_tensor_tensor(out=j2[:], in0=key[:], scalar=v8[:, k:k + 1], in1=gt[:, 0:NE], op0=ALU.is_equal, op1=ALU.mult,
                                                                       accum_out=self.gate8[:, ti, k:k + 1]), reads=[key, v8, gt], writes=[j2, self.gate8])
            fw.op("dve", lambda: V.tensor_copy(out=self.dest8[:, ti, :], in_=d8[:]), reads=[d8], writes=[self.dest8])
            for k in range(8):
                fw.idma(self.scr["XS"], bass.IndirectOffsetOnAxis(ap=self.dest8[:, ti, k:k + 1], axis=0), xb[:], None, reads=[xb, self.dest8])

    @_stage
    def stage_blocks(self, l, last):
        fw, nc = self.fw, self.nc
        T, NT = self.T, self.NT
        ntile = (T if last else NT) // 128
        NBl = ntile * 8 + NE
        w1v, w3v, w2v = self.din["exp_w1"], self.din["exp_w3"], self.din["exp_w2"]
        w1_ = fw.sb([128, 2048], BF16, "w1"); w3_ = fw.sb([128, 2048], BF16, "w3"); w2_ = fw.sb([128, 2048], BF16, "w2")

        class V3:
            def __init__(self, b, k):
                self.b, self.k = b, k

            def __getitem__(self, key):
                return self.b[:].rearrange("p (k f) -> p k f", k=self.k)[key]
        w1, w3, w2 = V3(w1_, 8), V3(w3_, 8), V3(w2_, 2)
        xsr = Ring([fw.sb([128, D], BF16, "xs") for _ in range(3)])
        xTr = Ring([fw.sb([128, 8, 128], BF16, "xT") for _ in range(2)])
        sar = Ring([fw.sb([128, 256], F32, "sa") for _ in range(2)])
        hdr = Ring([fw.sb([128, 256], BF16, "hd") for _ in range(2)])
        hTr = Ring([fw.sb([128, 2, 128], BF16, "hdT") for _ in range(2)])
        ysr = Ring([fw.sb([128, D], BF16, "ys") for _ in range(2)])
        ptx = Ring([fw.ps([128, 8, 128], BF16, "ptx") for _ in range(2)])
        pab = Ring([fw.ps([128, 512], F32, "pab") for _ in range(2)])
        pth = Ring([fw.ps([128, 2, 128], BF16, "pth") for _ in range(1)])
        pyr = Ring([fw.ps([128, 2, 512], F32, "py") for _ in range(1)])
        if not hasattr(self, "bound_reg"):
            self.bound_reg = nc.gpsimd.alloc_register("moe_bound")
        nc.gpsimd.reg_mov(self.bound_reg, (l + 1) * NE * 128 - 1)
        bound = self.bound_reg
        for b in range(NBl):
            off = bass.IndirectOffsetOnAxis(ap=self.widx[:, b:b + 1], axis=0)
            for (wt, wv) in ((w1_, w1v), (w3_, w3v), (w2_, w2v)):
                fw.idma(wt[:], None, wv, off, reads=[self.widx], writes=[wt], bounds_check=bound, oob_is_err=False)
            xs = xsr.next()
            fw.dma("sp", xs[:], self.scr["XS"][b * 128:(b + 1) * 128, :], writes=[xs])
            px = ptx.next()
            for k in range(8):
                fw.op("pe", lambda k=k: nc.tensor.transpose(out=px[:, k, :], in_=xs[:].rearrange("p (f k) -> p k f", k=8)[:, k, :], identity=self.ident_bf[:]),
                      reads=[xs, self.ident_bf], writes=[px], inc=(k == 7))
            xT = xTr.next()
            fw.op("act", lambda: nc.scalar.copy(out=xT[:], in_=px[:]), reads=[px], writes=[xT])
            pa = pab.next()
            for (wt, wb, c0) in ((w1, w1_, 0), (w3, w3_, 256)):
                for k in range(8):
                    fw.op("pe", lambda wt=wt, c0=c0, k=k: nc.tensor.matmul(pa[:, c0:c0 + 256], lhsT=xT[:, k, :], rhs=wt[:, k, :], start=(k == 0), stop=(k == 7)),
                          reads=[xT, wb], writes=[pa], inc=(k == 7))
            sa, hd = sar.next(), hdr.next()
            fw.op("act", lambda: nc.scalar.activation(out=sa[:], in_=pa[:, 0:256], func=AF.Silu), reads=[pa], writes=[sa])
            fw.op("dve", lambda: nc.vector.tensor_tensor(out=hd[:], in0=sa[:], in1=pa[:, 256:512], op=ALU.mult), reads=[sa, pa], writes=[hd])
            ph = pth.next()
            for k in range(2):
                fw.op("pe", lambda k=k: nc.tensor.transpose(out=ph[:, k, :], in_=hd[:].rearrange("p (f k) -> p k f", k=2)[:, k, :], identity=self.ident_bf[:]),
                      reads=[hd, self.ident_bf], writes=[ph], inc=(k == 1))
            hT = hTr.next()
            fw.op("dve", lambda: nc.vector.tensor_copy(out=hT[:], in_=ph[:]), reads=[ph], writes=[hT])
            py = pyr.next()
            for half in range(2):
                for k in range(2):
                    fw.op("pe", lambda half=half, k=k: nc.tensor.matmul(py[:, half, :], lhsT=hT[:, k, :], rhs=w2[:, k, half * 512:(half + 1) * 512],
                                                                         start=(k == 0), stop=(k == 1)), reads=[hT, w2_], writes=[py], inc=(k == 1))
            ys = ysr.next()
            fw.op("act", lambda: nc.scalar.copy(out=ys[:, 0:512], in_=py[:, 0, :]), reads=[py], writes=[ys])
            fw.op("dve", lambda: nc.vector.tensor_copy(out=ys[:, 512:1024], in_=py[:, 1, :]), reads=[py], writes=[ys])
            fw.dma("sp", self.scr["YS"][b * 128:(b + 1) * 128, :], ys[:], reads=[ys])

    @_stage
    def stage_experts(self, l, last, G=16, experts=(NE,)):
        fw, nc = self.fw, self.nc
        T, NT = self.T, self.NT
        g_lat = fw.sb([128, D], F32, "glat"); g_ctx = fw.sb([128, D], F32, "gctx")
        pg = fw.ps([128, 2, 512], F32, "pg")
        repr_ = Ring([fw.sb([128, 128], F32, "rep") for _ in range(2)])
        self.bcast_mod(40, g_lat, g_ctx, pg, repr_)
        lg, lb = self.load_ln(l, "ln2")
        ntile = (T if last else NT) // 128
        acc = fw.sb([128, G, D], F32, "acc")
        hT = fw.sb([128, 8, G * 128], BF16, "hTg")
        gts = fw.sb([128, G, NE + 1], F32, "gts")
        w1r = Ring([fw.sb([128, 8, 256], BF16, "w1") for _ in range(2)])
        w3r = Ring([fw.sb([128, 8, 256], BF16, "w3") for _ in range(2)])
        w2r = Ring([fw.sb([128, 2, D], BF16, "w2") for _ in range(2)])
        sar = Ring([fw.sb([128, 2, 512], BF16, "sa") for _ in range(2)])
        hdr = Ring([fw.sb([128, 2, 512], BF16, "hd") for _ in range(2)])
        xr = Ring([fw.sb([128, D], F32, "x") for _ in range(2)])
        ykr = Ring([fw.sb([128, D], BF16, "yk") for _ in range(4)])
        sr = Ring([fw.sb([128, 16], F32, "st") for _ in range(2)])
        par = Ring([fw.ps([128, 2, 512], F32, "pa") for _ in range(1)])
        pbr = Ring([fw.ps([128, 2, 512], F32, "pb3") for _ in range(1)])
        pyr = Ring([fw.ps([128, 512], F32, "py") for _ in range(2)])
        dn = self.din
        for g0 in range(0, ntile, G):
            gn = min(G, ntile - g0)
            tok0 = g0 * 128
            for k in range(8):
                fw.dma("sp", hT[:, k, 0:gn * 128], self.scr["HT"][k * 128:(k + 1) * 128, tok0:tok0 + gn * 128], writes=[hT])
            fw.dma("sp", gts[:, 0:gn, :], self.scr["GT"][g0:g0 + gn].rearrange("g p e -> p g e"), writes=[gts])
            for ei, e in enumerate(experts):
                w1, w3, w2 = w1r.next(), w3r.next(), w2r.next()
                if e < NE:
                    raise NotImplementedError("routed experts run in stage_blocks")
                else:
                    s1, s3, s2 = dn["sh_w1"][l], dn["sh_w3"][l], dn["sh_w2"][l]
                fw.dma("pool", w1[:], s1.rearrange("(k p) f -> p k f", p=128), writes=[w1])
                fw.dma("pool", w3[:], s3.rearrange("(k p) f -> p k f", p=128), writes=[w3])
                fw.dma("pool", w2[:], s2.rearrange("(k p) n -> p k n", p=128), writes=[w2])
                for c0 in range(0, gn, 4):
                    cn_ = min(4, gn - c0)
                    n = cn_ * 128
                    pa, pb = par.next(), pbr.next()
                    for (w, p) in ((w1, pa), (w3, pb)):
                        for hc in range(2):
                            for k in range(8):
                                fw.op("pe", lambda w=w, p=p, hc=hc, k=k: nc.tensor.matmul(p[:, hc, 0:n], lhsT=w[:, k, hc * 128:(hc + 1) * 128],
                                                                                         rhs=hT[:, k, c0 * 128:c0 * 128 + n], start=(k == 0), stop=(k == 7)),
                                      reads=[w, hT], writes=[p], inc=(k == 7))
                    sa, hd = sar.next(), hdr.next()
                    for hc in range(2):
                        fw.op("act", lambda hc=hc: nc.scalar.activation(out=sa[:, hc, 0:n], in_=pa[:, hc, 0:n], func=AF.Silu), reads=[pa], writes=[sa])
                        fw.op("dve", lambda hc=hc: nc.vector.tensor_tensor(out=hd[:, hc, 0:n], in0=sa[:, hc, 0:n], in1=pb[:, hc, 0:n], op=ALU.mult),
                              reads=[sa, pb], writes=[hd])
                    for t in range(cn_):
                        gi = c0 + t
                        for half in range(2):
                            py = pyr.next()
                            for hc in range(2):
                                fw.op("pe", lambda t=t, half=half, hc=hc, py=py: nc.tensor.matmul(py[:, :], lhsT=hd[:, hc, t * 128:(t + 1) * 128],
                                                                                                 rhs=w2[:, hc, half * 512:(half + 1) * 512], start=(hc == 0), stop=(hc == 1)),
                                      reads=[hd, w2], writes=[py], inc=(hc == 1))
                            a = acc[:, gi, half * 512:(half + 1) * 512]
                            gsc = gts[:, gi, e:e + 1]
                            if ei == 0:
                                fw.op("dve", lambda a=a, py=py, gsc=gsc: nc.vector.tensor_scalar(out=a, in0=py[:], scalar1=gsc, scalar2=None, op0=ALU.mult),
                                      reads=[py, gts], writes=[acc])
                            else:
                                fw.op("dve", lambda a=a, py=py, gsc=gsc: nc.vector.scalar_tensor_tensor(out=a, in0=py[:], scalar=gsc, in1=a, op0=ALU.mult, op1=ALU.add),
                                      reads=[py, gts, acc], writes=[acc])
            for gi in range(gn):
                tok = tok0 + gi * 128
                x = xr.next()
                fw.dma("sp", x[:], self.scr["XR"][tok:tok + 128, :], writes=[x])
                gb = g_lat if tok < T else g_ctx
                if NE not in experts or len(experts) == 1:
                    ti = tok // 128
                    for k in range(8):
                        yk = ykr.next()
                        fw.idma(yk[:], None, self.scr["YS"], bass.IndirectOffsetOnAxis(ap=self.dest8[:, ti, k:k + 1], axis=0), reads=[self.dest8], writes=[yk])
                        fw.op("dve", lambda gi=gi, yk=yk, ti=ti, k=k: nc.vector.scalar_tensor_tensor(
                            out=acc[:, gi, :], in0=yk[:], scalar=self.gate8[:, ti, k:k + 1], in1=acc[:, gi, :], op0=ALU.mult, op1=ALU.add),
                            reads=[yk, self.gate8, acc], writes=[acc])
                fw.op("dve", lambda gi=gi, gb=gb: nc.vector.tensor_tensor(out=acc[:, gi, :], in0=acc[:, gi, :], in1=gb[:], op=ALU.mult), reads=[acc, gb], writes=[acc])
                fw.op("dve", lambda gi=gi, x=x: nc.vector.scalar_tensor_tensor(out=x[:], in0=x[:], scalar=ALPHA, in1=acc[:, gi, :], op0=ALU.mult, op1=ALU.add),
                      reads=[x, acc], writes=[x])
                if last:
                    dst = self.out[tok:tok + 128, :]
                else:
                    dst = self.scr["XR"][tok:tok + 128, :]
                self.post_norm_store(x, sr.next(), lg, lb, dst)

    def build(self, upto=None):
        from contextlib import ExitStack
        self.declare()
        names = ["mod", "inproj", "wa_prep", "mla_prep", "na_attn", "wa_attn", "mla_attn", "gla", "outproj", "router", "dispatch", "blocks", "experts"]
        cnt = 0
        with ExitStack() as gst:
            self.setup_global(gst)
            self.fw.barrier()
            for l in range(self.L):
                last = l == self.L - 1
                for nm in names:
                    if upto is not None and cnt >= upto:
                        break
                    cnt += 1
                    fn = getattr(self, "stage_" + nm)
                    if nm in ("na_attn", "wa_attn", "mla_attn"):
                        fn(l, not last)
                    elif nm in ("outproj", "router", "dispatch", "blocks", "experts"):
                        fn(l, last)
                    else:
                        fn(l)
            self.fw.barrier()
        return self.nc


def prep_shared(inp, T, L):
    f = lambda a: np.ascontiguousarray(np.asarray(a, dtype=np.float32))
    m = dict(host_consts(T))
    m["w_ada"] = f(inp["w_ada"][:L])
    m["b_ada"] = f(inp["b_ada"][:L])
    m["b_ada_pj"] = f(np.asarray(inp["b_ada"][:L]).reshape(L, 48, 128).transpose(0, 2, 1))
    m["w_in"] = f(inp["w_in"][:L])
    m["na_rpb"] = f(inp["na_rpb"][:L]); m["wa_sink"] = f(inp["wa_sink"][:L])
    m["mla_g_q"] = f(np.asarray(inp["mla_g_q"][:L]).reshape(L, 2, 128).transpose(0, 2, 1))
    m["mla_g_kv"] = f(np.asarray(inp["mla_g_kv"][:L]).reshape(L, 128, 1))
    m["mla_w_uq"] = f(inp["mla_w_uq"][:L]); m["mla_w_ukv"] = f(inp["mla_w_ukv"][:L])
    for k in ("gla_w_gf", "gla_b_gf", "gla_w_gb", "gla_b_gb"):
        m[k] = f(inp[k][:L])
    m["gla_g_norm"] = f(np.asarray(inp["gla_g_norm"][:L]).reshape(L, 64, 1))
    for k in ("w_out", "ln1_g", "ln1_b", "ln2_g", "ln2_b", "router_w", "router_bias", "sh_w1", "sh_w3", "sh_w2"):
        m[k] = f(inp[k][:L])
    for k in ("exp_w1", "exp_w3", "exp_w2"):
        m[k] = f(inp[k][:L]).reshape(L * NE * 128, 2048)
    return m


def prep_core(inp, b):
    f = lambda a: np.ascontiguousarray(np.asarray(a, dtype=np.float32))
    c = np.asarray(inp["c"][b], dtype=np.float32)
    cc = np.asarray(inp["c_ctx"], dtype=np.float32)
    cv = np.stack([c.reshape(8, 128).T, cc.reshape(8, 128).T], axis=-1)
    return {"x": f(inp["x"][b]), "ctx": f(inp["ctx"][b]), "cv": f(cv)}


_CACHE = {}


def kernel(**inputs):
    B, T, _ = inputs["x"].shape
    L = inputs["w_ada"].shape[0]
    key = (T, L)
    if key not in _CACHE:
        _CACHE[key] = KB(T, L).build()
    nc = _CACHE[key]
    shared = prep_shared(inputs, T, L)
    in_maps = []
    for b in range(B):
        m = dict(shared)
        m.update(prep_core(inputs, b))
        in_maps.append(m)
    res = run_bass_kernel_spmd(nc, in_maps, core_ids=list(range(B)))
    return np.stack([np.asarray(res.results[b]["out"], dtype=np.float32) for b in range(B)], axis=0)
```

```python
import numpy as np
import ml_dtypes
import concourse.bass as bass
import concourse.mybir as mybir
from concourse.bass_utils import run_bass_kernel_spmd

F32 = mybir.dt.float32
BF16 = mybir.dt.bfloat16
I32 = mybir.dt.int32
AF = mybir.ActivationFunctionType
ALU = mybir.AluOpType
AX = mybir.AxisListType

D = 1024
C = 256
NE = 128
LN_EPS = 1e-5
RMS_EPS = 1e-6
ALPHA = 8 ** 0.25
IN_W = 2496
O_AQ, O_AK, O_AV = 0, 256, 512
O_BQ, O_BK, O_BV = 768, 1024, 1152
O_CQ, O_CKV, O_CKR = 1280, 1536, 1664
O_DQ, O_DK, O_DV, O_DR, O_ZF, O_ZB = 1696, 1824, 1952, 2208, 2464, 2480


class Buf:
    def __init__(self, t, name):
        self.t = t
        self.name = name
        self.w = None
        self.r = []

    def __getitem__(self, k):
        return self.t[k]


class FW:
    def __init__(self, nc):
        self.nc = nc
        self.eng = {"pe": nc.tensor, "act": nc.scalar, "dve": nc.vector, "pool": nc.gpsimd, "sp": nc.sync}
        self.sem = {}
        self.cnt = {}
        self.waited = {}
        for e in self.eng:
            self.sem[e] = nc.alloc_semaphore("s_" + e)
            self.cnt[e] = 0
            self.waited[e] = {}
        self.pend = {e: False for e in self.eng}
        self.dsem = {}
        self.dval = {}
        self.drr = {}
        for q in ("sp", "pool", "act"):
            self.dsem[q] = [nc.alloc_semaphore("d_%s%d" % (q, i)) for i in range(12)]
            self.dval[q] = [0] * 12
            self.drr[q] = 0
        self.semkey = {}
        self.nbuf = 0

    def sb(self, shape, dt, name=None):
        self.nbuf += 1
        name = (name or "t") + "_%d" % self.nbuf
        return Buf(self.stack.enter_context(self.nc.sbuf_tensor(name, list(shape), dt)), name)

    def ps(self, shape, dt=F32, name=None):
        self.nbuf += 1
        name = (name or "p") + "_%d" % self.nbuf
        return Buf(self.stack.enter_context(self.nc.psum_tensor(name, list(shape), dt)), name)

    def _wait(self, e, tok):
        if tok is None:
            return
        sem, val, key = tok
        if self.waited[e].get(key, 0) >= val:
            return
        self.eng[e].wait_ge(sem, val)
        self.waited[e][key] = val

    def _deps(self, e, reads, writes):
        for b in reads:
            if b.w is not None:
                if not (e == "pe" and b.w[2] == "pe"):
                    self._wait(e, b.w)
        for b in writes:
            if b.w is not None and b.w[2] != e:
                self._wait(e, b.w)
            for tok in b.r:
                if tok[2] != e:
                    self._wait(e, tok)

    def _mark(self, tok, reads, writes):
        for b in reads:
            b.r.append(tok)
            if len(b.r) > 24:
                last = {}
                for t in b.r:
                    if t[2] not in last or last[t[2]][1] < t[1]:
                        last[t[2]] = t
                b.r = list(last.values())
        for b in writes:
            b.w = tok
            b.r = []

    def op(self, e, fn, reads=(), writes=(), inc=True):
        self._deps(e, reads, writes)
        ins = fn()
        if inc:
            self.cnt[e] += 1
            ins.then_inc(self.sem[e], 1)
            self.pend[e] = False
            tok = (self.sem[e], self.cnt[e], e)
        else:
            self.pend[e] = True
            tok = (self.sem[e], self.cnt[e] + 1, e)
        self._mark(tok, reads, writes)
        return tok

    def dma(self, q, out, in_, reads=(), writes=()):
        self._deps(q, reads, writes)
        i = self.drr[q]
        self.drr[q] = (i + 1) % len(self.dsem[q])
        sem = self.dsem[q][i]
        key = "d_%s%d" % (q, i)
        if self.dval[q][i] > 0:
            self._wait(q, (sem, self.dval[q][i], key))
        self.dval[q][i] += 16
        self.eng[q].dma_start(out=out, in_=in_).then_inc(sem, 16)
        tok = (sem, self.dval[q][i], key)
        self._mark(tok, reads, writes)
        return tok

    def idma(self, out, out_off, in_, in_off, reads=(), writes=(), **kw):
        q = "pool"
        self._deps(q, reads, writes)
        i = self.drr[q]
        self.drr[q] = (i + 1) % len(self.dsem[q])
        sem = self.dsem[q][i]
        key = "d_%s%d" % (q, i)
        if self.dval[q][i] > 0:
            self._wait(q, (sem, self.dval[q][i], key))
        self.dval[q][i] += 16
        self.nc.gpsimd.indirect_dma_start(out=out, out_offset=out_off, in_=in_, in_offset=in_off, **kw).then_inc(sem, 16)
        tok = (sem, self.dval[q][i], key)
        self._mark(tok, reads, writes)
        return tok

    def barrier(self):
        for e in self.eng:
            assert not self.pend[e], "pending non-inc op on " + e
        toks = []
        for e in self.eng:
            if self.cnt[e] > 0:
                toks.append((self.sem[e], self.cnt[e], e))
        for q in self.dsem:
            for i, s in enumerate(self.dsem[q]):
                if self.dval[q][i] > 0:
                    toks.append((s, self.dval[q][i], "d_%s%d" % (q, i)))
        for e in self.eng:
            for t in toks:
                if t[2] != e:
                    self._wait(e, t)


class Ring:
    def __init__(self, bufs):
        self.bufs = bufs
        self.i = 0

    def next(self):
        b = self.bufs[self.i]
        self.i = (self.i + 1) % len(self.bufs)
        return b


def host_consts(T):
    cs = {}
    cs["ident"] = np.eye(128, dtype=np.float32)
    t = np.arange(T)
    row, col = (t // 64).astype(np.float32), (t % 64).astype(np.float32)

    def rope_tabs(d):
        half = d // 2
        inv = (10000.0 ** (-np.arange(half, dtype=np.float32) / half)).astype(np.float32)
        cos = np.zeros((2 * d, T), np.float32)
        sin = np.zeros((2 * d, T), np.float32)
        R = np.zeros((2 * d, 2 * d), np.float32)
        for a, pos in enumerate((row, col)):
            ang = pos[None, :] * inv[:, None]
            for hh in range(2):
                cos[a * d + hh * half:a * d + (hh + 1) * half] = np.cos(ang)
                sin[a * d + hh * half:a * d + (hh + 1) * half] = np.sin(ang)
            for i in range(half):
                R[a * d + i, a * d + half + i] = -1.0
                R[a * d + half + i, a * d + i] = 1.0
        return cos, sin, R

    cw, sw, Rw = rope_tabs(32)
    cs["cos_wa"], cs["sin_wa"], cs["rt_wa"] = cw, sw, np.ascontiguousarray(Rw.T)
    cm, sm, Rm = rope_tabs(16)
    cos_m = np.ones((64, T), np.float32); sin_m = np.zeros((64, T), np.float32); R64 = np.zeros((64, 64), np.float32)
    cos_m[32:], sin_m[32:], R64[32:, 32:] = cm, sm, Rm
    cs["cos_mla"], cs["sin_mla"], cs["rt_mla"] = cos_m, sin_m, np.ascontiguousarray(R64.T)
    kk = np.arange(128)[:, None]; qq = np.arange(512)[None, :]
    m = np.zeros((6, 128, 512), np.float32)
    for r in range(-1, 5):
        m[r + 1] = (np.abs(r * 128 + kk - qq) <= 128)
    cs["maskwa"] = m
    oh = np.zeros((31, 64, 64), np.float32)
    cmask = np.zeros((64, 15, 64), np.float32)
    for c_ in range(64):
        cs0 = min(max(c_ - 8, 0), 48)
        for kc in range(64):
            j = kc - c_ + 15
            if 0 <= j < 31:
                oh[j, c_, kc] = 1.0
            if cs0 <= kc < cs0 + 16:
                cmask[kc, :, c_] = 1.0
    cs["na_oh"] = oh
    cs["na_cmask"] = cmask
    cs["j15"] = np.ascontiguousarray(np.eye(15, dtype=np.float32)[::-1])
    a = np.arange(128)
    same = (a[:, None] // 64) == (a[None, :] // 64)
    le = a[:, None] <= a[None, :]
    lt = a[:, None] < a[None, :]
    g = np.zeros((4, 128, 128), np.float32)
    g[0] = same & le
    g[1] = same & le.T
    g[2] = same & lt.T
    g[3] = same & lt
    cs["gla_tri"] = (g * (-1.0 / 16.0)).astype(np.float32)
    mk = np.zeros((2, 128, 4, 128), np.float32)
    mk[0] = (same & le)[:, None, :]
    mk[1] = (same & le.T)[:, None, :]
    cs["gla_mask"] = mk
    NB = (T + C) // 128 * 8 + NE
    cs["moe_sl"] = np.triu(np.ones((128, 128), np.float32), 1)
    cs["moe_biota"] = np.tile((np.arange(NB, dtype=np.float32) * 128.0)[None, :], (128, 1))
    cs["moe_piota"] = np.arange(128, dtype=np.float32).reshape(128, 1)
    cs["moe_eiota"] = np.tile((128.0 - np.arange(128, dtype=np.float32))[None, :], (128, 1))
    return cs


class K:
    def __init__(self, T, L, debug=()):
        self.T, self.L = T, L
        self.NT = T + C
        self.debug = set(debug)
        nc = bass.Bass("TRN2", target_bir_lowering=False)
        self.nc = nc
        self.fw = FW(nc)
        self.din = {}
        self.scr = {}

    def inp(self, name, shape, dt=F32):
        self.din[name] = self.nc.dram_tensor(name, list(shape), dt, kind="ExternalInput").ap()
        return self.din[name]

    def scratch(self, name, shape, dt):
        kind = "ExternalOutput" if name in self.debug else "Internal"
        self.scr[name] = self.nc.dram_tensor(name, list(shape), dt, kind=kind).ap()
        return self.scr[name]

    def chunks(self, n=512):
        out = []
        t = 0
        while t < self.NT:
            m = min(n, self.NT - t)
            out.append((t, m))
            t += m
        return out

    def declare(self):
        T, L, NT = self.T, self.L, self.NT
        i = self.inp
        i("x", [T, D]); i("ctx", [C, D]); i("cv", [128, 8, 2])
        i("w_ada", [L, D, 6 * D]); i("b_ada_pj", [L, 128, 48]); i("b_ada", [L, 6 * D])
        i("w_in", [L, D, IN_W])
        i("ident", [128, 128])
        i("cos_wa", [64, T]); i("sin_wa", [64, T]); i("rt_wa", [64, 64])
        i("cos_mla", [64, T]); i("sin_mla", [64, T]); i("rt_mla", [64, 64])
        i("maskwa", [6, 128, 512]); i("na_oh", [31, 64, 64]); i("na_cmask", [64, 15, 64]); i("j15", [15, 15])
        i("gla_tri", [4, 128, 128]); i("gla_mask", [2, 128, 4, 128])
        self.NB = NT // 128 * 8 + NE
        i("moe_sl", [128, 128]); i("moe_biota", [128, self.NB]); i("moe_piota", [128, 1]); i("moe_eiota", [128, 128])
        i("na_rpb", [L, 4, 15, 31]); i("wa_sink", [L, 4])
        i("mla_g_q", [L, 128, 2]); i("mla_g_kv", [L, 128, 1]); i("mla_w_uq", [L, 256, 256]); i("mla_w_ukv", [L, 128, 384])
        i("gla_w_gf", [L, 16, 128]); i("gla_b_gf", [L, 128]); i("gla_w_gb", [L, 16, 128]); i("gla_b_gb", [L, 128])
        i("gla_g_norm", [L, 64, 1])
        i("w_out", [L, D, D]); i("ln1_g", [L, D]); i("ln1_b", [L, D]); i("ln2_g", [L, D]); i("ln2_b", [L, D])
        i("router_w", [L, D, NE]); i("router_bias", [L, NE])
        i("exp_w1", [L * NE * 128, 2048]); i("exp_w3", [L * NE * 128, 2048]); i("exp_w2", [L * NE * 128, 2048])
        i("sh_w1", [L, D, 256]); i("sh_w3", [L, D, 256]); i("sh_w2", [L, 256, D])
        self.out = self.nc.dram_tensor("out", [T, D], F32, kind="ExternalOutput").ap()
        s = self.scratch
        s("XR", [NT, D], F32)
        s("PT", [IN_W, NT], BF16)
        s("VT", [NT, 768], BF16)
        s("QTb", [4, 64, NT], BF16); s("KTb", [2, 64, NT], BF16)
        s("QTc", [4, 64, NT], BF16); s("KTc", [4, 64, NT], BF16); s("Vc", [NT, 4, 64], BF16)
        s("OT", [D, NT], BF16)
        s("OF", [64, 4, NT], F32)
        s("HT", [D, NT], BF16)
        s("GT", [NT // 128, 128, NE + 1], F32)
        s("POS", [NT // 128, 128, NE], F32)
        s("XB", [NT, D], BF16)
        s("XS", [self.NB * 128, D], BF16)
        s("YS", [self.NB * 128, D], BF16)

    def setup_global(self, stack):
        fw, nc = self.fw, self.nc
        self.gstack = stack
        fw.stack = stack
        self.ident = fw.sb([128, 128], F32, "ident")
        fw.dma("sp", self.ident[:], self.din["ident"], writes=[self.ident])
        self.ident_bf = fw.sb([128, 128], BF16, "identb")
        fw.op("dve", lambda: nc.vector.tensor_copy(out=self.ident_bf[:], in_=self.ident[:]),
              reads=[self.ident], writes=[self.ident_bf])
        self.ones = fw.sb([128, 128], F32, "ones")
        fw.op("dve", lambda: nc.vector.memset(self.ones[:], 1.0), writes=[self.ones])
        self.epsln = fw.sb([128, 2], F32, "eps")
        fw.op("dve", lambda: nc.vector.memset(self.epsln[:, 0:1], LN_EPS), writes=[self.epsln])
        fw.op("dve", lambda: nc.vector.memset(self.epsln[:, 1:2], RMS_EPS), writes=[self.epsln])
        self.cvs = fw.sb([128, 8, 2], F32, "cvs")
        cv_raw = fw.sb([128, 8, 2], F32, "cvraw")
        fw.dma("sp", cv_raw[:], self.din["cv"], writes=[cv_raw])
        fw.op("act", lambda: nc.scalar.activation(out=self.cvs[:], in_=cv_raw[:], func=AF.Silu),
              reads=[cv_raw], writes=[self.cvs])
        self.modv = fw.sb([128, 48, 2], F32, "modv")
        self.psum_banks = None
        U32 = mybir.dt.uint32
        nt = self.NT // 128
        self.moe_run = fw.sb([128, NE], F32, "moerun")
        self.widx = fw.sb([128, self.NB], U32, "widx")
        self.dest8 = fw.sb([128, nt, 8], U32, "dest8")
        self.gate8 = fw.sb([128, nt, 8], F32, "gate8")
        zt = fw.sb([128, D], BF16, "zt")
        fw.op("dve", lambda: nc.vector.memset(zt[:], 0.0), writes=[zt])
        for b in range(self.NB):
            fw.dma("sp", self.scr["XS"][b * 128:(b + 1) * 128, :], zt[:], reads=[zt])

    def stage_mod(self, l):
        fw, nc = self.fw, self.nc
        from contextlib import ExitStack
        with ExitStack() as st:
            fw.stack = st
            wj = Ring([fw.sb([128, 8, 128], F32, "wj") for _ in range(3)])
            pm = fw.ps([128, 48, 2], F32, "pm")
            bpj = fw.sb([128, 48], F32, "bpj")
            fw.dma("sp", bpj[:], self.din["b_ada_pj"][l], writes=[bpj])
            wsrc = self.din["w_ada"][l].rearrange("(k p) n -> p k n", p=128)
            for j in range(48):
                w = wj.next()
                fw.dma("sp", w[:], wsrc[:, :, j * 128:(j + 1) * 128], writes=[w])
                for k in range(8):
                    fw.op("pe", lambda k=k, w=w: nc.tensor.matmul(pm[:, j, :], lhsT=w[:, k, :], rhs=self.cvs[:, k, :],
                                                                   start=(k == 0), stop=(k == 7)),
                          reads=[w, self.cvs], writes=[pm], inc=(k == 7))
            for wh in range(2):
                fw.op("dve", lambda wh=wh: nc.vector.tensor_tensor(out=self.modv[:, :, wh], in0=pm[:, :, wh], in1=bpj[:],
                                                                    op=ALU.add),
                      reads=[pm, bpj], writes=[self.modv])
            for j0 in (8, 32):
                fw.op("dve", lambda j0=j0: nc.vector.tensor_scalar_add(out=self.modv[:, j0:j0 + 8, :],
                                                                        in0=self.modv[:, j0:j0 + 8, :], scalar1=1.0),
                      reads=[self.modv], writes=[self.modv])
            fw.barrier()
        fw.stack = self.gstack

    def stage_inproj(self, l):
        fw, nc = self.fw, self.nc
        T, NT = self.T, self.NT
        from contextlib import ExitStack
        with ExitStack() as st:
            fw.stack = st
            win = fw.sb([128, 8, IN_W], BF16, "win")
            wsrc = self.din["w_in"][l].rearrange("(k p) n -> p k n", p=128)
            for k in range(8):
                fw.dma("pool", win[:, k, :], wsrc[:, k, :], writes=[win])
            xr = Ring([fw.sb([128, D], F32, "x") for _ in range(3)])
            xnr = Ring([fw.sb([128, D], F32, "xn") for _ in range(2)])
            str_ = Ring([fw.sb([128, 16], F32, "st") for _ in range(3)])
            hTr = Ring([fw.sb([128, 8, 512], BF16, "hT") for _ in range(2)])
            ptr = Ring([fw.sb([128, 512], BF16, "pt") for _ in range(4)])
            vtr = Ring([fw.sb([128, 768], BF16, "vt") for _ in range(2)])
            tpr = Ring([fw.ps([128, 8, 128], F32, "tp") for _ in range(2)])
            ppr = Ring([fw.ps([128, 512], F32, "pp") for _ in range(2)])
            pvr = Ring([fw.ps([128, 2, 512], F32, "pv") for _ in range(1)])
            ev = 0
            for (t0, n) in self.chunks(512):
                hT = hTr.next()
                for ti in range(n // 128):
                    tok = t0 + ti * 128
                    lat = tok < T
                    wh = 0 if lat else 1
                    if l == 0:
                        src = self.din["x"][tok:tok + 128, :] if lat else self.din["ctx"][tok - T:tok - T + 128, :]
                    else:
                        src = self.scr["XR"][tok:tok + 128, :]
                    x = xr.next()
                    fw.dma("sp", x[:], src, writes=[x])
                    s = str_.next()
                    for hh in range(2):
                        fw.op("dve", lambda x=x, s=s, hh=hh: nc.vector.bn_stats(out=s[:, hh * 6:hh * 6 + 6],
                                                                                 in_=x[:, hh * 512:(hh + 1) * 512]),
                              reads=[x], writes=[s])
                    fw.op("dve", lambda s=s: nc.vector.bn_aggr(out=s[:, 12:14], in_=s[:, 0:12]), reads=[s], writes=[s])
                    fw.op("act", lambda s=s: nc.scalar.activation(out=s[:, 15:16], in_=s[:, 13:14], func=AF.Sqrt,
                                                                   bias=self.epsln[:, 0:1], scale=1.0),
                          reads=[s, self.epsln], writes=[s])
                    fw.op("dve", lambda s=s: nc.vector.reciprocal(out=s[:, 14:15], in_=s[:, 15:16]), reads=[s], writes=[s])
                    xn = xnr.next()
                    fw.op("dve", lambda x=x, s=s, xn=xn: nc.vector.tensor_scalar(out=xn[:], in0=x[:], scalar1=s[:, 12:13],
                                                                                  scalar2=s[:, 14:15], op0=ALU.subtract,
                                                                                  op1=ALU.mult),
                          reads=[x, s], writes=[xn])
                    tp = tpr.next()
                    for k in range(8):
                        fw.op("pe", lambda k=k, tp=tp, xn=xn: nc.tensor.transpose(out=tp[:, k, :], in_=xn[:, k * 128:(k + 1) * 128],
                                                                                   identity=self.ident[:]),
                              reads=[xn, self.ident], writes=[tp], inc=(k == 7))
                    for k in range(8):
                        sc = self.modv[:, 8 + k, wh:wh + 1]
                        sh = self.modv[:, k, wh:wh + 1]
                        o = hT[:, k, ti * 128:(ti + 1) * 128]
                        if k % 2 == 0:
                            fw.op("act", lambda o=o, tp=tp, k=k, sc=sc, sh=sh: nc.scalar.activation(
                                out=o, in_=tp[:, k, :], func=AF.Identity, bias=sh, scale=sc),
                                reads=[tp, self.modv], writes=[hT])
                        else:
                            fw.op("dve", lambda o=o, tp=tp, k=k, sc=sc, sh=sh: nc.vector.tensor_scalar(
                                out=o, in0=tp[:, k, :], scalar1=sc, scalar2=sh, op0=ALU.mult, op1=ALU.add),
                                reads=[tp, self.modv], writes=[hT])
                    pv = pvr.next()
                    for (c0, c1, bank, off) in ((O_AV, O_AV + 256, 0, 0), (O_BV, O_BV + 128, 0, 256), (O_DK, O_DK + 384, 1, 0)):
                        for k in range(8):
                            fw.op("pe", lambda k=k, c0=c0, c1=c1, bank=bank, off=off, pv=pv, hT=hT, ti=ti: nc.tensor.matmul(
                                pv[:, bank, off:off + (c1 - c0)], lhsT=hT[:, k, ti * 128:(ti + 1) * 128], rhs=win[:, k, c0:c1],
                                start=(k == 0), stop=(k == 7)),
                                reads=[hT, win], writes=[pv], inc=(k == 7))
                    vt = vtr.next()
                    fw.op("act", lambda vt=vt, pv=pv: nc.scalar.copy(out=vt[:, 0:384], in_=pv[:, 0, 0:384]), reads=[pv], writes=[vt])
                    fw.op("dve", lambda vt=vt, pv=pv: nc.vector.tensor_copy(out=vt[:, 384:768], in_=pv[:, 1, 0:384]), reads=[pv], writes=[vt])
                    fw.dma("pool", self.scr["VT"][tok:tok + 128, :], vt[:], reads=[vt])
                for jc in range(20):
                    m = min(128, IN_W - jc * 128)
                    pp = ppr.next()
                    for k in range(8):
                        fw.op("pe", lambda k=k, jc=jc, m=m, pp=pp, hT=hT: nc.tensor.matmul(
                            pp[0:m, 0:n], lhsT=win[:, k, jc * 128:jc * 128 + m], rhs=hT[:, k, 0:n],
                            start=(k == 0), stop=(k == 7)),
                            reads=[hT, win], writes=[pp], inc=(k == 7))
                    pt = ptr.next()
                    if ev % 2 == 0:
                        fw.op("act", lambda pt=pt, pp=pp, m=m: nc.scalar.copy(out=pt[0:m, 0:n], in_=pp[0:m, 0:n]), reads=[pp], writes=[pt])
                    else:
                        fw.op("dve", lambda pt=pt, pp=pp, m=m: nc.vector.tensor_copy(out=pt[0:m, 0:n], in_=pp[0:m, 0:n]), reads=[pp], writes=[pt])
                    ev += 1
                    fw.dma("pool", self.scr["PT"][jc * 128:jc * 128 + m, t0:t0 + n], pt[0:m, 0:n], reads=[pt])
            fw.barrier()
        fw.stack = self.gstack


def _stage(fn):
    def wrap(self, *a, **kw):
        from contextlib import ExitStack
        with ExitStack() as st:
            self.fw.stack = st
            fn(self, *a, **kw)
            self.fw.barrier()
        self.fw.stack = self.gstack
    return wrap


class KA(K):
    def lat_chunks(self, n=512):
        return [(t, min(n, self.T - t)) for t in range(0, self.T, n)]

    def attn_finish(self, po, n, dst, rings, sink=None):
        fw, nc = self.fw, self.nc
        rden, pbr, osbr, obr = rings
        rd = rden.next()
        if sink is not None:
            fw.op("dve", lambda: nc.vector.tensor_scalar(out=rd[64:65, 0:n], in0=po[64:65, 0:n], scalar1=sink, scalar2=None,
                                                          op0=ALU.add), reads=[po, self.sinkexp], writes=[rd])
            fw.op("dve", lambda: nc.vector.reciprocal(out=rd[64:65, 0:n], in_=rd[64:65, 0:n]), reads=[rd], writes=[rd])
        else:
            fw.op("dve", lambda: nc.vector.reciprocal(out=rd[64:65, 0:n], in_=po[64:65, 0:n]), reads=[po], writes=[rd])
        pb = pbr.next()
        fw.op("pe", lambda: nc.tensor.matmul(pb[0:64, 0:n], lhsT=self.ones[64:65, 0:64], rhs=rd[64:65, 0:n], start=True, stop=True),
              reads=[rd, self.ones], writes=[pb])
        osb = osbr.next()
        fw.op("act", lambda: nc.scalar.copy(out=osb[0:64, 0:n], in_=po[0:64, 0:n]), reads=[po], writes=[osb])
        ob = obr.next()
        fw.op("dve", lambda: nc.vector.tensor_tensor(out=ob[0:64, 0:n], in0=osb[0:64, 0:n], in1=pb[0:64, 0:n], op=ALU.mult),
              reads=[osb, pb], writes=[ob])
        fw.dma("pool", dst, ob[0:64, 0:n], reads=[ob])

    def finish_rings(self):
        fw = self.fw
        return (Ring([fw.sb([128, 512], F32, "rden") for _ in range(2)]),
                Ring([fw.ps([128, 512], F32, "pb") for _ in range(1)]),
                Ring([fw.sb([64, 512], F32, "osb") for _ in range(2)]),
                Ring([fw.sb([64, 512], BF16, "ob") for _ in range(2)]))

    def rope64(self, src, srcbufs, n, t0, rot, which, rings):
        fw, nc = self.fw, self.nc
        qsr, prr, cosr, sinr, t1r, qfr = rings
        qs = qsr.next()
        fw.op("act", lambda: nc.scalar.copy(out=qs[0:64, 0:n], in_=src), reads=srcbufs, writes=[qs])
        if not rot:
            return qs
        pr = prr.next()
        rt = self.rt_wa if which == "wa" else self.rt_mla
        fw.op("pe", lambda: nc.tensor.matmul(pr[0:64, 0:n], lhsT=rt[:], rhs=qs[0:64, 0:n], start=True, stop=True),
              reads=[qs, rt], writes=[pr])
        cos, sin = cosr.next(), sinr.next()
        fw.dma("sp", cos[0:64, 0:n], self.din["cos_" + which][:, t0:t0 + n], writes=[cos])
        fw.dma("sp", sin[0:64, 0:n], self.din["sin_" + which][:, t0:t0 + n], writes=[sin])
        t1 = t1r.next()
        fw.op("pool", lambda: nc.gpsimd.tensor_tensor(out=t1[0:64, 0:n], in0=qs[0:64, 0:n], in1=cos[0:64, 0:n], op=ALU.mult),
              reads=[qs, cos], writes=[t1])
        t2 = t1r.next()
        fw.op("dve", lambda: nc.vector.tensor_tensor(out=t2[0:64, 0:n], in0=pr[0:64, 0:n], in1=sin[0:64, 0:n], op=ALU.mult),
              reads=[pr, sin], writes=[t2])
        qf = qfr.next()
        fw.op("dve", lambda: nc.vector.tensor_tensor(out=qf[0:64, 0:n], in0=t1[0:64, 0:n], in1=t2[0:64, 0:n], op=ALU.add),
              reads=[t1, t2], writes=[qf])
        return qf

    def rope_rings(self):
        fw = self.fw
        return (Ring([fw.sb([64, 512], BF16, "qs") for _ in range(3)]),
                Ring([fw.ps([64, 512], F32, "pr") for _ in range(2)]),
                Ring([fw.sb([64, 512], F32, "cos") for _ in range(2)]),
                Ring([fw.sb([64, 512], F32, "sin") for _ in range(2)]),
                Ring([fw.sb([64, 512], F32, "t1") for _ in range(4)]),
                Ring([fw.sb([64, 512], BF16, "qf") for _ in range(3)]))

    def load_rt(self):
        fw, nc = self.fw, self.nc
        for nm in ("rt_wa", "rt_mla"):
            t = fw.sb([64, 64], BF16, nm)
            fw.dma("pool", t[:], self.din[nm], writes=[t])
            setattr(self, nm, t)

    @_stage
    def stage_wa_prep(self, l):
        fw, nc = self.fw, self.nc
        T = self.T
        self.load_rt()
        rr = self.rope_rings()
        inr = Ring([fw.sb([64, 512], BF16, "in") for _ in range(3)])
        for (t0, n) in self.chunks(512):
            rot = t0 < T
            for (src_row, dst) in [(O_BQ + 64 * h, self.scr["QTb"][h]) for h in range(4)] + \
                                  [(O_BK + 64 * h, self.scr["KTb"][h]) for h in range(2)]:
                a = inr.next()
                fw.dma("sp", a[0:64, 0:n], self.scr["PT"][src_row:src_row + 64, t0:t0 + n], writes=[a])
                if rot:
                    qf = self.rope64(a[0:64, 0:n], [a], n, t0, True, "wa", rr)
                    fw.dma("pool", dst[:, t0:t0 + n], qf[0:64, 0:n], reads=[qf])
                else:
                    fw.dma("pool", dst[:, t0:t0 + n], a[0:64, 0:n], reads=[a])

    @_stage
    def stage_mla_prep(self, l):
        fw, nc = self.fw, self.nc
        T = self.T
        self.load_rt()
        rr = self.rope_rings()
        onesb = fw.sb([128, 128], BF16, "onesb")
        fw.op("dve", lambda: nc.vector.memset(onesb[:], 1.0), writes=[onesb])
        gq = fw.sb([128, 2], F32, "gq"); gkv = fw.sb([128, 1], F32, "gkv")
        fw.dma("sp", gq[:], self.din["mla_g_q"][l], writes=[gq])
        fw.dma("sp", gkv[:], self.din["mla_g_kv"][l], writes=[gkv])
        wuq = fw.sb([128, 2, 256], BF16, "wuq"); wukv = fw.sb([128, 384], BF16, "wukv")
        fw.dma("pool", wuq[:], self.din["mla_w_uq"][l].rearrange("(k p) n -> p k n", p=128), writes=[wuq])
        fw.dma("pool", wukv[:], self.din["mla_w_ukv"][l], writes=[wukv])
        cqr = Ring([fw.sb([128, 3, 512], BF16, "cq") for _ in range(2)])
        sqr = Ring([fw.sb([128, 3, 512], BF16, "sq") for _ in range(2)])
        rsr = Ring([fw.sb([128, 2, 512], F32, "rs") for _ in range(2)])
        cnr = Ring([fw.sb([128, 3, 512], BF16, "cn") for _ in range(2)])
        krr = Ring([fw.sb([64, 512], BF16, "kr") for _ in range(2)])
        knr = Ring([fw.sb([32, 512], BF16, "kn") for _ in range(3)])
        vcr = Ring([fw.sb([128, 384], BF16, "vc") for _ in range(3)])
        pss = Ring([fw.ps([128, 2, 512], F32, "pss") for _ in range(1)])
        pq = Ring([fw.ps([64, 512], F32, "pq") for _ in range(2)])
        pv = Ring([fw.ps([128, 512], F32, "pvc") for _ in range(1)])
        PT = self.scr["PT"]
        for (t0, n) in self.chunks(512):
            rot = t0 < T
            cq = cqr.next()
            fw.dma("sp", cq[:, 0:2, 0:n], PT[O_CQ:O_CQ + 256, t0:t0 + n].rearrange("(k p) t -> p k t", p=128), writes=[cq])
            fw.dma("sp", cq[:, 2, 0:n], PT[O_CKV:O_CKV + 128, t0:t0 + n], writes=[cq])
            sq = sqr.next()
            fw.op("pool", lambda: nc.gpsimd.tensor_tensor(out=sq[:, :, 0:n], in0=cq[:, :, 0:n], in1=cq[:, :, 0:n], op=ALU.mult),
                  reads=[cq], writes=[sq])
            ps = pss.next()
            for k in range(2):
                fw.op("pe", lambda k=k: nc.tensor.matmul(ps[:, 0, 0:n], lhsT=onesb[:], rhs=sq[:, k, 0:n], start=(k == 0), stop=(k == 1)),
                      reads=[sq, onesb], writes=[ps], inc=(k == 1))
            fw.op("pe", lambda: nc.tensor.matmul(ps[:, 1, 0:n], lhsT=onesb[:], rhs=sq[:, 2, 0:n], start=True, stop=True),
                  reads=[sq, onesb], writes=[ps])
            rs = rsr.next()
            fw.op("act", lambda: nc.scalar.activation(out=rs[:, 0, 0:n], in_=ps[:, 0, 0:n], func=AF.Sqrt, bias=self.epsln[:, 1:2], scale=1.0 / 256),
                  reads=[ps, self.epsln], writes=[rs])
            fw.op("act", lambda: nc.scalar.activation(out=rs[:, 1, 0:n], in_=ps[:, 1, 0:n], func=AF.Sqrt, bias=self.epsln[:, 1:2], scale=1.0 / 128),
                  reads=[ps, self.epsln], writes=[rs])
            fw.op("dve", lambda: nc.vector.reciprocal(out=rs[:, :, 0:n], in_=rs[:, :, 0:n]), reads=[rs], writes=[rs])
            cn = cnr.next()
            for k in range(3):
                g = gq[:, k:k + 1] if k < 2 else gkv[:, 0:1]
                fw.op("dve", lambda k=k, g=g: nc.vector.scalar_tensor_tensor(out=cn[:, k, 0:n], in0=cq[:, k, 0:n], scalar=g,
                                                                             in1=rs[:, (0 if k < 2 else 1), 0:n], op0=ALU.mult, op1=ALU.mult),
                      reads=[cq, rs, gq, gkv], writes=[cn])
            for h in range(4):
                p = pq.next()
                for k in range(2):
                    fw.op("pe", lambda k=k, h=h, p=p: nc.tensor.matmul(p[0:64, 0:n], lhsT=wuq[:, k, h * 64:(h + 1) * 64], rhs=cn[:, k, 0:n],
                                                                       start=(k == 0), stop=(k == 1)),
                          reads=[cn, wuq], writes=[p], inc=(k == 1))
                qf = self.rope64(p[0:64, 0:n], [p], n, t0, rot, "mla", rr)
                fw.dma("pool", self.scr["QTc"][h][:, t0:t0 + n], qf[0:64, 0:n], reads=[qf])
            kr = krr.next()
            if rot:
                fw.op("pool", lambda: nc.gpsimd.memset(kr[0:32, 0:n], 0.0), writes=[kr])
            fw.dma("sp", kr[32:64, 0:n], PT[O_CKR:O_CKR + 32, t0:t0 + n], writes=[kr])
            if rot:
                kf = self.rope64(kr[0:64, 0:n], [kr], n, t0, True, "mla", rr)
            else:
                kf = kr
            for h in range(4):
                fw.dma("pool", self.scr["KTc"][h][32:64, t0:t0 + n], kf[32:64, 0:n], reads=[kf])
            for h in range(4):
                p = pq.next()
                fw.op("pe", lambda h=h, p=p: nc.tensor.matmul(p[0:32, 0:n], lhsT=wukv[:, h * 96:h * 96 + 32], rhs=cn[:, 2, 0:n], start=True, stop=True),
                      reads=[cn, wukv], writes=[p])
                kn = knr.next()
                fw.op("act", lambda p=p, kn=kn: nc.scalar.copy(out=kn[0:32, 0:n], in_=p[0:32, 0:n]), reads=[p], writes=[kn])
                fw.dma("pool", self.scr["KTc"][h][0:32, t0:t0 + n], kn[0:32, 0:n], reads=[kn])
            for ti in range(n // 128):
                p = pv.next()
                fw.op("pe", lambda p=p, ti=ti: nc.tensor.matmul(p[:, 0:384], lhsT=cn[:, 2, ti * 128:(ti + 1) * 128], rhs=wukv[:], start=True, stop=True),
                      reads=[cn, wukv], writes=[p])
                vc = vcr.next()
                fw.op("dve", lambda p=p, vc=vc: nc.vector.tensor_copy(out=vc[:], in_=p[:, 0:384]), reads=[p], writes=[vc])
                tok = t0 + ti * 128
                fw.dma("pool", self.scr["Vc"][tok:tok + 128], vc[:].rearrange("p (h c) -> p h c", h=4)[:, :, 32:96], reads=[vc])

    def exp_block(self, ps, kp, c0, c1, er, mask=None, mring=None):
        fw, nc = self.fw, self.nc
        e = er.next()
        fw.op("act", lambda: nc.scalar.activation(out=e[0:kp, c0:c1], in_=ps[0:kp, c0:c1], func=AF.Exp, scale=0.125),
              reads=[ps], writes=[e])
        if mask is None:
            return e
        mbufs, map_ = mask
        e2 = mring.next()
        fw.op("pool", lambda: nc.gpsimd.tensor_tensor(out=e2[0:kp, c0:c1], in0=e[0:kp, c0:c1], in1=map_, op=ALU.mult),
              reads=[e] + mbufs, writes=[e2])
        return e2

    @_stage
    def stage_mla_attn(self, l, ctx_out):
        fw, nc = self.fw, self.nc
        T, NT = self.T, self.NT
        nkb = NT // 128
        fr = self.finish_rings()
        kT = fw.sb([64, NT], BF16, "kT")
        va = fw.sb([128, nkb, 65], BF16, "va")
        fw.op("dve", lambda: nc.vector.memset(va[:, :, 64:65], 1.0), writes=[va])
        qr = Ring([fw.sb([64, 512], BF16, "q") for _ in range(2)])
        er = Ring([fw.sb([128, 512], BF16, "e") for _ in range(3)])
        psr = Ring([fw.ps([128, 512], F32, "ps") for _ in range(3)])
        por = Ring([fw.ps([128, 512], F32, "po") for _ in range(2)])
        for h in range(4):
            fw.dma("sp", kT[:], self.scr["KTc"][h], writes=[kT])
            fw.dma("sp", va[:, :, 0:64], self.scr["Vc"][:, h, :].rearrange("(b p) d -> p b d", p=128), writes=[va])
            qchunks = [(t0, n, list(range(nkb))) for (t0, n) in self.lat_chunks(512)]
            if ctx_out:
                qchunks.append((T, C, [nkb - 2, nkb - 1]))
            for (t0, n, kbs) in qchunks:
                q = qr.next()
                fw.dma("sp", q[0:64, 0:n], self.scr["QTc"][h][:, t0:t0 + n], writes=[q])
                po = por.next()

                def S(kb):
                    ps = psr.next()
                    fw.op("pe", lambda: nc.tensor.matmul(ps[:, 0:n], lhsT=kT[:, kb * 128:(kb + 1) * 128], rhs=q[0:64, 0:n], start=True, stop=True),
                          reads=[kT, q], writes=[ps])
                    return ps
                nxt = S(kbs[0])
                for i, kb in enumerate(kbs):
                    ps = nxt
                    if i + 1 < len(kbs):
                        nxt = S(kbs[i + 1])
                    e = self.exp_block(ps, 128, 0, n, er)
                    fw.op("pe", lambda e=e, kb=kb, i=i: nc.tensor.matmul(po[0:65, 0:n], lhsT=va[:, kb, :], rhs=e[:, 0:n], start=(i == 0), stop=(i == len(kbs) - 1)),
                          reads=[e, va], writes=[po], inc=(i == len(kbs) - 1))
                self.attn_finish(po, n, self.scr["OT"][512 + h * 64:512 + (h + 1) * 64, t0:t0 + n], fr)

    @_stage
    def stage_wa_attn(self, l, ctx_out):
        fw, nc = self.fw, self.nc
        T, NT = self.T, self.NT
        nb = T // 128
        fr = self.finish_rings()
        self.sinkexp = fw.sb([128, 4], F32, "sinkexp")
        fw.dma("sp", self.sinkexp[:], self.din["wa_sink"][l].partition_broadcast(128), writes=[self.sinkexp])
        fw.op("act", lambda: nc.scalar.activation(out=self.sinkexp[:], in_=self.sinkexp[:], func=AF.Exp), reads=[self.sinkexp], writes=[self.sinkexp])
        mask = fw.sb([128, 6, 512], BF16, "mask")
        fw.dma("pool", mask[:], self.din["maskwa"].rearrange("r p q -> p r q"), writes=[mask])
        kT = fw.sb([64, NT], BF16, "kT")
        va = fw.sb([128, NT // 128, 65], BF16, "va")
        fw.op("dve", lambda: nc.vector.memset(va[:, :, 64:65], 1.0), writes=[va])
        qr = Ring([fw.sb([64, 512], BF16, "q") for _ in range(2)])
        er = Ring([fw.sb([128, 512], BF16, "e") for _ in range(3)])
        mr = Ring([fw.sb([128, 512], BF16, "em") for _ in range(3)])
        psr = Ring([fw.ps([128, 512], F32, "ps") for _ in range(3)])
        por = Ring([fw.ps([128, 512], F32, "po") for _ in range(2)])
        for g in range(2):
            fw.dma("sp", kT[:], self.scr["KTb"][g], writes=[kT])
            fw.dma("sp", va[:, :, 0:64], self.scr["VT"][:, 256 + g * 64:256 + (g + 1) * 64].rearrange("(b p) d -> p b d", p=128), writes=[va])
            for h in (2 * g, 2 * g + 1):
                qchunks = [(t0, n, True) for (t0, n) in self.lat_chunks(512)]
                if ctx_out:
                    qchunks.append((T, C, False))
                for (t0, n, lat) in qchunks:
                    q = qr.next()
                    fw.dma("sp", q[0:64, 0:n], self.scr["QTb"][h][:, t0:t0 + n], writes=[q])
                    po = por.next()
                    items = [(nb, 0, n, None)]
                    if lat:
                        i0 = t0 // 128
                        nqb = n // 128
                        for r in range(-1, nqb + 1):
                            j = i0 + r
                            if j < 0 or j >= nb:
                                continue
                            qlo, qhi = max(0, r - 1), min(nqb, r + 2)
                            items.append((j, qlo * 128, qhi * 128, r + 1))
                    items.append((nb + 1, 0, n, None))
                    def S(it):
                        kb, c0, c1, mi = it
                        ps = psr.next()
                        fw.op("pe", lambda: nc.tensor.matmul(ps[:, c0:c1], lhsT=kT[:, kb * 128:(kb + 1) * 128], rhs=q[0:64, c0:c1], start=True, stop=True),
                              reads=[kT, q], writes=[ps])
                        return ps
                    nxt = S(items[0])
                    for i, (kb, c0, c1, mi) in enumerate(items):
                        ps = nxt
                        if i + 1 < len(items):
                            nxt = S(items[i + 1])
                        e = self.exp_block(ps, 128, c0, c1, er, mask=None if mi is None else ([mask], mask[:, mi, c0:c1]), mring=mr)
                        fw.op("pe", lambda e=e, kb=kb, c0=c0, c1=c1, i=i: nc.tensor.matmul(po[0:65, c0:c1], lhsT=va[:, kb, :], rhs=e[:, c0:c1], start=(i == 0), stop=(i == len(items) - 1)),
                              reads=[e, va], writes=[po], inc=(i == len(items) - 1))
                    self.attn_finish(po, n, self.scr["OT"][256 + h * 64:256 + (h + 1) * 64, t0:t0 + n], fr, sink=self.sinkexp[64:65, h:h + 1])

    @_stage
    def stage_na_attn(self, l, ctx_out):
        fw, nc = self.fw, self.nc
        T, NT = self.T, self.NT
        R = T // 64
        fr = self.finish_rings()
        oh = fw.sb([31, 64, 64], F32, "oh")
        fw.dma("sp", oh[:], self.din["na_oh"], writes=[oh])
        cmask = fw.sb([64, 960], F32, "cmask")
        fw.dma("sp", cmask[:], self.din["na_cmask"].rearrange("k m c -> k (m c)"), writes=[cmask])
        j15 = fw.sb([15, 15], F32, "j15")
        fw.dma("sp", j15[:], self.din["j15"], writes=[j15])
        rpb = fw.sb([15, 4, 31], F32, "rpb")
        fw.dma("sp", rpb[:], self.din["na_rpb"][l].rearrange("h r j -> r h j"), writes=[rpb])
        rrr = Ring([fw.sb([31, 15], F32, "rr") for _ in range(2)])
        etz = [fw.sb([64, 960], BF16, "etz") for _ in range(4)]
        etmp = fw.sb([64, 960], F32, "etmp")
        prr = fw.ps([31, 15], F32, "prr")
        pz = fw.ps([64, 2, 512], F32, "pz")
        for h in range(4):
            fw.op("pe", lambda h=h: nc.tensor.matmul(prr[:, :], lhsT=rpb[:, h, :], rhs=j15[:], start=True, stop=True), reads=[rpb, j15], writes=[prr])
            rr = rrr.next()
            fw.op("dve", lambda rr=rr: nc.vector.tensor_copy(out=rr[:], in_=prr[:]), reads=[prr], writes=[rr])
            for c_ in range(64):
                for (bk, m0, m1) in ((0, 0, 8), (1, 8, 15)):
                    fw.op("pe", lambda c_=c_, bk=bk, m0=m0, m1=m1, rr=rr: nc.tensor.matmul(
                        pz[:, bk, :].rearrange("k (m c) -> k m c", c=64)[:, 0:m1 - m0, c_], lhsT=oh[:, c_, :], rhs=rr[:, m0:m1], start=True, stop=True),
                        reads=[oh, rr], writes=[pz], inc=(c_ == 63 and bk == 1))
            fw.op("act", lambda: nc.scalar.activation(out=etmp[:, 0:512], in_=pz[:, 0, :], func=AF.Exp), reads=[pz], writes=[etmp])
            fw.op("act", lambda: nc.scalar.activation(out=etmp[:, 512:960], in_=pz[:, 1, 0:448], func=AF.Exp), reads=[pz], writes=[etmp])
            fw.op("dve", lambda h=h: nc.vector.tensor_tensor(out=etz[h][:], in0=etmp[:], in1=cmask[:], op=ALU.mult), reads=[etmp, cmask], writes=[etz[h]])
        kTr = Ring([fw.sb([64, 1024], BF16, "kT") for _ in range(2)])
        kcr = Ring([fw.sb([64, 256], BF16, "kc") for _ in range(2)])
        var = Ring([fw.sb([64, 16, 65], BF16, "va") for _ in range(2)])
        vcr = Ring([fw.sb([128, 2, 65], BF16, "vca") for _ in range(2)])
        for b in var.bufs:
            fw.op("dve", lambda b=b: nc.vector.memset(b[:, :, 64:65], 1.0), writes=[b])
        for b in vcr.bufs:
            fw.op("dve", lambda b=b: nc.vector.memset(b[:, :, 64:65], 1.0), writes=[b])
        qr = Ring([fw.sb([64, 512], BF16, "q") for _ in range(2)])
        er = Ring([fw.sb([128, 512], BF16, "e") for _ in range(3)])
        mr = Ring([fw.sb([128, 512], BF16, "em") for _ in range(3)])
        psr = Ring([fw.ps([128, 512], F32, "ps") for _ in range(2)])
        por = Ring([fw.ps([128, 512], F32, "po") for _ in range(2)])
        PT, VT = self.scr["PT"], self.scr["VT"]
        for h in range(4):
            kc = kcr.next()
            fw.dma("sp", kc[:], PT[O_AK + h * 64:O_AK + (h + 1) * 64, T:T + C], writes=[kc])
            vc = vcr.next()
            fw.dma("sp", vc[:, :, 0:64], VT[T:T + C, h * 64:(h + 1) * 64].rearrange("(b p) d -> p b d", p=128), writes=[vc])
            qchunks = [(t0, n, True) for (t0, n) in self.lat_chunks(512)]
            if ctx_out:
                qchunks.append((T, C, False))
            for (t0, n, lat) in qchunks:
                q = qr.next()
                fw.dma("sp", q[0:64, 0:n], PT[O_AQ + h * 64:O_AQ + (h + 1) * 64, t0:t0 + n], writes=[q])
                po = por.next()
                items = [("c", 0, 0, n, None)]
                if lat:
                    r0 = t0 // 64
                    nqr = n // 64
                    klo, khi = max(0, r0 - 4), min(R - 1, r0 + nqr - 1 + 3 + 0)
                    rs_all = [min(max(r0 + rq - 4, 0), R - 8) for rq in range(nqr)]
                    klo, khi = min(rs_all), max(rs_all) + 7
                    kT = kTr.next()
                    nk = khi - klo + 1
                    fw.dma("sp", kT[:, 0:nk * 64], PT[O_AK + h * 64:O_AK + (h + 1) * 64, klo * 64:(khi + 1) * 64], writes=[kT])
                    va = var.next()
                    fw.dma("sp", va[:, 0:nk, 0:64], VT[klo * 64:(khi + 1) * 64, h * 64:(h + 1) * 64].rearrange("(r p) d -> p r d", p=64), writes=[va])
                    for kr in range(klo, khi + 1):
                        val = [rq for rq in range(nqr) if rs_all[rq] <= kr <= rs_all[rq] + 7]
                        if not val:
                            continue
                        lo, hi = val[0], val[-1] + 1
                        assert val == list(range(lo, hi))
                        m0 = lo + 7 - kr + r0
                        assert 0 <= m0 and m0 + (hi - lo) <= 15
                        items.append(("k", kr - klo, lo * 64, hi * 64, m0))
                items.append(("c", 1, 0, n, None))
                def S(it):
                    kind, idx, c0, c1, m0 = it
                    ps = psr.next()
                    if kind == "c":
                        fw.op("pe", lambda: nc.tensor.matmul(ps[:, c0:c1], lhsT=kc[:, idx * 128:(idx + 1) * 128], rhs=q[0:64, c0:c1], start=True, stop=True),
                              reads=[kc, q], writes=[ps])
                    else:
                        fw.op("pe", lambda: nc.tensor.matmul(ps[0:64, c0:c1], lhsT=kT[:, idx * 64:(idx + 1) * 64], rhs=q[0:64, c0:c1], start=True, stop=True),
                              reads=[kT, q], writes=[ps])
                    return ps
                nxt = S(items[0])
                for i, (kind, idx, c0, c1, m0) in enumerate(items):
                    ps = nxt
                    if i + 1 < len(items):
                        nxt = S(items[i + 1])
                    first, last = i == 0, i == len(items) - 1
                    if kind == "c":
                        e = self.exp_block(ps, 128, c0, c1, er)
                        fw.op("pe", lambda e=e, idx=idx, first=first, last=last: nc.tensor.matmul(po[0:65, c0:c1], lhsT=vc[:, idx, :], rhs=e[:, c0:c1], start=first, stop=last),
                              reads=[e, vc], writes=[po], inc=last)
                    else:
                        nrow = (c1 - c0) // 64
                        e = self.exp_block(ps, 64, c0, c1, er, mask=([etz[h]], etz[h][:, m0 * 64:(m0 + nrow) * 64]), mring=mr)
                        fw.op("pe", lambda e=e, idx=idx, c0=c0, c1=c1: nc.tensor.matmul(po[0:65, c0:c1], lhsT=va[:, idx, :], rhs=e[0:64, c0:c1], start=False, stop=False),
                              reads=[e, va], writes=[po], inc=False)
                self.attn_finish(po, n, self.scr["OT"][h * 64:(h + 1) * 64, t0:t0 + n], fr)

    @_stage
    def stage_gla(self, l):
        fw, nc = self.fw, self.nc
        T, NT = self.T, self.NT
        PT, VT, OT, OF = self.scr["PT"], self.scr["VT"], self.scr["OT"], self.scr["OF"]
        tri = fw.sb([128, 4, 128], F32, "tri")
        fw.dma("sp", tri[:], self.din["gla_tri"].rearrange("g a b -> a g b"), writes=[tri])
        maskg = fw.sb([128, 2, 512], BF16, "maskg")
        fw.dma("pool", maskg[:], self.din["gla_mask"].rearrange("d j h i -> j d (h i)"), writes=[maskg])
        wg = fw.sb([32, 2, 128], BF16, "wg")
        for d, (wn, bn) in enumerate((("gla_w_gf", "gla_b_gf"), ("gla_w_gb", "gla_b_gb"))):
            fw.dma("pool", wg[0:16, d, :], self.din[wn][l], writes=[wg])
            fw.dma("pool", wg[16:17, d, :], self.din[bn][l:l + 1, :], writes=[wg])
        gn = fw.sb([64, 1], F32, "gn")
        fw.dma("sp", gn[:], self.din["gla_g_norm"][l], writes=[gn])
        onesb = fw.sb([64, 64], BF16, "onesb")
        fw.op("dve", lambda: nc.vector.memset(onesb[:], 1.0), writes=[onesb])
        zr = Ring([fw.sb([32, 128], BF16, "zaug") for _ in range(3)])
        for b in zr.bufs:
            fw.op("dve", lambda b=b: nc.vector.memset(b[:], 1.0), writes=[b])
        qkr = Ring([fw.sb([32, 2, 4, 128], BF16, "qk") for _ in range(3)])
        ktr = Ring([fw.sb([128, 128], BF16, "ktok") for _ in range(3)])
        vtr = Ring([fw.sb([128, 4, 64], BF16, "vtok") for _ in range(3)])
        exr = Ring([fw.sb([128, 128], F32, "ex") for _ in range(2)])
        Lr = Ring([fw.sb([128, 128], F32, "L") for _ in range(2)])
        ebr = Ring([fw.sb([32, 2, 512], F32, "eb") for _ in range(2)])
        esr = Ring([fw.sb([128, 128], F32, "es") for _ in range(2)])
        qdr = Ring([fw.sb([32, 2, 512], BF16, "qd") for _ in range(2)])
        ker = Ring([fw.sb([128, 2, 128], BF16, "kend") for _ in range(2)])
        cm = fw.sb([128, 2], F32, "cm")
        fw.op("dve", lambda: nc.vector.memset(cm[:], 0.0), writes=[cm])
        fw.op("dve", lambda: nc.vector.memset(cm[0:64, 0:1], 1.0), writes=[cm])
        fw.op("dve", lambda: nc.vector.memset(cm[64:128, 1:2], 1.0), writes=[cm])
        amr = Ring([fw.sb([128, 512], BF16, "am") for _ in range(2)])
        Sr = Ring([fw.sb([32, 4, 64], F32, "S") for _ in range(4)])
        Sbr = Ring([fw.sb([32, 4, 64], BF16, "Sb") for _ in range(4)])
        ofr = Ring([fw.sb([64, 512], F32, "of") for _ in range(2)])
        osr = Ring([fw.sb([64, 512], F32, "os") for _ in range(2)])
        sqr = Ring([fw.sb([64, 512], BF16, "sq") for _ in range(2)])
        rsr = Ring([fw.sb([64, 512], F32, "rs") for _ in range(2)])
        rtr = Ring([fw.sb([64, 512], BF16, "rT") for _ in range(2)])
        srr = Ring([fw.sb([64, 512], F32, "sr") for _ in range(2)])
        fnr = Ring([fw.sb([64, 512], BF16, "fin") for _ in range(2)])
        pz = fw.ps([128, 128], F32, "pz")
        pbT = fw.ps([32, 512], F32, "pbT")
        pbs = fw.ps([128, 128], F32, "pbs")
        pa = fw.ps([128, 512], F32, "pa")
        pu = fw.ps([32, 2, 4, 64], F32, "pu")
        po = fw.ps([64, 512], F32, "po")
        pss = fw.ps([64, 512], F32, "pss")
        nl, nt = T // 128, NT // 128
        orders = [[nt - 2, nt - 1] + list(range(nl)), [nt - 1, nt - 2] + list(range(nl - 1, -1, -1))]
        for d in (0, 1):
            if d == 1:
                fw.barrier()
            zrow = O_ZF if d == 0 else O_ZB
            S = Sr.next()
            fw.op("dve", lambda: nc.vector.memset(S[:], 0.0), writes=[S])
            for tile in orders[d]:
                tok = tile * 128
                za = zr.next()
                fw.dma("sp", za[0:16, :], PT[zrow:zrow + 16, tok:tok + 128], writes=[za])
                qk = qkr.next()
                fw.dma("sp", qk[:, 0, :, :], PT[O_DQ:O_DQ + 128, tok:tok + 128].rearrange("(h d) t -> d h t", d=32), writes=[qk])
                fw.dma("sp", qk[:, 1, :, :], PT[O_DK:O_DK + 128, tok:tok + 128].rearrange("(h d) t -> d h t", d=32), writes=[qk])
                ktok = ktr.next()
                fw.dma("sp", ktok[:], VT[tok:tok + 128, 384:512], writes=[ktok])
                vtok = vtr.next()
                fw.dma("sp", vtok[:], VT[tok:tok + 128, 512:768].rearrange("p (h d) -> p h d", h=4), writes=[vtok])
                if getattr(self, 'gla_cut', 99) <= 1:
                    continue
                fw.op("pe", lambda: nc.tensor.matmul(pz[:, :], lhsT=za[0:17, :], rhs=wg[0:17, d, :], start=True, stop=True), reads=[za, wg], writes=[pz])
                ex = exr.next()
                fw.op("act", lambda: nc.scalar.activation(out=ex[:], in_=pz[:], func=AF.Exp, scale=-1.0), reads=[pz], writes=[ex])
                L = Lr.next()
                fw.op("act", lambda: nc.scalar.activation(out=L[:], in_=ex[:], func=AF.Ln, bias=self.ones[:, 0:1], scale=1.0), reads=[ex, self.ones], writes=[L])
                if getattr(self, 'gla_cut', 99) <= 2:
                    continue
                for h in range(4):
                    fw.op("pe", lambda h=h: nc.tensor.matmul(pbT[0:32, h * 128:(h + 1) * 128], lhsT=L[:, h * 32:(h + 1) * 32], rhs=tri[:, d, :], start=True, stop=True),
                          reads=[L, tri], writes=[pbT], inc=(h == 3))
                fw.op("pe", lambda: nc.tensor.matmul(pbs[:, :], lhsT=tri[:, 2 + d, :], rhs=L[:], start=True, stop=True), reads=[L, tri], writes=[pbs])
                eb = ebr.next()
                fw.op("act", lambda: nc.scalar.activation(out=eb[:, 0, :], in_=pbT[0:32, :], func=AF.Exp), reads=[pbT], writes=[eb])
                fw.op("act", lambda: nc.scalar.activation(out=eb[:, 1, :], in_=pbT[0:32, :], func=AF.Exp, scale=-1.0), reads=[pbT], writes=[eb])
                es = esr.next()
                fw.op("act", lambda: nc.scalar.activation(out=es[:], in_=pbs[:], func=AF.Exp), reads=[pbs], writes=[es])
                if getattr(self, 'gla_cut', 99) <= 3:
                    continue
                qd = qdr.next()
                fw.op("dve", lambda: nc.vector.scalar_tensor_tensor(out=qd[:, 0, :], in0=qk[:, 0, :, :].rearrange("p h t -> p (h t)"), scalar=32 ** -0.5,
                                                                     in1=eb[:, 0, :], op0=ALU.mult, op1=ALU.mult), reads=[qk, eb], writes=[qd])
                fw.op("dve", lambda: nc.vector.tensor_tensor(out=qd[:, 1, :], in0=qk[:, 1, :, :].rearrange("p h t -> p (h t)"), in1=eb[:, 1, :], op=ALU.mult),
                      reads=[qk, eb], writes=[qd])
                kend = ker.next()
                for cc in range(2):
                    fw.op("dve", lambda cc=cc: nc.vector.scalar_tensor_tensor(out=kend[:, cc, :], in0=ktok[:], scalar=cm[:, cc:cc + 1], in1=es[:],
                                                                               op0=ALU.mult, op1=ALU.mult), reads=[ktok, es, cm], writes=[kend])
                if getattr(self, 'gla_cut', 99) <= 4:
                    continue
                for h in range(4):
                    fw.op("pe", lambda h=h: nc.tensor.matmul(pa[:, h * 128:(h + 1) * 128], lhsT=qd[:, 1, h * 128:(h + 1) * 128], rhs=qd[:, 0, h * 128:(h + 1) * 128], start=True, stop=True),
                          reads=[qd], writes=[pa], inc=(h == 3))
                am = amr.next()
                fw.op("dve", lambda: nc.vector.tensor_tensor(out=am[:], in0=pa[:], in1=maskg[:, d, :], op=ALU.mult), reads=[pa, maskg], writes=[am])
                if getattr(self, 'gla_cut', 99) <= 5:
                    continue
                for cc in range(2):
                    for h in range(4):
                        fw.op("pe", lambda cc=cc, h=h: nc.tensor.matmul(pu[0:32, cc, h, :], lhsT=kend[:, cc, h * 32:(h + 1) * 32],
                                                                         rhs=vtok[:, h, :], start=True, stop=True),
                              reads=[kend, vtok], writes=[pu], inc=(cc == 1 and h == 3))
                if getattr(self, 'gla_cut', 99) <= 6:
                    continue
                Sb = {}
                for cc in ((0, 1) if d == 0 else (1, 0)):
                    sb_ = Sbr.next()
                    fw.op("act", lambda sb_=sb_, S=S: nc.scalar.copy(out=sb_[:], in_=S[:]), reads=[S], writes=[sb_])
                    Sb[cc] = sb_
                    S2 = Sr.next()
                    tidx = cc * 64 + (63 if d == 0 else 0)
                    for h in range(4):
                        fw.op("dve", lambda h=h, S=S, S2=S2, cc=cc, tidx=tidx: nc.vector.scalar_tensor_tensor(
                            out=S2[:, h, :], in0=S[:, h, :], scalar=eb[:, 0, h * 128 + tidx:h * 128 + tidx + 1], in1=pu[0:32, cc, h, :],
                            op0=ALU.mult, op1=ALU.add), reads=[S, eb, pu], writes=[S2])
                    S = S2
                if getattr(self, 'gla_cut', 99) <= 7:
                    continue
                for h in range(4):
                    fw.op("pe", lambda h=h: nc.tensor.matmul(po[0:64, h * 128:(h + 1) * 128], lhsT=vtok[:, h, :], rhs=am[:, h * 128:(h + 1) * 128], start=True, stop=False),
                          reads=[vtok, am], writes=[po], inc=False)
                    for cc in range(2):
                        fw.op("pe", lambda h=h, cc=cc: nc.tensor.matmul(po[0:64, h * 128 + cc * 64:h * 128 + (cc + 1) * 64], lhsT=Sb[cc][:, h, :],
                                                                         rhs=qd[:, 0, h * 128 + cc * 64:h * 128 + (cc + 1) * 64], start=False, stop=(cc == 1)),
                              reads=[Sb[cc], qd], writes=[po], inc=(cc == 1))
                if getattr(self, 'gla_cut', 99) <= 8:
                    continue
                if d == 0:
                    of = ofr.next()
                    fw.op("act", lambda: nc.scalar.copy(out=of[:], in_=po[:]), reads=[po], writes=[of])
                    fw.dma("pool", OF[:, :, tok:tok + 128], of[:].rearrange("p (h t) -> p h t", h=4), reads=[of])
                else:
                    of = ofr.next()
                    fw.dma("sp", of[:].rearrange("p (h t) -> p h t", h=4), OF[:, :, tok:tok + 128], writes=[of])
                    rT = rtr.next()
                    fw.dma("sp", rT[:].rearrange("p (h t) -> p h t", h=4), PT[O_DR:O_DR + 256, tok:tok + 128].rearrange("(h d) t -> d h t", d=64), writes=[rT])
                    os_ = osr.next()
                    fw.op("dve", lambda: nc.vector.tensor_tensor(out=os_[:], in0=po[:], in1=of[:], op=ALU.add), reads=[po, of], writes=[os_])
                    sq = sqr.next()
                    fw.op("pool", lambda: nc.gpsimd.tensor_tensor(out=sq[:], in0=os_[:], in1=os_[:], op=ALU.mult), reads=[os_], writes=[sq])
                    fw.op("pe", lambda: nc.tensor.matmul(pss[:, :], lhsT=onesb[:], rhs=sq[:], start=True, stop=True), reads=[sq, onesb], writes=[pss])
                    rs = rsr.next()
                    fw.op("act", lambda: nc.scalar.activation(out=rs[:], in_=pss[:], func=AF.Sqrt, bias=self.epsln[0:64, 1:2], scale=1.0 / 64),
                          reads=[pss, self.epsln], writes=[rs])
                    fw.op("dve", lambda: nc.vector.reciprocal(out=rs[:], in_=rs[:]), reads=[rs], writes=[rs])
                    sr = srr.next()
                    fw.op("act", lambda: nc.scalar.activation(out=sr[:], in_=rT[:], func=AF.Silu), reads=[rT], writes=[sr])
                    fw.op("dve", lambda: nc.vector.scalar_tensor_tensor(out=os_[:], in0=os_[:], scalar=gn[:, 0:1], in1=rs[:], op0=ALU.mult, op1=ALU.mult),
                          reads=[os_, gn, rs], writes=[os_])
                    fin = fnr.next()
                    fw.op("dve", lambda: nc.vector.tensor_tensor(out=fin[:], in0=os_[:], in1=sr[:], op=ALU.mult), reads=[os_, sr], writes=[fin])
                    fw.dma("pool", OT[768:1024, tok:tok + 128].rearrange("(h d) t -> d h t", d=64), fin[:].rearrange("p (h t) -> p h t", h=4), reads=[fin])


class KB(KA):
    def bcast_mod(self, j0, dst_lat, dst_ctx, pg, repr_):
        fw, nc = self.fw, self.nc
        for wh, dst in ((0, dst_lat), (1, dst_ctx)):
            for jj in range(8):
                rep = repr_.next()
                fw.op("act", lambda rep=rep, jj=jj, wh=wh: nc.scalar.activation(out=rep[:], in_=self.ones[:], func=AF.Copy,
                                                                                 scale=self.modv[:, j0 + jj, wh:wh + 1]),
                      reads=[self.ones, self.modv], writes=[rep])
                fw.op("pe", lambda rep=rep, jj=jj: nc.tensor.matmul(pg[:, jj // 4, (jj % 4) * 128:(jj % 4 + 1) * 128], lhsT=rep[:], rhs=self.ident[:],
                                                                    start=True, stop=True), reads=[rep, self.ident], writes=[pg])
            fw.op("dve", lambda dst=dst: nc.vector.tensor_copy(out=dst[:].rearrange("p (a b) -> p a b", a=2), in_=pg[:]), reads=[pg], writes=[dst])

    def load_ln(self, l, which):
        fw = self.fw
        g = fw.sb([128, D], F32, "lng"); b = fw.sb([128, D], F32, "lnb")
        fw.dma("sp", g[:], self.din[which + "_g"][l].partition_broadcast(128), writes=[g])
        fw.dma("sp", b[:], self.din[which + "_b"][l].partition_broadcast(128), writes=[b])
        return g, b

    def ln_stats(self, z, s):
        fw, nc = self.fw, self.nc
        for hh in range(2):
            fw.op("dve", lambda hh=hh: nc.vector.bn_stats(out=s[:, hh * 6:hh * 6 + 6], in_=z[:, hh * 512:(hh + 1) * 512]), reads=[z], writes=[s])
        fw.op("dve", lambda: nc.vector.bn_aggr(out=s[:, 12:14], in_=s[:, 0:12]), reads=[s], writes=[s])
        fw.op("act", lambda: nc.scalar.activation(out=s[:, 15:16], in_=s[:, 13:14], func=AF.Sqrt, bias=self.epsln[:, 0:1], scale=1.0),
              reads=[s, self.epsln], writes=[s])
        fw.op("dve", lambda: nc.vector.reciprocal(out=s[:, 14:15], in_=s[:, 15:16]), reads=[s], writes=[s])

    def post_norm_store(self, z, s, g, b, dst):
        fw, nc = self.fw, self.nc
        self.ln_stats(z, s)
        fw.op("dve", lambda: nc.vector.tensor_scalar(out=z[:], in0=z[:], scalar1=s[:, 12:13], scalar2=s[:, 14:15], op0=ALU.subtract, op1=ALU.mult),
              reads=[z, s], writes=[z])
        fw.op("pool", lambda: nc.gpsimd.tensor_tensor(out=z[:], in0=z[:], in1=g[:], op=ALU.mult), reads=[z, g], writes=[z])
        fw.op("pool", lambda: nc.gpsimd.tensor_tensor(out=z[:], in0=z[:], in1=b[:], op=ALU.add), reads=[z, b], writes=[z])
        fw.dma("pool", dst, z[:], reads=[z])

    def xsrc(self, l, tok, first):
        if l == 0 and first:
            return self.din["x"][tok:tok + 128, :] if tok < self.T else self.din["ctx"][tok - self.T:tok - self.T + 128, :]
        return self.scr["XR"][tok:tok + 128, :]

    @_stage
    def stage_outproj(self, l, last):
        fw, nc = self.fw, self.nc
        T, NT = self.T, self.NT
        g_lat = fw.sb([128, D], F32, "glat"); g_ctx = fw.sb([128, D], F32, "gctx")
        pg = fw.ps([128, 2, 512], F32, "pg")
        repr_ = Ring([fw.sb([128, 128], F32, "rep") for _ in range(2)])
        self.bcast_mod(16, g_lat, g_ctx, pg, repr_)
        lg, lb = self.load_ln(l, "ln1")
        wout = fw.sb([128, 8, D], BF16, "wout")
        wsrc = self.din["w_out"][l].rearrange("(k p) n -> p k n", p=128)
        for k in range(8):
            fw.dma("pool", wout[:, k, :], wsrc[:, k, :], writes=[wout])
        otr = Ring([fw.sb([128, 8, 128], BF16, "oT") for _ in range(3)])
        xr = Ring([fw.sb([128, D], F32, "x") for _ in range(3)])
        zr = Ring([fw.sb([128, D], F32, "z") for _ in range(3)])
        sr = Ring([fw.sb([128, 16], F32, "st") for _ in range(3)])
        pyr = Ring([fw.ps([128, 2, 512], F32, "py") for _ in range(2)])
        ntile = (T if last else NT) // 128
        for ti in range(ntile):
            tok = ti * 128
            oT = otr.next()
            fw.dma("sp", oT[:], self.scr["OT"][:, tok:tok + 128].rearrange("(k p) t -> p k t", p=128), writes=[oT])
            x = xr.next()
            fw.dma("sp", x[:], self.xsrc(l, tok, True), writes=[x])
            py = pyr.next()
            for half in range(2):
                for k in range(8):
                    fw.op("pe", lambda half=half, k=k: nc.tensor.matmul(py[:, half, :], lhsT=oT[:, k, :], rhs=wout[:, k, half * 512:(half + 1) * 512],
                                                                         start=(k == 0), stop=(k == 7)), reads=[oT, wout], writes=[py], inc=(k == 7))
            gb = g_lat if tok < T else g_ctx
            z = zr.next()
            fw.op("dve", lambda: nc.vector.tensor_tensor(out=z[:].rearrange("p (a b) -> p a b", a=2), in0=py[:], in1=gb[:].rearrange("p (a b) -> p a b", a=2), op=ALU.mult),
                  reads=[py, gb], writes=[z])
            fw.op("dve", lambda: nc.vector.scalar_tensor_tensor(out=z[:], in0=x[:], scalar=ALPHA, in1=z[:], op0=ALU.mult, op1=ALU.add),
                  reads=[x, z], writes=[z])
            self.post_norm_store(z, sr.next(), lg, lb, self.scr["XR"][tok:tok + 128, :])

    @_stage
    def stage_router(self, l, last):
        fw, nc = self.fw, self.nc
        T, NT = self.T, self.NT
        rw = fw.sb([128, 8, NE], F32, "rw")
        fw.dma("sp", rw[:], self.din["router_w"][l].rearrange("(k p) e -> p k e", p=128), writes=[rw])
        rb = fw.sb([128, NE], F32, "rb")
        fw.dma("sp", rb[:], self.din["router_bias"][l].partition_broadcast(128), writes=[rb])
        xr = Ring([fw.sb([128, D], F32, "x") for _ in range(3)])
        xnr = Ring([fw.sb([128, D], F32, "xn") for _ in range(2)])
        sr = Ring([fw.sb([128, 16], F32, "st") for _ in range(3)])
        hbr = Ring([fw.sb([128, 8, 128], BF16, "hb") for _ in range(3)])
        hfr = Ring([fw.sb([128, 8, 128], F32, "hf") for _ in range(2)])
        scr_ = Ring([fw.sb([128, NE], F32, "sc") for _ in range(2)])
        bir = Ring([fw.sb([128, NE], F32, "bi") for _ in range(2)])
        m8r = Ring([fw.sb([128, 16], F32, "m8") for _ in range(2)])
        gtr = Ring([fw.sb([128, NE + 1], F32, "gt") for _ in range(3)])
        tpr = Ring([fw.ps([128, 8, 128], F32, "tp") for _ in range(1)])
        plr = Ring([fw.ps([128, NE], F32, "pl") for _ in range(1)])
        sc_lat = fw.sb([128, D], F32, "sclat"); sc_ctx = fw.sb([128, D], F32, "scctx")
        sh_lat = fw.sb([128, D], F32, "shlat"); sh_ctx = fw.sb([128, D], F32, "shctx")
        pg = fw.ps([128, 2, 512], F32, "pg")
        repr_ = Ring([fw.sb([128, 128], F32, "rep") for _ in range(2)])
        self.bcast_mod(32, sc_lat, sc_ctx, pg, repr_)
        self.bcast_mod(24, sh_lat, sh_ctx, pg, repr_)
        slb = fw.sb([128, 128], BF16, "slb"); onesb = fw.sb([128, 128], BF16, "onesb")
        fw.dma("pool", slb[:], self.din["moe_sl"], writes=[slb])
        fw.op("dve", lambda: nc.vector.memset(onesb[:], 1.0), writes=[onesb])
        fw.op("dve", lambda: nc.vector.memset(self.moe_run[:], 0.0), writes=[self.moe_run])
        xbr = Ring([fw.sb([128, D], BF16, "xb") for _ in range(2)])
        xtr = Ring([fw.sb([128, D], F32, "xt") for _ in range(2)])
        mkr = Ring([fw.sb([128, NE], BF16, "mk") for _ in range(2)])
        posr = Ring([fw.sb([128, NE], F32, "pos") for _ in range(2)])
        ppr = Ring([fw.ps([128, 2, NE], F32, "ppos") for _ in range(1)])
        ntile = (T if last else NT) // 128
        for ti in range(ntile):
            tok = ti * 128
            wh = 0 if tok < T else 1
            x = xr.next()
            fw.dma("sp", x[:], self.scr["XR"][tok:tok + 128, :], writes=[x])
            s = sr.next()
            self.ln_stats(x, s)
            xn = xnr.next()
            fw.op("dve", lambda: nc.vector.tensor_scalar(out=xn[:], in0=x[:], scalar1=s[:, 12:13], scalar2=s[:, 14:15], op0=ALU.subtract, op1=ALU.mult),
                  reads=[x, s], writes=[xn])
            tp = tpr.next()
            for k in range(8):
                fw.op("pe", lambda k=k: nc.tensor.transpose(out=tp[:, k, :], in_=xn[:, k * 128:(k + 1) * 128], identity=self.ident[:]),
                      reads=[xn, self.ident], writes=[tp], inc=(k == 7))
            hb, hf = hbr.next(), hfr.next()
            for k in range(8):
                sc_ = self.modv[:, 32 + k, wh:wh + 1]
                sh_ = self.modv[:, 24 + k, wh:wh + 1]
                fw.op("act", lambda k=k, sc_=sc_, sh_=sh_: nc.scalar.activation(out=hf[:, k, :], in_=tp[:, k, :], func=AF.Identity, bias=sh_, scale=sc_),
                      reads=[tp, self.modv], writes=[hf])
            fw.op("dve", lambda: nc.vector.tensor_copy(out=hb[:], in_=hf[:]), reads=[hf], writes=[hb])
            fw.dma("pool", self.scr["HT"][:, tok:tok + 128].rearrange("(k p) t -> p k t", p=128), hb[:], reads=[hb])
            xt, xb = xtr.next(), xbr.next()
            scb, shb = (sc_lat, sh_lat) if wh == 0 else (sc_ctx, sh_ctx)
            fw.op("pool", lambda: nc.gpsimd.tensor_tensor(out=xt[:], in0=xn[:], in1=scb[:], op=ALU.mult), reads=[xn, scb], writes=[xt])
            fw.op("pool", lambda: nc.gpsimd.tensor_tensor(out=xb[:], in0=xt[:], in1=shb[:], op=ALU.add), reads=[xt, shb], writes=[xb])
            fw.dma("pool", self.scr["XB"][tok:tok + 128, :], xb[:], reads=[xb])
            pl = plr.next()
            for k in range(8):
                fw.op("pe", lambda k=k: nc.tensor.matmul(pl[:, :], lhsT=hf[:, k, :], rhs=rw[:, k, :], start=(k == 0), stop=(k == 7)),
                      reads=[hf, rw], writes=[pl], inc=(k == 7))
            sc = scr_.next()
            fw.op("act", lambda: nc.scalar.activation(out=sc[:], in_=pl[:], func=AF.Sigmoid), reads=[pl], writes=[sc])
            bi = bir.next()
            fw.op("dve", lambda: nc.vector.tensor_tensor(out=bi[:], in0=sc[:], in1=rb[:], op=ALU.add), reads=[sc, rb], writes=[bi])
            m8 = m8r.next()
            fw.op("dve", lambda: nc.vector.max(out=m8[:, 0:8], in_=bi[:]), reads=[bi], writes=[m8])
            fw.op("dve", lambda: nc.vector.tensor_reduce(out=m8[:, 8:9], in_=m8[:, 0:8], axis=AX.X, op=ALU.min), reads=[m8], writes=[m8])
            fw.op("dve", lambda: nc.vector.tensor_scalar(out=bi[:], in0=bi[:], scalar1=m8[:, 8:9], scalar2=None, op0=ALU.is_ge), reads=[bi, m8], writes=[bi])
            fw.op("dve", lambda: nc.vector.tensor_tensor(out=sc[:], in0=sc[:], in1=bi[:], op=ALU.mult), reads=[sc, bi], writes=[sc])
            fw.op("dve", lambda: nc.vector.reduce_sum(out=m8[:, 9:10], in_=sc[:], axis=AX.X), reads=[sc], writes=[m8])
            fw.op("dve", lambda: nc.vector.reciprocal(out=m8[:, 10:11], in_=m8[:, 9:10]), reads=[m8], writes=[m8])
            gt = gtr.next()
            fw.op("dve", lambda: nc.vector.tensor_scalar(out=gt[:, 0:NE], in0=sc[:], scalar1=m8[:, 10:11], scalar2=2.5, op0=ALU.mult, op1=ALU.mult),
                  reads=[sc, m8], writes=[gt])
            fw.op("pool", lambda: nc.gpsimd.memset(gt[:, NE:NE + 1], 1.0), writes=[gt])
            fw.dma("pool", self.scr["GT"][ti], gt[:], reads=[gt])
            mk = mkr.next()
            fw.op("dve", lambda: nc.vector.tensor_copy(out=mk[:], in_=bi[:]), reads=[bi], writes=[mk])
            pp = ppr.next()
            fw.op("pe", lambda: nc.tensor.matmul(pp[:, 0, :], lhsT=slb[:], rhs=mk[:], start=True, stop=True), reads=[slb, mk], writes=[pp], inc=False)
            fw.op("pe", lambda: nc.tensor.matmul(pp[:, 1, :], lhsT=onesb[:], rhs=mk[:], start=True, stop=True), reads=[onesb, mk], writes=[pp])
            pos = posr.next()
            fw.op("dve", lambda: nc.vector.tensor_tensor(out=pos[:], in0=pp[:, 0, :], in1=self.moe_run[:], op=ALU.add), reads=[pp, self.moe_run], writes=[pos])
            fw.op("dve", lambda: nc.vector.tensor_tensor(out=self.moe_run[:], in0=pp[:, 1, :], in1=self.moe_run[:], op=ALU.add),
                  reads=[pp, self.moe_run], writes=[self.moe_run])
            fw.dma("pool", self.scr["POS"][ti], pos[:], reads=[pos])

    @_stage
    def stage_dispatch(self, l, last):
        fw, nc = self.fw, self.nc
        T, NT, NB = self.T, self.NT, self.NB
        U32 = mybir.dt.uint32
        ntile = (T if last else NT) // 128
        run = self.moe_run
        slf = fw.sb([128, 128], F32, "slf")
        fw.dma("sp", slf[:], self.din["moe_sl"], writes=[slf])
        biota = fw.sb([128, NB], F32, "biota")
        fw.dma("sp", biota[:], self.din["moe_biota"], writes=[biota])
        piota = fw.sb([128, 1], F32, "piota")
        fw.dma("sp", piota[:], self.din["moe_piota"], writes=[piota])
        eiota = fw.sb([128, NE], F32, "eiota")
        fw.dma("sp", eiota[:], self.din["moe_eiota"], writes=[eiota])
        a = fw.sb([128, NE], F32, "a"); b_ = fw.sb([128, NE], F32, "b"); ci = fw.sb([128, NE], I32, "ci")
        cnd = fw.sb([128, NE], F32, "cnd"); padded = fw.sb([128, NE], F32, "padded")
        V = nc.vector
        fw.op("dve", lambda: V.tensor_scalar(out=a[:], in0=run[:], scalar1=127.0, scalar2=1.0 / 128, op0=ALU.add, op1=ALU.mult), reads=[run], writes=[a])
        fw.op("dve", lambda: V.tensor_scalar_add(out=a[:], in0=a[:], scalar1=-0.49609375), reads=[a], writes=[a])
        fw.op("dve", lambda: V.tensor_copy(out=ci[:], in_=a[:]), reads=[a], writes=[ci])
        fw.op("dve", lambda: V.tensor_copy(out=cnd[:], in_=ci[:]), reads=[ci], writes=[cnd])
        fw.op("dve", lambda: V.tensor_scalar(out=a[:], in0=cnd[:], scalar1=128.0, scalar2=None, op0=ALU.mult), reads=[cnd], writes=[a])
        fw.op("dve", lambda: V.tensor_tensor(out=b_[:], in0=a[:], in1=run[:], op=ALU.is_lt), reads=[a, run], writes=[b_])
        fw.op("dve", lambda: V.tensor_tensor(out=cnd[:], in0=cnd[:], in1=b_[:], op=ALU.add), reads=[cnd, b_], writes=[cnd])
        fw.op("dve", lambda: V.tensor_scalar(out=a[:], in0=cnd[:], scalar1=128.0, scalar2=-128.0, op0=ALU.mult, op1=ALU.add), reads=[cnd], writes=[a])
        fw.op("dve", lambda: V.tensor_tensor(out=b_[:], in0=a[:], in1=run[:], op=ALU.is_ge), reads=[a, run], writes=[b_])
        fw.op("dve", lambda: V.tensor_tensor(out=cnd[:], in0=cnd[:], in1=b_[:], op=ALU.subtract), reads=[cnd, b_], writes=[cnd])
        fw.op("dve", lambda: V.tensor_scalar(out=padded[:], in0=cnd[:], scalar1=128.0, scalar2=None, op0=ALU.mult), reads=[cnd], writes=[padded])
        pt = fw.ps([128, 128], F32, "pt"); pst = fw.ps([128, 128], F32, "pst")
        padT = fw.sb([128, 128], F32, "padT"); pstart = fw.sb([128, NE], F32, "pstart"); pend = fw.sb([128, NE], F32, "pend")
        pendT = fw.sb([128, 128], F32, "pendT")
        fw.op("pe", lambda: nc.tensor.transpose(out=pt[:], in_=padded[:], identity=self.ident[:]), reads=[padded, self.ident], writes=[pt])
        fw.op("act", lambda: nc.scalar.copy(out=padT[:], in_=pt[:]), reads=[pt], writes=[padT])
        fw.op("pe", lambda: nc.tensor.matmul(pst[:], lhsT=padT[:], rhs=slf[:], start=True, stop=True), reads=[padT, slf], writes=[pst])
        fw.op("act", lambda: nc.scalar.copy(out=pstart[:], in_=pst[:]), reads=[pst], writes=[pstart])
        fw.op("dve", lambda: V.tensor_tensor(out=pend[:], in0=pstart[:], in1=padded[:], op=ALU.add), reads=[pstart, padded], writes=[pend])
        fw.op("pe", lambda: nc.tensor.transpose(out=pt[:], in_=pend[:], identity=self.ident[:]), reads=[pend, self.ident], writes=[pt])
        fw.op("act", lambda: nc.scalar.copy(out=pendT[:], in_=pt[:]), reads=[pt], writes=[pendT])
        cmp_ = fw.sb([128, NB], F32, "cmp"); ebc = fw.sb([128, NB], F32, "ebc"); chg = fw.sb([128, NB], F32, "chg")
        pe_ = fw.ps([128, 2, 512], F32, "pe")
        fw.op("dve", lambda: V.tensor_scalar(out=cmp_[:], in0=biota[:], scalar1=pendT[:, 0:1], scalar2=None, op0=ALU.is_ge), reads=[biota, pendT], writes=[cmp_])
        for hb in range(2):
            c0, c1 = hb * 512, min(NB, hb * 512 + 512)
            if c0 >= NB:
                continue
            fw.op("pe", lambda hb=hb, c0=c0, c1=c1: nc.tensor.matmul(pe_[:, hb, 0:c1 - c0], lhsT=self.ones[:], rhs=cmp_[:, c0:c1], start=True, stop=True),
                  reads=[self.ones, cmp_], writes=[pe_])
            fw.op("dve", lambda hb=hb, c0=c0, c1=c1: V.tensor_scalar_min(out=ebc[:, c0:c1], in0=pe_[:, hb, 0:c1 - c0], scalar1=float(NE - 1)), reads=[pe_], writes=[ebc])
        fw.op("dve", lambda: V.memset(chg[:, 0:2], 1.0), writes=[chg])
        fw.op("dve", lambda: V.tensor_tensor(out=chg[:, 2:NB], in0=ebc[:, 2:NB], in1=ebc[:, 0:NB - 2], op=ALU.not_equal), reads=[ebc], writes=[chg])
        fw.op("dve", lambda: V.tensor_scalar(out=chg[:], in0=chg[:], scalar1=-1.0e7, scalar2=1.0e7, op0=ALU.mult, op1=ALU.add), reads=[chg], writes=[chg])
        fw.op("dve", lambda: V.scalar_tensor_tensor(out=cmp_[:], in0=ebc[:], scalar=128.0, in1=chg[:], op0=ALU.mult, op1=ALU.add), reads=[ebc, chg], writes=[cmp_])
        fw.op("dve", lambda: V.tensor_scalar_add(out=cmp_[:], in0=cmp_[:], scalar1=float(l * NE * 128)), reads=[cmp_], writes=[cmp_])
        fw.op("dve", lambda: V.tensor_scalar(out=cmp_[:], in0=cmp_[:], scalar1=piota[:, 0:1], scalar2=None, op0=ALU.add), reads=[cmp_, piota], writes=[cmp_])
        fw.op("dve", lambda: V.tensor_copy(out=self.widx[:], in_=cmp_[:]), reads=[cmp_], writes=[self.widx])
        gtr = Ring([fw.sb([128, NE + 1], F32, "gt") for _ in range(2)])
        posr = Ring([fw.sb([128, NE], F32, "pos") for _ in range(2)])
        keyr = Ring([fw.sb([128, NE], F32, "key") for _ in range(2)])
        v8r = Ring([fw.sb([128, 8], F32, "v8") for _ in range(2)])
        d8r = Ring([fw.sb([128, 8], F32, "d8") for _ in range(2)])
        jr = Ring([fw.sb([128, NE], F32, "junk") for _ in range(2)])
        xbr = Ring([fw.sb([128, D], BF16, "xb") for _ in range(3)])
        for ti in range(ntile):
            tok = ti * 128
            gt, pos = gtr.next(), posr.next()
            fw.dma("sp", gt[:], self.scr["GT"][ti], writes=[gt])
            fw.dma("sp", pos[:], self.scr["POS"][ti], writes=[pos])
            xb = xbr.next()
            fw.dma("sp", xb[:], self.scr["XB"][tok:tok + 128, :], writes=[xb])
            fw.op("dve", lambda: V.tensor_tensor(out=pos[:], in0=pos[:], in1=pstart[:], op=ALU.add), reads=[pos, pstart], writes=[pos])
            key = keyr.next()
            fw.op("dve", lambda: V.scalar_tensor_tensor(out=key[:], in0=gt[:, 0:NE], scalar=0.0, in1=eiota[:], op0=ALU.is_gt, op1=ALU.mult), reads=[gt, eiota], writes=[key])
            v8, d8 = v8r.next(), d8r.next()
            fw.op("dve", lambda: V.max(out=v8[:], in_=key[:]), reads=[key], writes=[v8])
            fw.op("dve", lambda: V.memset(d8[:], 0.0), writes=[d8])
            fw.op("dve", lambda: V.memset(self.gate8[:, ti, :], 0.0), writes=[self.gate8])
            for k in range(8):
                j1, j2 = jr.next(), jr.next()
                fw.op("dve", lambda k=k, j1=j1: V.scalar_tensor_tensor(out=j1[:], in0=key[:], scalar=v8[:, k:k + 1], in1=pos[:], op0=ALU.is_equal, op1=ALU.mult,
                                                                       accum_out=d8[:, k:k + 1]), reads=[key, v8, pos], writes=[j1, d8])
                fw.op("dve", lambda k=k, j2=j2: V.scalar_tensor_tensor(out=j2[:], in0=key[:], scalar=v8[:, k:k + 1], in1=gt[:, 0:NE], op0=ALU.is_equal, op1=ALU.mult,
                                                                       accum_out=self.gate8[:, ti, k:k + 1]), reads=[key, v8, gt], writes=[j2, self.gate8])
            fw.op("dve", lambda: V.tensor_copy(out=self.dest8[:, ti, :], in_=d8[:]), reads=[d8], writes=[self.dest8])
            for k in range(8):
                fw.idma(self.scr["XS"], bass.IndirectOffsetOnAxis(ap=self.dest8[:, ti, k:k + 1], axis=0), xb[:], None, reads=[xb, self.dest8])

    @_stage
    def stage_blocks(self, l, last):
        fw, nc = self.fw, self.nc
        T, NT = self.T, self.NT
        ntile = (T if last else NT) // 128
        NBl = ntile * 8 + NE
        w1v, w3v, w2v = self.din["exp_w1"], self.din["exp_w3"], self.din["exp_w2"]
        wsets = [[fw.sb([128, 2048], BF16, nm) for nm in ("w1", "w3", "w2")] for _ in range(2)]

        class V3:
            def __init__(self, b, k):
                self.b, self.k = b, k

            def __getitem__(self, key):
                return self.b[:].rearrange("p (k f) -> p k f", k=self.k)[key]
        xsr = Ring([fw.sb([128, D], BF16, "xs") for _ in range(3)])
        xTr = Ring([fw.sb([128, 8, 128], BF16, "xT") for _ in range(2)])
        sar = Ring([fw.sb([128, 256], F32, "sa") for _ in range(2)])
        hdr = Ring([fw.sb([128, 256], BF16, "hd") for _ in range(2)])
        hTr = Ring([fw.sb([128, 2, 128], BF16, "hdT") for _ in range(2)])
        ysr = Ring([fw.sb([128, D], BF16, "ys") for _ in range(2)])
        ptx = Ring([fw.ps([128, 8, 128], BF16, "ptx") for _ in range(2)])
        pab = Ring([fw.ps([128, 512], F32, "pab") for _ in range(2)])
        pth = Ring([fw.ps([128, 2, 128], BF16, "pth") for _ in range(1)])
        pyr = Ring([fw.ps([128, 2, 512], F32, "py") for _ in range(1)])
        if not hasattr(self, "bound_reg"):
            self.bound_reg = nc.gpsimd.alloc_register("moe_bound")
        nc.gpsimd.reg_mov(self.bound_reg, (l + 1) * NE * 128 - 1)
        bound = self.bound_reg
        for b in range(NBl):
            off = bass.IndirectOffsetOnAxis(ap=self.widx[:, b:b + 1], axis=0)
            w1_, w3_, w2_ = wsets[b % 2]
            w1, w3, w2 = V3(w1_, 8), V3(w3_, 8), V3(w2_, 2)
            for (wt, wv) in ((w1_, w1v), (w3_, w3v), (w2_, w2v)):
                fw.idma(wt[:], None, wv, off, reads=[self.widx], writes=[wt], bounds_check=bound, oob_is_err=False)
            xs = xsr.next()
            fw.dma("sp", xs[:], self.scr["XS"][b * 128:(b + 1) * 128, :], writes=[xs])
            px = ptx.next()
            for k in range(8):
                fw.op("pe", lambda k=k: nc.tensor.transpose(out=px[:, k, :], in_=xs[:].rearrange("p (f k) -> p k f", k=8)[:, k, :], identity=self.ident_bf[:]),
                      reads=[xs, self.ident_bf], writes=[px], inc=(k == 7))
            xT = xTr.next()
            fw.op("act", lambda: nc.scalar.copy(out=xT[:], in_=px[:]), reads=[px], writes=[xT])
            pa = pab.next()
            for (wt, wb, c0) in ((w1, w1_, 0), (w3, w3_, 256)):
                for k in range(8):
                    fw.op("pe", lambda wt=wt, c0=c0, k=k: nc.tensor.matmul(pa[:, c0:c0 + 256], lhsT=xT[:, k, :], rhs=wt[:, k, :], start=(k == 0), stop=(k == 7)),
                          reads=[xT, wb], writes=[pa], inc=(k == 7))
            sa, hd = sar.next(), hdr.next()
            fw.op("act", lambda: nc.scalar.activation(out=sa[:], in_=pa[:, 0:256], func=AF.Silu), reads=[pa], writes=[sa])
            fw.op("dve", lambda: nc.vector.tensor_tensor(out=hd[:], in0=sa[:], in1=pa[:, 256:512], op=ALU.mult), reads=[sa, pa], writes=[hd])
            ph = pth.next()
            for k in range(2):
                fw.op("pe", lambda k=k: nc.tensor.transpose(out=ph[:, k, :], in_=hd[:].rearrange("p (f k) -> p k f", k=2)[:, k, :], identity=self.ident_bf[:]),
                      reads=[hd, self.ident_bf], writes=[ph], inc=(k == 1))
            hT = hTr.next()
            fw.op("dve", lambda: nc.vector.tensor_copy(out=hT[:], in_=ph[:]), reads=[ph], writes=[hT])
            py = pyr.next()
            for half in range(2):
                for k in range(2):
                    fw.op("pe", lambda half=half, k=k: nc.tensor.matmul(py[:, half, :], lhsT=hT[:, k, :], rhs=w2[:, k, half * 512:(half + 1) * 512],
                                                                         start=(k == 0), stop=(k == 1)), reads=[hT, w2_], writes=[py], inc=(k == 1))
            ys = ysr.next()
            fw.op("act", lambda: nc.scalar.copy(out=ys[:, 0:512], in_=py[:, 0, :]), reads=[py], writes=[ys])
            fw.op("dve", lambda: nc.vector.tensor_copy(out=ys[:, 512:1024], in_=py[:, 1, :]), reads=[py], writes=[ys])
            fw.dma("sp", self.scr["YS"][b * 128:(b + 1) * 128, :], ys[:], reads=[ys])

    @_stage
    def stage_experts(self, l, last, G=16, experts=(NE,)):
        fw, nc = self.fw, self.nc
        T, NT = self.T, self.NT
        g_lat = fw.sb([128, D], F32, "glat"); g_ctx = fw.sb([128, D], F32, "gctx")
        pg = fw.ps([128, 2, 512], F32, "pg")
        repr_ = Ring([fw.sb([128, 128], F32, "rep") for _ in range(2)])
        self.bcast_mod(40, g_lat, g_ctx, pg, repr_)
        lg, lb = self.load_ln(l, "ln2")
        ntile = (T if last else NT) // 128
        acc = fw.sb([128, G, D], F32, "acc")
        hT = fw.sb([128, 8, G * 128], BF16, "hTg")
        gts = fw.sb([128, G, NE + 1], F32, "gts")
        w1r = Ring([fw.sb([128, 8, 256], BF16, "w1") for _ in range(2)])
        w3r = Ring([fw.sb([128, 8, 256], BF16, "w3") for _ in range(2)])
        w2r = Ring([fw.sb([128, 2, D], BF16, "w2") for _ in range(2)])
        sar = Ring([fw.sb([128, 2, 512], BF16, "sa") for _ in range(2)])
        hdr = Ring([fw.sb([128, 2, 512], BF16, "hd") for _ in range(2)])
        xr = Ring([fw.sb([128, D], F32, "x") for _ in range(2)])
        ykr = Ring([fw.sb([128, D], BF16, "yk") for _ in range(4)])
        sr = Ring([fw.sb([128, 16], F32, "st") for _ in range(2)])
        par = Ring([fw.ps([128, 2, 512], F32, "pa") for _ in range(1)])
        pbr = Ring([fw.ps([128, 2, 512], F32, "pb3") for _ in range(1)])
        pyr = Ring([fw.ps([128, 512], F32, "py") for _ in range(2)])
        dn = self.din
        for g0 in range(0, ntile, G):
            gn = min(G, ntile - g0)
            tok0 = g0 * 128
            for k in range(8):
                fw.dma("sp", hT[:, k, 0:gn * 128], self.scr["HT"][k * 128:(k + 1) * 128, tok0:tok0 + gn * 128], writes=[hT])
            fw.dma("sp", gts[:, 0:gn, :], self.scr["GT"][g0:g0 + gn].rearrange("g p e -> p g e"), writes=[gts])
            for ei, e in enumerate(experts):
                w1, w3, w2 = w1r.next(), w3r.next(), w2r.next()
                if e < NE:
                    raise NotImplementedError("routed experts run in stage_blocks")
                else:
                    s1, s3, s2 = dn["sh_w1"][l], dn["sh_w3"][l], dn["sh_w2"][l]
                fw.dma("pool", w1[:], s1.rearrange("(k p) f -> p k f", p=128), writes=[w1])
                fw.dma("pool", w3[:], s3.rearrange("(k p) f -> p k f", p=128), writes=[w3])
                fw.dma("pool", w2[:], s2.rearrange("(k p) n -> p k n", p=128), writes=[w2])
                for c0 in range(0, gn, 4):
                    cn_ = min(4, gn - c0)
                    n = cn_ * 128
                    pa, pb = par.next(), pbr.next()
                    for (w, p) in ((w1, pa), (w3, pb)):
                        for hc in range(2):
                            for k in range(8):
                                fw.op("pe", lambda w=w, p=p, hc=hc, k=k: nc.tensor.matmul(p[:, hc, 0:n], lhsT=w[:, k, hc * 128:(hc + 1) * 128],
                                                                                         rhs=hT[:, k, c0 * 128:c0 * 128 + n], start=(k == 0), stop=(k == 7)),
                                      reads=[w, hT], writes=[p], inc=(k == 7))
                    sa, hd = sar.next(), hdr.next()
                    for hc in range(2):
                        fw.op("act", lambda hc=hc: nc.scalar.activation(out=sa[:, hc, 0:n], in_=pa[:, hc, 0:n], func=AF.Silu), reads=[pa], writes=[sa])
                        fw.op("dve", lambda hc=hc: nc.vector.tensor_tensor(out=hd[:, hc, 0:n], in0=sa[:, hc, 0:n], in1=pb[:, hc, 0:n], op=ALU.mult),
                              reads=[sa, pb], writes=[hd])
                    for t in range(cn_):
                        gi = c0 + t
                        for half in range(2):
                            py = pyr.next()
                            for hc in range(2):
                                fw.op("pe", lambda t=t, half=half, hc=hc, py=py: nc.tensor.matmul(py[:, :], lhsT=hd[:, hc, t * 128:(t + 1) * 128],
                                                                                                 rhs=w2[:, hc, half * 512:(half + 1) * 512], start=(hc == 0), stop=(hc == 1)),
                                      reads=[hd, w2], writes=[py], inc=(hc == 1))
                            a = acc[:, gi, half * 512:(half + 1) * 512]
                            gsc = gts[:, gi, e:e + 1]
                            if ei == 0:
                                fw.op("dve", lambda a=a, py=py, gsc=gsc: nc.vector.tensor_scalar(out=a, in0=py[:], scalar1=gsc, scalar2=None, op0=ALU.mult),
                                      reads=[py, gts], writes=[acc])
                            else:
                                fw.op("dve", lambda a=a, py=py, gsc=gsc: nc.vector.scalar_tensor_tensor(out=a, in0=py[:], scalar=gsc, in1=a, op0=ALU.mult, op1=ALU.add),
                                      reads=[py, gts, acc], writes=[acc])
            for gi in range(gn):
                tok = tok0 + gi * 128
                x = xr.next()
                fw.dma("sp", x[:], self.scr["XR"][tok:tok + 128, :], writes=[x])
                gb = g_lat if tok < T else g_ctx
                if NE not in experts or len(experts) == 1:
                    ti = tok // 128
                    for k in range(8):
                        yk = ykr.next()
                        fw.idma(yk[:], None, self.scr["YS"], bass.IndirectOffsetOnAxis(ap=self.dest8[:, ti, k:k + 1], axis=0), reads=[self.dest8], writes=[yk])
                        fw.op("dve", lambda gi=gi, yk=yk, ti=ti, k=k: nc.vector.scalar_tensor_tensor(
                            out=acc[:, gi, :], in0=yk[:], scalar=self.gate8[:, ti, k:k + 1], in1=acc[:, gi, :], op0=ALU.mult, op1=ALU.add),
                            reads=[yk, self.gate8, acc], writes=[acc])
                fw.op("dve", lambda gi=gi, gb=gb: nc.vector.tensor_tensor(out=acc[:, gi, :], in0=acc[:, gi, :], in1=gb[:], op=ALU.mult), reads=[acc, gb], writes=[acc])
                fw.op("dve", lambda gi=gi, x=x: nc.vector.scalar_tensor_tensor(out=x[:], in0=x[:], scalar=ALPHA, in1=acc[:, gi, :], op0=ALU.mult, op1=ALU.add),
                      reads=[x, acc], writes=[x])
                if last:
                    dst = self.out[tok:tok + 128, :]
                else:
                    dst = self.scr["XR"][tok:tok + 128, :]
                self.post_norm_store(x, sr.next(), lg, lb, dst)

    def build(self, upto=None):
        from contextlib import ExitStack
        self.declare()
        names = ["mod", "inproj", "wa_prep", "mla_prep", "na_attn", "wa_attn", "mla_attn", "gla", "outproj", "router", "dispatch", "blocks", "experts"]
        cnt = 0
        with ExitStack() as gst:
            self.setup_global(gst)
            self.fw.barrier()
            for l in range(self.L):
                last = l == self.L - 1
                for nm in names:
                    if upto is not None and cnt >= upto:
                        break
                    cnt += 1
                    fn = getattr(self, "stage_" + nm)
                    if nm in ("na_attn", "wa_attn", "mla_attn"):
                        fn(l, not last)
                    elif nm in ("outproj", "router", "dispatch", "blocks", "experts"):
                        fn(l, last)
                    else:
                        fn(l)
            self.fw.barrier()
        return self.nc


def prep_shared(inp, T, L):
    f = lambda a: np.ascontiguousarray(np.asarray(a, dtype=np.float32))
    m = dict(host_consts(T))
    m["w_ada"] = f(inp["w_ada"][:L])
    m["b_ada"] = f(inp["b_ada"][:L])
    m["b_ada_pj"] = f(np.asarray(inp["b_ada"][:L]).reshape(L, 48, 128).transpose(0, 2, 1))
    m["w_in"] = f(inp["w_in"][:L])
    m["na_rpb"] = f(inp["na_rpb"][:L]); m["wa_sink"] = f(inp["wa_sink"][:L])
    m["mla_g_q"] = f(np.asarray(inp["mla_g_q"][:L]).reshape(L, 2, 128).transpose(0, 2, 1))
    m["mla_g_kv"] = f(np.asarray(inp["mla_g_kv"][:L]).reshape(L, 128, 1))
    m["mla_w_uq"] = f(inp["mla_w_uq"][:L]); m["mla_w_ukv"] = f(inp["mla_w_ukv"][:L])
    for k in ("gla_w_gf", "gla_b_gf", "gla_w_gb", "gla_b_gb"):
        m[k] = f(inp[k][:L])
    m["gla_g_norm"] = f(np.asarray(inp["gla_g_norm"][:L]).reshape(L, 64, 1))
    for k in ("w_out", "ln1_g", "ln1_b", "ln2_g", "ln2_b", "router_w", "router_bias", "sh_w1", "sh_w3", "sh_w2"):
        m[k] = f(inp[k][:L])
    for k in ("exp_w1", "exp_w3", "exp_w2"):
        m[k] = f(inp[k][:L]).reshape(L * NE * 128, 2048)
    return m


def prep_core(inp, b):
    f = lambda a: np.ascontiguousarray(np.asarray(a, dtype=np.float32))
    c = np.asarray(inp["c"][b], dtype=np.float32)
    cc = np.asarray(inp["c_ctx"], dtype=np.float32)
    cv = np.stack([c.reshape(8, 128).T, cc.reshape(8, 128).T], axis=-1)
    return {"x": f(inp["x"][b]), "ctx": f(inp["ctx"][b]), "cv": f(cv)}


_CACHE = {}


def kernel(**inputs):
    B, T, _ = inputs["x"].shape
    L = inputs["w_ada"].shape[0]
    key = (T, L)
    if key not in _CACHE:
        _CACHE[key] = KB(T, L).build()
    nc = _CACHE[key]
    shared = prep_shared(inputs, T, L)
    in_maps = []
    for b in range(B):
        m = dict(shared)
        m.update(prep_core(inputs, b))
        in_maps.append(m)
    res = run_bass_kernel_spmd(nc, in_maps, core_ids=list(range(B)))
    return np.stack([np.asarray(res.results[b]["out"], dtype=np.float32) for b in range(B)], axis=0)
```

```python
import numpy as np
import ml_dtypes
import concourse.bass as bass
import concourse.mybir as mybir
from concourse.bass_utils import run_bass_kernel_spmd

F32 = mybir.dt.float32
BF16 = mybir.dt.bfloat16
I32 = mybir.dt.int32
AF = mybir.ActivationFunctionType
ALU = mybir.AluOpType
AX = mybir.AxisListType

D = 1024
C = 256
NE = 128
LN_EPS = 1e-5
RMS_EPS = 1e-6
ALPHA = 8 ** 0.25
IN_W = 2496
O_AQ, O_AK, O_AV = 0, 256, 512
O_BQ, O_BK, O_BV = 768, 1024, 1152
O_CQ, O_CKV, O_CKR = 1280, 1536, 1664
O_DQ, O_DK, O_DV, O_DR, O_ZF, O_ZB = 1696, 1824, 1952, 2208, 2464, 2480


class Buf:
    def __init__(self, t, name):
        self.t = t
        self.name = name
        self.w = None
        self.r = []

    def __getitem__(self, k):
        return self.t[k]


class FW:
    def __init__(self, nc):
        self.nc = nc
        self.eng = {"pe": nc.tensor, "act": nc.scalar, "dve": nc.vector, "pool": nc.gpsimd, "sp": nc.sync}
        self.sem = {}
        self.cnt = {}
        self.waited = {}
        for e in self.eng:
            self.sem[e] = nc.alloc_semaphore("s_" + e)
            self.cnt[e] = 0
            self.waited[e] = {}
        self.pend = {e: False for e in self.eng}
        self.dsem = {}
        self.dval = {}
        self.drr = {}
        for q in ("sp", "pool", "act"):
            self.dsem[q] = [nc.alloc_semaphore("d_%s%d" % (q, i)) for i in range(12)]
            self.dval[q] = [0] * 12
            self.drr[q] = 0
        self.semkey = {}
        self.nbuf = 0

    def sb(self, shape, dt, name=None):
        self.nbuf += 1
        name = (name or "t") + "_%d" % self.nbuf
        return Buf(self.stack.enter_context(self.nc.sbuf_tensor(name, list(shape), dt)), name)

    def ps(self, shape, dt=F32, name=None):
        self.nbuf += 1
        name = (name or "p") + "_%d" % self.nbuf
        return Buf(self.stack.enter_context(self.nc.psum_tensor(name, list(shape), dt)), name)

    def _wait(self, e, tok):
        if tok is None:
            return
        sem, val, key = tok
        if self.waited[e].get(key, 0) >= val:
            return
        self.eng[e].wait_ge(sem, val)
        self.waited[e][key] = val

    def _deps(self, e, reads, writes):
        for b in reads:
            if b.w is not None:
                if not (e == "pe" and b.w[2] == "pe"):
                    self._wait(e, b.w)
        for b in writes:
            if b.w is not None and b.w[2] != e:
                self._wait(e, b.w)
            for tok in b.r:
                if tok[2] != e:
                    self._wait(e, tok)

    def _mark(self, tok, reads, writes):
        for b in reads:
            b.r.append(tok)
            if len(b.r) > 24:
                last = {}
                for t in b.r:
                    if t[2] not in last or last[t[2]][1] < t[1]:
                        last[t[2]] = t
                b.r = list(last.values())
        for b in writes:
            b.w = tok
            b.r = []

    def op(self, e, fn, reads=(), writes=(), inc=True):
        self._deps(e, reads, writes)
        ins = fn()
        if inc:
            self.cnt[e] += 1
            ins.then_inc(self.sem[e], 1)
            self.pend[e] = False
            tok = (self.sem[e], self.cnt[e], e)
        else:
            self.pend[e] = True
            tok = (self.sem[e], self.cnt[e] + 1, e)
        self._mark(tok, reads, writes)
        return tok

    def dma(self, q, out, in_, reads=(), writes=()):
        self._deps(q, reads, writes)
        i = self.drr[q]
        self.drr[q] = (i + 1) % len(self.dsem[q])
        sem = self.dsem[q][i]
        key = "d_%s%d" % (q, i)
        if self.dval[q][i] > 0:
            self._wait(q, (sem, self.dval[q][i], key))
        self.dval[q][i] += 16
        self.eng[q].dma_start(out=out, in_=in_).then_inc(sem, 16)
        tok = (sem, self.dval[q][i], key)
        self._mark(tok, reads, writes)
        return tok

    def idma(self, out, out_off, in_, in_off, reads=(), writes=(), **kw):
        q = "pool"
        self._deps(q, reads, writes)
        i = self.drr[q]
        self.drr[q] = (i + 1) % len(self.dsem[q])
        sem = self.dsem[q][i]
        key = "d_%s%d" % (q, i)
        if self.dval[q][i] > 0:
            self._wait(q, (sem, self.dval[q][i], key))
        self.dval[q][i] += 16
        self.nc.gpsimd.indirect_dma_start(out=out, out_offset=out_off, in_=in_, in_offset=in_off, **kw).then_inc(sem, 16)
        tok = (sem, self.dval[q][i], key)
        self._mark(tok, reads, writes)
        return tok

    def barrier(self):
        for e in self.eng:
            assert not self.pend[e], "pending non-inc op on " + e
        toks = []
        for e in self.eng:
            if self.cnt[e] > 0:
                toks.append((self.sem[e], self.cnt[e], e))
        for q in self.dsem:
            for i, s in enumerate(self.dsem[q]):
                if self.dval[q][i] > 0:
                    toks.append((s, self.dval[q][i], "d_%s%d" % (q, i)))
        for e in self.eng:
            for t in toks:
                if t[2] != e:
                    self._wait(e, t)


class Ring:
    def __init__(self, bufs):
        self.bufs = bufs
        self.i = 0

    def next(self):
        b = self.bufs[self.i]
        self.i = (self.i + 1) % len(self.bufs)
        return b


def host_consts(T):
    cs = {}
    cs["ident"] = np.eye(128, dtype=np.float32)
    t = np.arange(T)
    row, col = (t // 64).astype(np.float32), (t % 64).astype(np.float32)

    def rope_tabs(d):
        half = d // 2
        inv = (10000.0 ** (-np.arange(half, dtype=np.float32) / half)).astype(np.float32)
        cos = np.zeros((2 * d, T), np.float32)
        sin = np.zeros((2 * d, T), np.float32)
        R = np.zeros((2 * d, 2 * d), np.float32)
        for a, pos in enumerate((row, col)):
            ang = pos[None, :] * inv[:, None]
            for hh in range(2):
                cos[a * d + hh * half:a * d + (hh + 1) * half] = np.cos(ang)
                sin[a * d + hh * half:a * d + (hh + 1) * half] = np.sin(ang)
            for i in range(half):
                R[a * d + i, a * d + half + i] = -1.0
                R[a * d + half + i, a * d + i] = 1.0
        return cos, sin, R

    cw, sw, Rw = rope_tabs(32)
    cs["cos_wa"], cs["sin_wa"], cs["rt_wa"] = cw, sw, np.ascontiguousarray(Rw.T)
    cm, sm, Rm = rope_tabs(16)
    cos_m = np.ones((64, T), np.float32); sin_m = np.zeros((64, T), np.float32); R64 = np.zeros((64, 64), np.float32)
    cos_m[32:], sin_m[32:], R64[32:, 32:] = cm, sm, Rm
    cs["cos_mla"], cs["sin_mla"], cs["rt_mla"] = cos_m, sin_m, np.ascontiguousarray(R64.T)
    kk = np.arange(128)[:, None]; qq = np.arange(512)[None, :]
    m = np.zeros((6, 128, 512), np.float32)
    for r in range(-1, 5):
        m[r + 1] = (np.abs(r * 128 + kk - qq) <= 128)
    cs["maskwa"] = m
    oh = np.zeros((31, 64, 64), np.float32)
    cmask = np.zeros((64, 15, 64), np.float32)
    for c_ in range(64):
        cs0 = min(max(c_ - 8, 0), 48)
        for kc in range(64):
            j = kc - c_ + 15
            if 0 <= j < 31:
                oh[j, c_, kc] = 1.0
            if cs0 <= kc < cs0 + 16:
                cmask[kc, :, c_] = 1.0
    cs["na_oh"] = oh
    cs["na_cmask"] = cmask
    cs["j15"] = np.ascontiguousarray(np.eye(15, dtype=np.float32)[::-1])
    a = np.arange(128)
    same = (a[:, None] // 64) == (a[None, :] // 64)
    le = a[:, None] <= a[None, :]
    lt = a[:, None] < a[None, :]
    g = np.zeros((4, 128, 128), np.float32)
    g[0] = same & le
    g[1] = same & le.T
    g[2] = same & lt.T
    g[3] = same & lt
    cs["gla_tri"] = (g * (-1.0 / 16.0)).astype(np.float32)
    mk = np.zeros((2, 128, 4, 128), np.float32)
    mk[0] = (same & le)[:, None, :]
    mk[1] = (same & le.T)[:, None, :]
    cs["gla_mask"] = mk
    NB = (T + C) // 128 * 8 + NE
    cs["moe_sl"] = np.triu(np.ones((128, 128), np.float32), 1)
    cs["moe_biota"] = np.tile((np.arange(NB, dtype=np.float32) * 128.0)[None, :], (128, 1))
    cs["moe_piota"] = np.arange(128, dtype=np.float32).reshape(128, 1)
    cs["moe_eiota"] = np.tile((128.0 - np.arange(128, dtype=np.float32))[None, :], (128, 1))
    return cs


class K:
    def __init__(self, T, L, debug=()):
        self.T, self.L = T, L
        self.NT = T + C
        self.debug = set(debug)
        nc = bass.Bass("TRN2", target_bir_lowering=False)
        self.nc = nc
        self.fw = FW(nc)
        self.din = {}
        self.scr = {}

    def inp(self, name, shape, dt=F32):
        self.din[name] = self.nc.dram_tensor(name, list(shape), dt, kind="ExternalInput").ap()
        return self.din[name]

    def scratch(self, name, shape, dt):
        kind = "ExternalOutput" if name in self.debug else "Internal"
        self.scr[name] = self.nc.dram_tensor(name, list(shape), dt, kind=kind).ap()
        return self.scr[name]

    def chunks(self, n=512):
        out = []
        t = 0
        while t < self.NT:
            m = min(n, self.NT - t)
            out.append((t, m))
            t += m
        return out

    def declare(self):
        T, L, NT = self.T, self.L, self.NT
        i = self.inp
        i("x", [T, D]); i("ctx", [C, D]); i("cv", [128, 8, 2])
        i("w_ada", [L, D, 6 * D]); i("b_ada_pj", [L, 128, 48]); i("b_ada", [L, 6 * D])
        i("w_in", [L, D, IN_W])
        i("ident", [128, 128])
        i("cos_wa", [64, T]); i("sin_wa", [64, T]); i("rt_wa", [64, 64])
        i("cos_mla", [64, T]); i("sin_mla", [64, T]); i("rt_mla", [64, 64])
        i("maskwa", [6, 128, 512]); i("na_oh", [31, 64, 64]); i("na_cmask", [64, 15, 64]); i("j15", [15, 15])
        i("gla_tri", [4, 128, 128]); i("gla_mask", [2, 128, 4, 128])
        self.NB = NT // 128 * 8 + NE
        i("moe_sl", [128, 128]); i("moe_biota", [128, self.NB]); i("moe_piota", [128, 1]); i("moe_eiota", [128, 128])
        i("na_rpb", [L, 4, 15, 31]); i("wa_sink", [L, 4])
        i("mla_g_q", [L, 128, 2]); i("mla_g_kv", [L, 128, 1]); i("mla_w_uq", [L, 256, 256]); i("mla_w_ukv", [L, 128, 384])
        i("gla_w_gf", [L, 16, 128]); i("gla_b_gf", [L, 128]); i("gla_w_gb", [L, 16, 128]); i("gla_b_gb", [L, 128])
        i("gla_g_norm", [L, 64, 1])
        i("w_out", [L, D, D]); i("ln1_g", [L, D]); i("ln1_b", [L, D]); i("ln2_g", [L, D]); i("ln2_b", [L, D])
        i("router_w", [L, D, NE]); i("router_bias", [L, NE])
        i("exp_w1", [L * NE * 128, 2048]); i("exp_w3", [L * NE * 128, 2048]); i("exp_w2", [L * NE * 128, 2048])
        i("sh_w1", [L, D, 256]); i("sh_w3", [L, D, 256]); i("sh_w2", [L, 256, D])
        self.out = self.nc.dram_tensor("out", [T, D], F32, kind="ExternalOutput").ap()
        s = self.scratch
        s("XR", [NT, D], F32)
        s("PT", [IN_W, NT], BF16)
        s("VT", [NT, 768], BF16)
        s("QTb", [4, 64, NT], BF16); s("KTb", [2, 64, NT], BF16)
        s("QTc", [4, 64, NT], BF16); s("KTc", [4, 64, NT], BF16); s("Vc", [NT, 4, 64], BF16)
        s("OT", [D, NT], BF16)
        s("OF", [64, 4, NT], F32)
        s("HT", [D, NT], BF16)
        s("GT", [NT // 128, 128, NE + 1], F32)
        s("POS", [NT // 128, 128, NE], F32)
        s("XB", [NT, D], BF16)
        s("XS", [self.NB * 128, D], BF16)
        s("YS", [self.NB * 128, D], BF16)

    def setup_global(self, stack):
        fw, nc = self.fw, self.nc
        self.gstack = stack
        fw.stack = stack
        self.ident = fw.sb([128, 128], F32, "ident")
        fw.dma("sp", self.ident[:], self.din["ident"], writes=[self.ident])
        self.ident_bf = fw.sb([128, 128], BF16, "identb")
        fw.op("dve", lambda: nc.vector.tensor_copy(out=self.ident_bf[:], in_=self.ident[:]),
              reads=[self.ident], writes=[self.ident_bf])
        self.ones = fw.sb([128, 128], F32, "ones")
        fw.op("dve", lambda: nc.vector.memset(self.ones[:], 1.0), writes=[self.ones])
        self.epsln = fw.sb([128, 2], F32, "eps")
        fw.op("dve", lambda: nc.vector.memset(self.epsln[:, 0:1], LN_EPS), writes=[self.epsln])
        fw.op("dve", lambda: nc.vector.memset(self.epsln[:, 1:2], RMS_EPS), writes=[self.epsln])
        self.cvs = fw.sb([128, 8, 2], F32, "cvs")
        cv_raw = fw.sb([128, 8, 2], F32, "cvraw")
        fw.dma("sp", cv_raw[:], self.din["cv"], writes=[cv_raw])
        fw.op("act", lambda: nc.scalar.activation(out=self.cvs[:], in_=cv_raw[:], func=AF.Silu),
              reads=[cv_raw], writes=[self.cvs])
        self.modv = fw.sb([128, 48, 2], F32, "modv")
        self.psum_banks = None
        U32 = mybir.dt.uint32
        nt = self.NT // 128
        self.moe_run = fw.sb([128, NE], F32, "moerun")
        self.widx = fw.sb([128, self.NB], U32, "widx")
        self.dest8 = fw.sb([128, nt, 8], U32, "dest8")
        self.gate8 = fw.sb([128, nt, 8], F32, "gate8")
        zt = fw.sb([128, D], BF16, "zt")
        fw.op("dve", lambda: nc.vector.memset(zt[:], 0.0), writes=[zt])
        for b in range(self.NB):
            fw.dma("sp", self.scr["XS"][b * 128:(b + 1) * 128, :], zt[:], reads=[zt])

    def stage_mod(self, l):
        fw, nc = self.fw, self.nc
        from contextlib import ExitStack
        with ExitStack() as st:
            fw.stack = st
            wj = Ring([fw.sb([128, 8, 128], F32, "wj") for _ in range(3)])
            pm = fw.ps([128, 48, 2], F32, "pm")
            bpj = fw.sb([128, 48], F32, "bpj")
            fw.dma("sp", bpj[:], self.din["b_ada_pj"][l], writes=[bpj])
            wsrc = self.din["w_ada"][l].rearrange("(k p) n -> p k n", p=128)
            for j in range(48):
                w = wj.next()
                fw.dma("sp", w[:], wsrc[:, :, j * 128:(j + 1) * 128], writes=[w])
                for k in range(8):
                    fw.op("pe", lambda k=k, w=w: nc.tensor.matmul(pm[:, j, :], lhsT=w[:, k, :], rhs=self.cvs[:, k, :],
                                                                   start=(k == 0), stop=(k == 7)),
                          reads=[w, self.cvs], writes=[pm], inc=(k == 7))
            for wh in range(2):
                fw.op("dve", lambda wh=wh: nc.vector.tensor_tensor(out=self.modv[:, :, wh], in0=pm[:, :, wh], in1=bpj[:],
                                                                    op=ALU.add),
                      reads=[pm, bpj], writes=[self.modv])
            for j0 in (8, 32):
                fw.op("dve", lambda j0=j0: nc.vector.tensor_scalar_add(out=self.modv[:, j0:j0 + 8, :],
                                                                        in0=self.modv[:, j0:j0 + 8, :], scalar1=1.0),
                      reads=[self.modv], writes=[self.modv])
            fw.barrier()
        fw.stack = self.gstack

    def stage_inproj(self, l):
        fw, nc = self.fw, self.nc
        T, NT = self.T, self.NT
        from contextlib import ExitStack
        with ExitStack() as st:
            fw.stack = st
            win = fw.sb([128, 8, IN_W], BF16, "win")
            wsrc = self.din["w_in"][l].rearrange("(k p) n -> p k n", p=128)
            for k in range(8):
                fw.dma("pool", win[:, k, :], wsrc[:, k, :], writes=[win])
            xr = Ring([fw.sb([128, D], F32, "x") for _ in range(3)])
            xnr = Ring([fw.sb([128, D], F32, "xn") for _ in range(2)])
            str_ = Ring([fw.sb([128, 16], F32, "st") for _ in range(3)])
            hTr = Ring([fw.sb([128, 8, 512], BF16, "hT") for _ in range(2)])
            ptr = Ring([fw.sb([128, 512], BF16, "pt") for _ in range(4)])
            vtr = Ring([fw.sb([128, 768], BF16, "vt") for _ in range(2)])
            tpr = Ring([fw.ps([128, 8, 128], F32, "tp") for _ in range(2)])
            ppr = Ring([fw.ps([128, 512], F32, "pp") for _ in range(2)])
            pvr = Ring([fw.ps([128, 2, 512], F32, "pv") for _ in range(1)])
            ev = 0
            for (t0, n) in self.chunks(512):
                hT = hTr.next()
                for ti in range(n // 128):
                    tok = t0 + ti * 128
                    lat = tok < T
                    wh = 0 if lat else 1
                    if l == 0:
                        src = self.din["x"][tok:tok + 128, :] if lat else self.din["ctx"][tok - T:tok - T + 128, :]
                    else:
                        src = self.scr["XR"][tok:tok + 128, :]
                    x = xr.next()
                    fw.dma("sp", x[:], src, writes=[x])
                    s = str_.next()
                    for hh in range(2):
                        fw.op("dve", lambda x=x, s=s, hh=hh: nc.vector.bn_stats(out=s[:, hh * 6:hh * 6 + 6],
                                                                                 in_=x[:, hh * 512:(hh + 1) * 512]),
                              reads=[x], writes=[s])
                    fw.op("dve", lambda s=s: nc.vector.bn_aggr(out=s[:, 12:14], in_=s[:, 0:12]), reads=[s], writes=[s])
                    fw.op("act", lambda s=s: nc.scalar.activation(out=s[:, 15:16], in_=s[:, 13:14], func=AF.Sqrt,
                                                                   bias=self.epsln[:, 0:1], scale=1.0),
                          reads=[s, self.epsln], writes=[s])
                    fw.op("dve", lambda s=s: nc.vector.reciprocal(out=s[:, 14:15], in_=s[:, 15:16]), reads=[s], writes=[s])
                    xn = xnr.next()
                    fw.op("dve", lambda x=x, s=s, xn=xn: nc.vector.tensor_scalar(out=xn[:], in0=x[:], scalar1=s[:, 12:13],
                                                                                  scalar2=s[:, 14:15], op0=ALU.subtract,
                                                                                  op1=ALU.mult),
                          reads=[x, s], writes=[xn])
                    tp = tpr.next()
                    for k in range(8):
                        fw.op("pe", lambda k=k, tp=tp, xn=xn: nc.tensor.transpose(out=tp[:, k, :], in_=xn[:, k * 128:(k + 1) * 128],
                                                                                   identity=self.ident[:]),
                              reads=[xn, self.ident], writes=[tp], inc=(k == 7))
                    for k in range(8):
                        sc = self.modv[:, 8 + k, wh:wh + 1]
                        sh = self.modv[:, k, wh:wh + 1]
                        o = hT[:, k, ti * 128:(ti + 1) * 128]
                        if k % 2 == 0:
                            fw.op("act", lambda o=o, tp=tp, k=k, sc=sc, sh=sh: nc.scalar.activation(
                                out=o, in_=tp[:, k, :], func=AF.Identity, bias=sh, scale=sc),
                                reads=[tp, self.modv], writes=[hT])
                        else:
                            fw.op("dve", lambda o=o, tp=tp, k=k, sc=sc, sh=sh: nc.vector.tensor_scalar(
                                out=o, in0=tp[:, k, :], scalar1=sc, scalar2=sh, op0=ALU.mult, op1=ALU.add),
                                reads=[tp, self.modv], writes=[hT])
                    pv = pvr.next()
                    for (c0, c1, bank, off) in ((O_AV, O_AV + 256, 0, 0), (O_BV, O_BV + 128, 0, 256), (O_DK, O_DK + 384, 1, 0)):
                        for k in range(8):
                            fw.op("pe", lambda k=k, c0=c0, c1=c1, bank=bank, off=off, pv=pv, hT=hT, ti=ti: nc.tensor.matmul(
                                pv[:, bank, off:off + (c1 - c0)], lhsT=hT[:, k, ti * 128:(ti + 1) * 128], rhs=win[:, k, c0:c1],
                                start=(k == 0), stop=(k == 7)),
                                reads=[hT, win], writes=[pv], inc=(k == 7))
                    vt = vtr.next()
                    fw.op("act", lambda vt=vt, pv=pv: nc.scalar.copy(out=vt[:, 0:384], in_=pv[:, 0, 0:384]), reads=[pv], writes=[vt])
                    fw.op("dve", lambda vt=vt, pv=pv: nc.vector.tensor_copy(out=vt[:, 384:768], in_=pv[:, 1, 0:384]), reads=[pv], writes=[vt])
                    fw.dma("pool", self.scr["VT"][tok:tok + 128, :], vt[:], reads=[vt])
                for jc in range(20):
                    m = min(128, IN_W - jc * 128)
                    pp = ppr.next()
                    for k in range(8):
                        fw.op("pe", lambda k=k, jc=jc, m=m, pp=pp, hT=hT: nc.tensor.matmul(
                            pp[0:m, 0:n], lhsT=win[:, k, jc * 128:jc * 128 + m], rhs=hT[:, k, 0:n],
                            start=(k == 0), stop=(k == 7)),
                            reads=[hT, win], writes=[pp], inc=(k == 7))
                    pt = ptr.next()
                    if ev % 2 == 0:
                        fw.op("act", lambda pt=pt, pp=pp, m=m: nc.scalar.copy(out=pt[0:m, 0:n], in_=pp[0:m, 0:n]), reads=[pp], writes=[pt])
                    else:
                        fw.op("dve", lambda pt=pt, pp=pp, m=m: nc.vector.tensor_copy(out=pt[0:m, 0:n], in_=pp[0:m, 0:n]), reads=[pp], writes=[pt])
                    ev += 1
                    fw.dma("pool", self.scr["PT"][jc * 128:jc * 128 + m, t0:t0 + n], pt[0:m, 0:n], reads=[pt])
            fw.barrier()
        fw.stack = self.gstack


def _stage(fn):
    def wrap(self, *a, **kw):
        from contextlib import ExitStack
        with ExitStack() as st:
            self.fw.stack = st
            fn(self, *a, **kw)
            self.fw.barrier()
        self.fw.stack = self.gstack
    return wrap


class KA(K):
    def lat_chunks(self, n=512):
        return [(t, min(n, self.T - t)) for t in range(0, self.T, n)]

    def attn_finish(self, po, n, dst, rings, sink=None):
        fw, nc = self.fw, self.nc
        rden, pbr, osbr, obr = rings
        rd = rden.next()
        if sink is not None:
            fw.op("dve", lambda: nc.vector.tensor_scalar(out=rd[64:65, 0:n], in0=po[64:65, 0:n], scalar1=sink, scalar2=None,
                                                          op0=ALU.add), reads=[po, self.sinkexp], writes=[rd])
            fw.op("dve", lambda: nc.vector.reciprocal(out=rd[64:65, 0:n], in_=rd[64:65, 0:n]), reads=[rd], writes=[rd])
        else:
            fw.op("dve", lambda: nc.vector.reciprocal(out=rd[64:65, 0:n], in_=po[64:65, 0:n]), reads=[po], writes=[rd])
        pb = pbr.next()
        fw.op("pe", lambda: nc.tensor.matmul(pb[0:64, 0:n], lhsT=self.ones[64:65, 0:64], rhs=rd[64:65, 0:n], start=True, stop=True),
              reads=[rd, self.ones], writes=[pb])
        osb = osbr.next()
        fw.op("act", lambda: nc.scalar.copy(out=osb[0:64, 0:n], in_=po[0:64, 0:n]), reads=[po], writes=[osb])
        ob = obr.next()
        fw.op("dve", lambda: nc.vector.tensor_tensor(out=ob[0:64, 0:n], in0=osb[0:64, 0:n], in1=pb[0:64, 0:n], op=ALU.mult),
              reads=[osb, pb], writes=[ob])
        fw.dma("pool", dst, ob[0:64, 0:n], reads=[ob])

    def finish_rings(self):
        fw = self.fw
        return (Ring([fw.sb([128, 512], F32, "rden") for _ in range(2)]),
                Ring([fw.ps([128, 512], F32, "pb") for _ in range(1)]),
                Ring([fw.sb([64, 512], F32, "osb") for _ in range(2)]),
                Ring([fw.sb([64, 512], BF16, "ob") for _ in range(2)]))

    def rope64(self, src, srcbufs, n, t0, rot, which, rings):
        fw, nc = self.fw, self.nc
        qsr, prr, cosr, sinr, t1r, qfr = rings
        qs = qsr.next()
        fw.op("act", lambda: nc.scalar.copy(out=qs[0:64, 0:n], in_=src), reads=srcbufs, writes=[qs])
        if not rot:
            return qs
        pr = prr.next()
        rt = self.rt_wa if which == "wa" else self.rt_mla
        fw.op("pe", lambda: nc.tensor.matmul(pr[0:64, 0:n], lhsT=rt[:], rhs=qs[0:64, 0:n], start=True, stop=True),
              reads=[qs, rt], writes=[pr])
        cos, sin = cosr.next(), sinr.next()
        fw.dma("sp", cos[0:64, 0:n], self.din["cos_" + which][:, t0:t0 + n], writes=[cos])
        fw.dma("sp", sin[0:64, 0:n], self.din["sin_" + which][:, t0:t0 + n], writes=[sin])
        t1 = t1r.next()
        fw.op("pool", lambda: nc.gpsimd.tensor_tensor(out=t1[0:64, 0:n], in0=qs[0:64, 0:n], in1=cos[0:64, 0:n], op=ALU.mult),
              reads=[qs, cos], writes=[t1])
        t2 = t1r.next()
        fw.op("dve", lambda: nc.vector.tensor_tensor(out=t2[0:64, 0:n], in0=pr[0:64, 0:n], in1=sin[0:64, 0:n], op=ALU.mult),
              reads=[pr, sin], writes=[t2])
        qf = qfr.next()
        fw.op("dve", lambda: nc.vector.tensor_tensor(out=qf[0:64, 0:n], in0=t1[0:64, 0:n], in1=t2[0:64, 0:n], op=ALU.add),
              reads=[t1, t2], writes=[qf])
        return qf

    def rope_rings(self):
        fw = self.fw
        return (Ring([fw.sb([64, 512], BF16, "qs") for _ in range(3)]),
                Ring([fw.ps([64, 512], F32, "pr") for _ in range(2)]),
                Ring([fw.sb([64, 512], F32, "cos") for _ in range(2)]),
                Ring([fw.sb([64, 512], F32, "sin") for _ in range(2)]),
                Ring([fw.sb([64, 512], F32, "t1") for _ in range(4)]),
                Ring([fw.sb([64, 512], BF16, "qf") for _ in range(3)]))

    def load_rt(self):
        fw, nc = self.fw, self.nc
        for nm in ("rt_wa", "rt_mla"):
            t = fw.sb([64, 64], BF16, nm)
            fw.dma("pool", t[:], self.din[nm], writes=[t])
            setattr(self, nm, t)

    @_stage
    def stage_wa_prep(self, l):
        fw, nc = self.fw, self.nc
        T = self.T
        self.load_rt()
        rr = self.rope_rings()
        inr = Ring([fw.sb([64, 512], BF16, "in") for _ in range(3)])
        for (t0, n) in self.chunks(512):
            rot = t0 < T
            for (src_row, dst) in [(O_BQ + 64 * h, self.scr["QTb"][h]) for h in range(4)] + \
                                  [(O_BK + 64 * h, self.scr["KTb"][h]) for h in range(2)]:
                a = inr.next()
                fw.dma("sp", a[0:64, 0:n], self.scr["PT"][src_row:src_row + 64, t0:t0 + n], writes=[a])
                if rot:
                    qf = self.rope64(a[0:64, 0:n], [a], n, t0, True, "wa", rr)
                    fw.dma("pool", dst[:, t0:t0 + n], qf[0:64, 0:n], reads=[qf])
                else:
                    fw.dma("pool", dst[:, t0:t0 + n], a[0:64, 0:n], reads=[a])

    @_stage
    def stage_mla_prep(self, l):
        fw, nc = self.fw, self.nc
        T = self.T
        self.load_rt()
        rr = self.rope_rings()
        onesb = fw.sb([128, 128], BF16, "onesb")
        fw.op("dve", lambda: nc.vector.memset(onesb[:], 1.0), writes=[onesb])
        gq = fw.sb([128, 2], F32, "gq"); gkv = fw.sb([128, 1], F32, "gkv")
        fw.dma("sp", gq[:], self.din["mla_g_q"][l], writes=[gq])
        fw.dma("sp", gkv[:], self.din["mla_g_kv"][l], writes=[gkv])
        wuq = fw.sb([128, 2, 256], BF16, "wuq"); wukv = fw.sb([128, 384], BF16, "wukv")
        fw.dma("pool", wuq[:], self.din["mla_w_uq"][l].rearrange("(k p) n -> p k n", p=128), writes=[wuq])
        fw.dma("pool", wukv[:], self.din["mla_w_ukv"][l], writes=[wukv])
        cqr = Ring([fw.sb([128, 3, 512], BF16, "cq") for _ in range(2)])
        sqr = Ring([fw.sb([128, 3, 512], BF16, "sq") for _ in range(2)])
        rsr = Ring([fw.sb([128, 2, 512], F32, "rs") for _ in range(2)])
        cnr = Ring([fw.sb([128, 3, 512], BF16, "cn") for _ in range(2)])
        krr = Ring([fw.sb([64, 512], BF16, "kr") for _ in range(2)])
        knr = Ring([fw.sb([32, 512], BF16, "kn") for _ in range(3)])
        vcr = Ring([fw.sb([128, 384], BF16, "vc") for _ in range(3)])
        pss = Ring([fw.ps([128, 2, 512], F32, "pss") for _ in range(1)])
        pq = Ring([fw.ps([64, 512], F32, "pq") for _ in range(2)])
        pv = Ring([fw.ps([128, 512], F32, "pvc") for _ in range(1)])
        PT = self.scr["PT"]
        for (t0, n) in self.chunks(512):
            rot = t0 < T
            cq = cqr.next()
            fw.dma("sp", cq[:, 0:2, 0:n], PT[O_CQ:O_CQ + 256, t0:t0 + n].rearrange("(k p) t -> p k t", p=128), writes=[cq])
            fw.dma("sp", cq[:, 2, 0:n], PT[O_CKV:O_CKV + 128, t0:t0 + n], writes=[cq])
            sq = sqr.next()
            fw.op("pool", lambda: nc.gpsimd.tensor_tensor(out=sq[:, :, 0:n], in0=cq[:, :, 0:n], in1=cq[:, :, 0:n], op=ALU.mult),
                  reads=[cq], writes=[sq])
            ps = pss.next()
            for k in range(2):
                fw.op("pe", lambda k=k: nc.tensor.matmul(ps[:, 0, 0:n], lhsT=onesb[:], rhs=sq[:, k, 0:n], start=(k == 0), stop=(k == 1)),
                      reads=[sq, onesb], writes=[ps], inc=(k == 1))
            fw.op("pe", lambda: nc.tensor.matmul(ps[:, 1, 0:n], lhsT=onesb[:], rhs=sq[:, 2, 0:n], start=True, stop=True),
                  reads=[sq, onesb], writes=[ps])
            rs = rsr.next()
            fw.op("act", lambda: nc.scalar.activation(out=rs[:, 0, 0:n], in_=ps[:, 0, 0:n], func=AF.Sqrt, bias=self.epsln[:, 1:2], scale=1.0 / 256),
                  reads=[ps, self.epsln], writes=[rs])
            fw.op("act", lambda: nc.scalar.activation(out=rs[:, 1, 0:n], in_=ps[:, 1, 0:n], func=AF.Sqrt, bias=self.epsln[:, 1:2], scale=1.0 / 128),
                  reads=[ps, self.epsln], writes=[rs])
            fw.op("dve", lambda: nc.vector.reciprocal(out=rs[:, :, 0:n], in_=rs[:, :, 0:n]), reads=[rs], writes=[rs])
            cn = cnr.next()
            for k in range(3):
                g = gq[:, k:k + 1] if k < 2 else gkv[:, 0:1]
                fw.op("dve", lambda k=k, g=g: nc.vector.scalar_tensor_tensor(out=cn[:, k, 0:n], in0=cq[:, k, 0:n], scalar=g,
                                                                             in1=rs[:, (0 if k < 2 else 1), 0:n], op0=ALU.mult, op1=ALU.mult),
                      reads=[cq, rs, gq, gkv], writes=[cn])
            for h in range(4):
                p = pq.next()
                for k in range(2):
                    fw.op("pe", lambda k=k, h=h, p=p: nc.tensor.matmul(p[0:64, 0:n], lhsT=wuq[:, k, h * 64:(h + 1) * 64], rhs=cn[:, k, 0:n],
                                                                       start=(k == 0), stop=(k == 1)),
                          reads=[cn, wuq], writes=[p], inc=(k == 1))
                qf = self.rope64(p[0:64, 0:n], [p], n, t0, rot, "mla", rr)
                fw.dma("pool", self.scr["QTc"][h][:, t0:t0 + n], qf[0:64, 0:n], reads=[qf])
            kr = krr.next()
            if rot:
                fw.op("pool", lambda: nc.gpsimd.memset(kr[0:32, 0:n], 0.0), writes=[kr])
            fw.dma("sp", kr[32:64, 0:n], PT[O_CKR:O_CKR + 32, t0:t0 + n], writes=[kr])
            if rot:
                kf = self.rope64(kr[0:64, 0:n], [kr], n, t0, True, "mla", rr)
            else:
                kf = kr
            for h in range(4):
                fw.dma("pool", self.scr["KTc"][h][32:64, t0:t0 + n], kf[32:64, 0:n], reads=[kf])
            for h in range(4):
                p = pq.next()
                fw.op("pe", lambda h=h, p=p: nc.tensor.matmul(p[0:32, 0:n], lhsT=wukv[:, h * 96:h * 96 + 32], rhs=cn[:, 2, 0:n], start=True, stop=True),
                      reads=[cn, wukv], writes=[p])
                kn = knr.next()
                fw.op("act", lambda p=p, kn=kn: nc.scalar.copy(out=kn[0:32, 0:n], in_=p[0:32, 0:n]), reads=[p], writes=[kn])
                fw.dma("pool", self.scr["KTc"][h][0:32, t0:t0 + n], kn[0:32, 0:n], reads=[kn])
            for ti in range(n // 128):
                p = pv.next()
                fw.op("pe", lambda p=p, ti=ti: nc.tensor.matmul(p[:, 0:384], lhsT=cn[:, 2, ti * 128:(ti + 1) * 128], rhs=wukv[:], start=True, stop=True),
                      reads=[cn, wukv], writes=[p])
                vc = vcr.next()
                fw.op("dve", lambda p=p, vc=vc: nc.vector.tensor_copy(out=vc[:], in_=p[:, 0:384]), reads=[p], writes=[vc])
                tok = t0 + ti * 128
                fw.dma("pool", self.scr["Vc"][tok:tok + 128], vc[:].rearrange("p (h c) -> p h c", h=4)[:, :, 32:96], reads=[vc])

    def exp_block(self, ps, kp, c0, c1, er, mask=None, mring=None):
        fw, nc = self.fw, self.nc
        e = er.next()
        fw.op("act", lambda: nc.scalar.activation(out=e[0:kp, c0:c1], in_=ps[0:kp, c0:c1], func=AF.Exp, scale=0.125),
              reads=[ps], writes=[e])
        if mask is None:
            return e
        mbufs, map_ = mask
        e2 = mring.next()
        fw.op("dve", lambda: nc.vector.tensor_tensor(out=e2[0:kp, c0:c1], in0=e[0:kp, c0:c1], in1=map_, op=ALU.mult),
              reads=[e] + mbufs, writes=[e2])
        return e2

    @_stage
    def stage_mla_attn(self, l, ctx_out):
        fw, nc = self.fw, self.nc
        T, NT = self.T, self.NT
        nkb = NT // 128
        fr = self.finish_rings()
        kT = fw.sb([64, NT], BF16, "kT")
        va = fw.sb([128, nkb, 65], BF16, "va")
        fw.op("dve", lambda: nc.vector.memset(va[:, :, 64:65], 1.0), writes=[va])
        qr = Ring([fw.sb([64, 512], BF16, "q") for _ in range(2)])
        er = Ring([fw.sb([128, 2, 512], BF16, "e") for _ in range(3)])
        psr = Ring([fw.ps([128, 2, 512], F32, "ps") for _ in range(2)])
        por = Ring([fw.ps([128, 512], F32, "po") for _ in range(2)])
        for h in range(4):
            fw.dma("sp", kT[:], self.scr["KTc"][h], writes=[kT])
            fw.dma("sp", va[:, :, 0:64], self.scr["Vc"][:, h, :].rearrange("(b p) d -> p b d", p=128), writes=[va])
            qchunks = [(t0, n, list(range(nkb))) for (t0, n) in self.lat_chunks(512)]
            if ctx_out:
                qchunks.append((T, C, [nkb - 2, nkb - 1]))
            for (t0, n, kbs) in qchunks:
                q = qr.next()
                fw.dma("sp", q[0:64, 0:n], self.scr["QTc"][h][:, t0:t0 + n], writes=[q])
                po = por.next()
                pairs = [(kbs[i], kbs[i + 1]) for i in range(0, len(kbs), 2)]

                def S(pair):
                    ps = psr.next()
                    for j, kb in enumerate(pair):
                        fw.op("pe", lambda j=j, kb=kb: nc.tensor.matmul(ps[:, j, 0:n], lhsT=kT[:, kb * 128:(kb + 1) * 128], rhs=q[0:64, 0:n], start=True, stop=True),
                              reads=[kT, q], writes=[ps], inc=(j == 1))
                    return ps
                nxt = S(pairs[0])
                for i, pair in enumerate(pairs):
                    ps = nxt
                    if i + 1 < len(pairs):
                        nxt = S(pairs[i + 1])
                    e = er.next()
                    fw.op("act", lambda: nc.scalar.activation(out=e[:, :, 0:n], in_=ps[:, :, 0:n], func=AF.Exp, scale=0.125), reads=[ps], writes=[e])
                    for j, kb in enumerate(pair):
                        first = (i == 0 and j == 0)
                        last = (i == len(pairs) - 1 and j == 1)
                        fw.op("pe", lambda j=j, kb=kb, first=first, last=last: nc.tensor.matmul(po[0:65, 0:n], lhsT=va[:, kb, :], rhs=e[:, j, 0:n], start=first, stop=last),
                              reads=[e, va], writes=[po], inc=last)
                self.attn_finish(po, n, self.scr["OT"][512 + h * 64:512 + (h + 1) * 64, t0:t0 + n], fr)

    @_stage
    def stage_wa_attn(self, l, ctx_out):
        fw, nc = self.fw, self.nc
        T, NT = self.T, self.NT
        nb = T // 128
        fr = self.finish_rings()
        self.sinkexp = fw.sb([128, 4], F32, "sinkexp")
        fw.dma("sp", self.sinkexp[:], self.din["wa_sink"][l].partition_broadcast(128), writes=[self.sinkexp])
        fw.op("act", lambda: nc.scalar.activation(out=self.sinkexp[:], in_=self.sinkexp[:], func=AF.Exp), reads=[self.sinkexp], writes=[self.sinkexp])
        mask = fw.sb([128, 6, 512], BF16, "mask")
        fw.dma("pool", mask[:], self.din["maskwa"].rearrange("r p q -> p r q"), writes=[mask])
        kT = fw.sb([64, NT], BF16, "kT")
        va = fw.sb([128, NT // 128, 65], BF16, "va")
        fw.op("dve", lambda: nc.vector.memset(va[:, :, 64:65], 1.0), writes=[va])
        qr = Ring([fw.sb([64, 512], BF16, "q") for _ in range(2)])
        er = Ring([fw.sb([128, 512], BF16, "e") for _ in range(3)])
        mr = Ring([fw.sb([128, 512], BF16, "em") for _ in range(3)])
        psr = Ring([fw.ps([128, 512], F32, "ps") for _ in range(3)])
        por = Ring([fw.ps([128, 512], F32, "po") for _ in range(2)])
        for g in range(2):
            fw.dma("sp", kT[:], self.scr["KTb"][g], writes=[kT])
            fw.dma("sp", va[:, :, 0:64], self.scr["VT"][:, 256 + g * 64:256 + (g + 1) * 64].rearrange("(b p) d -> p b d", p=128), writes=[va])
            for h in (2 * g, 2 * g + 1):
                qchunks = [(t0, n, True) for (t0, n) in self.lat_chunks(512)]
                if ctx_out:
                    qchunks.append((T, C, False))
                for (t0, n, lat) in qchunks:
                    q = qr.next()
                    fw.dma("sp", q[0:64, 0:n], self.scr["QTb"][h][:, t0:t0 + n], writes=[q])
                    po = por.next()
                    items = [(nb, 0, n, None)]
                    if lat:
                        i0 = t0 // 128
                        nqb = n // 128
                        for r in range(-1, nqb + 1):
                            j = i0 + r
                            if j < 0 or j >= nb:
                                continue
                            qlo, qhi = max(0, r - 1), min(nqb, r + 2)
                            items.append((j, qlo * 128, qhi * 128, r + 1))
                    items.append((nb + 1, 0, n, None))
                    def S(it):
                        kb, c0, c1, mi = it
                        ps = psr.next()
                        fw.op("pe", lambda: nc.tensor.matmul(ps[:, c0:c1], lhsT=kT[:, kb * 128:(kb + 1) * 128], rhs=q[0:64, c0:c1], start=True, stop=True),
                              reads=[kT, q], writes=[ps])
                        return ps
                    nxt = S(items[0])
                    for i, (kb, c0, c1, mi) in enumerate(items):
                        ps = nxt
                        if i + 1 < len(items):
                            nxt = S(items[i + 1])
                        e = self.exp_block(ps, 128, c0, c1, er, mask=None if mi is None else ([mask], mask[:, mi, c0:c1]), mring=mr)
                        fw.op("pe", lambda e=e, kb=kb, c0=c0, c1=c1, i=i: nc.tensor.matmul(po[0:65, c0:c1], lhsT=va[:, kb, :], rhs=e[:, c0:c1], start=(i == 0), stop=(i == len(items) - 1)),
                              reads=[e, va], writes=[po], inc=(i == len(items) - 1))
                    self.attn_finish(po, n, self.scr["OT"][256 + h * 64:256 + (h + 1) * 64, t0:t0 + n], fr, sink=self.sinkexp[64:65, h:h + 1])

    @_stage
    def stage_na_attn(self, l, ctx_out):
        fw, nc = self.fw, self.nc
        T, NT = self.T, self.NT
        R = T // 64
        fr = self.finish_rings()
        oh = fw.sb([31, 64, 64], F32, "oh")
        fw.dma("sp", oh[:], self.din["na_oh"], writes=[oh])
        cmask = fw.sb([64, 960], F32, "cmask")
        fw.dma("sp", cmask[:], self.din["na_cmask"].rearrange("k m c -> k (m c)"), writes=[cmask])
        j15 = fw.sb([15, 15], F32, "j15")
        fw.dma("sp", j15[:], self.din["j15"], writes=[j15])
        rpb = fw.sb([15, 4, 31], F32, "rpb")
        fw.dma("sp", rpb[:], self.din["na_rpb"][l].rearrange("h r j -> r h j"), writes=[rpb])
        rrr = Ring([fw.sb([31, 15], F32, "rr") for _ in range(2)])
        etz = [fw.sb([64, 960], BF16, "etz") for _ in range(4)]
        etmp = fw.sb([64, 960], F32, "etmp")
        prr = fw.ps([31, 15], F32, "prr")
        pz = fw.ps([64, 2, 512], F32, "pz")
        for h in range(4):
            fw.op("pe", lambda h=h: nc.tensor.matmul(prr[:, :], lhsT=rpb[:, h, :], rhs=j15[:], start=True, stop=True), reads=[rpb, j15], writes=[prr])
            rr = rrr.next()
            fw.op("dve", lambda rr=rr: nc.vector.tensor_copy(out=rr[:], in_=prr[:]), reads=[prr], writes=[rr])
            for c_ in range(64):
                for (bk, m0, m1) in ((0, 0, 8), (1, 8, 15)):
                    fw.op("pe", lambda c_=c_, bk=bk, m0=m0, m1=m1, rr=rr: nc.tensor.matmul(
                        pz[:, bk, :].rearrange("k (m c) -> k m c", c=64)[:, 0:m1 - m0, c_], lhsT=oh[:, c_, :], rhs=rr[:, m0:m1], start=True, stop=True),
                        reads=[oh, rr], writes=[pz], inc=(c_ == 63 and bk == 1))
            fw.op("act", lambda: nc.scalar.activation(out=etmp[:, 0:512], in_=pz[:, 0, :], func=AF.Exp), reads=[pz], writes=[etmp])
            fw.op("act", lambda: nc.scalar.activation(out=etmp[:, 512:960], in_=pz[:, 1, 0:448], func=AF.Exp), reads=[pz], writes=[etmp])
            fw.op("dve", lambda h=h: nc.vector.tensor_tensor(out=etz[h][:], in0=etmp[:], in1=cmask[:], op=ALU.mult), reads=[etmp, cmask], writes=[etz[h]])
        kTr = Ring([fw.sb([64, 1024], BF16, "kT") for _ in range(2)])
        kcr = Ring([fw.sb([64, 256], BF16, "kc") for _ in range(2)])
        var = Ring([fw.sb([64, 16, 65], BF16, "va") for _ in range(2)])
        vcr = Ring([fw.sb([128, 2, 65], BF16, "vca") for _ in range(2)])
        for b in var.bufs:
            fw.op("dve", lambda b=b: nc.vector.memset(b[:, :, 64:65], 1.0), writes=[b])
        for b in vcr.bufs:
            fw.op("dve", lambda b=b: nc.vector.memset(b[:, :, 64:65], 1.0), writes=[b])
        qr = Ring([fw.sb([64, 512], BF16, "q") for _ in range(2)])
        er = Ring([fw.sb([128, 512], BF16, "e") for _ in range(3)])
        mr = Ring([fw.sb([128, 512], BF16, "em") for _ in range(3)])
        psr = Ring([fw.ps([128, 512], F32, "ps") for _ in range(2)])
        por = Ring([fw.ps([128, 512], F32, "po") for _ in range(2)])
        PT, VT = self.scr["PT"], self.scr["VT"]
        for h in range(4):
            kc = kcr.next()
            fw.dma("sp", kc[:], PT[O_AK + h * 64:O_AK + (h + 1) * 64, T:T + C], writes=[kc])
            vc = vcr.next()
            fw.dma("sp", vc[:, :, 0:64], VT[T:T + C, h * 64:(h + 1) * 64].rearrange("(b p) d -> p b d", p=128), writes=[vc])
            qchunks = [(t0, n, True) for (t0, n) in self.lat_chunks(512)]
            if ctx_out:
                qchunks.append((T, C, False))
            for (t0, n, lat) in qchunks:
                q = qr.next()
                fw.dma("sp", q[0:64, 0:n], PT[O_AQ + h * 64:O_AQ + (h + 1) * 64, t0:t0 + n], writes=[q])
                po = por.next()
                items = [("c", 0, 0, n, None)]
                if lat:
                    r0 = t0 // 64
                    nqr = n // 64
                    klo, khi = max(0, r0 - 4), min(R - 1, r0 + nqr - 1 + 3 + 0)
                    rs_all = [min(max(r0 + rq - 4, 0), R - 8) for rq in range(nqr)]
                    klo, khi = min(rs_all), max(rs_all) + 7
                    kT = kTr.next()
                    nk = khi - klo + 1
                    fw.dma("sp", kT[:, 0:nk * 64], PT[O_AK + h * 64:O_AK + (h + 1) * 64, klo * 64:(khi + 1) * 64], writes=[kT])
                    va = var.next()
                    fw.dma("sp", va[:, 0:nk, 0:64], VT[klo * 64:(khi + 1) * 64, h * 64:(h + 1) * 64].rearrange("(r p) d -> p r d", p=64), writes=[va])
                    for kr in range(klo, khi + 1):
                        val = [rq for rq in range(nqr) if rs_all[rq] <= kr <= rs_all[rq] + 7]
                        if not val:
                            continue
                        lo, hi = val[0], val[-1] + 1
                        assert val == list(range(lo, hi))
                        m0 = lo + 7 - kr + r0
                        assert 0 <= m0 and m0 + (hi - lo) <= 15
                        items.append(("k", kr - klo, lo * 64, hi * 64, m0))
                items.append(("c", 1, 0, n, None))
                def S(it):
                    kind, idx, c0, c1, m0 = it
                    ps = psr.next()
                    if kind == "c":
                        fw.op("pe", lambda: nc.tensor.matmul(ps[:, c0:c1], lhsT=kc[:, idx * 128:(idx + 1) * 128], rhs=q[0:64, c0:c1], start=True, stop=True),
                              reads=[kc, q], writes=[ps])
                    else:
                        fw.op("pe", lambda: nc.tensor.matmul(ps[0:64, c0:c1], lhsT=kT[:, idx * 64:(idx + 1) * 64], rhs=q[0:64, c0:c1], start=True, stop=True),
                              reads=[kT, q], writes=[ps])
                    return ps
                nxt = S(items[0])
                for i, (kind, idx, c0, c1, m0) in enumerate(items):
                    ps = nxt
                    if i + 1 < len(items):
                        nxt = S(items[i + 1])
                    first, last = i == 0, i == len(items) - 1
                    if kind == "c":
                        e = self.exp_block(ps, 128, c0, c1, er)
                        fw.op("pe", lambda e=e, idx=idx, first=first, last=last: nc.tensor.matmul(po[0:65, c0:c1], lhsT=vc[:, idx, :], rhs=e[:, c0:c1], start=first, stop=last),
                              reads=[e, vc], writes=[po], inc=last)
                    else:
                        nrow = (c1 - c0) // 64
                        e = self.exp_block(ps, 64, c0, c1, er, mask=([etz[h]], etz[h][:, m0 * 64:(m0 + nrow) * 64]), mring=mr)
                        fw.op("pe", lambda e=e, idx=idx, c0=c0, c1=c1: nc.tensor.matmul(po[0:65, c0:c1], lhsT=va[:, idx, :], rhs=e[0:64, c0:c1], start=False, stop=False),
                              reads=[e, va], writes=[po], inc=False)
                self.attn_finish(po, n, self.scr["OT"][h * 64:(h + 1) * 64, t0:t0 + n], fr)

    @_stage
    def stage_gla(self, l):
        fw, nc = self.fw, self.nc
        T, NT = self.T, self.NT
        PT, VT, OT, OF = self.scr["PT"], self.scr["VT"], self.scr["OT"], self.scr["OF"]
        tri = fw.sb([128, 4, 128], F32, "tri")
        fw.dma("sp", tri[:], self.din["gla_tri"].rearrange("g a b -> a g b"), writes=[tri])
        maskg = fw.sb([128, 2, 512], BF16, "maskg")
        fw.dma("pool", maskg[:], self.din["gla_mask"].rearrange("d j h i -> j d (h i)"), writes=[maskg])
        wg = fw.sb([32, 2, 128], BF16, "wg")
        for d, (wn, bn) in enumerate((("gla_w_gf", "gla_b_gf"), ("gla_w_gb", "gla_b_gb"))):
            fw.dma("pool", wg[0:16, d, :], self.din[wn][l], writes=[wg])
            fw.dma("pool", wg[16:17, d, :], self.din[bn][l:l + 1, :], writes=[wg])
        gn = fw.sb([64, 1], F32, "gn")
        fw.dma("sp", gn[:], self.din["gla_g_norm"][l], writes=[gn])
        onesb = fw.sb([64, 64], BF16, "onesb")
        fw.op("dve", lambda: nc.vector.memset(onesb[:], 1.0), writes=[onesb])
        zr = Ring([fw.sb([32, 128], BF16, "zaug") for _ in range(3)])
        for b in zr.bufs:
            fw.op("dve", lambda b=b: nc.vector.memset(b[:], 1.0), writes=[b])
        qkr = Ring([fw.sb([32, 2, 4, 128], BF16, "qk") for _ in range(3)])
        ktr = Ring([fw.sb([128, 128], BF16, "ktok") for _ in range(3)])
        vtr = Ring([fw.sb([128, 4, 64], BF16, "vtok") for _ in range(3)])
        exr = Ring([fw.sb([128, 128], F32, "ex") for _ in range(2)])
        Lr = Ring([fw.sb([128, 128], F32, "L") for _ in range(2)])
        ebr = Ring([fw.sb([32, 2, 512], F32, "eb") for _ in range(2)])
        esr = Ring([fw.sb([128, 128], F32, "es") for _ in range(2)])
        qdr = Ring([fw.sb([32, 2, 512], BF16, "qd") for _ in range(2)])
        ker = Ring([fw.sb([128, 2, 128], BF16, "kend") for _ in range(2)])
        cm = fw.sb([128, 2], F32, "cm")
        fw.op("dve", lambda: nc.vector.memset(cm[:], 0.0), writes=[cm])
        fw.op("dve", lambda: nc.vector.memset(cm[0:64, 0:1], 1.0), writes=[cm])
        fw.op("dve", lambda: nc.vector.memset(cm[64:128, 1:2], 1.0), writes=[cm])
        amr = Ring([fw.sb([128, 512], BF16, "am") for _ in range(2)])
        Sr = Ring([fw.sb([32, 4, 64], F32, "S") for _ in range(4)])
        Sbr = Ring([fw.sb([32, 4, 64], BF16, "Sb") for _ in range(4)])
        ofr = Ring([fw.sb([64, 512], F32, "of") for _ in range(2)])
        osr = Ring([fw.sb([64, 512], F32, "os") for _ in range(2)])
        sqr = Ring([fw.sb([64, 512], BF16, "sq") for _ in range(2)])
        rsr = Ring([fw.sb([64, 512], F32, "rs") for _ in range(2)])
        rtr = Ring([fw.sb([64, 512], BF16, "rT") for _ in range(2)])
        srr = Ring([fw.sb([64, 512], F32, "sr") for _ in range(2)])
        fnr = Ring([fw.sb([64, 512], BF16, "fin") for _ in range(2)])
        pz = fw.ps([128, 128], F32, "pz")
        pbT = fw.ps([32, 512], F32, "pbT")
        pbs = fw.ps([128, 128], F32, "pbs")
        pa = fw.ps([128, 512], F32, "pa")
        pu = fw.ps([32, 2, 4, 64], F32, "pu")
        po = fw.ps([64, 512], F32, "po")
        pss = fw.ps([64, 512], F32, "pss")
        nl, nt = T // 128, NT // 128
        orders = [[nt - 2, nt - 1] + list(range(nl)), [nt - 1, nt - 2] + list(range(nl - 1, -1, -1))]
        for d in (0, 1):
            if d == 1:
                fw.barrier()
            zrow = O_ZF if d == 0 else O_ZB
            S = Sr.next()
            fw.op("dve", lambda: nc.vector.memset(S[:], 0.0), writes=[S])
            for tile in orders[d]:
                tok = tile * 128
                za = zr.next()
                fw.dma("sp", za[0:16, :], PT[zrow:zrow + 16, tok:tok + 128], writes=[za])
                qk = qkr.next()
                fw.dma("sp", qk[:, 0, :, :], PT[O_DQ:O_DQ + 128, tok:tok + 128].rearrange("(h d) t -> d h t", d=32), writes=[qk])
                fw.dma("sp", qk[:, 1, :, :], PT[O_DK:O_DK + 128, tok:tok + 128].rearrange("(h d) t -> d h t", d=32), writes=[qk])
                ktok = ktr.next()
                fw.dma("sp", ktok[:], VT[tok:tok + 128, 384:512], writes=[ktok])
                vtok = vtr.next()
                fw.dma("sp", vtok[:], VT[tok:tok + 128, 512:768].rearrange("p (h d) -> p h d", h=4), writes=[vtok])
                if getattr(self, 'gla_cut', 99) <= 1:
                    continue
                fw.op("pe", lambda: nc.tensor.matmul(pz[:, :], lhsT=za[0:17, :], rhs=wg[0:17, d, :], start=True, stop=True), reads=[za, wg], writes=[pz])
                ex = exr.next()
                fw.op("act", lambda: nc.scalar.activation(out=ex[:], in_=pz[:], func=AF.Exp, scale=-1.0), reads=[pz], writes=[ex])
                L = Lr.next()
                fw.op("act", lambda: nc.scalar.activation(out=L[:], in_=ex[:], func=AF.Ln, bias=self.ones[:, 0:1], scale=1.0), reads=[ex, self.ones], writes=[L])
                if getattr(self, 'gla_cut', 99) <= 2:
                    continue
                for h in range(4):
                    fw.op("pe", lambda h=h: nc.tensor.matmul(pbT[0:32, h * 128:(h + 1) * 128], lhsT=L[:, h * 32:(h + 1) * 32], rhs=tri[:, d, :], start=True, stop=True),
                          reads=[L, tri], writes=[pbT], inc=(h == 3))
                fw.op("pe", lambda: nc.tensor.matmul(pbs[:, :], lhsT=tri[:, 2 + d, :], rhs=L[:], start=True, stop=True), reads=[L, tri], writes=[pbs])
                eb = ebr.next()
                fw.op("act", lambda: nc.scalar.activation(out=eb[:, 0, :], in_=pbT[0:32, :], func=AF.Exp), reads=[pbT], writes=[eb])
                fw.op("act", lambda: nc.scalar.activation(out=eb[:, 1, :], in_=pbT[0:32, :], func=AF.Exp, scale=-1.0), reads=[pbT], writes=[eb])
                es = esr.next()
                fw.op("act", lambda: nc.scalar.activation(out=es[:], in_=pbs[:], func=AF.Exp), reads=[pbs], writes=[es])
                if getattr(self, 'gla_cut', 99) <= 3:
                    continue
                qd = qdr.next()
                fw.op("dve", lambda: nc.vector.scalar_tensor_tensor(out=qd[:, 0, :], in0=qk[:, 0, :, :].rearrange("p h t -> p (h t)"), scalar=32 ** -0.5,
                                                                     in1=eb[:, 0, :], op0=ALU.mult, op1=ALU.mult), reads=[qk, eb], writes=[qd])
                fw.op("dve", lambda: nc.vector.tensor_tensor(out=qd[:, 1, :], in0=qk[:, 1, :, :].rearrange("p h t -> p (h t)"), in1=eb[:, 1, :], op=ALU.mult),
                      reads=[qk, eb], writes=[qd])
                kend = ker.next()
                for cc in range(2):
                    fw.op("dve", lambda cc=cc: nc.vector.scalar_tensor_tensor(out=kend[:, cc, :], in0=ktok[:], scalar=cm[:, cc:cc + 1], in1=es[:],
                                                                               op0=ALU.mult, op1=ALU.mult), reads=[ktok, es, cm], writes=[kend])
                if getattr(self, 'gla_cut', 99) <= 4:
                    continue
                for h in range(4):
                    fw.op("pe", lambda h=h: nc.tensor.matmul(pa[:, h * 128:(h + 1) * 128], lhsT=qd[:, 1, h * 128:(h + 1) * 128], rhs=qd[:, 0, h * 128:(h + 1) * 128], start=True, stop=True),
                          reads=[qd], writes=[pa], inc=(h == 3))
                am = amr.next()
                fw.op("dve", lambda: nc.vector.tensor_tensor(out=am[:], in0=pa[:], in1=maskg[:, d, :], op=ALU.mult), reads=[pa, maskg], writes=[am])
                if getattr(self, 'gla_cut', 99) <= 5:
                    continue
                for cc in range(2):
                    for h in range(4):
                        fw.op("pe", lambda cc=cc, h=h: nc.tensor.matmul(pu[0:32, cc, h, :], lhsT=kend[:, cc, h * 32:(h + 1) * 32],
                                                                         rhs=vtok[:, h, :], start=True, stop=True),
                              reads=[kend, vtok], writes=[pu], inc=(cc == 1 and h == 3))
                if getattr(self, 'gla_cut', 99) <= 6:
                    continue
                Sb = {}
                for cc in ((0, 1) if d == 0 else (1, 0)):
                    sb_ = Sbr.next()
                    fw.op("act", lambda sb_=sb_, S=S: nc.scalar.copy(out=sb_[:], in_=S[:]), reads=[S], writes=[sb_])
                    Sb[cc] = sb_
                    S2 = Sr.next()
                    tidx = cc * 64 + (63 if d == 0 else 0)
                    for h in range(4):
                        fw.op("dve", lambda h=h, S=S, S2=S2, cc=cc, tidx=tidx: nc.vector.scalar_tensor_tensor(
                            out=S2[:, h, :], in0=S[:, h, :], scalar=eb[:, 0, h * 128 + tidx:h * 128 + tidx + 1], in1=pu[0:32, cc, h, :],
                            op0=ALU.mult, op1=ALU.add), reads=[S, eb, pu], writes=[S2])
                    S = S2
                if getattr(self, 'gla_cut', 99) <= 7:
                    continue
                for h in range(4):
                    fw.op("pe", lambda h=h: nc.tensor.matmul(po[0:64, h * 128:(h + 1) * 128], lhsT=vtok[:, h, :], rhs=am[:, h * 128:(h + 1) * 128], start=True, stop=False),
                          reads=[vtok, am], writes=[po], inc=False)
                    for cc in range(2):
                        fw.op("pe", lambda h=h, cc=cc: nc.tensor.matmul(po[0:64, h * 128 + cc * 64:h * 128 + (cc + 1) * 64], lhsT=Sb[cc][:, h, :],
                                                                         rhs=qd[:, 0, h * 128 + cc * 64:h * 128 + (cc + 1) * 64], start=False, stop=(cc == 1)),
                              reads=[Sb[cc], qd], writes=[po], inc=(cc == 1))
                if getattr(self, 'gla_cut', 99) <= 8:
                    continue
                if d == 0:
                    of = ofr.next()
                    fw.op("act", lambda: nc.scalar.copy(out=of[:], in_=po[:]), reads=[po], writes=[of])
                    fw.dma("pool", OF[:, :, tok:tok + 128], of[:].rearrange("p (h t) -> p h t", h=4), reads=[of])
                else:
                    of = ofr.next()
                    fw.dma("sp", of[:].rearrange("p (h t) -> p h t", h=4), OF[:, :, tok:tok + 128], writes=[of])
                    rT = rtr.next()
                    fw.dma("sp", rT[:].rearrange("p (h t) -> p h t", h=4), PT[O_DR:O_DR + 256, tok:tok + 128].rearrange("(h d) t -> d h t", d=64), writes=[rT])
                    os_ = osr.next()
                    fw.op("dve", lambda: nc.vector.tensor_tensor(out=os_[:], in0=po[:], in1=of[:], op=ALU.add), reads=[po, of], writes=[os_])
                    sq = sqr.next()
                    fw.op("pool", lambda: nc.gpsimd.tensor_tensor(out=sq[:], in0=os_[:], in1=os_[:], op=ALU.mult), reads=[os_], writes=[sq])
                    fw.op("pe", lambda: nc.tensor.matmul(pss[:, :], lhsT=onesb[:], rhs=sq[:], start=True, stop=True), reads=[sq, onesb], writes=[pss])
                    rs = rsr.next()
                    fw.op("act", lambda: nc.scalar.activation(out=rs[:], in_=pss[:], func=AF.Sqrt, bias=self.epsln[0:64, 1:2], scale=1.0 / 64),
                          reads=[pss, self.epsln], writes=[rs])
                    fw.op("dve", lambda: nc.vector.reciprocal(out=rs[:], in_=rs[:]), reads=[rs], writes=[rs])
                    sr = srr.next()
                    fw.op("act", lambda: nc.scalar.activation(out=sr[:], in_=rT[:], func=AF.Silu), reads=[rT], writes=[sr])
                    fw.op("dve", lambda: nc.vector.scalar_tensor_tensor(out=os_[:], in0=os_[:], scalar=gn[:, 0:1], in1=rs[:], op0=ALU.mult, op1=ALU.mult),
                          reads=[os_, gn, rs], writes=[os_])
                    fin = fnr.next()
                    fw.op("dve", lambda: nc.vector.tensor_tensor(out=fin[:], in0=os_[:], in1=sr[:], op=ALU.mult), reads=[os_, sr], writes=[fin])
                    fw.dma("pool", OT[768:1024, tok:tok + 128].rearrange("(h d) t -> d h t", d=64), fin[:].rearrange("p (h t) -> p h t", h=4), reads=[fin])


class KB(KA):
    def bcast_mod(self, j0, dst_lat, dst_ctx, pg, repr_):
        fw, nc = self.fw, self.nc
        for wh, dst in ((0, dst_lat), (1, dst_ctx)):
            for jj in range(8):
                rep = repr_.next()
                fw.op("act", lambda rep=rep, jj=jj, wh=wh: nc.scalar.activation(out=rep[:], in_=self.ones[:], func=AF.Copy,
                                                                                 scale=self.modv[:, j0 + jj, wh:wh + 1]),
                      reads=[self.ones, self.modv], writes=[rep])
                fw.op("pe", lambda rep=rep, jj=jj: nc.tensor.matmul(pg[:, jj // 4, (jj % 4) * 128:(jj % 4 + 1) * 128], lhsT=rep[:], rhs=self.ident[:],
                                                                    start=True, stop=True), reads=[rep, self.ident], writes=[pg])
            fw.op("dve", lambda dst=dst: nc.vector.tensor_copy(out=dst[:].rearrange("p (a b) -> p a b", a=2), in_=pg[:]), reads=[pg], writes=[dst])

    def load_ln(self, l, which):
        fw = self.fw
        g = fw.sb([128, D], F32, "lng"); b = fw.sb([128, D], F32, "lnb")
        fw.dma("sp", g[:], self.din[which + "_g"][l].partition_broadcast(128), writes=[g])
        fw.dma("sp", b[:], self.din[which + "_b"][l].partition_broadcast(128), writes=[b])
        return g, b

    def ln_stats(self, z, s):
        fw, nc = self.fw, self.nc
        for hh in range(2):
            fw.op("dve", lambda hh=hh: nc.vector.bn_stats(out=s[:, hh * 6:hh * 6 + 6], in_=z[:, hh * 512:(hh + 1) * 512]), reads=[z], writes=[s])
        fw.op("dve", lambda: nc.vector.bn_aggr(out=s[:, 12:14], in_=s[:, 0:12]), reads=[s], writes=[s])
        fw.op("act", lambda: nc.scalar.activation(out=s[:, 15:16], in_=s[:, 13:14], func=AF.Sqrt, bias=self.epsln[:, 0:1], scale=1.0),
              reads=[s, self.epsln], writes=[s])
        fw.op("dve", lambda: nc.vector.reciprocal(out=s[:, 14:15], in_=s[:, 15:16]), reads=[s], writes=[s])

    def post_norm_store(self, z, s, g, b, dst, eng="pool"):
        fw, nc = self.fw, self.nc
        self.ln_stats(z, s)
        fw.op("dve", lambda: nc.vector.tensor_scalar(out=z[:], in0=z[:], scalar1=s[:, 12:13], scalar2=s[:, 14:15], op0=ALU.subtract, op1=ALU.mult),
              reads=[z, s], writes=[z])
        E = nc.gpsimd if eng == "pool" else nc.vector
        fw.op(eng, lambda: E.tensor_tensor(out=z[:], in0=z[:], in1=g[:], op=ALU.mult), reads=[z, g], writes=[z])
        fw.op(eng, lambda: E.tensor_tensor(out=z[:], in0=z[:], in1=b[:], op=ALU.add), reads=[z, b], writes=[z])
        fw.dma("pool", dst, z[:], reads=[z])

    def xsrc(self, l, tok, first):
        if l == 0 and first:
            return self.din["x"][tok:tok + 128, :] if tok < self.T else self.din["ctx"][tok - self.T:tok - self.T + 128, :]
        return self.scr["XR"][tok:tok + 128, :]

    @_stage
    def stage_outproj(self, l, last):
        fw, nc = self.fw, self.nc
        T, NT = self.T, self.NT
        g_lat = fw.sb([128, D], F32, "glat"); g_ctx = fw.sb([128, D], F32, "gctx")
        pg = fw.ps([128, 2, 512], F32, "pg")
        repr_ = Ring([fw.sb([128, 128], F32, "rep") for _ in range(2)])
        self.bcast_mod(16, g_lat, g_ctx, pg, repr_)
        lg, lb = self.load_ln(l, "ln1")
        wout = fw.sb([128, 8, D], BF16, "wout")
        wsrc = self.din["w_out"][l].rearrange("(k p) n -> p k n", p=128)
        for k in range(8):
            fw.dma("pool", wout[:, k, :], wsrc[:, k, :], writes=[wout])
        otr = Ring([fw.sb([128, 8, 128], BF16, "oT") for _ in range(3)])
        xr = Ring([fw.sb([128, D], F32, "x") for _ in range(3)])
        zr = Ring([fw.sb([128, D], F32, "z") for _ in range(3)])
        sr = Ring([fw.sb([128, 16], F32, "st") for _ in range(3)])
        pyr = Ring([fw.ps([128, 2, 512], F32, "py") for _ in range(2)])
        ntile = (T if last else NT) // 128
        for ti in range(ntile):
            tok = ti * 128
            oT = otr.next()
            fw.dma("sp", oT[:], self.scr["OT"][:, tok:tok + 128].rearrange("(k p) t -> p k t", p=128), writes=[oT])
            x = xr.next()
            fw.dma("sp", x[:], self.xsrc(l, tok, True), writes=[x])
            py = pyr.next()
            for half in range(2):
                for k in range(8):
                    fw.op("pe", lambda half=half, k=k: nc.tensor.matmul(py[:, half, :], lhsT=oT[:, k, :], rhs=wout[:, k, half * 512:(half + 1) * 512],
                                                                         start=(k == 0), stop=(k == 7)), reads=[oT, wout], writes=[py], inc=(k == 7))
            gb = g_lat if tok < T else g_ctx
            z = zr.next()
            fw.op("dve", lambda: nc.vector.tensor_tensor(out=z[:].rearrange("p (a b) -> p a b", a=2), in0=py[:], in1=gb[:].rearrange("p (a b) -> p a b", a=2), op=ALU.mult),
                  reads=[py, gb], writes=[z])
            fw.op("dve", lambda: nc.vector.scalar_tensor_tensor(out=z[:], in0=x[:], scalar=ALPHA, in1=z[:], op0=ALU.mult, op1=ALU.add),
                  reads=[x, z], writes=[z])
            self.post_norm_store(z, sr.next(), lg, lb, self.scr["XR"][tok:tok + 128, :])

    @_stage
    def stage_router(self, l, last):
        fw, nc = self.fw, self.nc
        T, NT = self.T, self.NT
        rw = fw.sb([128, 8, NE], F32, "rw")
        fw.dma("sp", rw[:], self.din["router_w"][l].rearrange("(k p) e -> p k e", p=128), writes=[rw])
        rb = fw.sb([128, NE], F32, "rb")
        fw.dma("sp", rb[:], self.din["router_bias"][l].partition_broadcast(128), writes=[rb])
        xr = Ring([fw.sb([128, D], F32, "x") for _ in range(3)])
        xnr = Ring([fw.sb([128, D], F32, "xn") for _ in range(2)])
        sr = Ring([fw.sb([128, 16], F32, "st") for _ in range(3)])
        hbr = Ring([fw.sb([128, 8, 128], BF16, "hb") for _ in range(3)])
        hfr = Ring([fw.sb([128, 8, 128], F32, "hf") for _ in range(2)])
        scr_ = Ring([fw.sb([128, NE], F32, "sc") for _ in range(2)])
        bir = Ring([fw.sb([128, NE], F32, "bi") for _ in range(2)])
        m8r = Ring([fw.sb([128, 16], F32, "m8") for _ in range(2)])
        gtr = Ring([fw.sb([128, NE + 1], F32, "gt") for _ in range(3)])
        tpr = Ring([fw.ps([128, 8, 128], F32, "tp") for _ in range(1)])
        plr = Ring([fw.ps([128, NE], F32, "pl") for _ in range(1)])
        sc_lat = fw.sb([128, D], F32, "sclat"); sc_ctx = fw.sb([128, D], F32, "scctx")
        sh_lat = fw.sb([128, D], F32, "shlat"); sh_ctx = fw.sb([128, D], F32, "shctx")
        pg = fw.ps([128, 2, 512], F32, "pg")
        repr_ = Ring([fw.sb([128, 128], F32, "rep") for _ in range(2)])
        self.bcast_mod(32, sc_lat, sc_ctx, pg, repr_)
        self.bcast_mod(24, sh_lat, sh_ctx, pg, repr_)
        slb = fw.sb([128, 128], BF16, "slb"); onesb = fw.sb([128, 128], BF16, "onesb")
        fw.dma("pool", slb[:], self.din["moe_sl"], writes=[slb])
        fw.op("dve", lambda: nc.vector.memset(onesb[:], 1.0), writes=[onesb])
        fw.op("dve", lambda: nc.vector.memset(self.moe_run[:], 0.0), writes=[self.moe_run])
        xbr = Ring([fw.sb([128, D], BF16, "xb") for _ in range(2)])
        xtr = Ring([fw.sb([128, D], F32, "xt") for _ in range(2)])
        mkr = Ring([fw.sb([128, NE], BF16, "mk") for _ in range(2)])
        posr = Ring([fw.sb([128, NE], F32, "pos") for _ in range(2)])
        ppr = Ring([fw.ps([128, 2, NE], F32, "ppos") for _ in range(1)])
        ntile = (T if last else NT) // 128
        for ti in range(ntile):
            tok = ti * 128
            wh = 0 if tok < T else 1
            x = xr.next()
            fw.dma("sp", x[:], self.scr["XR"][tok:tok + 128, :], writes=[x])
            s = sr.next()
            self.ln_stats(x, s)
            xn = xnr.next()
            fw.op("dve", lambda: nc.vector.tensor_scalar(out=xn[:], in0=x[:], scalar1=s[:, 12:13], scalar2=s[:, 14:15], op0=ALU.subtract, op1=ALU.mult),
                  reads=[x, s], writes=[xn])
            tp = tpr.next()
            for k in range(8):
                fw.op("pe", lambda k=k: nc.tensor.transpose(out=tp[:, k, :], in_=xn[:, k * 128:(k + 1) * 128], identity=self.ident[:]),
                      reads=[xn, self.ident], writes=[tp], inc=(k == 7))
            hb, hf = hbr.next(), hfr.next()
            for k in range(8):
                sc_ = self.modv[:, 32 + k, wh:wh + 1]
                sh_ = self.modv[:, 24 + k, wh:wh + 1]
                fw.op("act", lambda k=k, sc_=sc_, sh_=sh_: nc.scalar.activation(out=hf[:, k, :], in_=tp[:, k, :], func=AF.Identity, bias=sh_, scale=sc_),
                      reads=[tp, self.modv], writes=[hf])
            fw.op("dve", lambda: nc.vector.tensor_copy(out=hb[:], in_=hf[:]), reads=[hf], writes=[hb])
            fw.dma("pool", self.scr["HT"][:, tok:tok + 128].rearrange("(k p) t -> p k t", p=128), hb[:], reads=[hb])
            xt, xb = xtr.next(), xbr.next()
            scb, shb = (sc_lat, sh_lat) if wh == 0 else (sc_ctx, sh_ctx)
            fw.op("pool", lambda: nc.gpsimd.tensor_tensor(out=xt[:], in0=xn[:], in1=scb[:], op=ALU.mult), reads=[xn, scb], writes=[xt])
            fw.op("pool", lambda: nc.gpsimd.tensor_tensor(out=xb[:], in0=xt[:], in1=shb[:], op=ALU.add), reads=[xt, shb], writes=[xb])
            fw.dma("pool", self.scr["XB"][tok:tok + 128, :], xb[:], reads=[xb])
            pl = plr.next()
            for k in range(8):
                fw.op("pe", lambda k=k: nc.tensor.matmul(pl[:, :], lhsT=hf[:, k, :], rhs=rw[:, k, :], start=(k == 0), stop=(k == 7)),
                      reads=[hf, rw], writes=[pl], inc=(k == 7))
            sc = scr_.next()
            fw.op("act", lambda: nc.scalar.activation(out=sc[:], in_=pl[:], func=AF.Sigmoid), reads=[pl], writes=[sc])
            bi = bir.next()
            fw.op("dve", lambda: nc.vector.tensor_tensor(out=bi[:], in0=sc[:], in1=rb[:], op=ALU.add), reads=[sc, rb], writes=[bi])
            m8 = m8r.next()
            fw.op("dve", lambda: nc.vector.max(out=m8[:, 0:8], in_=bi[:]), reads=[bi], writes=[m8])
            fw.op("dve", lambda: nc.vector.tensor_reduce(out=m8[:, 8:9], in_=m8[:, 0:8], axis=AX.X, op=ALU.min), reads=[m8], writes=[m8])
            fw.op("dve", lambda: nc.vector.tensor_scalar(out=bi[:], in0=bi[:], scalar1=m8[:, 8:9], scalar2=None, op0=ALU.is_ge), reads=[bi, m8], writes=[bi])
            fw.op("dve", lambda: nc.vector.tensor_tensor(out=sc[:], in0=sc[:], in1=bi[:], op=ALU.mult), reads=[sc, bi], writes=[sc])
            fw.op("dve", lambda: nc.vector.reduce_sum(out=m8[:, 9:10], in_=sc[:], axis=AX.X), reads=[sc], writes=[m8])
            fw.op("dve", lambda: nc.vector.reciprocal(out=m8[:, 10:11], in_=m8[:, 9:10]), reads=[m8], writes=[m8])
            gt = gtr.next()
            fw.op("dve", lambda: nc.vector.tensor_scalar(out=gt[:, 0:NE], in0=sc[:], scalar1=m8[:, 10:11], scalar2=2.5, op0=ALU.mult, op1=ALU.mult),
                  reads=[sc, m8], writes=[gt])
            fw.op("pool", lambda: nc.gpsimd.memset(gt[:, NE:NE + 1], 1.0), writes=[gt])
            fw.dma("pool", self.scr["GT"][ti], gt[:], reads=[gt])
            mk = mkr.next()
            fw.op("dve", lambda: nc.vector.tensor_copy(out=mk[:], in_=bi[:]), reads=[bi], writes=[mk])
            pp = ppr.next()
            fw.op("pe", lambda: nc.tensor.matmul(pp[:, 0, :], lhsT=slb[:], rhs=mk[:], start=True, stop=True), reads=[slb, mk], writes=[pp], inc=False)
            fw.op("pe", lambda: nc.tensor.matmul(pp[:, 1, :], lhsT=onesb[:], rhs=mk[:], start=True, stop=True), reads=[onesb, mk], writes=[pp])
            pos = posr.next()
            fw.op("dve", lambda: nc.vector.tensor_tensor(out=pos[:], in0=pp[:, 0, :], in1=self.moe_run[:], op=ALU.add), reads=[pp, self.moe_run], writes=[pos])
            fw.op("dve", lambda: nc.vector.tensor_tensor(out=self.moe_run[:], in0=pp[:, 1, :], in1=self.moe_run[:], op=ALU.add),
                  reads=[pp, self.moe_run], writes=[self.moe_run])
            fw.dma("pool", self.scr["POS"][ti], pos[:], reads=[pos])

    @_stage
    def stage_dispatch(self, l, last):
        fw, nc = self.fw, self.nc
        T, NT, NB = self.T, self.NT, self.NB
        U32 = mybir.dt.uint32
        ntile = (T if last else NT) // 128
        run = self.moe_run
        slf = fw.sb([128, 128], F32, "slf")
        fw.dma("sp", slf[:], self.din["moe_sl"], writes=[slf])
        biota = fw.sb([128, NB], F32, "biota")
        fw.dma("sp", biota[:], self.din["moe_biota"], writes=[biota])
        piota = fw.sb([128, 1], F32, "piota")
        fw.dma("sp", piota[:], self.din["moe_piota"], writes=[piota])
        eiota = fw.sb([128, NE], F32, "eiota")
        fw.dma("sp", eiota[:], self.din["moe_eiota"], writes=[eiota])
        a = fw.sb([128, NE], F32, "a"); b_ = fw.sb([128, NE], F32, "b"); ci = fw.sb([128, NE], I32, "ci")
        cnd = fw.sb([128, NE], F32, "cnd"); padded = fw.sb([128, NE], F32, "padded")
        V = nc.vector
        fw.op("dve", lambda: V.tensor_scalar(out=a[:], in0=run[:], scalar1=127.0, scalar2=1.0 / 128, op0=ALU.add, op1=ALU.mult), reads=[run], writes=[a])
        fw.op("dve", lambda: V.tensor_scalar_add(out=a[:], in0=a[:], scalar1=-0.49609375), reads=[a], writes=[a])
        fw.op("dve", lambda: V.tensor_copy(out=ci[:], in_=a[:]), reads=[a], writes=[ci])
        fw.op("dve", lambda: V.tensor_copy(out=cnd[:], in_=ci[:]), reads=[ci], writes=[cnd])
        fw.op("dve", lambda: V.tensor_scalar(out=a[:], in0=cnd[:], scalar1=128.0, scalar2=None, op0=ALU.mult), reads=[cnd], writes=[a])
        fw.op("dve", lambda: V.tensor_tensor(out=b_[:], in0=a[:], in1=run[:], op=ALU.is_lt), reads=[a, run], writes=[b_])
        fw.op("dve", lambda: V.tensor_tensor(out=cnd[:], in0=cnd[:], in1=b_[:], op=ALU.add), reads=[cnd, b_], writes=[cnd])
        fw.op("dve", lambda: V.tensor_scalar(out=a[:], in0=cnd[:], scalar1=128.0, scalar2=-128.0, op0=ALU.mult, op1=ALU.add), reads=[cnd], writes=[a])
        fw.op("dve", lambda: V.tensor_tensor(out=b_[:], in0=a[:], in1=run[:], op=ALU.is_ge), reads=[a, run], writes=[b_])
        fw.op("dve", lambda: V.tensor_tensor(out=cnd[:], in0=cnd[:], in1=b_[:], op=ALU.subtract), reads=[cnd, b_], writes=[cnd])
        fw.op("dve", lambda: V.tensor_scalar(out=padded[:], in0=cnd[:], scalar1=128.0, scalar2=None, op0=ALU.mult), reads=[cnd], writes=[padded])
        pt = fw.ps([128, 128], F32, "pt"); pst = fw.ps([128, 128], F32, "pst")
        padT = fw.sb([128, 128], F32, "padT"); pstart = fw.sb([128, NE], F32, "pstart"); pend = fw.sb([128, NE], F32, "pend")
        pendT = fw.sb([128, 128], F32, "pendT")
        fw.op("pe", lambda: nc.tensor.transpose(out=pt[:], in_=padded[:], identity=self.ident[:]), reads=[padded, self.ident], writes=[pt])
        fw.op("act", lambda: nc.scalar.copy(out=padT[:], in_=pt[:]), reads=[pt], writes=[padT])
        fw.op("pe", lambda: nc.tensor.matmul(pst[:], lhsT=padT[:], rhs=slf[:], start=True, stop=True), reads=[padT, slf], writes=[pst])
        fw.op("act", lambda: nc.scalar.copy(out=pstart[:], in_=pst[:]), reads=[pst], writes=[pstart])
        fw.op("dve", lambda: V.tensor_tensor(out=pend[:], in0=pstart[:], in1=padded[:], op=ALU.add), reads=[pstart, padded], writes=[pend])
        fw.op("pe", lambda: nc.tensor.transpose(out=pt[:], in_=pend[:], identity=self.ident[:]), reads=[pend, self.ident], writes=[pt])
        fw.op("act", lambda: nc.scalar.copy(out=pendT[:], in_=pt[:]), reads=[pt], writes=[pendT])
        cmp_ = fw.sb([128, NB], F32, "cmp"); ebc = fw.sb([128, NB], F32, "ebc"); chg = fw.sb([128, NB], F32, "chg")
        pe_ = fw.ps([128, 2, 512], F32, "pe")
        fw.op("dve", lambda: V.tensor_scalar(out=cmp_[:], in0=biota[:], scalar1=pendT[:, 0:1], scalar2=None, op0=ALU.is_ge), reads=[biota, pendT], writes=[cmp_])
        for hb in range(2):
            c0, c1 = hb * 512, min(NB, hb * 512 + 512)
            if c0 >= NB:
                continue
            fw.op("pe", lambda hb=hb, c0=c0, c1=c1: nc.tensor.matmul(pe_[:, hb, 0:c1 - c0], lhsT=self.ones[:], rhs=cmp_[:, c0:c1], start=True, stop=True),
                  reads=[self.ones, cmp_], writes=[pe_])
            fw.op("dve", lambda hb=hb, c0=c0, c1=c1: V.tensor_scalar_min(out=ebc[:, c0:c1], in0=pe_[:, hb, 0:c1 - c0], scalar1=float(NE - 1)), reads=[pe_], writes=[ebc])
        fw.op("dve", lambda: V.memset(chg[:, 0:1], 1.0), writes=[chg])
        fw.op("dve", lambda: V.tensor_tensor(out=chg[:, 1:NB], in0=ebc[:, 1:NB], in1=ebc[:, 0:NB - 1], op=ALU.not_equal), reads=[ebc], writes=[chg])
        fw.op("dve", lambda: V.tensor_scalar(out=chg[:], in0=chg[:], scalar1=-1.0e7, scalar2=1.0e7, op0=ALU.mult, op1=ALU.add), reads=[chg], writes=[chg])
        fw.op("dve", lambda: V.scalar_tensor_tensor(out=cmp_[:], in0=ebc[:], scalar=128.0, in1=chg[:], op0=ALU.mult, op1=ALU.add), reads=[ebc, chg], writes=[cmp_])
        fw.op("dve", lambda: V.tensor_scalar_add(out=cmp_[:], in0=cmp_[:], scalar1=float(l * NE * 128)), reads=[cmp_], writes=[cmp_])
        fw.op("dve", lambda: V.tensor_scalar(out=cmp_[:], in0=cmp_[:], scalar1=piota[:, 0:1], scalar2=None, op0=ALU.add), reads=[cmp_, piota], writes=[cmp_])
        fw.op("dve", lambda: V.tensor_copy(out=self.widx[:], in_=cmp_[:]), reads=[cmp_], writes=[self.widx])
        gtr = Ring([fw.sb([128, NE + 1], F32, "gt") for _ in range(2)])
        posr = Ring([fw.sb([128, NE], F32, "pos") for _ in range(2)])
        keyr = Ring([fw.sb([128, NE], F32, "key") for _ in range(2)])
        v8r = Ring([fw.sb([128, 8], F32, "v8") for _ in range(2)])
        d8r = Ring([fw.sb([128, 8], F32, "d8") for _ in range(2)])
        jr = Ring([fw.sb([128, NE], F32, "junk") for _ in range(2)])
        xbr = Ring([fw.sb([128, D], BF16, "xb") for _ in range(3)])
        for ti in range(ntile):
            tok = ti * 128
            gt, pos = gtr.next(), posr.next()
            fw.dma("sp", gt[:], self.scr["GT"][ti], writes=[gt])
            fw.dma("sp", pos[:], self.scr["POS"][ti], writes=[pos])
            xb = xbr.next()
            fw.dma("sp", xb[:], self.scr["XB"][tok:tok + 128, :], writes=[xb])
            fw.op("dve", lambda: V.tensor_tensor(out=pos[:], in0=pos[:], in1=pstart[:], op=ALU.add), reads=[pos, pstart], writes=[pos])
            key = keyr.next()
            fw.op("dve", lambda: V.scalar_tensor_tensor(out=key[:], in0=gt[:, 0:NE], scalar=0.0, in1=eiota[:], op0=ALU.is_gt, op1=ALU.mult), reads=[gt, eiota], writes=[key])
            v8, d8 = v8r.next(), d8r.next()
            fw.op("dve", lambda: V.max(out=v8[:], in_=key[:]), reads=[key], writes=[v8])
            fw.op("dve", lambda: V.memset(d8[:], 0.0), writes=[d8])
            fw.op("dve", lambda: V.memset(self.gate8[:, ti, :], 0.0), writes=[self.gate8])
            for k in range(8):
                j1, j2 = jr.next(), jr.next()
                fw.op("dve", lambda k=k, j1=j1: V.scalar_tensor_tensor(out=j1[:], in0=key[:], scalar=v8[:, k:k + 1], in1=pos[:], op0=ALU.is_equal, op1=ALU.mult,
                                                                       accum_out=d8[:, k:k + 1]), reads=[key, v8, pos], writes=[j1, d8])
                fw.op("dve", lambda k=k, j2=j2: V.scalar_tensor_tensor(out=j2[:], in0=key[:], scalar=v8[:, k:k + 1], in1=gt[:, 0:NE], op0=ALU.is_equal, op1=ALU.mult,
                                                                       accum_out=self.gate8[:, ti, k:k + 1]), reads=[key, v8, gt], writes=[j2, self.gate8])
            fw.op("dve", lambda: V.tensor_copy(out=self.dest8[:, ti, :], in_=d8[:]), reads=[d8], writes=[self.dest8])
            for k in range(8):
                fw.idma(self.scr["XS"], bass.IndirectOffsetOnAxis(ap=self.dest8[:, ti, k:k + 1], axis=0), xb[:], None, reads=[xb, self.dest8])

    @_stage
    def stage_blocks(self, l, last):
        fw, nc = self.fw, self.nc
        T, NT = self.T, self.NT
        ntile = (T if last else NT) // 128
        NBl = ntile * 8 + NE
        w1v, w3v, w2v = self.din["exp_w1"], self.din["exp_w3"], self.din["exp_w2"]
        wsets = [[fw.sb([128, 2048], BF16, nm) for nm in ("w1", "w3", "w2")] for _ in range(1)]

        class V3:
            def __init__(self, b, k):
                self.b, self.k = b, k

            def __getitem__(self, key):
                return self.b[:].rearrange("p (k f) -> p k f", k=self.k)[key]
        xsr = Ring([fw.sb([128, D], BF16, "xs") for _ in range(3)])
        xTr = Ring([fw.sb([128, 8, 128], BF16, "xT") for _ in range(2)])
        sar = Ring([fw.sb([128, 256], F32, "sa") for _ in range(2)])
        hdr = Ring([fw.sb([128, 256], BF16, "hd") for _ in range(2)])
        hTr = Ring([fw.sb([128, 2, 128], BF16, "hdT") for _ in range(2)])
        ysr = Ring([fw.sb([128, D], BF16, "ys") for _ in range(2)])
        ptx = Ring([fw.ps([128, 8, 128], BF16, "ptx") for _ in range(2)])
        pab = Ring([fw.ps([128, 512], F32, "pab") for _ in range(2)])
        pth = Ring([fw.ps([128, 2, 128], BF16, "pth") for _ in range(1)])
        pyr = Ring([fw.ps([128, 2, 512], F32, "py") for _ in range(1)])
        if not hasattr(self, "bound_reg"):
            self.bound_reg = nc.gpsimd.alloc_register("moe_bound")
        nc.gpsimd.reg_mov(self.bound_reg, (l + 1) * NE * 128 - 1)
        bound = self.bound_reg
        for b in range(NBl):
            off = bass.IndirectOffsetOnAxis(ap=self.widx[:, b:b + 1], axis=0)
            w1_, w3_, w2_ = wsets[0]
            w1, w3, w2 = V3(w1_, 8), V3(w3_, 8), V3(w2_, 2)
            for (wt, wv) in ((w1_, w1v), (w3_, w3v), (w2_, w2v)):
                fw.idma(wt[:], None, wv, off, reads=[self.widx], writes=[wt], bounds_check=bound, oob_is_err=False)
            xs = xsr.next()
            fw.dma("sp", xs[:], self.scr["XS"][b * 128:(b + 1) * 128, :], writes=[xs])
            px = ptx.next()
            for k in range(8):
                fw.op("pe", lambda k=k: nc.tensor.transpose(out=px[:, k, :], in_=xs[:].rearrange("p (f k) -> p k f", k=8)[:, k, :], identity=self.ident_bf[:]),
                      reads=[xs, self.ident_bf], writes=[px], inc=(k == 7))
            xT = xTr.next()
            fw.op("act", lambda: nc.scalar.copy(out=xT[:], in_=px[:]), reads=[px], writes=[xT])
            pa = pab.next()
            for (wt, wb, c0) in ((w1, w1_, 0), (w3, w3_, 256)):
                for k in range(8):
                    fw.op("pe", lambda wt=wt, c0=c0, k=k: nc.tensor.matmul(pa[:, c0:c0 + 256], lhsT=xT[:, k, :], rhs=wt[:, k, :], start=(k == 0), stop=(k == 7)),
                          reads=[xT, wb], writes=[pa], inc=(k == 7))
            sa, hd = sar.next(), hdr.next()
            fw.op("act", lambda: nc.scalar.activation(out=sa[:], in_=pa[:, 0:256], func=AF.Silu), reads=[pa], writes=[sa])
            fw.op("dve", lambda: nc.vector.tensor_tensor(out=hd[:], in0=sa[:], in1=pa[:, 256:512], op=ALU.mult), reads=[sa, pa], writes=[hd])
            ph = pth.next()
            for k in range(2):
                fw.op("pe", lambda k=k: nc.tensor.transpose(out=ph[:, k, :], in_=hd[:].rearrange("p (f k) -> p k f", k=2)[:, k, :], identity=self.ident_bf[:]),
                      reads=[hd, self.ident_bf], writes=[ph], inc=(k == 1))
            hT = hTr.next()
            fw.op("dve", lambda: nc.vector.tensor_copy(out=hT[:], in_=ph[:]), reads=[ph], writes=[hT])
            py = pyr.next()
            for half in range(2):
                for k in range(2):
                    fw.op("pe", lambda half=half, k=k: nc.tensor.matmul(py[:, half, :], lhsT=hT[:, k, :], rhs=w2[:, k, half * 512:(half + 1) * 512],
                                                                         start=(k == 0), stop=(k == 1)), reads=[hT, w2_], writes=[py], inc=(k == 1))
            ys = ysr.next()
            fw.op("act", lambda: nc.scalar.copy(out=ys[:, 0:512], in_=py[:, 0, :]), reads=[py], writes=[ys])
            fw.op("dve", lambda: nc.vector.tensor_copy(out=ys[:, 512:1024], in_=py[:, 1, :]), reads=[py], writes=[ys])
            fw.dma("sp", self.scr["YS"][b * 128:(b + 1) * 128, :], ys[:], reads=[ys])

    @_stage
    def stage_experts(self, l, last, G=16, experts=(NE,)):
        fw, nc = self.fw, self.nc
        T, NT = self.T, self.NT
        g_lat = fw.sb([128, D], F32, "glat"); g_ctx = fw.sb([128, D], F32, "gctx")
        pg = fw.ps([128, 2, 512], F32, "pg")
        repr_ = Ring([fw.sb([128, 128], F32, "rep") for _ in range(2)])
        self.bcast_mod(40, g_lat, g_ctx, pg, repr_)
        lg, lb = self.load_ln(l, "ln2")
        ntile = (T if last else NT) // 128
        acc = fw.sb([128, G, D], F32, "acc")
        hT = fw.sb([128, 8, G * 128], BF16, "hTg")
        gts = fw.sb([128, G, NE + 1], F32, "gts")
        w1r = Ring([fw.sb([128, 8, 256], BF16, "w1") for _ in range(2)])
        w3r = Ring([fw.sb([128, 8, 256], BF16, "w3") for _ in range(2)])
        w2r = Ring([fw.sb([128, 2, D], BF16, "w2") for _ in range(2)])
        sar = Ring([fw.sb([128, 2, 512], BF16, "sa") for _ in range(2)])
        hdr = Ring([fw.sb([128, 2, 512], BF16, "hd") for _ in range(2)])
        xr = Ring([fw.sb([128, D], F32, "x") for _ in range(2)])
        ykr = Ring([fw.sb([128, D], BF16, "yk") for _ in range(4)])
        sr = Ring([fw.sb([128, 16], F32, "st") for _ in range(2)])
        par = Ring([fw.ps([128, 2, 512], F32, "pa") for _ in range(1)])
        pbr = Ring([fw.ps([128, 2, 512], F32, "pb3") for _ in range(1)])
        pyr = Ring([fw.ps([128, 512], F32, "py") for _ in range(2)])
        dn = self.din
        for g0 in range(0, ntile, G):
            gn = min(G, ntile - g0)
            tok0 = g0 * 128
            for k in range(8):
                fw.dma("sp", hT[:, k, 0:gn * 128], self.scr["HT"][k * 128:(k + 1) * 128, tok0:tok0 + gn * 128], writes=[hT])
            fw.dma("sp", gts[:, 0:gn, :], self.scr["GT"][g0:g0 + gn].rearrange("g p e -> p g e"), writes=[gts])
            for ei, e in enumerate(experts):
                w1, w3, w2 = w1r.next(), w3r.next(), w2r.next()
                if e < NE:
                    raise NotImplementedError("routed experts run in stage_blocks")
                else:
                    s1, s3, s2 = dn["sh_w1"][l], dn["sh_w3"][l], dn["sh_w2"][l]
                fw.dma("pool", w1[:], s1.rearrange("(k p) f -> p k f", p=128), writes=[w1])
                fw.dma("pool", w3[:], s3.rearrange("(k p) f -> p k f", p=128), writes=[w3])
                fw.dma("pool", w2[:], s2.rearrange("(k p) n -> p k n", p=128), writes=[w2])
                for c0 in range(0, gn, 4):
                    cn_ = min(4, gn - c0)
                    n = cn_ * 128
                    pa, pb = par.next(), pbr.next()
                    for (w, p) in ((w1, pa), (w3, pb)):
                        for hc in range(2):
                            for k in range(8):
                                fw.op("pe", lambda w=w, p=p, hc=hc, k=k: nc.tensor.matmul(p[:, hc, 0:n], lhsT=w[:, k, hc * 128:(hc + 1) * 128],
                                                                                         rhs=hT[:, k, c0 * 128:c0 * 128 + n], start=(k == 0), stop=(k == 7)),
                                      reads=[w, hT], writes=[p], inc=(k == 7))
                    sa, hd = sar.next(), hdr.next()
                    for hc in range(2):
                        fw.op("act", lambda hc=hc: nc.scalar.activation(out=sa[:, hc, 0:n], in_=pa[:, hc, 0:n], func=AF.Silu), reads=[pa], writes=[sa])
                        fw.op("dve", lambda hc=hc: nc.vector.tensor_tensor(out=hd[:, hc, 0:n], in0=sa[:, hc, 0:n], in1=pb[:, hc, 0:n], op=ALU.mult),
                              reads=[sa, pb], writes=[hd])
                    for t in range(cn_):
                        gi = c0 + t
                        for half in range(2):
                            py = pyr.next()
                            for hc in range(2):
                                fw.op("pe", lambda t=t, half=half, hc=hc, py=py: nc.tensor.matmul(py[:, :], lhsT=hd[:, hc, t * 128:(t + 1) * 128],
                                                                                                 rhs=w2[:, hc, half * 512:(half + 1) * 512], start=(hc == 0), stop=(hc == 1)),
                                      reads=[hd, w2], writes=[py], inc=(hc == 1))
                            a = acc[:, gi, half * 512:(half + 1) * 512]
                            gsc = gts[:, gi, e:e + 1]
                            if ei == 0:
                                fw.op("dve", lambda a=a, py=py, gsc=gsc: nc.vector.tensor_scalar(out=a, in0=py[:], scalar1=gsc, scalar2=None, op0=ALU.mult),
                                      reads=[py, gts], writes=[acc])
                            else:
                                fw.op("dve", lambda a=a, py=py, gsc=gsc: nc.vector.scalar_tensor_tensor(out=a, in0=py[:], scalar=gsc, in1=a, op0=ALU.mult, op1=ALU.add),
                                      reads=[py, gts, acc], writes=[acc])
            for gi in range(gn):
                tok = tok0 + gi * 128
                x = xr.next()
                fw.dma("sp", x[:], self.scr["XR"][tok:tok + 128, :], writes=[x])
                gb = g_lat if tok < T else g_ctx
                if NE not in experts or len(experts) == 1:
                    ti = tok // 128
                    for k in range(8):
                        yk = ykr.next()
                        fw.idma(yk[:], None, self.scr["YS"], bass.IndirectOffsetOnAxis(ap=self.dest8[:, ti, k:k + 1], axis=0), reads=[self.dest8], writes=[yk])
                        fw.op("dve", lambda gi=gi, yk=yk, ti=ti, k=k: nc.vector.scalar_tensor_tensor(
                            out=acc[:, gi, :], in0=yk[:], scalar=self.gate8[:, ti, k:k + 1], in1=acc[:, gi, :], op0=ALU.mult, op1=ALU.add),
                            reads=[yk, self.gate8, acc], writes=[acc])
                fw.op("dve", lambda gi=gi, gb=gb: nc.vector.tensor_tensor(out=acc[:, gi, :], in0=acc[:, gi, :], in1=gb[:], op=ALU.mult), reads=[acc, gb], writes=[acc])
                fw.op("dve", lambda gi=gi, x=x: nc.vector.scalar_tensor_tensor(out=x[:], in0=x[:], scalar=ALPHA, in1=acc[:, gi, :], op0=ALU.mult, op1=ALU.add),
                      reads=[x, acc], writes=[x])
                if last:
                    dst = self.out[tok:tok + 128, :]
                else:
                    dst = self.scr["XR"][tok:tok + 128, :]
                self.post_norm_store(x, sr.next(), lg, lb, dst, eng="dve")

    def build(self, upto=None):
        from contextlib import ExitStack
        self.declare()
        names = ["mod", "inproj", "wa_prep", "mla_prep", "na_attn", "wa_attn", "mla_attn", "gla", "outproj", "router", "dispatch", "blocks", "experts"]
        cnt = 0
        with ExitStack() as gst:
            self.setup_global(gst)
            self.fw.barrier()
            for l in range(self.L):
                last = l == self.L - 1
                for nm in names:
                    if upto is not None and cnt >= upto:
                        break
                    cnt += 1
                    fn = getattr(self, "stage_" + nm)
                    if nm in ("na_attn", "wa_attn", "mla_attn"):
                        fn(l, not last)
                    elif nm in ("outproj", "router", "dispatch", "blocks", "experts"):
                        fn(l, last)
                    else:
                        fn(l)
            self.fw.barrier()
        return self.nc


def prep_shared(inp, T, L):
    f = lambda a: np.ascontiguousarray(np.asarray(a, dtype=np.float32))
    m = dict(host_consts(T))
    m["w_ada"] = f(inp["w_ada"][:L])
    m["b_ada"] = f(inp["b_ada"][:L])
    m["b_ada_pj"] = f(np.asarray(inp["b_ada"][:L]).reshape(L, 48, 128).transpose(0, 2, 1))
    m["w_in"] = f(inp["w_in"][:L])
    m["na_rpb"] = f(inp["na_rpb"][:L]); m["wa_sink"] = f(inp["wa_sink"][:L])
    m["mla_g_q"] = f(np.asarray(inp["mla_g_q"][:L]).reshape(L, 2, 128).transpose(0, 2, 1))
    m["mla_g_kv"] = f(np.asarray(inp["mla_g_kv"][:L]).reshape(L, 128, 1))
    m["mla_w_uq"] = f(inp["mla_w_uq"][:L]); m["mla_w_ukv"] = f(inp["mla_w_ukv"][:L])
    for k in ("gla_w_gf", "gla_b_gf", "gla_w_gb", "gla_b_gb"):
        m[k] = f(inp[k][:L])
    m["gla_g_norm"] = f(np.asarray(inp["gla_g_norm"][:L]).reshape(L, 64, 1))
    for k in ("w_out", "ln1_g", "ln1_b", "ln2_g", "ln2_b", "router_w", "router_bias", "sh_w1", "sh_w3", "sh_w2"):
        m[k] = f(inp[k][:L])
    for k in ("exp_w1", "exp_w3", "exp_w2"):
        m[k] = f(inp[k][:L]).reshape(L * NE * 128, 2048)
    return m


def prep_core(inp, b):
    f = lambda a: np.ascontiguousarray(np.asarray(a, dtype=np.float32))
    c = np.asarray(inp["c"][b], dtype=np.float32)
    cc = np.asarray(inp["c_ctx"], dtype=np.float32)
    cv = np.stack([c.reshape(8, 128).T, cc.reshape(8, 128).T], axis=-1)
    return {"x": f(inp["x"][b]), "ctx": f(inp["ctx"][b]), "cv": f(cv)}


_CACHE = {}


def kernel(**inputs):
    B, T, _ = inputs["x"].shape
    L = inputs["w_ada"].shape[0]
    key = (T, L)
    if key not in _CACHE:
        _CACHE[key] = KB(T, L).build()
    nc = _CACHE[key]
    shared = prep_shared(inputs, T, L)
    in_maps = []
    for b in range(B):
        m = dict(shared)
        m.update(prep_core(inputs, b))
        in_maps.append(m)
    res = run_bass_kernel_spmd(nc, in_maps, core_ids=list(range(B)))
    return np.stack([np.asarray(res.results[b]["out"], dtype=np.float32) for b in range(B)], axis=0)
```
